# Optimizing a Trainium2 kernel written in Bass

```python
import jax
import jax.numpy as jnp
from jax import lax
import numpy as np

D_MODEL = 1024
BATCH = 8
SEQ = 4096
DEPTH = 4

GRID_W = 64
CTX_LEN = 256
N_MIXERS = 3
EPS = 1e-6
D_FF = 4 * D_MODEL
N_DIRS = 2

GLA_HEADS = 4
GLA_DK = D_MODEL // 2
GLA_DV = D_MODEL
GLA_HK = GLA_DK // GLA_HEADS
GLA_HV = GLA_DV // GLA_HEADS
GLA_IN = 2 * GLA_DK + 2 * GLA_DV
GLA_GATE_RANK = 16
GLA_TAU = 16.0
GLA_CHUNK = 64

MLSTM_HEADS = 4
MLSTM_INNER = 2 * D_MODEL
MLSTM_HD = MLSTM_INNER // MLSTM_HEADS
MLSTM_QKV_BLOCK = 4
MLSTM_N_BLOCKS = MLSTM_INNER // MLSTM_QKV_BLOCK
MLSTM_CONV = 4
MLSTM_CHUNK = 64

POOL_WINDOWS = (2, 4, 8, 16)
POOL_GROUP = D_MODEL // len(POOL_WINDOWS)

kernel_name = 'hybrid_gla_mlstm_pool_flow_block'


def rmsnorm(x, g):
    xf = x.astype(jnp.float32)
    y = xf * lax.rsqrt(jnp.mean(xf * xf, axis=-1, keepdims=True) + EPS)
    return (y * g.astype(jnp.float32)).astype(x.dtype)


def modulate(h, shift, scale):
    return h * (1 + scale) + shift


def head_rmsnorm(o, g):
    return o * lax.rsqrt(jnp.mean(o * o, axis=-1, keepdims=True) + EPS) * g.astype(jnp.float32)


def head_layernorm(o, g):
    mu = jnp.mean(o, axis=-1, keepdims=True)
    oc = o - mu
    return oc * lax.rsqrt(jnp.mean(oc * oc, axis=-1, keepdims=True) + EPS) * g.astype(jnp.float32)


def to_chunks(t, size):
    b, n = t.shape[0], t.shape[1] // size
    t = t.reshape((b, n, size) + t.shape[2:])
    return t.transpose((1, 0, 3, 2) + tuple(range(4, t.ndim)))


def from_chunks(t):
    n, b, h, c, d = t.shape
    return t.transpose(1, 0, 3, 2, 4).reshape(b, n * c, h, d)


def gla_chunk_scan(q, k, v, log_a, s0):
    f32 = jnp.float32
    qc, kc, vc, gc = (to_chunks(t.astype(f32), GLA_CHUNK) for t in (q, k, v, log_a))
    tri = jnp.tril(jnp.ones((GLA_CHUNK, GLA_CHUNK), dtype=bool))

    def step(s, blk):
        qb, kb, vb, gb = blk
        b = jnp.cumsum(gb, axis=2)
        b_last = b[:, :, -1:, :]
        q_dec = qb * jnp.exp(b)
        att = jnp.where(tri, jnp.einsum('bhtd,bhsd->bhts', q_dec, kb * jnp.exp(-b)), 0.0)
        o = jnp.einsum('bhts,bhsv->bhtv', att, vb) + jnp.einsum('bhtd,bhdv->bhtv', q_dec, s)
        s_new = (jnp.exp(b_last[:, :, 0, :])[..., None] * s
                 + jnp.einsum('bhsd,bhsv->bhdv', kb * jnp.exp(b_last - b), vb))
        return s_new, o

    s_fin, o = lax.scan(step, s0, (qc, kc, vc, gc))
    return from_chunks(o), s_fin


def mlstm_chunk_scan(q, k, v, log_i, log_f, state):
    f32 = jnp.float32
    qc, kc, vc = (to_chunks(t.astype(f32), MLSTM_CHUNK) for t in (q, k, v))
    ic, fc = (to_chunks(t.astype(f32), MLSTM_CHUNK) for t in (log_i, log_f))
    tri = jnp.tril(jnp.ones((MLSTM_CHUNK, MLSTM_CHUNK), dtype=bool))

    def step(carry, blk):
        c_bar, n_bar, m_prev = carry
        qb, kb, vb, ib, fb = blk
        b = jnp.cumsum(fb, axis=-1)
        d_mat = jnp.where(tri, b[..., :, None] - b[..., None, :] + ib[..., None, :], -jnp.inf)
        inter = b + m_prev[..., None]
        m_row = jnp.maximum(jnp.max(d_mat, axis=-1), inter)
        w_qk = jnp.einsum('bhtd,bhsd->bhts', qb, kb) * jnp.exp(d_mat - m_row[..., None])
        w_inter = jnp.exp(inter - m_row)
        num = (jnp.einsum('bhts,bhsv->bhtv', w_qk, vb)
               + w_inter[..., None] * jnp.einsum('bhtd,bhdv->bhtv', qb, c_bar))
        den = jnp.sum(w_qk, axis=-1) + w_inter * jnp.einsum('bhtd,bhd->bht', qb, n_bar)
        h = num / jnp.maximum(jnp.abs(den), jnp.exp(-m_row))[..., None]
        carry_log = b[..., -1] + m_prev
        tok_log = b[..., -1:] - b + ib
        m_new = jnp.maximum(carry_log, jnp.max(tok_log, axis=-1))
        w_tok = jnp.exp(tok_log - m_new[..., None])
        w_carry = jnp.exp(carry_log - m_new)
        k_w = kb * w_tok[..., None]
        c_new = w_carry[..., None, None] * c_bar + jnp.einsum('bhsd,bhsv->bhdv', k_w, vb)
        n_new = w_carry[..., None] * n_bar + jnp.sum(k_w, axis=2)
        return (c_new, n_new, m_new), h

    state_fin, h = lax.scan(step, state, (qc, kc, vc, ic, fc))
    return from_chunks(h), state_fin


def bidirectional_scan(scan_fn, ctx_dirs, lat_dirs, init):
    rev = lambda t: jnp.flip(t, axis=1)
    out_ctx, out_lat = [], []
    for direction in range(N_DIRS):
        ca, la = ctx_dirs[direction], lat_dirs[direction]
        if direction == 1:
            ca = tuple(rev(t) for t in ca)
            la = tuple(rev(t) for t in la)
        o_c, state = scan_fn(*ca, init)
        o_l, _ = scan_fn(*la, state)
        if direction == 1:
            o_c, o_l = rev(o_c), rev(o_l)
        out_ctx.append(o_c)
        out_lat.append(o_l)
    return out_ctx[0] + out_ctx[1], out_lat[0] + out_lat[1]


def gla_mixer(a_ctx, a_lat, w_in, w_a1, w_a2, b_a, g_head, w_o, need_ctx):
    def project(a):
        B, T, _ = a.shape
        q, k, v, r = jnp.split(a @ w_in, [GLA_DK, 2 * GLA_DK, 2 * GLA_DK + GLA_DV], axis=-1)
        q = q.reshape(B, T, GLA_HEADS, GLA_HK) * GLA_HK ** -0.5
        k = k.reshape(B, T, GLA_HEADS, GLA_HK)
        v = v.reshape(B, T, GLA_HEADS, GLA_HV)
        dirs = []
        for d in range(N_DIRS):
            z = (a @ w_a1[d]) @ w_a2[d] + b_a[d]
            log_a = jax.nn.log_sigmoid(z.astype(jnp.float32)) / GLA_TAU
            dirs.append((q, k, v, log_a.reshape(B, T, GLA_HEADS, GLA_HK)))
        return dirs, r

    def finish(o, r):
        B, T = o.shape[:2]
        o = head_rmsnorm(o, g_head).reshape(B, T, GLA_DV).astype(r.dtype)
        return (o * jax.nn.silu(r)) @ w_o

    ctx_dirs, r_ctx = project(a_ctx)
    lat_dirs, r_lat = project(a_lat)
    s0 = jnp.zeros((a_lat.shape[0], GLA_HEADS, GLA_HK, GLA_HV), jnp.float32)
    o_ctx, o_lat = bidirectional_scan(gla_chunk_scan, ctx_dirs, lat_dirs, s0)
    out_ctx = finish(o_ctx, r_ctx) if need_ctx else None
    return out_ctx, finish(o_lat, r_lat)


def centred_conv(x, w, b):
    K = w.shape[0]
    y = lax.conv_general_dilated(x, w[:, None, :], window_strides=(1,),
                                 padding=[(K // 2, K - 1 - K // 2)],
                                 dimension_numbers=('NWC', 'WIO', 'NWC'),
                                 feature_group_count=x.shape[-1])
    return y + b


def headwise(t, w):
    B, T, _ = t.shape
    t = t.reshape(B, T, MLSTM_N_BLOCKS, MLSTM_QKV_BLOCK)
    return jnp.einsum('btni,nij->btnj', t, w).reshape(B, T, MLSTM_INNER)


def mlstm_mixer(a_ctx, a_lat, w_up, conv_w, conv_b, w_qkv, w_gate, b_gate, g_norm, skip, w_down, need_ctx):
    H = MLSTM_HEADS

    def project(a):
        B, T, _ = a.shape
        xm, z = jnp.split(a @ w_up, 2, axis=-1)
        xc = jax.nn.silu(centred_conv(xm, conv_w, conv_b))
        q = headwise(xc, w_qkv[0])
        k = headwise(xc, w_qkv[1])
        v = headwise(xm, w_qkv[2])
        gin = jnp.concatenate([q, k, v], axis=-1)
        heads = lambda t: t.reshape(B, T, H, MLSTM_HD)
        qh, kh, vh = heads(q), heads(k) * MLSTM_HD ** -0.5, heads(v)
        dirs = []
        for d in range(N_DIRS):
            g = (gin @ w_gate[d] + b_gate[d]).astype(jnp.float32)
            dirs.append((qh, kh, vh, g[..., :H], jax.nn.log_sigmoid(g[..., H:])))
        return dirs, xc, z

    def finish(h, xc, z):
        B, T = h.shape[:2]
        hn = head_layernorm(h, g_norm).reshape(B, T, MLSTM_INNER).astype(xc.dtype)
        return ((hn + skip * xc) * jax.nn.silu(z)) @ w_down

    ctx_dirs, xc_ctx, z_ctx = project(a_ctx)
    lat_dirs, xc_lat, z_lat = project(a_lat)
    B = a_lat.shape[0]
    init = (jnp.zeros((B, H, MLSTM_HD, MLSTM_HD), jnp.float32),
            jnp.zeros((B, H, MLSTM_HD), jnp.float32),
            jnp.zeros((B, H), jnp.float32))
    h_ctx, h_lat = bidirectional_scan(mlstm_chunk_scan, ctx_dirs, lat_dirs, init)
    out_ctx = finish(h_ctx, xc_ctx, z_ctx) if need_ctx else None
    return out_ctx, finish(h_lat, xc_lat, z_lat)


def box_mean(x, axis, window):
    length = x.shape[axis]
    pad = [(0, 0)] * x.ndim
    pad[axis] = (1, 0)
    cs = jnp.pad(jnp.cumsum(x.astype(jnp.float32), axis=axis), pad)
    pos = jnp.arange(length)
    lo = jnp.maximum(pos - window // 2, 0)
    hi = jnp.minimum(pos + (window - window // 2), length)
    total = jnp.take(cs, hi, axis=axis) - jnp.take(cs, lo, axis=axis)
    shape = [1] * x.ndim
    shape[axis] = length
    return total / (hi - lo).astype(jnp.float32).reshape(shape)


def pool_mixer(a_ctx, a_lat, w_pool, b_pool, scale, need_ctx):
    B, T, D = a_lat.shape
    rows = T // GRID_W
    grid = a_lat.reshape(B, rows, GRID_W, D)
    lat_parts, ctx_parts = [], []
    for g, win in enumerate(POOL_WINDOWS):
        sl = slice(g * POOL_GROUP, (g + 1) * POOL_GROUP)
        hg = grid[..., sl]
        pooled = box_mean(box_mean(hg, 2, win), 1, win).astype(hg.dtype) - hg
        lat_parts.append(pooled.reshape(B, T, POOL_GROUP) @ w_pool[g] + b_pool[g])
        if need_ctx:
            cg = a_ctx[..., sl]
            pooled_c = box_mean(cg, 1, win).astype(cg.dtype) - cg
            ctx_parts.append(pooled_c @ w_pool[g] + b_pool[g])
    out_lat = jnp.concatenate(lat_parts, axis=-1) * scale
    out_ctx = jnp.concatenate(ctx_parts, axis=-1) * scale if need_ctx else None
    return out_ctx, out_lat


def sqrelu_mlp(h, w1, w2):
    return jnp.square(jax.nn.relu(h @ w1)) @ w2


def setup_inputs(seed: int = 0) -> dict:
    key = jax.random.key(seed)
    keys = iter(jax.random.split(key, 48))

    def normal(shape, scale):
        return jax.random.normal(next(keys), shape, jnp.float32) * scale

    def gain(shape):
        return 1.0 + normal(shape, 0.02)

    n_gla = sum(1 for i in range(DEPTH) if i % N_MIXERS == 0)
    n_mlstm = sum(1 for i in range(DEPTH) if i % N_MIXERS == 1)
    n_pool = sum(1 for i in range(DEPTH) if i % N_MIXERS == 2)
    d = D_MODEL
    gate_i = normal((n_mlstm, N_DIRS, MLSTM_HEADS), 0.1)
    gate_f = jnp.linspace(3.0, 6.0, MLSTM_HEADS, dtype=jnp.float32) + normal((n_mlstm, N_DIRS, MLSTM_HEADS), 0.1)
    return {
        'x': normal((BATCH, SEQ, d), 1.0),
        'c': normal((BATCH, d), 1.0),
        'ctx': normal((BATCH, CTX_LEN, d), 1.0),
        'c_ctx': normal((d,), 1.0),
        'ada_w': normal((DEPTH, d, 6 * d), 0.5 * d ** -0.5),
        'ada_b': normal((DEPTH, 6 * d), 0.01),
        'norm1_g': gain((DEPTH, d)),
        'norm2_g': gain((DEPTH, d)),
        'mlp_w1': normal((DEPTH, d, D_FF), d ** -0.5),
        'mlp_w2': normal((DEPTH, D_FF, d), D_FF ** -0.5),
        'gla_w_in': normal((n_gla, d, GLA_IN), d ** -0.5),
        'gla_w_a1': normal((n_gla, N_DIRS, d, GLA_GATE_RANK), d ** -0.5),
        'gla_w_a2': normal((n_gla, N_DIRS, GLA_GATE_RANK, GLA_DK), GLA_GATE_RANK ** -0.5),
        'gla_b_a': normal((n_gla, N_DIRS, GLA_DK), 0.1),
        'gla_g_head': gain((n_gla, GLA_HEADS, GLA_HV)),
        'gla_w_o': normal((n_gla, GLA_DV, d), GLA_DV ** -0.5),
        'mlstm_w_up': normal((n_mlstm, d, 2 * MLSTM_INNER), d ** -0.5),
        'mlstm_conv_w': normal((n_mlstm, MLSTM_CONV, MLSTM_INNER), MLSTM_CONV ** -0.5),
        'mlstm_conv_b': normal((n_mlstm, MLSTM_INNER), 0.01),
        'mlstm_w_qkv': normal((n_mlstm, 3, MLSTM_N_BLOCKS, MLSTM_QKV_BLOCK, MLSTM_QKV_BLOCK), MLSTM_QKV_BLOCK ** -0.5),
        'mlstm_w_gate': normal((n_mlstm, N_DIRS, 3 * MLSTM_INNER, 2 * MLSTM_HEADS), (3 * MLSTM_INNER) ** -0.5),
        'mlstm_b_gate': jnp.concatenate([gate_i, gate_f], axis=-1),
        'mlstm_g_norm': gain((n_mlstm, MLSTM_HEADS, MLSTM_HD)),
        'mlstm_skip': gain((n_mlstm, MLSTM_INNER)),
        'mlstm_w_down': normal((n_mlstm, MLSTM_INNER, d), MLSTM_INNER ** -0.5),
        'pool_w': normal((n_pool, len(POOL_WINDOWS), POOL_GROUP, POOL_GROUP), POOL_GROUP ** -0.5),
        'pool_b': normal((n_pool, len(POOL_WINDOWS), POOL_GROUP), 0.01),
        'pool_scale': gain((n_pool, d)),
        'final_g': gain((d,)),
    }


def reference(x, c, ctx, c_ctx, ada_w, ada_b, norm1_g, norm2_g, mlp_w1, mlp_w2,
              gla_w_in, gla_w_a1, gla_w_a2, gla_b_a, gla_g_head, gla_w_o,
              mlstm_w_up, mlstm_conv_w, mlstm_conv_b, mlstm_w_qkv, mlstm_w_gate, mlstm_b_gate,
              mlstm_g_norm, mlstm_skip, mlstm_w_down,
              pool_w, pool_b, pool_scale, final_g):
    silu_c = jax.nn.silu(c)
    silu_cc = jax.nn.silu(c_ctx)
    h_lat, h_ctx = x, ctx
    for i in range(DEPTH):
        kind, j = i % N_MIXERS, i // N_MIXERS
        need_ctx = i < DEPTH - 1
        mod_l = jnp.split((silu_c @ ada_w[i] + ada_b[i])[:, None, :], 6, axis=-1)
        mod_c = jnp.split((silu_cc @ ada_w[i] + ada_b[i])[None, None, :], 6, axis=-1)
        a_lat = modulate(rmsnorm(h_lat, norm1_g[i]), mod_l[0], mod_l[1])
        a_ctx = modulate(rmsnorm(h_ctx, norm1_g[i]), mod_c[0], mod_c[1])
        if kind == 0:
            o_ctx, o_lat = gla_mixer(a_ctx, a_lat, gla_w_in[j], gla_w_a1[j], gla_w_a2[j], gla_b_a[j],
                                     gla_g_head[j], gla_w_o[j], need_ctx)
        elif kind == 1:
            o_ctx, o_lat = mlstm_mixer(a_ctx, a_lat, mlstm_w_up[j], mlstm_conv_w[j], mlstm_conv_b[j],
                                       mlstm_w_qkv[j], mlstm_w_gate[j], mlstm_b_gate[j],
                                       mlstm_g_norm[j], mlstm_skip[j], mlstm_w_down[j], need_ctx)
        else:
            o_ctx, o_lat = pool_mixer(a_ctx, a_lat, pool_w[j], pool_b[j], pool_scale[j], need_ctx)
        h_lat = h_lat + mod_l[2] * o_lat
        h_lat = h_lat + mod_l[5] * sqrelu_mlp(modulate(rmsnorm(h_lat, norm2_g[i]), mod_l[3], mod_l[4]),
                                              mlp_w1[i], mlp_w2[i])
        if need_ctx:
            h_ctx = h_ctx + mod_c[2] * o_ctx
            h_ctx = h_ctx + mod_c[5] * sqrelu_mlp(modulate(rmsnorm(h_ctx, norm2_g[i]), mod_c[3], mod_c[4]),
                                                  mlp_w1[i], mlp_w2[i])
    return rmsnorm(h_lat, final_g)
```

```python
from contextlib import ExitStack
import numpy as np
import ml_dtypes
import concourse.bass as bass
import concourse.mybir as mybir
from concourse.bass_utils import run_bass_kernel_spmd

F32 = mybir.dt.float32
BF16 = mybir.dt.bfloat16
AF = mybir.ActivationFunctionType
ALU = mybir.AluOpType
AX = mybir.AxisListType

COMPUTE = ("pe", "act", "dve", "pool")
EPOCH = 20000
NDMASEM = 64

D = 1024
KC = 8
TC = 256
DFF = 4096
EPS = 1e-6


class Prog:
    def __init__(self, nc):
        self.nc = nc
        self.es = ExitStack()
        self.ops = []
        self.nname = 0

    def sb(self, shape, dtype, name=None):
        self.nname += 1
        name = (name or "sb") + f"_{self.nname}"
        return self.es.enter_context(self.nc.sbuf_tensor(name, list(shape), dtype))

    def ps(self, shape, dtype, name=None):
        self.nname += 1
        name = (name or "ps") + f"_{self.nname}"
        return self.es.enter_context(self.nc.psum_tensor(name, list(shape), dtype))

    def op(self, eng, fn, reads=(), writes=()):
        self.ops.append((eng, fn, tuple(reads), tuple(writes)))

    def barrier(self):
        self.ops.append(("barrier", None, (), ()))

    def phase_begin(self):
        self._saved_es = self.es
        self.es = ExitStack()

    def phase_end(self):
        self.barrier()
        self.es.close()
        self.es = self._saved_es

    def _engine(self, eng):
        nc = self.nc
        return {"pe": nc.tensor, "act": nc.scalar, "dve": nc.vector, "pool": nc.gpsimd,
                "q_sp": nc.sync, "q_act": nc.scalar, "q_pool": nc.gpsimd}[eng]

    def _seq(self, stream):
        nc = self.nc
        return {"pe": nc.tensor, "act": nc.scalar, "dve": nc.vector, "pool": nc.gpsimd,
                "sp": nc.sync}[stream]

    @staticmethod
    def _stream(eng):
        return {"q_sp": "sp", "q_act": "act", "q_pool": "pool"}.get(eng, eng)

    def emit(self, final_keys=()):
        nc = self.nc
        ops = self.ops
        n = len(ops)
        last_w = {}
        rd_eng = {}
        rd_dma = {}
        deps = [None] * n
        needed = [False] * n
        bar_deps = {}
        last_on = {}
        dma_since = []
        for i, (eng, fn, reads, writes) in enumerate(ops):
            if eng == "barrier":
                bd = list(last_on.values()) + dma_since
                bar_deps[i] = bd
                for j in bd:
                    needed[j] = True
                deps[i] = []
                last_w, rd_eng, rd_dma = {}, {}, {}
                dma_since = []
                continue
            if eng.startswith("q_"):
                dma_since.append(i)
            else:
                last_on[eng] = i
            isdma_i = eng.startswith("q_")
            d = set()
            raw = set()
            for k in reads:
                w = last_w.get(k)
                if w is not None:
                    d.add(w)
                    raw.add(w)
            for k in writes:
                w = last_w.get(k)
                if w is not None:
                    d.add(w)
                for r in rd_eng.get(k, {}).values():
                    d.add(r)
                for r in rd_dma.get(k, ()):
                    d.add(r)
            d.discard(i)
            dd = []
            for j in d:
                ej = ops[j][0]
                if (not isdma_i) and ej == eng:
                    if eng == "pe":
                        continue
                    if j not in raw:
                        continue
                dd.append(j)
            deps[i] = dd
            for j in dd:
                needed[j] = True
            for k in reads:
                if isdma_i:
                    rd_dma.setdefault(k, []).append(i)
                else:
                    rd_eng.setdefault(k, {})[eng] = i
            for k in writes:
                last_w[k] = i
                rd_eng[k] = {}
                rd_dma[k] = []
        fin = [last_w[k] for k in final_keys if k in last_w]
        for j in fin:
            needed[j] = True

        cnt = {e: 0 for e in COMPUTE}
        sig = [None] * n
        dma_cnt = [0] * NDMASEM
        ndma = 0
        nsw = 0
        NHW = 48
        for i, (eng, fn, reads, writes) in enumerate(ops):
            if not needed[i] or eng == "barrier":
                continue
            if eng.startswith("q_"):
                if eng == "q_pool":
                    s = NHW + nsw % (NDMASEM - NHW)
                    nsw += 1
                else:
                    s = ndma % NHW
                    ndma += 1
                dma_cnt[s] += 1
                sig[i] = ("dma", s, dma_cnt[s] * 16)
            else:
                cnt[eng] += 1
                sig[i] = ("eng", eng, cnt[eng])
        sems = {}
        for e in COMPUTE:
            ne = max(1, (cnt[e] + EPOCH - 1) // EPOCH)
            sems[e] = [self.es.enter_context(nc.semaphore(f"s_{e}{k}")) for k in range(ne)]
        dsems = [self.es.enter_context(nc.semaphore(f"s_dma{k}")) for k in range(NDMASEM)]
        assert max(dma_cnt + [0]) * 16 < 60000, dma_cnt
        self.stats = dict(cnt=dict(cnt), ndma=ndma, nops=n)
        known = {}

        def do_wait(stream, s):
            if s[0] == "dma":
                key = ("dma", s[1])
                val = s[2]
                if known.get((stream, key), 0) >= val:
                    return
                known[(stream, key)] = val
                self._seq(stream).wait_ge(dsems[s[1]], val)
            else:
                e, idx = s[1], s[2]
                key = ("eng", e)
                if known.get((stream, key), 0) >= idx:
                    return
                known[(stream, key)] = idx
                ep = (idx - 1) // EPOCH
                self._seq(stream).wait_ge(sems[e][ep], idx - ep * EPOCH)

        for i, (eng, fn, reads, writes) in enumerate(ops):
            if eng == "barrier":
                for stream in ("pe", "act", "dve", "pool", "sp"):
                    for j in bar_deps[i]:
                        if sig[j][0] == "eng" and sig[j][1] == stream:
                            continue
                        do_wait(stream, sig[j])
                continue
            stream = self._stream(eng)
            for j in sorted(deps[i]):
                do_wait(stream, sig[j])
            if needed[i] and sig[i][0] == "dma" and sig[i][2] > 16:
                do_wait(stream, ("dma", sig[i][1], sig[i][2] - 16))
            ins = fn(self._engine(eng))
            if needed[i]:
                s = sig[i]
                if s[0] == "dma":
                    ins.then_inc(dsems[s[1]], 16)
                else:
                    ep = (s[2] - 1) // EPOCH
                    ins.then_inc(sems[s[1]][ep], 1)
        for j in fin:
            do_wait("sp", sig[j])

    def close(self):
        self.es.close()


class Rot:
    def __init__(self, P, n, shape, dtype, name, psum=False):
        self.bufs = [(P.ps if psum else P.sb)(shape, dtype, f"{name}{i}") for i in range(n)]
        self.keys = [(name, i) for i in range(n)]
        self.i = -1

    def next(self):
        self.i = (self.i + 1) % len(self.bufs)
        return self.bufs[self.i], self.keys[self.i]


class Builder:
    def __init__(self, T_lat, depth, kinds=None):
        self.TL = T_lat
        self.T = TC + T_lat
        self.depth = depth
        self.kinds = kinds if kinds is not None else [i % 3 for i in range(depth)]
        self.nc = bass.Bass("TRN2", target_bir_lowering=False)
        self.P = Prog(self.nc)
        self.sts = [(0, TC)] + [(TC + 512 * i, 512) for i in range(T_lat // 512)]
        self.din = {}

    def mm(self, out, lhsT, rhs, start, stop, r, w, sgc=False):
        self.P.op("pe", lambda e: e.matmul(out, lhsT=lhsT, rhs=rhs, start=start, stop=stop, skip_group_check=sgc), r, w)

    def tr(self, out, in_, ident, r, w):
        self.P.op("pe", lambda e: e.transpose(out, in_, ident), r, w)

    def act(self, out, in_, func, r, w, bias=None, scale=None, accum=None):
        kw = {}
        if bias is not None:
            kw["bias"] = bias
        if scale is not None:
            kw["scale"] = scale
        if accum is not None:
            kw["accum_out"] = accum
        self.P.op("act", lambda e: e.activation(out=out, in_=in_, func=func, **kw), r, w)

    def tt(self, eng, out, a, b, op, r, w):
        self.P.op(eng, lambda e: e.tensor_tensor(out=out, in0=a, in1=b, op=op), r, w)

    def ts(self, eng, out, a, s1, s2, op0, op1, r, w):
        if s2 is None:
            self.P.op(eng, lambda e: e.tensor_scalar(out=out, in0=a, scalar1=s1, scalar2=None, op0=op0), r, w)
        else:
            self.P.op(eng, lambda e: e.tensor_scalar(out=out, in0=a, scalar1=s1, scalar2=s2, op0=op0, op1=op1), r, w)

    def stt(self, eng, out, in0, scalar, in1, op0, op1, r, w):
        self.P.op(eng, lambda e: e.scalar_tensor_tensor(out=out, in0=in0, scalar=scalar, in1=in1, op0=op0, op1=op1), r, w)

    def cp(self, eng, out, in_, r, w):
        if eng == "act":
            self.P.op("act", lambda e: e.copy(out, in_), r, w)
        else:
            self.P.op(eng, lambda e: e.tensor_copy(out, in_), r, w)

    def dma(self, q, out, in_, r, w, slow=False):
        if slow:
            self.P.op(q, lambda e: e.dma_start(out=out, in_=in_, allow_slow_non_contiguous=True), r, w)
        else:
            self.P.op(q, lambda e: e.dma_start(out=out, in_=in_), r, w)

    def scan(self, out, d0, d1, init, op0, op1, r, w):
        self.P.op("dve", lambda e: e.tensor_tensor_scan(out=out, data0=d0, data1=d1, initial=init, op0=op0, op1=op1), r, w)

    def memset(self, eng, ap, val, w):
        self.P.op(eng, lambda e: e.memset(ap, val), (), w)

    def inp(self, name, shape, dtype=F32):
        t = self.nc.dram_tensor(name, list(shape), dtype, kind="ExternalInput")
        self.din[name] = t
        return t

    def scratch(self, name, shape, dtype):
        return self.nc.dram_tensor(name, list(shape), dtype, kind="Internal")

    def setup(self):
        P = self.P
        TL, T = self.TL, self.T
        self.x = self.inp("x", [TL, D])
        self.ctx = self.inp("ctx", [TC, D])
        self.cT = self.inp("cT", [128, KC, 2])
        self.ada_w = self.inp("ada_w", [4, D, 6 * D])
        self.ada_b = self.inp("ada_b", [4, 6 * D])
        self.norm1_g = self.inp("norm1_g", [4, D])
        self.norm2_g = self.inp("norm2_g", [4, D])
        self.mlp_w1 = self.inp("mlp_w1", [4, D, DFF])
        self.mlp_w2 = self.inp("mlp_w2", [4, DFF, D])
        self.final_g = self.inp("final_g", [1, D])
        self.identb = self.inp("identb", [128, 128], BF16)
        self.out = self.nc.dram_tensor("out", [TL, D], F32, kind="ExternalOutput")
        self.hres = self.scratch("hres", [T, D], F32)
        self.modv = self.scratch("modv", [4, 2, 6 * D], F32)
        self.U = self.scratch("U", [32, 128, T], BF16)
        ng = max(1, self.kinds.count(0))
        self.gla_w_in = self.inp("gla_w_in", [ng, D, 3072])
        self.gla_wa1 = self.inp("gla_wa1", [ng, D, 32])
        self.gla_wa2 = self.inp("gla_wa2", [ng, 32, 1024])
        self.gla_ba = self.inp("gla_ba", [ng, 128, 8])
        self.gla_gh = self.inp("gla_gh", [ng, 128, 8])
        self.gla_w_o = self.inp("gla_w_o", [ng, D, D])
        self.glacf = self.inp("glacf", [128, 768])
        self.onesb = self.inp("onesb", [128, 128], BF16)
        self.QD = self.scratch("QD", [2, 4, 128, T], BF16)
        self.KD = self.scratch("KD", [2, 4, 128, T], BF16)
        self.KW_tm = self.scratch("KW_tm", [2, T, 512], BF16)
        self.V_tm = self.scratch("V_tm", [T, D], BF16)
        self.SR = self.scratch("SR", [8, 128, T], BF16)
        self.DEC = self.scratch("DEC", [2, 4, 128, T // 64], F32)
        self.OO = self.scratch("OO", [2, 8, 128, T], F32)
        nm_ = max(1, self.kinds.count(1))
        self.ml_w_up = self.inp("ml_w_up", [nm_, D, 4096])
        self.ml_bd = self.inp("ml_bd", [nm_, 3, 16, 128, 128])
        self.ml_wgI = self.inp("ml_wgI", [nm_, 128, 48, 64])
        self.ml_wgF = self.inp("ml_wgF", [nm_, 128, 48, 64])
        self.ml_convw = self.inp("ml_convw", [nm_, 128, 16, 4])
        self.ml_convb = self.inp("ml_convb", [nm_, 128, 16])
        self.ml_bgI = self.inp("ml_bgI", [nm_, 64, 1])
        self.ml_bgF = self.inp("ml_bgF", [nm_, 64, 1])
        self.ml_gn = self.inp("ml_gn", [nm_, 128, 16])
        self.ml_skip = self.inp("ml_skip", [nm_, 128, 16])
        self.ml_w_down = self.inp("ml_w_down", [nm_, 2048, D])
        self.ml_sel = self.inp("ml_sel", [64, 8, 128])
        self.identf_d = self.inp("identf", [128, 128])
        self.XM = self.scratch("XM", [16, 128, T], BF16)
        self.SZ = self.scratch("SZ", [16, 128, T], BF16)
        self.XC = self.scratch("XC", [16, 128, T], BF16)
        self.KT = self.scratch("KT", [16, 128, T], BF16)
        self.QB = self.scratch("QB", [2, 16, 128, T], BF16)
        self.VX = self.scratch("VX", [T, 2560], BF16)
        self.KWm = self.scratch("KWm", [2, T, 2048], BF16)
        self.CWT = self.scratch("CWT", [T, 128], F32)
        self.CARB = self.scratch("CARB", [2, 4, 128, T // 64], F32)
        self.HT = self.scratch("HT", [2, 16, 128, T], F32)
        self.A_tm = self.scratch("A_tm", [T, D], BF16)
        self.CP_tm = self.scratch("CP_tm", [T, D], BF16)
        npool = max(1, self.kinds.count(2))
        self.pool_w = self.inp("pool_w", [npool, 4, 256, 256])
        self.pool_b = self.inp("pool_b", [npool, D])
        self.pool_scale = self.inp("pool_scale", [npool, D])
        self.poolcb = self.inp("poolcb", [128, 36, 128], BF16)
        self.poolcf = self.inp("poolcf", [128, 16, 128])
        self.h_in_src = True

    def hrow(self, j):
        return self.hres[j * 128:(j + 1) * 128, :]

    def src_row(self, j):
        if j < 2:
            return self.ctx[j * 128:(j + 1) * 128, :]
        return self.x[(j - 2) * 128:(j - 1) * 128, :]

    def load_h(self, h, hk, j):
        if self.h_in_src:
            self.dma("q_sp", h[:], self.src_row(j), [], [hk])
        else:
            self.dma("q_sp", h[:], self.hrow(j), [("hres", j)], [hk])

    def load_ident(self):
        self.ident = self.P.sb([128, 128], BF16, "ident")
        self.dma("q_sp", self.ident[:], self.identb[:], (), ["ident"])

    def ada_phase(self):
        P = self.P
        P.phase_begin()
        cs = P.sb([128, KC, 2], F32, "cs")
        cin = P.sb([128, KC, 2], F32, "cin")
        psm = Rot(P, 2, [128, 512], F32, "psm", psum=True)
        self.dma("q_sp", cin[:], self.cT[:], (), ["cin"])
        self.act(cs[:], cin[:], AF.Silu, ["cin"], ["cs"])
        wst = Rot(P, 2, [128, KC, 512], F32, "adaw")
        bsb = P.sb([2, 6 * D], F32, "adab")
        msb = Rot(P, 1, [2, 6 * D], F32, "adam")
        for l in range(self.depth):
            self.dma("q_sp", bsb[:], self.ada_b[l:l + 1, :].partition_broadcast(2), (), ["adab"])
            mt, mk = msb.next()
            for n in range(12):
                wt, wk = wst.next()
                self.dma("q_sp", wt[:], self.ada_w[l, :, n * 512:(n + 1) * 512].rearrange("(k p) n -> p k n", p=128), (), [wk])
                pt, pk = psm.next()
                for k in range(KC):
                    self.mm(pt[0:2, :], cs[:, k, :], wt[:, k, :], k == 0, k == KC - 1, [wk, "cs"], [pk])
                self.tt("dve", mt[:, n * 512:(n + 1) * 512], pt[0:2, :], bsb[:, n * 512:(n + 1) * 512], ALU.add, [pk, "adab"], [(mk, n)])
            self.dma("q_sp", self.modv[l], mt[:], [(mk, n) for n in range(12)], [("modv", l)])
        P.phase_end()

    def alloc_norm(self, need_aT=True):
        P = self.P
        self.load_ident()
        self.hbuf = Rot(P, 4, [128, D], F32, "hbuf")
        self.t1buf = Rot(P, 2, [128, D], F32, "t1buf")
        self.ssb = Rot(P, 4, [128, 2], F32, "ssb")
        self.thalf = Rot(P, 3, [128, 512], F32, "thalf")
        self.modt = {nm: P.sb([128, D], F32, nm) for nm in ("gs", "sh", "gt")}
        self.abuf = Rot(P, 2, [128, D], BF16, "abuf")
        if need_aT:
            self.aT = Rot(P, 2, [128, KC, 512], BF16, "aT")
            self.ptr = Rot(P, 2, [128, 1024], BF16, "ptr", psum=True)

    def load_mod(self, l, which, norm_g, row):
        m = self.modt
        base = 3 * which
        mv = self.modv[l, row:row + 1, :]
        tmps, tmpsk = self.t1buf.next()
        tmpg, tmpgk = self.t1buf.next()
        self.dma("q_sp", m["sh"][:], mv[:, (base + 0) * D:(base + 1) * D].partition_broadcast(128), [], ["sh"])
        self.dma("q_sp", tmps[:], mv[:, (base + 1) * D:(base + 2) * D].partition_broadcast(128), [], [tmpsk])
        self.dma("q_sp", m["gt"][:], mv[:, (base + 2) * D:(base + 3) * D].partition_broadcast(128), [], ["gt"])
        if norm_g is not None:
            self.dma("q_sp", tmpg[:], norm_g[l:l + 1, :].partition_broadcast(128), (), [tmpgk])
            self.stt("dve", m["gs"][:], tmps[:], 1.0, tmpg[:], ALU.add, ALU.mult, [tmpsk, tmpgk], ["gs"])

    def rstd(self, ss, ssk, n=D):
        self.act(ss[:, 1:2], ss[:, 0:1], AF.Sqrt, [ssk], [ssk], scale=1.0 / n, bias=EPS)
        self.P.op("dve", lambda e: e.reciprocal(ss[:, 1:2], ss[:, 1:2]), [ssk], [ssk])

    def norm_tile(self, j):
        m = self.modt
        h, hk = self.hbuf.next()
        self.load_h(h, hk, j)
        t1, t1k = self.t1buf.next()
        ss, ssk = self.ssb.next()
        self.act(t1[:], h[:], AF.Square, [hk], [t1k, ssk], accum=ss[:, 0:1])
        self.rstd(ss, ssk)
        self.stt("dve", t1[:], h[:], ss[:, 1:2], m["gs"][:], ALU.mult, ALU.mult, [hk, ssk, "gs", t1k], [t1k])
        a, ak = self.abuf.next()
        self.tt("pool", a[:], t1[:], m["sh"][:], ALU.add, [t1k, "sh"], [ak])
        return a, ak

    def norm_stage(self, si):
        s0, NT = self.sts[si]
        aT, aTk = self.aT.next()
        for jj in range(NT // 128):
            j = s0 // 128 + jj
            a, ak = self.norm_tile(j)
            pt, ptk = self.ptr.next()
            for k in range(KC):
                self.tr(pt[:, k * 128:(k + 1) * 128], a[:, k * 128:(k + 1) * 128], self.ident[:], [ak, "ident"], [ptk])
            self.cp("act", aT[:, :, jj * 128:(jj + 1) * 128], pt[:].rearrange("p (k t) -> p k t", k=KC), [ptk], [(aTk, jj)])
        return aT, aTk

    def load_w(self, dst_view, src_view, key, ncols_piece=2048):
        K = dst_view.shape[1]
        N = dst_view.shape[2]
        for k in range(K):
            for c in range(0, N, ncols_piece):
                ce = min(N, c + ncols_piece)
                self.dma("q_pool", dst_view[:, k, c:ce], src_view[:, k, c:ce], (), [(key, k, c // ncols_piece)])

    def sis(self, skip_ctx):
        return [si for si in range(len(self.sts)) if not (skip_ctx and si == 0)]

    def pipelined(self, sis, stage_a, stage_b, l, which, norm_g):
        prev = None
        cur_row = None
        for si in sis:
            row = 1 if si == 0 else 0
            if row != cur_row:
                if prev is not None:
                    stage_b(*prev)
                    prev = None
                self.load_mod(l, which, norm_g, row)
                cur_row = row
            a = stage_a(si)
            if prev is not None:
                stage_b(*prev)
            prev = (si,) + tuple(a)
        if prev is not None:
            stage_b(*prev)

    def mlp1_phase(self, l, skip_ctx=False):
        P = self.P
        P.phase_begin()
        wb = P.sb([128, KC * DFF], BF16, "w1")
        wkey = "w1"
        w1 = wb[:, :].rearrange("p (k n) -> p k n", k=KC)
        self.load_w(w1, self.mlp_w1[l].rearrange("(k p) n -> p k n", p=128), wkey)
        self.alloc_norm()
        pacc = Rot(P, 4, [128, 512], F32, "pacc", psum=True)
        rbuf = Rot(P, 3, [128, 512], F32, "rbuf")
        ubuf = Rot(P, 2, [128, 4, 512], BF16, "ubuf")

        def stage_b(si, aT, aTk):
            s0, NT = self.sts[si]
            nj = NT // 128
            for fo4 in range(8):
                ut, uk = ubuf.next()
                for q in range(4):
                    fo = fo4 * 4 + q
                    pa, pk = pacc.next()
                    for k in range(KC):
                        self.mm(pa[:, :NT], w1[:, k, fo * 128:(fo + 1) * 128], aT[:, k, :NT], k == 0, k == KC - 1,
                                [(wkey, k, fo // 16)] + [(aTk, jj) for jj in range(nj)], [pk])
                    rt, rk = rbuf.next()
                    self.act(rt[:, :NT], pa[:, :NT], AF.Relu, [pk], [rk])
                    self.tt("dve", ut[:, q, :NT], rt[:, :NT], rt[:, :NT], ALU.mult, [rk], [(uk, q)])
                self.dma("q_sp", self.U[fo4 * 4:(fo4 + 1) * 4, :, s0:s0 + NT].rearrange("f p t -> p f t"), ut[:, :, :NT],
                         [(uk, q) for q in range(4)], [("U", si, fo4)])

        self.pipelined(self.sis(skip_ctx), self.norm_stage, stage_b, l, 1, self.norm2_g)
        P.phase_end()

    def mlp2_phase(self, l, skip_ctx=False, final=False):
        P = self.P
        P.phase_begin()
        wb = P.sb([128, 32 * D], BF16, "w2")
        wkey = "w2"
        w2 = wb[:, :].rearrange("p (k n) -> p k n", k=32)
        self.load_w(w2, self.mlp_w2[l].rearrange("(k p) n -> p k n", p=128), wkey, ncols_piece=1024)
        self.alloc_norm(need_aT=False)
        pacc = Rot(P, 4, [128, 512], F32, "pacc", psum=True)
        u2buf = Rot(P, 2, [128, 32, 512], BF16, "u2buf")
        m = self.modt
        fg = None
        if final:
            fg = P.sb([128, D], F32, "fg")
            self.dma("q_sp", fg[:], self.final_g[0:1, :].partition_broadcast(128), (), ["fg"])
        cur_row = None
        for si in self.sis(skip_ctx):
            row = 1 if si == 0 else 0
            if row != cur_row:
                self.load_mod(l, 1, None, row)
                cur_row = row
            s0, NT = self.sts[si]
            nj = NT // 128
            ut, uk = u2buf.next()
            for f8 in range(4):
                self.dma("q_sp", ut[:, f8 * 8:(f8 + 1) * 8, :NT], self.U[f8 * 8:(f8 + 1) * 8, :, s0:s0 + NT].rearrange("f p t -> p f t"),
                         [("U", si, f8 * 2), ("U", si, f8 * 2 + 1)], [(uk, f8)])
            for jj in range(nj):
                j = s0 // 128 + jj
                h, hk = self.hbuf.next()
                self.load_h(h, hk, j)
                t1, t1k = self.t1buf.next()
                for nh in range(2):
                    pa, pk = pacc.next()
                    th, thk = self.thalf.next()
                    for k in range(32):
                        self.mm(pa[:, :], ut[:, k, jj * 128:(jj + 1) * 128], w2[:, k, nh * 512:(nh + 1) * 512], k == 0, k == 31,
                                [(wkey, k, 0), (uk, k // 8)], [pk])
                    self.tt("dve", th[:, :], pa[:, :], m["gt"][:, nh * 512:(nh + 1) * 512], ALU.mult,
                            [pk, "gt"], [thk])
                    self.tt("pool", h[:, nh * 512:(nh + 1) * 512], th[:, :], h[:, nh * 512:(nh + 1) * 512], ALU.add,
                            [thk, hk], [hk])
                if not final:
                    self.dma("q_sp", self.hrow(j), h[:], [hk], [("hres", j)])
                else:
                    t2, t2k = self.t1buf.next()
                    ss, ssk = self.ssb.next()
                    self.act(t2[:], h[:], AF.Square, [hk], [t2k, ssk], accum=ss[:, 0:1])
                    self.rstd(ss, ssk)
                    self.stt("dve", t2[:], h[:], ss[:, 1:2], fg[:], ALU.mult, ALU.mult, [hk, ssk, "fg", t2k], [t2k])
                    self.dma("q_sp", self.out[(j - 2) * 128:(j - 1) * 128, :], t2[:], [t2k], [("out", j)])
        P.phase_end()
        self.h_in_src = False

    def gla_phase_p(self, l):
        P = self.P
        jg = self.kinds[:l + 1].count(0) - 1
        P.phase_begin()
        wb = P.sb([128, KC * 3072], BF16, "win")
        win = wb[:, :].rearrange("p (k n) -> p k n", k=KC)
        self.load_w(win, self.gla_w_in[jg].rearrange("(k p) n -> p k n", p=128), "win", ncols_piece=1024)
        wa1 = P.sb([128, KC, 32], BF16, "wa1")
        self.dma("q_pool", wa1[:], self.gla_wa1[jg].rearrange("(k p) n -> p k n", p=128), (), ["wa1"])
        wa2 = P.sb([32, 1024], BF16, "wa2")
        self.dma("q_pool", wa2[:], self.gla_wa2[jg], (), ["wa2"])
        nba = P.sb([128, 8], F32, "nba")
        self.dma("q_sp", nba[:], self.gla_ba[jg], (), ["nba"])
        self.ts("dve", nba[:], nba[:], -1.0, None, ALU.mult, None, ["nba"], ["nba"])
        gc = P.sb([128, 768], F32, "gc")
        self.dma("q_sp", gc[:], self.glacf[:], (), ["gc"])
        self.alloc_norm()
        pacc = Rot(P, 4, [128, 512], F32, "pacc", psum=True)
        pz = Rot(P, 2, [128, 512], F32, "pz", psum=True)
        qkb = Rot(P, 2, [128, 8, 512], F32, "qkb")
        srb = Rot(P, 2, [128, 8, 512], BF16, "srb")
        vtb = Rot(P, 2, [128, D], BF16, "vtb")
        utb = Rot(P, 2, [32, 512], BF16, "utb")
        tA = Rot(P, 3, [128, 512], F32, "tA")
        tB = Rot(P, 2, [128, 512], F32, "tB")
        tC = Rot(P, 2, [128, 512], F32, "tC")
        tD = Rot(P, 2, [128, 512], F32, "tD")
        ob = Rot(P, 4, [128, 512], BF16, "ob")
        kw4 = Rot(P, 2, [128, 4, 512], BF16, "kw4")
        kwt = Rot(P, 2, [128, 512], BF16, "kwt")
        decb = Rot(P, 2, [128, 32], F32, "decb")

        def stage_b(si, aT, aTk):
            s0, NT = self.sts[si]
            nj = NT // 128
            nch = NT // 64
            aks = [(aTk, jj) for jj in range(nj)]
            qk, qkk = qkb.next()
            for i in range(8):
                pa, pk = pacc.next()
                for k in range(KC):
                    self.mm(pa[:, :NT], win[:, k, i * 128:(i + 1) * 128], aT[:, k, :NT], k == 0, k == KC - 1, [("win", k, 0)] + aks, [pk])
                self.act(qk[:, i, :NT], pa[:, :NT], AF.Copy, [pk], [(qkk, i)], scale=(128 ** -0.5 if i < 4 else 1.0))
            sr, srk = srb.next()
            for i in range(8):
                pa, pk = pacc.next()
                for k in range(KC):
                    self.mm(pa[:, :NT], win[:, k, 2048 + i * 128:2048 + (i + 1) * 128], aT[:, k, :NT], k == 0, k == KC - 1, [("win", k, 2)] + aks, [pk])
                self.act(sr[:, i, :NT], pa[:, :NT], AF.Silu, [pk], [(srk, i)])
            self.dma("q_sp", self.SR[:, :, s0:s0 + NT].rearrange("f p t -> p f t"), sr[:, :, :NT], [(srk, i) for i in range(8)], [])
            for jj in range(nj):
                vt, vtk = vtb.next()
                for nh in range(2):
                    pa, pk = pacc.next()
                    th, thk = self.thalf.next()
                    for k in range(KC):
                        self.mm(pa[:, :], aT[:, k, jj * 128:(jj + 1) * 128], win[:, k, 1024 + nh * 512:1024 + (nh + 1) * 512], k == 0, k == KC - 1,
                                [("win", k, 1), (aTk, jj)], [pk])
                    self.cp("dve", vt[:, nh * 512:(nh + 1) * 512], pa[:, :], [pk], [(vtk, nh)])
                self.dma("q_sp", self.V_tm[s0 + jj * 128:s0 + (jj + 1) * 128, :], vt[:], [(vtk, 0), (vtk, 1)], [])
            ut, utk = utb.next()
            pu, puk = pz.next()
            for k in range(KC):
                self.mm(pu[0:32, :NT], wa1[:, k, :], aT[:, k, :NT], k == 0, k == KC - 1, ["wa1"] + aks, [puk])
            self.cp("dve", ut[:, :NT], pu[0:32, :NT], [puk], [utk])
            for d in range(2):
                kw, kwk = kw4.next()
                dec, deck = decb.next()
                for h in range(4):
                    q = qk[:, h, :NT]
                    kk = qk[:, 4 + h, :NT]
                    pzt, pzk = pz.next()
                    c0 = d * 512 + h * 128
                    self.mm(pzt[:, :NT], wa2[0:32, c0:c0 + 128], ut[0:32, :NT], True, True, ["wa2", utk], [pzk])
                    e, ek = tA.next()
                    self.act(e[:, :NT], pzt[:, :NT], AF.Exp, [pzk, "nba"], [ek], scale=-1.0, bias=nba[:, d * 4 + h:d * 4 + h + 1])
                    sp, spk = tB.next()
                    self.act(sp[:, :NT], e[:, :NT], AF.Ln, [ek], [spk], bias=1.0)
                    cs, csk = tC.next()
                    self.scan(cs[:, :NT], gc[:, :NT], sp[:, :NT], 0.0, ALU.mult, ALU.add, ["gc", spk], [csk])
                    cs3 = cs[:, :NT].rearrange("p (c t) -> p c t", t=64)
                    cl_b = cs3[:, :, 63:64].to_broadcast([128, nch, 64])
                    dd, ddk = tD.next()
                    dd3 = dd[:, :NT].rearrange("p (c t) -> p c t", t=64)
                    sp3 = sp[:, :NT].rearrange("p (c t) -> p c t", t=64)
                    if d == 0:
                        xq, xs = cs, -1.0 / 16
                        xk, xks = cs, 1.0 / 16
                        self.tt("dve", dd3, cs3, cl_b, ALU.subtract, [csk], [ddk])
                        xw, xws = dd, 1.0 / 16
                        xqk = xkk = csk
                        xwk = ddk
                    else:
                        t1, t1k_ = tD.next()
                        t13 = t1[:, :NT].rearrange("p (c t) -> p c t", t=64)
                        self.tt("dve", t13, sp3, cs3, ALU.subtract, [csk, spk], [t1k_])
                        self.tt("dve", dd3, t13, cl_b, ALU.add, [t1k_, csk], [ddk])
                        xq, xs, xqk = dd, -1.0 / 16, ddk
                        xk, xks, xkk = dd, 1.0 / 16, ddk
                        xw, xws, xwk = t1, 1.0 / 16, t1k_
                    e1, e1k = tA.next()
                    self.act(e1[:, :NT], xq[:, :NT], AF.Exp, [xqk], [e1k], scale=xs)
                    o1, o1k = ob.next()
                    self.tt("pool", o1[:, :NT], q, e1[:, :NT], ALU.mult, [(qkk, h), e1k], [o1k])
                    self.dma("q_sp", self.QD[d, h, :, s0:s0 + NT], o1[:, :NT], [o1k], [])
                    e2, e2k = tA.next()
                    self.act(e2[:, :NT], xk[:, :NT], AF.Exp, [xkk], [e2k], scale=xks)
                    o2, o2k = ob.next()
                    self.tt("pool", o2[:, :NT], kk, e2[:, :NT], ALU.mult, [(qkk, 4 + h), e2k], [o2k])
                    self.dma("q_sp", self.KD[d, h, :, s0:s0 + NT], o2[:, :NT], [o2k], [])
                    e3, e3k = tA.next()
                    self.act(e3[:, :NT], xw[:, :NT], AF.Exp, [xwk], [e3k], scale=xws)
                    self.tt("pool", kw[:, h, :NT], kk, e3[:, :NT], ALU.mult, [(qkk, 4 + h), e3k], [(kwk, h)])
                    self.act(self._decv(dec, h, nch), cs3[:, :, 63], AF.Exp, [csk], [(deck, h)], scale=-1.0 / 16)
                self.dma("q_sp", self.DEC[d, :, :, s0 // 64:s0 // 64 + nch].rearrange("h p c -> p h c"),
                         self._decall(dec, nch), [(deck, h) for h in range(4)], [])
                for jj in range(nj):
                    pt, ptk = self.ptr.next()
                    for h in range(4):
                        self.tr(pt[:, h * 128:(h + 1) * 128], kw[:, h, jj * 128:(jj + 1) * 128], self.ident[:], [(kwk, h), "ident"], [ptk])
                    kt, ktk = kwt.next()
                    self.cp("act", kt[:], pt[:, 0:512], [ptk], [ktk])
                    self.dma("q_sp", self.KW_tm[d, s0 + jj * 128:s0 + (jj + 1) * 128, :], kt[:], [ktk], [])

        self.pipelined(self.sis(False), self.norm_stage, stage_b, l, 0, self.norm1_g)
        P.phase_end()

    def _decv(self, dec, h, nch):
        return dec[:, h * 8:h * 8 + nch]

    def _decall(self, dec, nch):
        return dec[:, :].rearrange("p (h c) -> p h c", h=4)[:, :, :nch]

    def tile_order(self, d):
        sis = list(range(len(self.sts)))
        if d == 1:
            sis = [0] + sis[:0:-1]
        return sis

    def gla_phase_s(self, l, d):
        P = self.P
        P.phase_begin()
        gc = P.sb([128, 768], F32, "gc")
        self.dma("q_sp", gc[:], self.glacf[:], (), ["gc"])
        mask = gc[:, 512 + d * 128:512 + (d + 1) * 128]
        qdb = Rot(P, 2, [128, 4, 512], BF16, "qdb")
        kdb = Rot(P, 2, [128, 4, 512], BF16, "kdb")
        kwb = Rot(P, 2, [128, 4, 512], BF16, "kwb")
        vtb = Rot(P, 2, [128, 4, D], BF16, "vtb")
        decb = Rot(P, 2, [128, 4, 8], F32, "decb")
        S = P.sb([128, 4, 256], F32, "S")
        Sb = P.sb([128, 4, 256], BF16, "Sb")
        attb = Rot(P, 4, [128, 128], BF16, "attb")
        otb = Rot(P, 2, [128, 8, 512], F32, "otb")
        patt = Rot(P, 2, [128, 512], F32, "patt", psum=True)
        po = Rot(P, 3, [128, 512], F32, "po", psum=True)
        pst = Rot(P, 3, [128, 512], F32, "pst", psum=True)
        for h in range(4):
            self.memset("dve", S[:, h, :], 0.0, [("S", h)])
            self.memset("pool", Sb[:, h, :], 0.0, [("Sb", h)])
        for si in self.tile_order(d):
            s0, NT = self.sts[si]
            nj = NT // 128
            nch = NT // 64
            qd, qdk = qdb.next()
            kd, kdk = kdb.next()
            kw, kwk = kwb.next()
            vt, vtk = vtb.next()
            dec, deck = decb.next()
            ot, otk = otb.next()
            self.dma("q_sp", qd[:, :, :NT], self.QD[d, :, :, s0:s0 + NT].rearrange("h p t -> p h t"), [], [qdk])
            self.dma("q_sp", kd[:, :, :NT], self.KD[d, :, :, s0:s0 + NT].rearrange("h p t -> p h t"), [], [kdk])
            self.dma("q_sp", kw[:, :nj, :], self.KW_tm[d, s0:s0 + NT, :].rearrange("(j p) f -> p j f", p=128), [], [kwk])
            self.dma("q_sp", vt[:, :nj, :], self.V_tm[s0:s0 + NT, :].rearrange("(j p) f -> p j f", p=128), [], [vtk])
            self.dma("q_sp", dec[:, :, :nch], self.DEC[d, :, :, s0 // 64:s0 // 64 + nch].rearrange("h p c -> p h c"), [], [deck])
            jjs = list(range(nj)) if d == 0 else list(range(nj - 1, -1, -1))
            cs_ = (0, 1) if d == 0 else (1, 0)
            for jj in jjs:
                tsl = slice(jj * 128, (jj + 1) * 128)
                pos = []
                for h in range(4):
                    pa, pak = patt.next()
                    self.mm(pa[:, 0:128], kd[:, h, tsl], qd[:, h, tsl], True, True, [kdk, qdk], [pak])
                    at, atk = attb.next()
                    self.tt("dve", at[:], pa[:, 0:128], mask, ALU.mult, [pak, "gc"], [atk])
                    p_ob, pok = po.next()
                    p_o = p_ob[:, 0:256].rearrange("p (v t) -> p v t", v=2)
                    for vc in range(2):
                        self.mm(p_o[:, vc, :], vt[:, jj, h * 256 + vc * 128:h * 256 + (vc + 1) * 128], at[:], vc == 0, False, [vtk, atk], [pok], sgc=True)
                    pos.append((p_o, pok))
                    for ci, c in enumerate(cs_):
                        csl = slice(jj * 128 + c * 64, jj * 128 + (c + 1) * 64)
                        rows = slice(c * 64, (c + 1) * 64)
                        for vc in range(2):
                            self.mm(p_o[:, vc, c * 64:(c + 1) * 64], Sb[:, h, vc * 128:(vc + 1) * 128], qd[:, h, csl], False, ci == 1,
                                    [("Sb", h), qdk], [pok], sgc=True)
                        ps, psk = pst.next()
                        self.mm(ps[:, 0:256], kw[rows, jj, h * 128:(h + 1) * 128], vt[rows, jj, h * 256:(h + 1) * 256], True, True, [kwk, vtk], [psk])
                        ch = jj * 2 + c
                        self.stt("dve", S[:, h, :], S[:, h, :], dec[:, h, ch:ch + 1], ps[:, 0:256], ALU.mult, ALU.add, [("S", h), deck, psk], [("S", h)])
                        self.cp("act", Sb[:, h, :], S[:, h, :], [("S", h)], [("Sb", h)])
                    self.cp("act", ot[:, 2 * h:2 * h + 2, tsl], p_o[:, :, :], [pok], [(otk, jj, h)])
            self.dma("q_sp", self.OO[d, :, :, s0:s0 + NT].rearrange("f p t -> p f t"), ot[:, :, :NT],
                     [(otk, jj, h) for jj in range(nj) for h in range(4)], [])
        P.phase_end()

    def gla_phase_o(self, l, need_ctx):
        P = self.P
        jg = self.kinds[:l + 1].count(0) - 1
        P.phase_begin()
        wb = P.sb([128, KC * D], BF16, "wo")
        wo = wb[:, :].rearrange("p (k n) -> p k n", k=KC)
        self.load_w(wo, self.gla_w_o[jg].rearrange("(k p) n -> p k n", p=128), "wo", ncols_piece=1024)
        gh = P.sb([128, 8], F32, "gh")
        self.dma("q_sp", gh[:], self.gla_gh[jg], (), ["gh"])
        ones = P.sb([128, 128], BF16, "ones")
        self.dma("q_sp", ones[:], self.onesb[:], (), ["ones"])
        self.alloc_norm(need_aT=False)
        m = self.modt
        pacc = Rot(P, 4, [128, 512], F32, "pacc", psum=True)
        o0b = Rot(P, 2, [128, 8, 512], F32, "o0b")
        o1b = Rot(P, 1, [128, 8, 512], F32, "o1b")
        srb = Rot(P, 2, [128, 8, 512], BF16, "srb")
        sqb = Rot(P, 1, [128, 8, 512], BF16, "sqb")
        yTb = Rot(P, 2, [128, 8, 512], BF16, "yTb")
        rsb = Rot(P, 2, [128, 4, 512], F32, "rsb")
        tmpb = Rot(P, 2, [128, 512], F32, "tmpb")
        cur_row = None
        for si in self.sis(not need_ctx):
            row = 1 if si == 0 else 0
            if row != cur_row:
                self.load_mod(l, 0, None, row)
                cur_row = row
            s0, NT = self.sts[si]
            nj = NT // 128
            o0, o0k = o0b.next()
            o1, o1k = o1b.next()
            sr, srk = srb.next()
            self.dma("q_sp", o0[:, :, :NT], self.OO[0, :, :, s0:s0 + NT].rearrange("f p t -> p f t"), [], [o0k])
            self.dma("q_sp", o1[:, :, :NT], self.OO[1, :, :, s0:s0 + NT].rearrange("f p t -> p f t"), [], [o1k])
            self.dma("q_sp", sr[:, :, :NT], self.SR[:, :, s0:s0 + NT].rearrange("f p t -> p f t"), [], [srk])
            self.tt("pool", o0[:, :, :NT], o0[:, :, :NT], o1[:, :, :NT], ALU.add, [o0k, o1k], [o0k])
            sq, sqk = sqb.next()
            self.act(sq[:, :, :NT], o0[:, :, :NT], AF.Square, [o0k], [sqk])
            rs, rsk = rsb.next()
            for h in range(4):
                pa, pk = pacc.next()
                for vc in range(2):
                    self.mm(pa[:, :NT], ones[:], sq[:, 2 * h + vc, :NT], vc == 0, vc == 1, ["ones", sqk], [pk])
                self.act(rs[:, h, :NT], pa[:, :NT], AF.Sqrt, [pk], [(rsk, h)], scale=1.0 / 256, bias=EPS)
                self.P.op("dve", (lambda rs=rs, h=h, NT=NT: (lambda e: e.reciprocal(rs[:, h, :NT], rs[:, h, :NT])))(), [(rsk, h)], [(rsk, h)])
            yT, yTk = yTb.next()
            for i in range(8):
                tm, tmk = tmpb.next()
                self.stt("dve", tm[:, :NT], o0[:, i, :NT], gh[:, i:i + 1], rs[:, i // 2, :NT], ALU.mult, ALU.mult, [o0k, "gh", (rsk, i // 2)], [tmk])
                self.tt("pool", yT[:, i, :NT], tm[:, :NT], sr[:, i, :NT], ALU.mult, [tmk, srk], [(yTk, i)])
            yks = [(yTk, i) for i in range(8)]
            for jj in range(nj):
                j = s0 // 128 + jj
                h_, hk = self.hbuf.next()
                self.load_h(h_, hk, j)
                t1, t1k = self.t1buf.next()
                for nh in range(2):
                    pa, pk = pacc.next()
                    th, thk = self.thalf.next()
                    for k in range(KC):
                        self.mm(pa[:, :], yT[:, k, jj * 128:(jj + 1) * 128], wo[:, k, nh * 512:(nh + 1) * 512], k == 0, k == KC - 1,
                                [("wo", k, 0), (yTk, k)], [pk])
                    self.tt("dve", th[:, :], pa[:, :], m["gt"][:, nh * 512:(nh + 1) * 512], ALU.mult, [pk, "gt"], [thk])
                    self.tt("pool", h_[:, nh * 512:(nh + 1) * 512], th[:, :], h_[:, nh * 512:(nh + 1) * 512], ALU.add,
                            [thk, hk], [hk])
                self.dma("q_sp", self.hrow(j), h_[:], [hk], [])
        P.phase_end()
        self.h_in_src = False

    def units256(self):
        return [(s0, 256) for s0 in range(0, self.T, 256)]

    def mlstm_phase_p1(self, l):
        P = self.P
        jm = self.kinds[:l + 1].count(1) - 1
        P.phase_begin()
        wb = P.sb([128, KC * 4096], BF16, "wup")
        wup = wb[:, :].rearrange("p (k n) -> p k n", k=KC)
        self.load_w(wup, self.ml_w_up[jm].rearrange("(k p) n -> p k n", p=128), "wup")
        self.alloc_norm()
        pacc = Rot(P, 4, [128, 512], F32, "pacc", psum=True)
        obuf = Rot(P, 3, [128, 4, 512], BF16, "obuf")

        def stage_b(si, aT, aTk):
            s0, NT = self.sts[si]
            nj = NT // 128
            aks = [(aTk, jj) for jj in range(nj)]
            for i4 in range(8):
                ot, otk = obuf.next()
                for q in range(4):
                    i = i4 * 4 + q
                    pa, pk = pacc.next()
                    for k in range(KC):
                        self.mm(pa[:, :NT], wup[:, k, i * 128:(i + 1) * 128], aT[:, k, :NT], k == 0, k == KC - 1, [("wup", k, i // 16)] + aks, [pk])
                    if i < 16:
                        self.cp("act", ot[:, q, :NT], pa[:, :NT], [pk], [(otk, q)])
                    else:
                        self.act(ot[:, q, :NT], pa[:, :NT], AF.Silu, [pk], [(otk, q)])
                dst = self.XM if i4 < 4 else self.SZ
                i0 = (i4 % 4) * 4
                self.dma("q_sp", dst[i0:i0 + 4, :, s0:s0 + NT].rearrange("f p t -> p f t"), ot[:, :, :NT], [(otk, q) for q in range(4)], [])

        self.pipelined(self.sis(False), self.norm_stage, stage_b, l, 0, self.norm1_g)
        P.phase_end()

    def mlstm_phase_p2(self, l):
        P = self.P
        jm = self.kinds[:l + 1].count(1) - 1
        P.phase_begin()
        self.load_ident()
        NT = 256
        nj, nch = 2, 4
        bd = P.sb([128, 48, 128], BF16, "bd")
        for m_ in range(3):
            self.dma("q_pool", bd[:, m_ * 16:(m_ + 1) * 16, :], self.ml_bd[jm, m_].rearrange("c p n -> p c n"), (), ["bd"])
        wgI = P.sb([128, 48, 64], BF16, "wgI")
        wgF = P.sb([128, 48, 64], BF16, "wgF")
        self.dma("q_pool", wgI[:], self.ml_wgI[jm], (), ["wg"])
        self.dma("q_pool", wgF[:], self.ml_wgF[jm], (), ["wg"])
        cw = P.sb([128, 16, 4], F32, "cw")
        cbias = P.sb([128, 16], F32, "cbias")
        self.dma("q_sp", cw[:], self.ml_convw[jm], (), ["cw"])
        self.dma("q_sp", cbias[:], self.ml_convb[jm], (), ["cw"])
        bI = P.sb([64, 1], F32, "bI")
        nbF = P.sb([64, 1], F32, "nbF")
        self.dma("q_sp", bI[:], self.ml_bgI[jm], (), ["bI"])
        self.dma("q_sp", nbF[:], self.ml_bgF[jm], (), ["nbF"])
        self.ts("dve", nbF[:], nbF[:], -1.0, None, ALU.mult, None, ["nbF"], ["nbF"])
        gc = P.sb([128, 768], F32, "gc")
        self.dma("q_sp", gc[:], self.glacf[:], (), ["gc"])
        sel = P.sb([64, 8, 128], F32, "sel")
        self.dma("q_sp", sel[:], self.ml_sel[:], (), ["sel"])
        identf = P.sb([128, 128], F32, "identf")
        self.dma("q_sp", identf[:], self.identf_d[:], (), ["identf"])
        pacc = Rot(P, 2, [128, 512], F32, "pacc", psum=True)
        pgI = P.ps([128, 512], F32, "pgI")
        pgF = P.ps([128, 512], F32, "pgF")
        pb = Rot(P, 1, [128, 512], F32, "pb", psum=True)
        pcx = P.ps([128, 512], F32, "pcx")
        ptkv = P.ps([128, 2048], BF16, "ptkv")
        xwb = Rot(P, 2, [128, 16, NT + 32], BF16, "xwb")
        xcb = Rot(P, 2, [128, 16, NT], BF16, "xcb")
        qkvb = Rot(P, 1, [128, 48, NT], BF16, "qkvb")
        qsb = Rot(P, 1, [128, 16, NT], BF16, "qsb")
        qbb = Rot(P, 3, [128, 4, NT], BF16, "qbb")
        accb = Rot(P, 4, [128, NT], F32, "accb")
        gt_ = {nm: P.sb([64, NT], F32, "g" + nm) for nm in ("LI", "E", "SP", "CS", "BN", "EB", "T1", "COL", "T2", "CW")}
        car = P.sb([64, 8], F32, "car")
        carb = Rot(P, 2, [128, 8, nch], F32, "carb")
        cwtb = Rot(P, 2, [128, 128], F32, "cwtb")
        vxb = Rot(P, 2, [128, 4, 640], BF16, "vxb")
        for i in range(2):
            self.memset("pool", vxb.bufs[i][:, :, 512:640], 1.0, [(vxb.keys[i], "ones")])
        kwb = Rot(P, 2, [128, 2048], BF16, "kwb")
        s_q = 512.0 ** -0.5
        nunits = self.T // 256
        for u in range(nunits):
            s0 = u * 256
            seq_lo, seq_hi = (0, TC) if u == 0 else (TC, self.T)
            xw, xwk = xwb.next()
            lo = max(s0 - 2, seq_lo)
            hi = min(s0 + NT + 1, seq_hi)
            if lo > s0 - 2:
                self.memset("pool", xw[:, :, 14:16], 0.0, [(xwk, "L")])
            if hi < s0 + NT + 1:
                self.memset("pool", xw[:, :, NT + 16:NT + 17], 0.0, [(xwk, "R")])
            self.dma("q_sp", xw[:, :, 14 + lo - (s0 - 2):14 + hi - (s0 - 2)], self.XM[:, :, lo:hi].rearrange("c p t -> p c t"), [],
                     [(xwk, "L"), (xwk, "M"), (xwk, "R")])
            xwks = [(xwk, "L"), (xwk, "M"), (xwk, "R")]
            xc, xck = xcb.next()
            for c in range(16):
                eng = "dve"
                ac, ack = accb.next()
                self.ts(eng, ac[:, :], xw[:, c, 14:14 + NT], cw[:, c, 0:1], None, ALU.mult, None, xwks + ["cw"], [ack])
                for j in range(1, 4):
                    self.stt(eng, ac[:, :], xw[:, c, 14 + j:14 + NT + j], cw[:, c, j:j + 1], ac[:, :], ALU.mult, ALU.add, xwks + ["cw", ack], [ack])
                self.act(xc[:, c, :], ac[:, :], AF.Silu, [ack, "cw"], [(xck, c)], bias=cbias[:, c:c + 1])
            xcks = [(xck, c) for c in range(16)]
            self.dma("q_sp", self.XC[:, :, s0:s0 + NT].rearrange("c p t -> p c t"), xc[:, :, :], xcks, [])
            qkv, qkvk = qkvb.next()
            qs, qsk = qsb.next()
            for m_ in range(3):
                for c in range(16):
                    pa, pk = pacc.next()
                    if m_ < 2:
                        self.mm(pa[:, :NT], bd[:, m_ * 16 + c, :], xc[:, c, :], True, True, ["bd", (xck, c)], [pk])
                    else:
                        self.mm(pa[:, :NT], bd[:, m_ * 16 + c, :], xw[:, c, 16:NT + 16], True, True, ["bd"] + xwks, [pk])
                    self.cp("act", qkv[:, m_ * 16 + c, :], pa[:, :NT], [pk], [(qkvk, m_ * 16 + c)])
                    if m_ == 0:
                        self.ts("pool", qs[:, c, :], qkv[:, c, :], s_q, None, ALU.mult, None, [(qkvk, c)], [(qsk, c)])
            self.dma("q_sp", self.KT[:, :, s0:s0 + NT].rearrange("c p t -> p c t"), qkv[:, 16:32, :], [(qkvk, 16 + c) for c in range(16)], [])
            for c in range(48):
                self.mm(pgI[0:64, :NT], wgI[:, c, :], qkv[:, c, :], c == 0, c == 47, ["wg", (qkvk, c)], ["pgI"])
            for c in range(48):
                self.mm(pgF[0:64, :NT], wgF[:, c, :], qkv[:, c, :], c == 0, c == 47, ["wg", (qkvk, c)], ["pgF"])
            g = gt_
            self.act(g["LI"][:], pgI[0:64, :NT], AF.Identity, ["pgI", "bI"], ["LI"], bias=bI[:, 0:1])
            self.act(g["E"][:], pgF[0:64, :NT], AF.Exp, ["pgF", "nbF"], ["E"], scale=-1.0, bias=nbF[:, 0:1])
            self.act(g["SP"][:], g["E"][:], AF.Ln, ["E"], ["SP"], bias=1.0)
            self.scan(g["CS"][:], gc[0:64, :NT], g["SP"][:], 0.0, ALU.mult, ALU.add, ["gc", "SP"], ["CS"])
            cs3 = g["CS"][:].rearrange("p (c t) -> p c t", t=64)
            self.cp("act", g["BN"][0:32, :], g["CS"][0:32, :], ["CS"], [("BN", 0)])
            self.tt("dve", g["BN"][32:64, :], g["SP"][32:64, :], g["CS"][32:64, :], ALU.subtract, ["SP", "CS"], [("BN", 1)])
            bn3 = g["BN"][:].rearrange("p (c t) -> p c t", t=64)
            self.tt("dve", bn3[32:64], bn3[32:64], cs3[32:64, :, 63:64].to_broadcast([32, nch, 64]), ALU.add, [("BN", 1), "CS"], [("BN", 1)])
            bnk = [("BN", 0), ("BN", 1)]
            self.act(g["EB"][:], g["BN"][:], AF.Exp, bnk, ["EB"], scale=-1.0)
            self.tt("dve", g["T1"][:], g["LI"][:], g["BN"][:], ALU.add, ["LI"] + bnk, ["T1"])
            self.act(g["COL"][:], g["T1"][:], AF.Exp, ["T1"], ["COL"])
            t13 = g["T1"][:].rearrange("p (c t) -> p c t", t=64)
            t23 = g["T2"][:].rearrange("p (c t) -> p c t", t=64)
            self.tt("dve", t23, t13, cs3[:, :, 63:64].to_broadcast([64, nch, 64]), ALU.subtract, ["T1", "CS"], ["T2"])
            self.act(g["CW"][:], g["T2"][:], AF.Exp, ["T2"], ["CW"])
            self.act(car[:, 0:nch], cs3[:, :, 63], AF.Exp, ["CS"], ["car"], scale=-1.0)
            for r8 in range(8):
                d, h = r8 // 4, r8 % 4
                pbt, pbk = pb.next()
                self.mm(pbt[:, :NT], sel[:, r8, :], g["EB"][:, :], True, True, ["sel", "EB"], [pbk])
                qb, qbk = qbb.next()
                self.tt("dve", qb[:, :, :], qs[:, h * 4:(h + 1) * 4, :], pbt[:, :NT].unsqueeze(1).to_broadcast([128, 4, NT]), ALU.mult,
                        [pbk] + [(qsk, h * 4 + i) for i in range(4)], [qbk])
                self.dma("q_sp", self.QB[d, h * 4:(h + 1) * 4, :, s0:s0 + NT].rearrange("c p t -> p c t"), qb[:, :, :], [qbk], [])
            for r8 in range(8):
                self.mm(pcx[:, 256 + r8 * nch:256 + (r8 + 1) * nch], sel[:, r8, :], car[:, 0:nch], r8 == 0, r8 == 7, ["sel", "car"], ["pcx"], sgc=True)
            cb_, cbk = carb.next()
            self.cp("dve", cb_[:, :, :], pcx[:, 256:256 + 8 * nch].rearrange("p (r c) -> p r c", c=nch), ["pcx"], [cbk])
            for d in range(2):
                self.dma("q_sp", self.CARB[d, :, :, s0 // 64:s0 // 64 + nch].rearrange("h p c -> p h c"), cb_[:, d * 4:(d + 1) * 4, :], [cbk], [])
            for jj in range(nj):
                tsl = slice(jj * 128, (jj + 1) * 128)
                self.tr(pcx[:, 0:64], g["COL"][:, tsl], identf[0:64, 0:64], ["COL", "identf"], ["pcx"])
                self.tr(pcx[:, 64:128], g["CW"][:, tsl], identf[0:64, 0:64], ["CW", "identf"], ["pcx"])
                ct, ctk = cwtb.next()
                self.cp("dve", ct[:, :], pcx[:, 0:128], ["pcx"], [ctk])
                self.dma("q_sp", self.CWT[s0 + jj * 128:s0 + (jj + 1) * 128, :], ct[:, :], [ctk], [])
                for c in range(16):
                    self.tr(ptkv[:, c * 128:(c + 1) * 128], qkv[:, 32 + c, tsl], self.ident[:], [(qkvk, 32 + c), "ident"], ["ptkv"])
                vx, vxk = vxb.next()
                self.cp("act", vx[:, :, 0:512], ptkv[:, :].rearrange("p (h v) -> p h v", h=4), ["ptkv"], [(vxk, "v")])
                self.dma("q_sp", self.VX[s0 + jj * 128:s0 + (jj + 1) * 128, :], vx[:, :, :].rearrange("p h v -> p (h v)"), [(vxk, "v"), (vxk, "ones")], [])
                for c in range(16):
                    self.tr(ptkv[:, c * 128:(c + 1) * 128], qkv[:, 16 + c, tsl], self.ident[:], [(qkvk, 16 + c), "ident"], ["ptkv"])
                for d in range(2):
                    kw, kwk = kwb.next()
                    for h in range(4):
                        col = ct[:, 64 + 32 * d + h:64 + 32 * d + h + 1]
                        if h < 2:
                            self.act(kw[:, h * 512:(h + 1) * 512], ptkv[:, h * 512:(h + 1) * 512], AF.Copy, ["ptkv", ctk], [(kwk, h)], scale=col)
                        else:
                            self.ts("dve", kw[:, h * 512:(h + 1) * 512], ptkv[:, h * 512:(h + 1) * 512], col, None, ALU.mult, None, ["ptkv", ctk], [(kwk, h)])
                    self.dma("q_sp", self.KWm[d, s0 + jj * 128:s0 + (jj + 1) * 128, :], kw[:, :], [(kwk, h) for h in range(4)], [])
        P.phase_end()

    def mlstm_phase_s(self, l, d):
        P = self.P
        P.phase_begin()
        NT, nj, nch = 256, 2, 4
        gc = P.sb([128, 768], F32, "gc")
        self.dma("q_sp", gc[:], self.glacf[:], (), ["gc"])
        mask = gc[:, 512 + d * 128:512 + (d + 1) * 128]
        qbb = Rot(P, 2, [128, 16, NT], BF16, "qbb")
        ktb = Rot(P, 2, [128, 16, NT], BF16, "ktb")
        vxb = Rot(P, 2, [128, nj, 2560], BF16, "vxb")
        kwb = Rot(P, 2, [128, nj, 2048], BF16, "kwb")
        cwb = Rot(P, 2, [128, nj, 128], F32, "cwb")
        crb = Rot(P, 2, [128, 4, nch], F32, "crb")
        C = P.sb([128, 16, 640], F32, "C")
        Cb = P.sb([128, 16, 640], BF16, "Cb")
        wtb = Rot(P, 3, [128, 128], BF16, "wtb")
        rdb = Rot(P, 2, [128, 128], F32, "rdb")
        htb = Rot(P, 2, [128, 16, NT], F32, "htb")
        pqk = Rot(P, 2, [128, 512], F32, "pqk", psum=True)
        pn = Rot(P, 2, [128, 1024], F32, "pn", psum=True)
        pst = Rot(P, 1, [128, 1024], F32, "pst", psum=True)
        for i in range(16):
            self.memset("dve", C[:, i, :], 0.0, [("C", i)])
            self.memset("pool", Cb[:, i, :], 0.0, [("Cb", i)])
        nunits = self.T // 256
        order = list(range(nunits)) if d == 0 else [0] + list(range(nunits - 1, 0, -1))
        for u in order:
            s0 = u * 256
            qb, qbk = qbb.next()
            kt, ktk = ktb.next()
            vx, vxk = vxb.next()
            kw, kwk = kwb.next()
            cw, cwk = cwb.next()
            cr, crk = crb.next()
            ht, htk = htb.next()
            self.dma("q_sp", qb[:, :, :], self.QB[d, :, :, s0:s0 + NT].rearrange("c p t -> p c t"), [], [qbk])
            self.dma("q_sp", kt[:, :, :], self.KT[:, :, s0:s0 + NT].rearrange("c p t -> p c t"), [], [ktk])
            self.dma("q_sp", vx[:, :, :], self.VX[s0:s0 + NT, :].rearrange("(j p) f -> p j f", p=128), [], [vxk])
            self.dma("q_sp", kw[:, :, :], self.KWm[d, s0:s0 + NT, :].rearrange("(j p) f -> p j f", p=128), [], [kwk])
            self.dma("q_sp", cw[:, :, :], self.CWT[s0:s0 + NT, :].rearrange("(j p) f -> p j f", p=128), [], [cwk])
            self.dma("q_sp", cr[:, :, :], self.CARB[d, :, :, s0 // 64:s0 // 64 + nch].rearrange("h p c -> p h c"), [], [crk])
            jjs = list(range(nj)) if d == 0 else list(range(nj - 1, -1, -1))
            cs_ = (0, 1) if d == 0 else (1, 0)
            for jj in jjs:
                tsl = slice(jj * 128, (jj + 1) * 128)
                pns = {}

                def step_A(h):
                    pq, pqk_ = pqk.next()
                    for dc in range(4):
                        self.mm(pq[:, 0:128], kt[:, h * 4 + dc, tsl], qb[:, h * 4 + dc, tsl], dc == 0, dc == 3, [ktk, qbk], [pqk_])
                    wt, wtk = wtb.next()
                    self.stt("dve", wt[:, :], pq[:, 0:128], cw[:, jj, 32 * d + h:32 * d + h + 1], mask, ALU.mult, ALU.mult, [pqk_, cwk, "gc"], [wtk])
                    pnt, pnk = pn.next()
                    pns[h] = (pnt, pnk)
                    for vc in range(4):
                        self.mm(pnt[:, vc * 128:(vc + 1) * 128], vx[:, jj, h * 640 + vc * 128:h * 640 + (vc + 1) * 128], wt[:, :], vc == 0, False,
                                [vxk, wtk], [pnk], sgc=True)
                    self.mm(pnt[:, 512:640], vx[:, jj, h * 640 + 512:h * 640 + 640], wt[:, :], True, False, [vxk, wtk], [pnk], sgc=True)

                def step_c(h, ci):
                    c = cs_[ci]
                    pnt, pnk = pns[h]
                    csl = slice(jj * 128 + c * 64, jj * 128 + (c + 1) * 64)
                    rows = slice(c * 64, (c + 1) * 64)
                    for vc in range(5):
                        dst = pnt[:, vc * 128 + c * 64:vc * 128 + (c + 1) * 64] if vc < 4 else pnt[:, 512 + c * 64:512 + (c + 1) * 64]
                        for dc in range(4):
                            self.mm(dst, Cb[:, h * 4 + dc, vc * 128:(vc + 1) * 128], qb[:, h * 4 + dc, csl], False, (ci == 1 and dc == 3),
                                    [("Cb", h * 4 + dc), qbk], [pnk], sgc=True)
                    ch = jj * 2 + c
                    for dc in range(4):
                        ps, psk = pst.next()
                        lhs = kw[rows, jj, h * 512 + dc * 128:h * 512 + (dc + 1) * 128]
                        self.mm(ps[:, 0:512], lhs, vx[rows, jj, h * 640:h * 640 + 512], True, True, [kwk, vxk], [psk])
                        self.mm(ps[:, 512:640], lhs, vx[rows, jj, h * 640 + 512:h * 640 + 640], True, True, [kwk, vxk], [psk])
                        i = h * 4 + dc
                        self.stt("dve", C[:, i, :], C[:, i, :], cr[:, h, ch:ch + 1], ps[:, 0:640], ALU.mult, ALU.add, [("C", i), crk, psk], [("C", i)])
                        self.cp("act", Cb[:, i, :], C[:, i, :], [("C", i)], [("Cb", i)])

                def step_E(h):
                    pnt, pnk = pns[h]
                    rd, rdk = rdb.next()
                    self.act(rd[:, :], pnt[:, 512:640], AF.Abs, [pnk], [rdk])
                    self.ts("dve", rd[:, :], rd[:, :], 1.0, None, ALU.max, None, [rdk], [rdk])
                    self.P.op("dve", (lambda rd=rd: (lambda e: e.reciprocal(rd[:, :], rd[:, :])))(), [rdk], [rdk])
                    self.tt("dve", ht[:, h * 4:(h + 1) * 4, tsl], pnt[:, 0:512].rearrange("p (v t) -> p v t", v=4),
                            rd[:, :].unsqueeze(1).to_broadcast([128, 4, 128]), ALU.mult, [pnk, rdk], [(htk, jj, h)])

                for h in range(4):
                    step_A(h)
                    step_c(h, 0)
                    if h >= 1:
                        step_c(h - 1, 1)
                        step_E(h - 1)
                step_c(3, 1)
                step_E(3)
            self.dma("q_sp", self.HT[d, :, :, s0:s0 + NT].rearrange("c p t -> p c t"), ht[:, :, :],
                     [(htk, jj, h) for jj in range(nj) for h in range(4)], [])
        P.phase_end()

    def mlstm_phase_o(self, l, need_ctx):
        P = self.P
        jm = self.kinds[:l + 1].count(1) - 1
        P.phase_begin()
        NT, nj = 256, 2
        wb = P.sb([128, 16 * D], BF16, "wdn")
        wd = wb[:, :].rearrange("p (k n) -> p k n", k=16)
        self.load_w(wd, self.ml_w_down[jm].rearrange("(k p) n -> p k n", p=128), "wdn", ncols_piece=1024)
        gn = P.sb([128, 16], F32, "gn")
        sk = P.sb([128, 16], F32, "sk")
        self.dma("q_sp", gn[:], self.ml_gn[jm], (), ["gn"])
        self.dma("q_sp", sk[:], self.ml_skip[jm], (), ["sk"])
        ones = P.sb([128, 128], BF16, "ones")
        self.dma("q_sp", ones[:], self.onesb[:], (), ["ones"])
        self.alloc_norm(need_aT=False)
        m = self.modt
        pacc = Rot(P, 4, [128, 512], F32, "pacc", psum=True)
        pm_ = Rot(P, 2, [128, 512], F32, "pm", psum=True)
        pq_ = Rot(P, 2, [128, 512], F32, "pq", psum=True)
        h0b = Rot(P, 1, [128, 16, NT], F32, "h0b")
        h1b = Rot(P, 1, [128, 16, NT], F32, "h1b")
        xcb = Rot(P, 1, [128, 16, NT], BF16, "xcb")
        szb = Rot(P, 1, [128, 16, NT], BF16, "szb")
        hbb = Rot(P, 1, [128, 16, NT], BF16, "hbb")
        sqb = Rot(P, 1, [128, 16, NT], BF16, "sqb")
        yTb = Rot(P, 2, [128, 16, NT], BF16, "yTb")
        mnb = Rot(P, 2, [128, 4, NT], F32, "mnb")
        rsb = Rot(P, 2, [128, 4, NT], F32, "rsb")
        tma = Rot(P, 3, [128, NT], F32, "tma")
        cur_row = None
        nunits = self.T // 256
        for u in range(nunits):
            if u == 0 and not need_ctx:
                continue
            row = 1 if u == 0 else 0
            if row != cur_row:
                self.load_mod(l, 0, None, row)
                cur_row = row
            s0 = u * 256
            h0, h0k = h0b.next()
            h1, h1k = h1b.next()
            xc, xck = xcb.next()
            sz, szk = szb.next()
            self.dma("q_sp", h0[:, :, :], self.HT[0, :, :, s0:s0 + NT].rearrange("c p t -> p c t"), [], [h0k])
            self.dma("q_sp", h1[:, :, :], self.HT[1, :, :, s0:s0 + NT].rearrange("c p t -> p c t"), [], [h1k])
            self.dma("q_sp", xc[:, :, :], self.XC[:, :, s0:s0 + NT].rearrange("c p t -> p c t"), [], [xck])
            self.dma("q_sp", sz[:, :, :], self.SZ[:, :, s0:s0 + NT].rearrange("c p t -> p c t"), [], [szk])
            self.tt("pool", h0[:, :, :], h0[:, :, :], h1[:, :, :], ALU.add, [h0k, h1k], [h0k])
            hb, hbk = hbb.next()
            sq, sqk = sqb.next()
            self.cp("act", hb[:, :, :], h0[:, :, :], [h0k], [hbk])
            self.act(sq[:, :, :], h0[:, :, :], AF.Square, [h0k], [sqk])
            mn, mnk = mnb.next()
            rs, rsk = rsb.next()
            for h in range(4):
                p1, p1k = pm_.next()
                p2, p2k = pq_.next()
                for vc in range(4):
                    self.mm(p1[:, :NT], ones[:], hb[:, 4 * h + vc, :], vc == 0, vc == 3, ["ones", hbk], [p1k])
                for vc in range(4):
                    self.mm(p2[:, :NT], ones[:], sq[:, 4 * h + vc, :], vc == 0, vc == 3, ["ones", sqk], [p2k])
                self.act(mn[:, h, :], p1[:, :NT], AF.Copy, [p1k], [(mnk, h)], scale=1.0 / 512)
                tq, tqk = tma.next()
                self.tt("dve", tq[:, :], mn[:, h, :], mn[:, h, :], ALU.mult, [(mnk, h)], [tqk])
                self.stt("dve", tq[:, :], p2[:, :NT], 1.0 / 512, tq[:, :], ALU.mult, ALU.subtract, [p2k, tqk], [tqk])
                self.act(rs[:, h, :], tq[:, :], AF.Sqrt, [tqk], [(rsk, h)], bias=EPS)
                self.P.op("dve", (lambda rs=rs, h=h: (lambda e: e.reciprocal(rs[:, h, :], rs[:, h, :])))(), [(rsk, h)], [(rsk, h)])
            yT, yTk = yTb.next()
            for i in range(16):
                h = i // 4
                eng = "dve" if i % 2 == 0 else "pool"
                ta, tak = tma.next()
                self.tt("pool", ta[:, :], h0[:, i, :], mn[:, h, :], ALU.subtract, [h0k, (mnk, h)], [tak])
                self.stt("dve", ta[:, :], ta[:, :], gn[:, i:i + 1], rs[:, h, :], ALU.mult, ALU.mult, [tak, "gn", (rsk, h)], [tak])
                self.stt("dve", ta[:, :], xc[:, i, :], sk[:, i:i + 1], ta[:, :], ALU.mult, ALU.add, [xck, "sk", tak], [tak])
                self.tt("pool", yT[:, i, :], ta[:, :], sz[:, i, :], ALU.mult, [tak, szk], [(yTk, i)])
            for jj in range(nj):
                j = s0 // 128 + jj
                h_, hk = self.hbuf.next()
                self.load_h(h_, hk, j)
                t1, t1k = self.t1buf.next()
                for nh in range(2):
                    pa, pk = pacc.next()
                    th, thk = self.thalf.next()
                    for k in range(16):
                        self.mm(pa[:, :], yT[:, k, jj * 128:(jj + 1) * 128], wd[:, k, nh * 512:(nh + 1) * 512], k == 0, k == 15,
                                [("wdn", k, 0), (yTk, k)], [pk])
                    self.tt("dve", th[:, :], pa[:, :], m["gt"][:, nh * 512:(nh + 1) * 512], ALU.mult, [pk, "gt"], [thk])
                    self.tt("pool", h_[:, nh * 512:(nh + 1) * 512], th[:, :], h_[:, nh * 512:(nh + 1) * 512], ALU.add,
                            [thk, hk], [hk])
                self.dma("q_sp", self.hrow(j), h_[:], [hk], [])
        P.phase_end()
        self.h_in_src = False

    def pool_phase_p(self, l):
        P = self.P
        P.phase_begin()
        self.alloc_norm(need_aT=False)
        cb = P.sb([128, 36, 128], BF16, "cb")
        cf = P.sb([128, 16, 128], F32, "cf")
        self.dma("q_sp", cb[:], self.poolcb[:], (), ["cb"])
        self.dma("q_sp", cf[:], self.poolcf[:], (), ["cf"])
        pp = Rot(P, 2, [128, 1024], F32, "pp", psum=True)
        cpb = Rot(P, 2, [128, D], BF16, "cpb")
        cur_row = None
        for j in range(self.T // 128):
            row = 1 if j < 2 else 0
            if row != cur_row:
                self.load_mod(l, 0, self.norm1_g, row)
                cur_row = row
            a, ak = self.norm_tile(j)
            self.dma("q_sp", self.A_tm[j * 128:(j + 1) * 128, :], a[:], [ak], [("A", j)])
            if j >= 2:
                pt, ptk = pp.next()
                cp_, cpk = cpb.next()
                for g in range(4):
                    self.mm(pt[:, g * 256:(g + 1) * 256], cb[:, g, :], a[:, g * 256:(g + 1) * 256], True, True, ["cb", ak], [(ptk, g // 2)])
                for g in range(4):
                    self.act(cp_[:, g * 256:(g + 1) * 256], pt[:, g * 256:(g + 1) * 256], AF.Copy, [(ptk, g // 2), "cf"], [(cpk, g)], scale=cf[:, 12 + g, 0:1])
                self.dma("q_sp", self.CP_tm[j * 128:(j + 1) * 128, :], cp_[:], [(cpk, g) for g in range(4)], [("CP", j)])
        P.phase_end()

    def pool_phase_q(self, l, need_ctx):
        P = self.P
        jp = self.kinds[:l + 1].count(2) - 1
        P.phase_begin()
        self.load_ident()
        R = self.TL // 64
        cpt = 128 // R
        cb = P.sb([128, 36, 128], BF16, "cb")
        cf = P.sb([128, 16, 128], F32, "cf")
        self.dma("q_sp", cb[:], self.poolcb[:], (), ["cb"])
        self.dma("q_sp", cf[:], self.poolcf[:], (), ["cf"])
        wp = P.sb([128, 4, 2, 256], BF16, "wp")
        for g in range(4):
            self.dma("q_pool", wp[:, g, :, :], self.pool_w[jp, g].rearrange("(k p) n -> p k n", p=128), (), [("wp", g)])
        gt = P.sb([128, D], F32, "gt")
        sg = P.sb([128, D], F32, "sg")
        bsg = P.sb([128, D], F32, "bsg")
        tmpa = P.sb([128, D], F32, "tmpa")
        ppt = Rot(P, 2, [128, 8, 128], F32, "ppt", psum=True)
        ppo = Rot(P, 2, [128, 1024], F32, "ppo", psum=True)
        cpb = Rot(P, 2, [128, D], BF16, "cpb")
        ab = Rot(P, 3, [128, D], BF16, "ab")
        hb = Rot(P, 3, [128, D], F32, "hb")
        tb = Rot(P, 2, [128, D], F32, "tb")
        plT = Rot(P, 2, [128, 8, 128], BF16, "plT")

        def load_gate(row):
            mv = self.modv[l, row:row + 1, :]
            self.dma("q_sp", gt[:], mv[:, 2 * D:3 * D].partition_broadcast(128), [], ["gt"])
            self.dma("q_sp", tmpa[:], self.pool_scale[jp:jp + 1, :].partition_broadcast(128), [], ["tmpa"])
            self.tt("dve", sg[:], gt[:], tmpa[:], ALU.mult, ["gt", "tmpa"], ["sg"])
            self.dma("q_sp", tmpa[:], self.pool_b[jp:jp + 1, :].partition_broadcast(128), [], ["tmpa"])
            self.tt("dve", bsg[:], sg[:], tmpa[:], ALU.mult, ["sg", "tmpa"], ["bsg"])

        def finish(pt, ptk, rr_idx, h, hks, store):
            pl, plk = plT.next()
            for g in range(4):
                self.tt("dve", pl[:, 2 * g:2 * g + 2, :], pt[:, 2 * g:2 * g + 2, :],
                        cf[:, rr_idx(g):rr_idx(g) + 1, :].to_broadcast([128, 2, 128]), ALU.mult, [ptk, "cf"], [(plk, g)])
            po, pok = ppo.next()
            for g in range(4):
                for kc in range(2):
                    self.mm(po[:, g * 256:(g + 1) * 256], pl[:, 2 * g + kc, :], wp[:, g, kc, :], kc == 0, kc == 1, [(plk, g), ("wp", g)], [pok])
            t, tk = tb.next()
            self.tt("dve", t[:], po[:], sg[:], ALU.mult, [pok, "sg"], [tk])
            self.tt("pool", t[:], t[:], bsg[:], ALU.add, [tk, "bsg"], [tk])
            self.tt("pool", h[:], t[:], h[:], ALU.add, [tk] + hks, hks)
            store(h, hks)

        if need_ctx:
            load_gate(1)
            a2 = []
            for j in range(2):
                a, ak = ab.next()
                aks_ = [(ak, cc) for cc in range(cpt)]
                self.dma("q_sp", a[:], self.A_tm[j * 128:(j + 1) * 128, :], [], aks_)
                a2.append((a, aks_))
            for j2 in range(2):
                h, hk = hb.next()
                hks_ = [(hk, cc) for cc in range(cpt)]
                self.dma("q_sp", h[:], self.src_row(j2) if self.h_in_src else self.hrow(j2), [], hks_)
                pt, ptk = ppt.next()
                for fc in range(8):
                    g = fc // 2
                    for j in range(2):
                        self.mm(pt[:, fc, :], a2[j][0][:, fc * 128:(fc + 1) * 128], cb[:, 12 + g * 4 + j * 2 + j2, :], j == 0, False,
                                a2[j][1] + ["cb"], [ptk])
                    self.mm(pt[:, fc, :], a2[j2][0][:, fc * 128:(fc + 1) * 128], cb[:, 28 + g * 2 + j2, :], False, True, a2[j2][1] + ["cb"], [ptk])

                def store_c(h, hks, j2=j2):
                    self.dma("q_sp", self.hrow(j2), h[:], hks, [])
                finish(pt, ptk, lambda g, j2=j2: 4 + g * 2 + j2, h, hks_, store_c)
        load_gate(0)
        cp_v = self.CP_tm[TC:, :].rearrange("(r c) f -> c r f", c=64)
        a_v = self.A_tm[TC:, :].rearrange("(r c) f -> c r f", c=64)
        hsrc = self.x[:, :] if self.h_in_src else self.hres[TC:, :]
        hs_v = hsrc.rearrange("(r c) f -> c r f", c=64)
        hd_v = self.hres[TC:, :].rearrange("(r c) f -> c r f", c=64)
        for m_ in range(64 // cpt):
            c0 = m_ * cpt
            cp_, cpk = cpb.next()
            a, ak = ab.next()
            h, hk = hb.next()
            for cc in range(cpt):
                rows = slice(cc * R, (cc + 1) * R)
                self.dma("q_sp", cp_[rows, :], cp_v[c0 + cc], [], [(cpk, cc)])
                self.dma("q_sp", a[rows, :], a_v[c0 + cc], [], [(ak, cc)])
                self.dma("q_sp", h[rows, :], hs_v[c0 + cc], [], [(hk, cc)])
            pt, ptk = ppt.next()
            cpks = [(cpk, cc) for cc in range(cpt)]
            aks = [(ak, cc) for cc in range(cpt)]
            hks = [(hk, cc) for cc in range(cpt)]
            for fc in range(8):
                g = fc // 2
                self.mm(pt[:, fc, :], cp_[:, fc * 128:(fc + 1) * 128], cb[:, 4 + g, :], True, False, cpks + ["cb"], [ptk])
                self.mm(pt[:, fc, :], a[:, fc * 128:(fc + 1) * 128], cb[:, 8 + g, :], False, True, aks + ["cb"], [ptk])

            def store_l(h, hks_, c0=c0):
                for cc in range(cpt):
                    self.dma("q_sp", hd_v[c0 + cc], h[cc * R:(cc + 1) * R, :], hks_, [])
            finish(pt, ptk, lambda g: g, h, hks, store_l)
        P.phase_end()
        self.h_in_src = False

    def build(self):
        self.setup()
        self.ada_phase()
        for l in range(self.depth):
            kind = self.kinds[l]
            last = l == self.depth - 1
            if kind == 0:
                self.gla_phase_p(l)
                self.gla_phase_s(l, 0)
                self.gla_phase_s(l, 1)
                self.gla_phase_o(l, need_ctx=not last)
            elif kind == 1:
                self.mlstm_phase_p1(l)
                self.mlstm_phase_p2(l)
                self.mlstm_phase_s(l, 0)
                self.mlstm_phase_s(l, 1)
                self.mlstm_phase_o(l, need_ctx=not last)
            elif kind == 2:
                self.pool_phase_p(l)
                self.pool_phase_q(l, need_ctx=not last)
            elif kind is not None:
                raise NotImplementedError
            self.mlp1_phase(l, skip_ctx=last)
            self.mlp2_phase(l, skip_ctx=last, final=last)
        self.P.emit()
        self.P.close()
        return self.nc


def _box(L, w):
    pos = np.arange(L)
    lo = np.maximum(pos - w // 2, 0)
    hi = np.minimum(pos + (w - w // 2), L)
    M = ((pos[:, None] >= lo[None, :]) & (pos[:, None] < hi[None, :])).astype(np.float32)
    return M, (hi - lo).astype(np.float32)


def _consts(T_lat):
    R = T_lat // 64
    cpt = 128 // R
    cb = np.zeros((128, 36, 128), np.float32)
    cf = np.zeros((128, 16, 128), np.float32)
    for g, w in enumerate((2, 4, 8, 16)):
        Mc, cc_ = _box(64, w)
        cb[:, g, :] = np.kron(np.eye(2, dtype=np.float32), Mc)
        cf[:, 12 + g, 0] = 1.0 / np.tile(cc_, 2)
        Mr, cr = _box(R, w)
        cb[:, 4 + g, :] = np.kron(np.eye(cpt, dtype=np.float32), Mr)
        cb[:, 8 + g, :] = -np.diag(np.tile(cr, cpt))
        cf[:, g, :] = (1.0 / np.tile(cr, cpt))[None, :]
        Mx, cx = _box(TC, w)
        for j in range(2):
            for j2 in range(2):
                cb[:, 12 + g * 4 + j * 2 + j2, :] = Mx[j * 128:(j + 1) * 128, j2 * 128:(j2 + 1) * 128]
        for j2 in range(2):
            cb[:, 28 + g * 2 + j2, :] = -np.diag(cx[j2 * 128:(j2 + 1) * 128])
            cf[:, 4 + g * 2 + j2, :] = (1.0 / cx[j2 * 128:(j2 + 1) * 128])[None, :]
    glacf = np.ones((128, 768), np.float32)
    glacf[:, 0:512:64] = 0.0
    si_, ti_ = np.meshgrid(np.arange(128), np.arange(128), indexing="ij")
    same = (si_ // 64) == (ti_ // 64)
    glacf[:, 512:640] = (same & (ti_ >= si_)).astype(np.float32)
    glacf[:, 640:768] = (same & (ti_ <= si_)).astype(np.float32)
    sel = np.zeros((64, 8, 128), np.float32)
    for r8 in range(8):
        sel[32 * (r8 // 4) + r8 % 4, r8, :] = 1.0
    return {"ml_sel": sel, "identf": np.eye(128, dtype=np.float32), "glacf": glacf, "onesb": np.ones((128, 128), np.float32).astype(ml_dtypes.bfloat16),
            "identb": np.eye(128, dtype=np.float32).astype(ml_dtypes.bfloat16),
            "poolcb": cb.astype(ml_dtypes.bfloat16), "poolcf": cf}


def _bd(w_qkv):
    n = w_qkv.shape[0]
    o = np.zeros((n, 3, 16, 128, 128), np.float32)
    w = w_qkv.reshape(n, 3, 16, 32, 4, 4)
    for b in range(32):
        o[:, :, :, 4 * b:4 * b + 4, 4 * b:4 * b + 4] = w[:, :, :, b]
    return o


def _wg(w_gate, off):
    n = w_gate.shape[0]
    o = np.zeros((n, 128, 48, 64), np.float32)
    w = w_gate.reshape(n, 2, 48, 128, 8)
    for d in range(2):
        o[:, :, :, 32 * d:32 * d + 4] = w[:, d, :, :, off:off + 4].transpose(0, 2, 1, 3)
    return o


def _bg(b_gate, off):
    n = b_gate.shape[0]
    o = np.zeros((n, 64, 1), np.float32)
    for d in range(2):
        o[:, 32 * d:32 * d + 4, 0] = b_gate[:, d, off:off + 4]
    return o


def _wa2blk(w_a2):
    n = w_a2.shape[0]
    o = np.zeros((n, 32, 1024), np.float32)
    o[:, 0:16, 0:512] = w_a2[:, 0]
    o[:, 16:32, 512:1024] = w_a2[:, 1]
    return o


def make_in_maps(inputs, T_lat, depth):
    B = inputs["x"].shape[0]
    consts = _consts(T_lat)
    maps = []
    for b in range(B):
        cT = np.stack([inputs["c"][b], inputs["c_ctx"]], axis=1)
        cT = np.ascontiguousarray(cT.reshape(KC, 128, 2).transpose(1, 0, 2))
        m = {
            "x": np.ascontiguousarray(inputs["x"][b]),
            "ctx": np.ascontiguousarray(inputs["ctx"][b]),
            "cT": cT.astype(np.float32),
            "ada_w": inputs["ada_w"], "ada_b": inputs["ada_b"],
            "norm1_g": inputs["norm1_g"], "norm2_g": inputs["norm2_g"],
            "mlp_w1": inputs["mlp_w1"], "mlp_w2": inputs["mlp_w2"],
            "final_g": inputs["final_g"].reshape(1, D),
            "gla_w_in": inputs["gla_w_in"],
            "gla_wa1": np.ascontiguousarray(np.concatenate([inputs["gla_w_a1"][:, 0], inputs["gla_w_a1"][:, 1]], axis=-1)),
            "gla_wa2": _wa2blk(inputs["gla_w_a2"]),
            "gla_ba": np.ascontiguousarray(inputs["gla_b_a"].reshape(-1, 8, 128).transpose(0, 2, 1)),
            "gla_gh": np.ascontiguousarray(inputs["gla_g_head"].reshape(-1, 8, 128).transpose(0, 2, 1)),
            "gla_w_o": inputs["gla_w_o"],
            "ml_w_up": inputs["mlstm_w_up"], "ml_w_down": inputs["mlstm_w_down"],
            "ml_bd": _bd(inputs["mlstm_w_qkv"]),
            "ml_wgI": _wg(inputs["mlstm_w_gate"], 0), "ml_wgF": _wg(inputs["mlstm_w_gate"], 4),
            "ml_convw": np.ascontiguousarray(inputs["mlstm_conv_w"].reshape(-1, 4, 16, 128).transpose(0, 3, 2, 1)),
            "ml_convb": np.ascontiguousarray(inputs["mlstm_conv_b"].reshape(-1, 16, 128).transpose(0, 2, 1)),
            "ml_bgI": _bg(inputs["mlstm_b_gate"], 0), "ml_bgF": _bg(inputs["mlstm_b_gate"], 4),
            "ml_gn": np.ascontiguousarray(inputs["mlstm_g_norm"].reshape(-1, 16, 128).transpose(0, 2, 1)),
            "ml_skip": np.ascontiguousarray(inputs["mlstm_skip"].reshape(-1, 16, 128).transpose(0, 2, 1)),
            "pool_w": inputs["pool_w"], "pool_b": inputs["pool_b"].reshape(-1, D),
            "pool_scale": inputs["pool_scale"],
        }
        m.update(consts)
        maps.append(m)
    return maps


def run(inputs, T_lat, depth, kinds=None, trace=False):
    inputs = {k: np.asarray(v) for k, v in inputs.items()}
    bld = Builder(T_lat, depth, kinds)
    nc = bld.build()
    maps = make_in_maps(inputs, T_lat, depth)
    maps = [{k: v for k, v in m.items() if k in bld.din} for m in maps]
    res = run_bass_kernel_spmd(nc, maps, core_ids=list(range(len(maps))), trace=trace)
    out = np.stack([r["out"] for r in res.results], axis=0)
    return out.astype(np.float32), res, bld


def kernel(**inputs):
    out, _, _ = run(inputs, 4096, 4)
    return out
```

```python
from contextlib import ExitStack
import numpy as np
import ml_dtypes
import concourse.bass as bass
import concourse.mybir as mybir
from concourse.bass_utils import run_bass_kernel_spmd

F32 = mybir.dt.float32
BF16 = mybir.dt.bfloat16
AF = mybir.ActivationFunctionType
ALU = mybir.AluOpType
AX = mybir.AxisListType

COMPUTE = ("pe", "act", "dve", "pool")
EPOCH = 20000
NDMASEM = 64

D = 1024
KC = 8
TC = 256
DFF = 4096
EPS = 1e-6


class Prog:
    def __init__(self, nc):
        self.nc = nc
        self.es = ExitStack()
        self.ops = []
        self.nname = 0

    def sb(self, shape, dtype, name=None):
        self.nname += 1
        name = (name or "sb") + f"_{self.nname}"
        return self.es.enter_context(self.nc.sbuf_tensor(name, list(shape), dtype))

    def ps(self, shape, dtype, name=None):
        self.nname += 1
        name = (name or "ps") + f"_{self.nname}"
        return self.es.enter_context(self.nc.psum_tensor(name, list(shape), dtype))

    def op(self, eng, fn, reads=(), writes=()):
        self.ops.append((eng, fn, tuple(reads), tuple(writes)))

    def barrier(self):
        self.ops.append(("barrier", None, (), ()))

    def phase_begin(self):
        self._saved_es = self.es
        self.es = ExitStack()

    def phase_end(self):
        self.barrier()
        self.es.close()
        self.es = self._saved_es

    def _engine(self, eng):
        nc = self.nc
        return {"pe": nc.tensor, "act": nc.scalar, "dve": nc.vector, "pool": nc.gpsimd,
                "q_sp": nc.sync, "q_act": nc.scalar, "q_pool": nc.gpsimd}[eng]

    def _seq(self, stream):
        nc = self.nc
        return {"pe": nc.tensor, "act": nc.scalar, "dve": nc.vector, "pool": nc.gpsimd,
                "sp": nc.sync}[stream]

    @staticmethod
    def _stream(eng):
        return {"q_sp": "sp", "q_act": "act", "q_pool": "pool"}.get(eng, eng)

    def emit(self, final_keys=()):
        nc = self.nc
        ops = self.ops
        n = len(ops)
        last_w = {}
        rd_eng = {}
        rd_dma = {}
        deps = [None] * n
        needed = [False] * n
        bar_deps = {}
        last_on = {}
        dma_since = []
        for i, (eng, fn, reads, writes) in enumerate(ops):
            if eng == "barrier":
                bd = list(last_on.values()) + dma_since
                bar_deps[i] = bd
                for j in bd:
                    needed[j] = True
                deps[i] = []
                last_w, rd_eng, rd_dma = {}, {}, {}
                dma_since = []
                continue
            if eng.startswith("q_"):
                dma_since.append(i)
            else:
                last_on[eng] = i
            isdma_i = eng.startswith("q_")
            d = set()
            raw = set()
            for k in reads:
                w = last_w.get(k)
                if w is not None:
                    d.add(w)
                    raw.add(w)
            for k in writes:
                w = last_w.get(k)
                if w is not None:
                    d.add(w)
                for r in rd_eng.get(k, {}).values():
                    d.add(r)
                for r in rd_dma.get(k, ()):
                    d.add(r)
            d.discard(i)
            dd = []
            for j in d:
                ej = ops[j][0]
                if (not isdma_i) and ej == eng and eng == "pe":
                    continue
                dd.append(j)
            deps[i] = dd
            for j in dd:
                needed[j] = True
            for k in reads:
                if isdma_i:
                    rd_dma.setdefault(k, []).append(i)
                else:
                    rd_eng.setdefault(k, {})[eng] = i
            for k in writes:
                last_w[k] = i
                rd_eng[k] = {}
                rd_dma[k] = []
        fin = [last_w[k] for k in final_keys if k in last_w]
        for j in fin:
            needed[j] = True

        cnt = {e: 0 for e in COMPUTE}
        sig = [None] * n
        dma_cnt = [0] * NDMASEM
        ndma = 0
        nsw = 0
        NHW = 48
        for i, (eng, fn, reads, writes) in enumerate(ops):
            if not needed[i] or eng == "barrier":
                continue
            if eng.startswith("q_"):
                if eng == "q_pool":
                    s = NHW + nsw % (NDMASEM - NHW)
                    nsw += 1
                else:
                    s = ndma % NHW
                    ndma += 1
                dma_cnt[s] += 1
                sig[i] = ("dma", s, dma_cnt[s] * 16)
            else:
                cnt[eng] += 1
                sig[i] = ("eng", eng, cnt[eng])
        sems = {}
        for e in COMPUTE:
            ne = max(1, (cnt[e] + EPOCH - 1) // EPOCH)
            sems[e] = [self.es.enter_context(nc.semaphore(f"s_{e}{k}")) for k in range(ne)]
        dsems = [self.es.enter_context(nc.semaphore(f"s_dma{k}")) for k in range(NDMASEM)]
        assert max(dma_cnt + [0]) * 16 < 60000, dma_cnt
        self.stats = dict(cnt=dict(cnt), ndma=ndma, nops=n)
        known = {}

        def do_wait(stream, s):
            if s[0] == "dma":
                key = ("dma", s[1])
                val = s[2]
                if known.get((stream, key), 0) >= val:
                    return
                known[(stream, key)] = val
                self._seq(stream).wait_ge(dsems[s[1]], val)
            else:
                e, idx = s[1], s[2]
                key = ("eng", e)
                if known.get((stream, key), 0) >= idx:
                    return
                known[(stream, key)] = idx
                ep = (idx - 1) // EPOCH
                self._seq(stream).wait_ge(sems[e][ep], idx - ep * EPOCH)

        for i, (eng, fn, reads, writes) in enumerate(ops):
            if eng == "barrier":
                for stream in ("pe", "act", "dve", "pool", "sp"):
                    for j in bar_deps[i]:
                        if sig[j][0] == "eng" and sig[j][1] == stream:
                            continue
                        do_wait(stream, sig[j])
                continue
            stream = self._stream(eng)
            for j in sorted(deps[i]):
                do_wait(stream, sig[j])
            if needed[i] and sig[i][0] == "dma" and sig[i][2] > 16:
                do_wait(stream, ("dma", sig[i][1], sig[i][2] - 16))
            ins = fn(self._engine(eng))
            if needed[i]:
                s = sig[i]
                if s[0] == "dma":
                    ins.then_inc(dsems[s[1]], 16)
                else:
                    ep = (s[2] - 1) // EPOCH
                    ins.then_inc(sems[s[1]][ep], 1)
        for j in fin:
            do_wait("sp", sig[j])

    def close(self):
        self.es.close()


class Rot:
    def __init__(self, P, n, shape, dtype, name, psum=False):
        self.bufs = [(P.ps if psum else P.sb)(shape, dtype, f"{name}{i}") for i in range(n)]
        self.keys = [(name, i) for i in range(n)]
        self.i = -1

    def next(self):
        self.i = (self.i + 1) % len(self.bufs)
        return self.bufs[self.i], self.keys[self.i]


class Builder:
    def __init__(self, T_lat, depth, kinds=None):
        self.TL = T_lat
        self.T = TC + T_lat
        self.depth = depth
        self.kinds = kinds if kinds is not None else [i % 3 for i in range(depth)]
        self.nc = bass.Bass("TRN2", target_bir_lowering=False)
        self.P = Prog(self.nc)
        self.sts = [(0, TC)] + [(TC + 512 * i, 512) for i in range(T_lat // 512)]
        self.din = {}

    def mm(self, out, lhsT, rhs, start, stop, r, w, sgc=False):
        self.P.op("pe", lambda e: e.matmul(out, lhsT=lhsT, rhs=rhs, start=start, stop=stop, skip_group_check=sgc), r, w)

    def tr(self, out, in_, ident, r, w):
        self.P.op("pe", lambda e: e.transpose(out, in_, ident), r, w)

    def act(self, out, in_, func, r, w, bias=None, scale=None, accum=None):
        kw = {}
        if bias is not None:
            kw["bias"] = bias
        if scale is not None:
            kw["scale"] = scale
        if accum is not None:
            kw["accum_out"] = accum
        self.P.op("act", lambda e: e.activation(out=out, in_=in_, func=func, **kw), r, w)

    def tt(self, eng, out, a, b, op, r, w):
        self.P.op(eng, lambda e: e.tensor_tensor(out=out, in0=a, in1=b, op=op), r, w)

    def ts(self, eng, out, a, s1, s2, op0, op1, r, w):
        if s2 is None:
            self.P.op(eng, lambda e: e.tensor_scalar(out=out, in0=a, scalar1=s1, scalar2=None, op0=op0), r, w)
        else:
            self.P.op(eng, lambda e: e.tensor_scalar(out=out, in0=a, scalar1=s1, scalar2=s2, op0=op0, op1=op1), r, w)

    def stt(self, eng, out, in0, scalar, in1, op0, op1, r, w):
        self.P.op(eng, lambda e: e.scalar_tensor_tensor(out=out, in0=in0, scalar=scalar, in1=in1, op0=op0, op1=op1), r, w)

    def cp(self, eng, out, in_, r, w):
        if eng == "act":
            self.P.op("act", lambda e: e.copy(out, in_), r, w)
        else:
            self.P.op(eng, lambda e: e.tensor_copy(out, in_), r, w)

    def dma(self, q, out, in_, r, w, slow=False):
        if slow:
            self.P.op(q, lambda e: e.dma_start(out=out, in_=in_, allow_slow_non_contiguous=True), r, w)
        else:
            self.P.op(q, lambda e: e.dma_start(out=out, in_=in_), r, w)

    def scan(self, out, d0, d1, init, op0, op1, r, w):
        self.P.op("dve", lambda e: e.tensor_tensor_scan(out=out, data0=d0, data1=d1, initial=init, op0=op0, op1=op1), r, w)

    def memset(self, eng, ap, val, w):
        self.P.op(eng, lambda e: e.memset(ap, val), (), w)

    def inp(self, name, shape, dtype=F32):
        t = self.nc.dram_tensor(name, list(shape), dtype, kind="ExternalInput")
        self.din[name] = t
        return t

    def scratch(self, name, shape, dtype):
        return self.nc.dram_tensor(name, list(shape), dtype, kind="Internal")

    def setup(self):
        P = self.P
        TL, T = self.TL, self.T
        self.x = self.inp("x", [TL, D])
        self.ctx = self.inp("ctx", [TC, D])
        self.cT = self.inp("cT", [128, KC, 2])
        self.ada_w = self.inp("ada_w", [4, D, 6 * D])
        self.ada_b = self.inp("ada_b", [4, 6 * D])
        self.norm1_g = self.inp("norm1_g", [4, D])
        self.norm2_g = self.inp("norm2_g", [4, D])
        self.mlp_w1 = self.inp("mlp_w1", [4, D, DFF])
        self.mlp_w2 = self.inp("mlp_w2", [4, DFF, D])
        self.final_g = self.inp("final_g", [1, D])
        self.identb = self.inp("identb", [128, 128], BF16)
        self.out = self.nc.dram_tensor("out", [TL, D], F32, kind="ExternalOutput")
        self.hres = self.scratch("hres", [T, D], F32)
        self.modv = self.scratch("modv", [4, 2, 6 * D], F32)
        self.U = self.scratch("U", [32, 128, T], BF16)
        ng = max(1, self.kinds.count(0))
        self.gla_w_in = self.inp("gla_w_in", [ng, D, 3072])
        self.gla_wa1 = self.inp("gla_wa1", [ng, D, 32])
        self.gla_wa2 = self.inp("gla_wa2", [ng, 32, 1024])
        self.gla_ba = self.inp("gla_ba", [ng, 128, 8])
        self.gla_gh = self.inp("gla_gh", [ng, 128, 8])
        self.gla_w_o = self.inp("gla_w_o", [ng, D, D])
        self.glacf = self.inp("glacf", [128, 768])
        self.onesb = self.inp("onesb", [128, 128], BF16)
        self.QD = self.scratch("QD", [2, 4, 128, T], BF16)
        self.KD = self.scratch("KD", [2, 4, 128, T], BF16)
        self.KW_tm = self.scratch("KW_tm", [2, T, 512], BF16)
        self.V_tm = self.scratch("V_tm", [T, D], BF16)
        self.SR = self.scratch("SR", [8, 128, T], BF16)
        self.DEC = self.scratch("DEC", [2, 4, 128, T // 64], F32)
        self.OO = self.scratch("OO", [2, 8, 128, T], F32)
        nm_ = max(1, self.kinds.count(1))
        self.ml_w_up = self.inp("ml_w_up", [nm_, D, 4096])
        self.ml_bd = self.inp("ml_bd", [nm_, 3, 16, 128, 128])
        self.ml_wgI = self.inp("ml_wgI", [nm_, 128, 48, 64])
        self.ml_wgF = self.inp("ml_wgF", [nm_, 128, 48, 64])
        self.ml_convw = self.inp("ml_convw", [nm_, 128, 16, 4])
        self.ml_convb = self.inp("ml_convb", [nm_, 128, 16])
        self.ml_bgI = self.inp("ml_bgI", [nm_, 64, 1])
        self.ml_bgF = self.inp("ml_bgF", [nm_, 64, 1])
        self.ml_gn = self.inp("ml_gn", [nm_, 128, 16])
        self.ml_skip = self.inp("ml_skip", [nm_, 128, 16])
        self.ml_w_down = self.inp("ml_w_down", [nm_, 2048, D])
        self.ml_sel = self.inp("ml_sel", [64, 8, 128])
        self.identf_d = self.inp("identf", [128, 128])
        self.XM = self.scratch("XM", [16, 128, T], BF16)
        self.SZ = self.scratch("SZ", [16, 128, T], BF16)
        self.XC = self.scratch("XC", [16, 128, T], BF16)
        self.KT = self.scratch("KT", [16, 128, T], BF16)
        self.QB = self.scratch("QB", [2, 16, 128, T], BF16)
        self.VX = self.scratch("VX", [T, 2560], BF16)
        self.KWm = self.scratch("KWm", [2, T, 2048], BF16)
        self.CWT = self.scratch("CWT", [T, 128], F32)
        self.CARB = self.scratch("CARB", [2, 4, 128, T // 64], F32)
        self.HT = self.scratch("HT", [2, 16, 128, T], F32)
        self.A_tm = self.scratch("A_tm", [T, D], BF16)
        self.CP_tm = self.scratch("CP_tm", [T, D], BF16)
        npool = max(1, self.kinds.count(2))
        self.pool_w = self.inp("pool_w", [npool, 4, 256, 256])
        self.pool_b = self.inp("pool_b", [npool, D])
        self.pool_scale = self.inp("pool_scale", [npool, D])
        self.poolcb = self.inp("poolcb", [128, 36, 128], BF16)
        self.poolcf = self.inp("poolcf", [128, 16, 128])
        self.h_in_src = True

    def hrow(self, j):
        return self.hres[j * 128:(j + 1) * 128, :]

    def src_row(self, j):
        if j < 2:
            return self.ctx[j * 128:(j + 1) * 128, :]
        return self.x[(j - 2) * 128:(j - 1) * 128, :]

    def load_h(self, h, hk, j):
        if self.h_in_src:
            self.dma("q_sp", h[:], self.src_row(j), [], [hk])
        else:
            self.dma("q_sp", h[:], self.hrow(j), [("hres", j)], [hk])

    def load_ident(self):
        self.ident = self.P.sb([128, 128], BF16, "ident")
        self.dma("q_sp", self.ident[:], self.identb[:], (), ["ident"])

    def ada_phase(self):
        P = self.P
        P.phase_begin()
        cs = P.sb([128, KC, 2], F32, "cs")
        cin = P.sb([128, KC, 2], F32, "cin")
        psm = Rot(P, 2, [128, 512], F32, "psm", psum=True)
        self.dma("q_sp", cin[:], self.cT[:], (), ["cin"])
        self.act(cs[:], cin[:], AF.Silu, ["cin"], ["cs"])
        wst = Rot(P, 2, [128, KC, 512], F32, "adaw")
        bsb = P.sb([2, 6 * D], F32, "adab")
        msb = Rot(P, 1, [2, 6 * D], F32, "adam")
        for l in range(self.depth):
            self.dma("q_sp", bsb[:], self.ada_b[l:l + 1, :].partition_broadcast(2), (), ["adab"])
            mt, mk = msb.next()
            for n in range(12):
                wt, wk = wst.next()
                self.dma("q_sp", wt[:], self.ada_w[l, :, n * 512:(n + 1) * 512].rearrange("(k p) n -> p k n", p=128), (), [wk])
                pt, pk = psm.next()
                for k in range(KC):
                    self.mm(pt[0:2, :], cs[:, k, :], wt[:, k, :], k == 0, k == KC - 1, [wk, "cs"], [pk])
                self.tt("dve", mt[:, n * 512:(n + 1) * 512], pt[0:2, :], bsb[:, n * 512:(n + 1) * 512], ALU.add, [pk, "adab"], [(mk, n)])
            self.dma("q_pool", self.modv[l], mt[:], [(mk, n) for n in range(12)], [("modv", l)])
        P.phase_end()

    def alloc_norm(self, need_aT=True):
        P = self.P
        self.load_ident()
        self.hbuf = Rot(P, 4, [128, D], F32, "hbuf")
        self.t1buf = Rot(P, 2, [128, D], F32, "t1buf")
        self.ssb = Rot(P, 4, [128, 2], F32, "ssb")
        self.thalf = Rot(P, 3, [128, 512], F32, "thalf")
        self.modt = {nm: P.sb([128, D], F32, nm) for nm in ("gs", "sh", "gt")}
        self.abuf = Rot(P, 2, [128, D], BF16, "abuf")
        if need_aT:
            self.aT = Rot(P, 2, [128, KC, 512], BF16, "aT")
            self.ptr = Rot(P, 2, [128, 1024], BF16, "ptr", psum=True)

    def load_mod(self, l, which, norm_g, row):
        m = self.modt
        base = 3 * which
        mv = self.modv[l, row:row + 1, :]
        tmps, tmpsk = self.t1buf.next()
        tmpg, tmpgk = self.t1buf.next()
        self.dma("q_sp", m["sh"][:], mv[:, (base + 0) * D:(base + 1) * D].partition_broadcast(128), [], ["sh"])
        self.dma("q_sp", tmps[:], mv[:, (base + 1) * D:(base + 2) * D].partition_broadcast(128), [], [tmpsk])
        self.dma("q_sp", m["gt"][:], mv[:, (base + 2) * D:(base + 3) * D].partition_broadcast(128), [], ["gt"])
        if norm_g is not None:
            self.dma("q_sp", tmpg[:], norm_g[l:l + 1, :].partition_broadcast(128), (), [tmpgk])
            self.stt("dve", m["gs"][:], tmps[:], 1.0, tmpg[:], ALU.add, ALU.mult, [tmpsk, tmpgk], ["gs"])

    def rstd(self, ss, ssk, n=D):
        self.act(ss[:, 1:2], ss[:, 0:1], AF.Sqrt, [ssk], [ssk], scale=1.0 / n, bias=EPS)
        self.P.op("dve", lambda e: e.reciprocal(ss[:, 1:2], ss[:, 1:2]), [ssk], [ssk])

    def norm_tile(self, j):
        m = self.modt
        h, hk = self.hbuf.next()
        self.load_h(h, hk, j)
        t1, t1k = self.t1buf.next()
        ss, ssk = self.ssb.next()
        self.act(t1[:], h[:], AF.Square, [hk], [t1k, ssk], accum=ss[:, 0:1])
        self.rstd(ss, ssk)
        self.stt("dve", t1[:], h[:], ss[:, 1:2], m["gs"][:], ALU.mult, ALU.mult, [hk, ssk, "gs", t1k], [t1k])
        a, ak = self.abuf.next()
        self.tt("pool", a[:], t1[:], m["sh"][:], ALU.add, [t1k, "sh"], [ak])
        return a, ak

    def norm_stage(self, si):
        s0, NT = self.sts[si]
        aT, aTk = self.aT.next()
        for jj in range(NT // 128):
            j = s0 // 128 + jj
            a, ak = self.norm_tile(j)
            pt, ptk = self.ptr.next()
            for k in range(KC):
                self.tr(pt[:, k * 128:(k + 1) * 128], a[:, k * 128:(k + 1) * 128], self.ident[:], [ak, "ident"], [ptk])
            self.cp("act", aT[:, :, jj * 128:(jj + 1) * 128], pt[:].rearrange("p (k t) -> p k t", k=KC), [ptk], [(aTk, jj)])
        return aT, aTk

    def load_w(self, dst_view, src_view, key, ncols_piece=2048):
        K = dst_view.shape[1]
        N = dst_view.shape[2]
        for k in range(K):
            for c in range(0, N, ncols_piece):
                ce = min(N, c + ncols_piece)
                self.dma("q_pool", dst_view[:, k, c:ce], src_view[:, k, c:ce], (), [(key, k, c // ncols_piece)])

    def sis(self, skip_ctx):
        return [si for si in range(len(self.sts)) if not (skip_ctx and si == 0)]

    def pipelined(self, sis, stage_a, stage_b, l, which, norm_g):
        prev = None
        cur_row = None
        for si in sis:
            row = 1 if si == 0 else 0
            if row != cur_row:
                if prev is not None:
                    stage_b(*prev)
                    prev = None
                self.load_mod(l, which, norm_g, row)
                cur_row = row
            a = stage_a(si)
            if prev is not None:
                stage_b(*prev)
            prev = (si,) + tuple(a)
        if prev is not None:
            stage_b(*prev)

    def mlp1_phase(self, l, skip_ctx=False):
        P = self.P
        P.phase_begin()
        wb = P.sb([128, KC * DFF], BF16, "w1")
        wkey = "w1"
        w1 = wb[:, :].rearrange("p (k n) -> p k n", k=KC)
        self.load_w(w1, self.mlp_w1[l].rearrange("(k p) n -> p k n", p=128), wkey)
        self.alloc_norm()
        pacc = Rot(P, 4, [128, 512], F32, "pacc", psum=True)
        rbuf = Rot(P, 3, [128, 512], F32, "rbuf")
        ubuf = Rot(P, 2, [128, 4, 512], BF16, "ubuf")

        def stage_b(si, aT, aTk):
            s0, NT = self.sts[si]
            nj = NT // 128
            for fo4 in range(8):
                ut, uk = ubuf.next()
                for q in range(4):
                    fo = fo4 * 4 + q
                    pa, pk = pacc.next()
                    for k in range(KC):
                        self.mm(pa[:, :NT], w1[:, k, fo * 128:(fo + 1) * 128], aT[:, k, :NT], k == 0, k == KC - 1,
                                [(wkey, k, fo // 16)] + [(aTk, jj) for jj in range(nj)], [pk])
                    rt, rk = rbuf.next()
                    self.act(rt[:, :NT], pa[:, :NT], AF.Relu, [pk], [rk])
                    self.tt("dve", ut[:, q, :NT], rt[:, :NT], rt[:, :NT], ALU.mult, [rk], [(uk, q)])
                self.dma("q_pool", self.U[fo4 * 4:(fo4 + 1) * 4, :, s0:s0 + NT].rearrange("f p t -> p f t"), ut[:, :, :NT],
                         [(uk, q) for q in range(4)], [("U", si, fo4)])

        self.pipelined(self.sis(skip_ctx), self.norm_stage, stage_b, l, 1, self.norm2_g)
        P.phase_end()

    def mlp2_phase(self, l, skip_ctx=False, final=False):
        P = self.P
        P.phase_begin()
        wb = P.sb([128, 32 * D], BF16, "w2")
        wkey = "w2"
        w2 = wb[:, :].rearrange("p (k n) -> p k n", k=32)
        self.load_w(w2, self.mlp_w2[l].rearrange("(k p) n -> p k n", p=128), wkey, ncols_piece=1024)
        self.alloc_norm(need_aT=False)
        pacc = Rot(P, 4, [128, 512], F32, "pacc", psum=True)
        u2buf = Rot(P, 2, [128, 32, 512], BF16, "u2buf")
        m = self.modt
        fg = None
        if final:
            fg = P.sb([128, D], F32, "fg")
            self.dma("q_sp", fg[:], self.final_g[0:1, :].partition_broadcast(128), (), ["fg"])
        cur_row = None
        for si in self.sis(skip_ctx):
            row = 1 if si == 0 else 0
            if row != cur_row:
                self.load_mod(l, 1, None, row)
                cur_row = row
            s0, NT = self.sts[si]
            nj = NT // 128
            ut, uk = u2buf.next()
            for f8 in range(4):
                self.dma("q_sp", ut[:, f8 * 8:(f8 + 1) * 8, :NT], self.U[f8 * 8:(f8 + 1) * 8, :, s0:s0 + NT].rearrange("f p t -> p f t"),
                         [("U", si, f8 * 2), ("U", si, f8 * 2 + 1)], [(uk, f8)])
            for jj in range(nj):
                j = s0 // 128 + jj
                h, hk = self.hbuf.next()
                self.load_h(h, hk, j)
                t1, t1k = self.t1buf.next()
                for nh in range(2):
                    pa, pk = pacc.next()
                    th, thk = self.thalf.next()
                    for k in range(32):
                        self.mm(pa[:, :], ut[:, k, jj * 128:(jj + 1) * 128], w2[:, k, nh * 512:(nh + 1) * 512], k == 0, k == 31,
                                [(wkey, k, 0), (uk, k // 8)], [pk])
                    self.tt("dve", th[:, :], pa[:, :], m["gt"][:, nh * 512:(nh + 1) * 512], ALU.mult,
                            [pk, "gt"], [thk])
                    self.tt("pool", h[:, nh * 512:(nh + 1) * 512], th[:, :], h[:, nh * 512:(nh + 1) * 512], ALU.add,
                            [thk, hk], [hk])
                if not final:
                    self.dma("q_pool", self.hrow(j), h[:], [hk], [("hres", j)])
                else:
                    t2, t2k = self.t1buf.next()
                    ss, ssk = self.ssb.next()
                    self.act(t2[:], h[:], AF.Square, [hk], [t2k, ssk], accum=ss[:, 0:1])
                    self.rstd(ss, ssk)
                    self.stt("dve", t2[:], h[:], ss[:, 1:2], fg[:], ALU.mult, ALU.mult, [hk, ssk, "fg", t2k], [t2k])
                    self.dma("q_pool", self.out[(j - 2) * 128:(j - 1) * 128, :], t2[:], [t2k], [("out", j)])
        P.phase_end()
        self.h_in_src = False

    def gla_phase_p(self, l):
        P = self.P
        jg = self.kinds[:l + 1].count(0) - 1
        P.phase_begin()
        wb = P.sb([128, KC * 3072], BF16, "win")
        win = wb[:, :].rearrange("p (k n) -> p k n", k=KC)
        self.load_w(win, self.gla_w_in[jg].rearrange("(k p) n -> p k n", p=128), "win", ncols_piece=1024)
        wa1 = P.sb([128, KC, 32], BF16, "wa1")
        self.dma("q_pool", wa1[:], self.gla_wa1[jg].rearrange("(k p) n -> p k n", p=128), (), ["wa1"])
        wa2 = P.sb([32, 1024], BF16, "wa2")
        self.dma("q_pool", wa2[:], self.gla_wa2[jg], (), ["wa2"])
        nba = P.sb([128, 8], F32, "nba")
        self.dma("q_sp", nba[:], self.gla_ba[jg], (), ["nba"])
        self.ts("dve", nba[:], nba[:], -1.0, None, ALU.mult, None, ["nba"], ["nba"])
        gc = P.sb([128, 768], F32, "gc")
        self.dma("q_sp", gc[:], self.glacf[:], (), ["gc"])
        self.alloc_norm()
        pacc = Rot(P, 4, [128, 512], F32, "pacc", psum=True)
        pz = Rot(P, 2, [128, 512], F32, "pz", psum=True)
        qkb = Rot(P, 2, [128, 8, 512], F32, "qkb")
        srb = Rot(P, 2, [128, 8, 512], BF16, "srb")
        vtb = Rot(P, 2, [128, D], BF16, "vtb")
        utb = Rot(P, 2, [32, 512], BF16, "utb")
        tA = Rot(P, 3, [128, 512], F32, "tA")
        tB = Rot(P, 2, [128, 512], F32, "tB")
        tC = Rot(P, 2, [128, 512], F32, "tC")
        tD = Rot(P, 2, [128, 512], F32, "tD")
        ob = Rot(P, 4, [128, 512], BF16, "ob")
        kw4 = Rot(P, 2, [128, 4, 512], BF16, "kw4")
        kwt = Rot(P, 2, [128, 512], BF16, "kwt")
        decb = Rot(P, 2, [128, 32], F32, "decb")

        def stage_b(si, aT, aTk):
            s0, NT = self.sts[si]
            nj = NT // 128
            nch = NT // 64
            aks = [(aTk, jj) for jj in range(nj)]
            qk, qkk = qkb.next()
            for i in range(8):
                pa, pk = pacc.next()
                for k in range(KC):
                    self.mm(pa[:, :NT], win[:, k, i * 128:(i + 1) * 128], aT[:, k, :NT], k == 0, k == KC - 1, [("win", k, 0)] + aks, [pk])
                self.act(qk[:, i, :NT], pa[:, :NT], AF.Copy, [pk], [(qkk, i)], scale=(128 ** -0.5 if i < 4 else 1.0))
            sr, srk = srb.next()
            for i in range(8):
                pa, pk = pacc.next()
                for k in range(KC):
                    self.mm(pa[:, :NT], win[:, k, 2048 + i * 128:2048 + (i + 1) * 128], aT[:, k, :NT], k == 0, k == KC - 1, [("win", k, 2)] + aks, [pk])
                self.act(sr[:, i, :NT], pa[:, :NT], AF.Silu, [pk], [(srk, i)])
            self.dma("q_pool", self.SR[:, :, s0:s0 + NT].rearrange("f p t -> p f t"), sr[:, :, :NT], [(srk, i) for i in range(8)], [])
            for jj in range(nj):
                vt, vtk = vtb.next()
                for nh in range(2):
                    pa, pk = pacc.next()
                    th, thk = self.thalf.next()
                    for k in range(KC):
                        self.mm(pa[:, :], aT[:, k, jj * 128:(jj + 1) * 128], win[:, k, 1024 + nh * 512:1024 + (nh + 1) * 512], k == 0, k == KC - 1,
                                [("win", k, 1), (aTk, jj)], [pk])
                    self.cp("dve", vt[:, nh * 512:(nh + 1) * 512], pa[:, :], [pk], [(vtk, nh)])
                self.dma("q_pool", self.V_tm[s0 + jj * 128:s0 + (jj + 1) * 128, :], vt[:], [(vtk, 0), (vtk, 1)], [])
            ut, utk = utb.next()
            pu, puk = pz.next()
            for k in range(KC):
                self.mm(pu[0:32, :NT], wa1[:, k, :], aT[:, k, :NT], k == 0, k == KC - 1, ["wa1"] + aks, [puk])
            self.cp("dve", ut[:, :NT], pu[0:32, :NT], [puk], [utk])
            for d in range(2):
                kw, kwk = kw4.next()
                dec, deck = decb.next()
                for h in range(4):
                    q = qk[:, h, :NT]
                    kk = qk[:, 4 + h, :NT]
                    pzt, pzk = pz.next()
                    c0 = d * 512 + h * 128
                    self.mm(pzt[:, :NT], wa2[0:32, c0:c0 + 128], ut[0:32, :NT], True, True, ["wa2", utk], [pzk])
                    e, ek = tA.next()
                    self.act(e[:, :NT], pzt[:, :NT], AF.Exp, [pzk, "nba"], [ek], scale=-1.0, bias=nba[:, d * 4 + h:d * 4 + h + 1])
                    sp, spk = tB.next()
                    self.act(sp[:, :NT], e[:, :NT], AF.Ln, [ek], [spk], bias=1.0)
                    cs, csk = tC.next()
                    self.scan(cs[:, :NT], gc[:, :NT], sp[:, :NT], 0.0, ALU.mult, ALU.add, ["gc", spk], [csk])
                    cs3 = cs[:, :NT].rearrange("p (c t) -> p c t", t=64)
                    cl_b = cs3[:, :, 63:64].to_broadcast([128, nch, 64])
                    dd, ddk = tD.next()
                    dd3 = dd[:, :NT].rearrange("p (c t) -> p c t", t=64)
                    sp3 = sp[:, :NT].rearrange("p (c t) -> p c t", t=64)
                    if d == 0:
                        xq, xs = cs, -1.0 / 16
                        xk, xks = cs, 1.0 / 16
                        self.tt("dve", dd3, cs3, cl_b, ALU.subtract, [csk], [ddk])
                        xw, xws = dd, 1.0 / 16
                        xqk = xkk = csk
                        xwk = ddk
                    else:
                        t1, t1k_ = tD.next()
                        t13 = t1[:, :NT].rearrange("p (c t) -> p c t", t=64)
                        self.tt("dve", t13, sp3, cs3, ALU.subtract, [csk, spk], [t1k_])
                        self.tt("dve", dd3, t13, cl_b, ALU.add, [t1k_, csk], [ddk])
                        xq, xs, xqk = dd, -1.0 / 16, ddk
                        xk, xks, xkk = dd, 1.0 / 16, ddk
                        xw, xws, xwk = t1, 1.0 / 16, t1k_
                    e1, e1k = tA.next()
                    self.act(e1[:, :NT], xq[:, :NT], AF.Exp, [xqk], [e1k], scale=xs)
                    o1, o1k = ob.next()
                    self.tt("pool", o1[:, :NT], q, e1[:, :NT], ALU.mult, [(qkk, h), e1k], [o1k])
                    self.dma("q_pool", self.QD[d, h, :, s0:s0 + NT], o1[:, :NT], [o1k], [])
                    e2, e2k = tA.next()
                    self.act(e2[:, :NT], xk[:, :NT], AF.Exp, [xkk], [e2k], scale=xks)
                    o2, o2k = ob.next()
                    self.tt("pool", o2[:, :NT], kk, e2[:, :NT], ALU.mult, [(qkk, 4 + h), e2k], [o2k])
                    self.dma("q_pool", self.KD[d, h, :, s0:s0 + NT], o2[:, :NT], [o2k], [])
                    e3, e3k = tA.next()
                    self.act(e3[:, :NT], xw[:, :NT], AF.Exp, [xwk], [e3k], scale=xws)
                    self.tt("pool", kw[:, h, :NT], kk, e3[:, :NT], ALU.mult, [(qkk, 4 + h), e3k], [(kwk, h)])
                    self.act(self._decv(dec, h, nch), cs3[:, :, 63], AF.Exp, [csk], [(deck, h)], scale=-1.0 / 16)
                self.dma("q_pool", self.DEC[d, :, :, s0 // 64:s0 // 64 + nch].rearrange("h p c -> p h c"),
                         self._decall(dec, nch), [(deck, h) for h in range(4)], [])
                for jj in range(nj):
                    pt, ptk = self.ptr.next()
                    for h in range(4):
                        self.tr(pt[:, h * 128:(h + 1) * 128], kw[:, h, jj * 128:(jj + 1) * 128], self.ident[:], [(kwk, h), "ident"], [ptk])
                    kt, ktk = kwt.next()
                    self.cp("act", kt[:], pt[:, 0:512], [ptk], [ktk])
                    self.dma("q_pool", self.KW_tm[d, s0 + jj * 128:s0 + (jj + 1) * 128, :], kt[:], [ktk], [])

        self.pipelined(self.sis(False), self.norm_stage, stage_b, l, 0, self.norm1_g)
        P.phase_end()

    def _decv(self, dec, h, nch):
        return dec[:, h * 8:h * 8 + nch]

    def _decall(self, dec, nch):
        return dec[:, :].rearrange("p (h c) -> p h c", h=4)[:, :, :nch]

    def tile_order(self, d):
        sis = list(range(len(self.sts)))
        if d == 1:
            sis = [0] + sis[:0:-1]
        return sis

    def gla_phase_s(self, l, d):
        P = self.P
        P.phase_begin()
        gc = P.sb([128, 768], F32, "gc")
        self.dma("q_sp", gc[:], self.glacf[:], (), ["gc"])
        mask = gc[:, 512 + d * 128:512 + (d + 1) * 128]
        qdb = Rot(P, 2, [128, 4, 512], BF16, "qdb")
        kdb = Rot(P, 2, [128, 4, 512], BF16, "kdb")
        kwb = Rot(P, 2, [128, 4, 512], BF16, "kwb")
        vtb = Rot(P, 2, [128, 4, D], BF16, "vtb")
        decb = Rot(P, 2, [128, 4, 8], F32, "decb")
        S = P.sb([128, 4, 256], F32, "S")
        Sb = P.sb([128, 4, 256], BF16, "Sb")
        attb = Rot(P, 4, [128, 128], BF16, "attb")
        otb = Rot(P, 2, [128, 8, 512], F32, "otb")
        patt = Rot(P, 2, [128, 512], F32, "patt", psum=True)
        po = Rot(P, 3, [128, 512], F32, "po", psum=True)
        pst = Rot(P, 3, [128, 512], F32, "pst", psum=True)
        for h in range(4):
            self.memset("dve", S[:, h, :], 0.0, [("S", h)])
            self.memset("pool", Sb[:, h, :], 0.0, [("Sb", h)])
        for si in self.tile_order(d):
            s0, NT = self.sts[si]
            nj = NT // 128
            nch = NT // 64
            qd, qdk = qdb.next()
            kd, kdk = kdb.next()
            kw, kwk = kwb.next()
            vt, vtk = vtb.next()
            dec, deck = decb.next()
            ot, otk = otb.next()
            self.dma("q_sp", qd[:, :, :NT], self.QD[d, :, :, s0:s0 + NT].rearrange("h p t -> p h t"), [], [qdk])
            self.dma("q_sp", kd[:, :, :NT], self.KD[d, :, :, s0:s0 + NT].rearrange("h p t -> p h t"), [], [kdk])
            self.dma("q_sp", kw[:, :nj, :], self.KW_tm[d, s0:s0 + NT, :].rearrange("(j p) f -> p j f", p=128), [], [kwk])
            self.dma("q_sp", vt[:, :nj, :], self.V_tm[s0:s0 + NT, :].rearrange("(j p) f -> p j f", p=128), [], [vtk])
            self.dma("q_sp", dec[:, :, :nch], self.DEC[d, :, :, s0 // 64:s0 // 64 + nch].rearrange("h p c -> p h c"), [], [deck])
            jjs = list(range(nj)) if d == 0 else list(range(nj - 1, -1, -1))
            cs_ = (0, 1) if d == 0 else (1, 0)
            for jj in jjs:
                tsl = slice(jj * 128, (jj + 1) * 128)
                pos = []
                for h in range(4):
                    pa, pak = patt.next()
                    self.mm(pa[:, 0:128], kd[:, h, tsl], qd[:, h, tsl], True, True, [kdk, qdk], [pak])
                    at, atk = attb.next()
                    self.tt("dve", at[:], pa[:, 0:128], mask, ALU.mult, [pak, "gc"], [atk])
                    p_ob, pok = po.next()
                    p_o = p_ob[:, 0:256].rearrange("p (v t) -> p v t", v=2)
                    for vc in range(2):
                        self.mm(p_o[:, vc, :], vt[:, jj, h * 256 + vc * 128:h * 256 + (vc + 1) * 128], at[:], vc == 0, False, [vtk, atk], [pok], sgc=True)
                    pos.append((p_o, pok))
                    for ci, c in enumerate(cs_):
                        csl = slice(jj * 128 + c * 64, jj * 128 + (c + 1) * 64)
                        rows = slice(c * 64, (c + 1) * 64)
                        for vc in range(2):
                            self.mm(p_o[:, vc, c * 64:(c + 1) * 64], Sb[:, h, vc * 128:(vc + 1) * 128], qd[:, h, csl], False, ci == 1,
                                    [("Sb", h), qdk], [pok], sgc=True)
                        ps, psk = pst.next()
                        self.mm(ps[:, 0:256], kw[rows, jj, h * 128:(h + 1) * 128], vt[rows, jj, h * 256:(h + 1) * 256], True, True, [kwk, vtk], [psk])
                        ch = jj * 2 + c
                        self.stt("dve", S[:, h, :], S[:, h, :], dec[:, h, ch:ch + 1], ps[:, 0:256], ALU.mult, ALU.add, [("S", h), deck, psk], [("S", h)])
                        self.cp("act", Sb[:, h, :], S[:, h, :], [("S", h)], [("Sb", h)])
                    self.cp("act", ot[:, 2 * h:2 * h + 2, tsl], p_o[:, :, :], [pok], [(otk, jj, h)])
            self.dma("q_pool", self.OO[d, :, :, s0:s0 + NT].rearrange("f p t -> p f t"), ot[:, :, :NT],
                     [(otk, jj, h) for jj in range(nj) for h in range(4)], [])
        P.phase_end()

    def gla_phase_o(self, l, need_ctx):
        P = self.P
        jg = self.kinds[:l + 1].count(0) - 1
        P.phase_begin()
        wb = P.sb([128, KC * D], BF16, "wo")
        wo = wb[:, :].rearrange("p (k n) -> p k n", k=KC)
        self.load_w(wo, self.gla_w_o[jg].rearrange("(k p) n -> p k n", p=128), "wo", ncols_piece=1024)
        gh = P.sb([128, 8], F32, "gh")
        self.dma("q_sp", gh[:], self.gla_gh[jg], (), ["gh"])
        ones = P.sb([128, 128], BF16, "ones")
        self.dma("q_sp", ones[:], self.onesb[:], (), ["ones"])
        self.alloc_norm(need_aT=False)
        m = self.modt
        pacc = Rot(P, 4, [128, 512], F32, "pacc", psum=True)
        o0b = Rot(P, 2, [128, 8, 512], F32, "o0b")
        o1b = Rot(P, 1, [128, 8, 512], F32, "o1b")
        srb = Rot(P, 2, [128, 8, 512], BF16, "srb")
        sqb = Rot(P, 1, [128, 8, 512], BF16, "sqb")
        yTb = Rot(P, 2, [128, 8, 512], BF16, "yTb")
        rsb = Rot(P, 2, [128, 4, 512], F32, "rsb")
        tmpb = Rot(P, 2, [128, 512], F32, "tmpb")
        cur_row = None
        for si in self.sis(not need_ctx):
            row = 1 if si == 0 else 0
            if row != cur_row:
                self.load_mod(l, 0, None, row)
                cur_row = row
            s0, NT = self.sts[si]
            nj = NT // 128
            o0, o0k = o0b.next()
            o1, o1k = o1b.next()
            sr, srk = srb.next()
            self.dma("q_sp", o0[:, :, :NT], self.OO[0, :, :, s0:s0 + NT].rearrange("f p t -> p f t"), [], [o0k])
            self.dma("q_sp", o1[:, :, :NT], self.OO[1, :, :, s0:s0 + NT].rearrange("f p t -> p f t"), [], [o1k])
            self.dma("q_sp", sr[:, :, :NT], self.SR[:, :, s0:s0 + NT].rearrange("f p t -> p f t"), [], [srk])
            self.tt("pool", o0[:, :, :NT], o0[:, :, :NT], o1[:, :, :NT], ALU.add, [o0k, o1k], [o0k])
            sq, sqk = sqb.next()
            self.act(sq[:, :, :NT], o0[:, :, :NT], AF.Square, [o0k], [sqk])
            rs, rsk = rsb.next()
            for h in range(4):
                pa, pk = pacc.next()
                for vc in range(2):
                    self.mm(pa[:, :NT], ones[:], sq[:, 2 * h + vc, :NT], vc == 0, vc == 1, ["ones", sqk], [pk])
                self.act(rs[:, h, :NT], pa[:, :NT], AF.Sqrt, [pk], [(rsk, h)], scale=1.0 / 256, bias=EPS)
                self.P.op("dve", (lambda rs=rs, h=h, NT=NT: (lambda e: e.reciprocal(rs[:, h, :NT], rs[:, h, :NT])))(), [(rsk, h)], [(rsk, h)])
            yT, yTk = yTb.next()
            for i in range(8):
                tm, tmk = tmpb.next()
                self.stt("dve", tm[:, :NT], o0[:, i, :NT], gh[:, i:i + 1], rs[:, i // 2, :NT], ALU.mult, ALU.mult, [o0k, "gh", (rsk, i // 2)], [tmk])
                self.tt("pool", yT[:, i, :NT], tm[:, :NT], sr[:, i, :NT], ALU.mult, [tmk, srk], [(yTk, i)])
            yks = [(yTk, i) for i in range(8)]
            for jj in range(nj):
                j = s0 // 128 + jj
                h_, hk = self.hbuf.next()
                self.load_h(h_, hk, j)
                t1, t1k = self.t1buf.next()
                for nh in range(2):
                    pa, pk = pacc.next()
                    th, thk = self.thalf.next()
                    for k in range(KC):
                        self.mm(pa[:, :], yT[:, k, jj * 128:(jj + 1) * 128], wo[:, k, nh * 512:(nh + 1) * 512], k == 0, k == KC - 1,
                                [("wo", k, 0), (yTk, k)], [pk])
                    self.tt("dve", th[:, :], pa[:, :], m["gt"][:, nh * 512:(nh + 1) * 512], ALU.mult, [pk, "gt"], [thk])
                    self.tt("pool", h_[:, nh * 512:(nh + 1) * 512], th[:, :], h_[:, nh * 512:(nh + 1) * 512], ALU.add,
                            [thk, hk], [hk])
                self.dma("q_pool", self.hrow(j), h_[:], [hk], [])
        P.phase_end()
        self.h_in_src = False

    def units256(self):
        return [(s0, 256) for s0 in range(0, self.T, 256)]

    def mlstm_phase_p1(self, l):
        P = self.P
        jm = self.kinds[:l + 1].count(1) - 1
        P.phase_begin()
        wb = P.sb([128, KC * 4096], BF16, "wup")
        wup = wb[:, :].rearrange("p (k n) -> p k n", k=KC)
        self.load_w(wup, self.ml_w_up[jm].rearrange("(k p) n -> p k n", p=128), "wup")
        self.alloc_norm()
        pacc = Rot(P, 4, [128, 512], F32, "pacc", psum=True)
        obuf = Rot(P, 3, [128, 4, 512], BF16, "obuf")

        def stage_b(si, aT, aTk):
            s0, NT = self.sts[si]
            nj = NT // 128
            aks = [(aTk, jj) for jj in range(nj)]
            for i4 in range(8):
                ot, otk = obuf.next()
                for q in range(4):
                    i = i4 * 4 + q
                    pa, pk = pacc.next()
                    for k in range(KC):
                        self.mm(pa[:, :NT], wup[:, k, i * 128:(i + 1) * 128], aT[:, k, :NT], k == 0, k == KC - 1, [("wup", k, i // 16)] + aks, [pk])
                    if i < 16:
                        self.cp("act", ot[:, q, :NT], pa[:, :NT], [pk], [(otk, q)])
                    else:
                        self.act(ot[:, q, :NT], pa[:, :NT], AF.Silu, [pk], [(otk, q)])
                dst = self.XM if i4 < 4 else self.SZ
                i0 = (i4 % 4) * 4
                self.dma("q_pool", dst[i0:i0 + 4, :, s0:s0 + NT].rearrange("f p t -> p f t"), ot[:, :, :NT], [(otk, q) for q in range(4)], [])

        self.pipelined(self.sis(False), self.norm_stage, stage_b, l, 0, self.norm1_g)
        P.phase_end()

    def mlstm_phase_p2(self, l):
        P = self.P
        jm = self.kinds[:l + 1].count(1) - 1
        P.phase_begin()
        self.load_ident()
        NT = 256
        nj, nch = 2, 4
        bd = P.sb([128, 48, 128], BF16, "bd")
        for m_ in range(3):
            self.dma("q_pool", bd[:, m_ * 16:(m_ + 1) * 16, :], self.ml_bd[jm, m_].rearrange("c p n -> p c n"), (), ["bd"])
        wgI = P.sb([128, 48, 64], BF16, "wgI")
        wgF = P.sb([128, 48, 64], BF16, "wgF")
        self.dma("q_pool", wgI[:], self.ml_wgI[jm], (), ["wg"])
        self.dma("q_pool", wgF[:], self.ml_wgF[jm], (), ["wg"])
        cw = P.sb([128, 16, 4], F32, "cw")
        cbias = P.sb([128, 16], F32, "cbias")
        self.dma("q_sp", cw[:], self.ml_convw[jm], (), ["cw"])
        self.dma("q_sp", cbias[:], self.ml_convb[jm], (), ["cw"])
        bI = P.sb([64, 1], F32, "bI")
        nbF = P.sb([64, 1], F32, "nbF")
        self.dma("q_sp", bI[:], self.ml_bgI[jm], (), ["bI"])
        self.dma("q_sp", nbF[:], self.ml_bgF[jm], (), ["nbF"])
        self.ts("dve", nbF[:], nbF[:], -1.0, None, ALU.mult, None, ["nbF"], ["nbF"])
        gc = P.sb([128, 768], F32, "gc")
        self.dma("q_sp", gc[:], self.glacf[:], (), ["gc"])
        sel = P.sb([64, 8, 128], F32, "sel")
        self.dma("q_sp", sel[:], self.ml_sel[:], (), ["sel"])
        identf = P.sb([128, 128], F32, "identf")
        self.dma("q_sp", identf[:], self.identf_d[:], (), ["identf"])
        pacc = Rot(P, 2, [128, 512], F32, "pacc", psum=True)
        pgI = P.ps([128, 512], F32, "pgI")
        pgF = P.ps([128, 512], F32, "pgF")
        pb = Rot(P, 1, [128, 512], F32, "pb", psum=True)
        pcx = P.ps([128, 512], F32, "pcx")
        ptkv = P.ps([128, 2048], BF16, "ptkv")
        xwb = Rot(P, 2, [128, 16, NT + 32], BF16, "xwb")
        xcb = Rot(P, 2, [128, 16, NT], BF16, "xcb")
        qkvb = Rot(P, 1, [128, 48, NT], BF16, "qkvb")
        qsb = Rot(P, 1, [128, 16, NT], BF16, "qsb")
        qbb = Rot(P, 3, [128, 4, NT], BF16, "qbb")
        accb = Rot(P, 4, [128, NT], F32, "accb")
        gt_ = {nm: P.sb([64, NT], F32, "g" + nm) for nm in ("LI", "E", "SP", "CS", "BN", "EB", "T1", "COL", "T2", "CW")}
        car = P.sb([64, 8], F32, "car")
        carb = Rot(P, 2, [128, 8, nch], F32, "carb")
        cwtb = Rot(P, 2, [128, 128], F32, "cwtb")
        vxb = Rot(P, 2, [128, 4, 640], BF16, "vxb")
        for i in range(2):
            self.memset("pool", vxb.bufs[i][:, :, 512:640], 1.0, [(vxb.keys[i], "ones")])
        kwb = Rot(P, 2, [128, 2048], BF16, "kwb")
        s_q = 512.0 ** -0.5
        nunits = self.T // 256
        for u in range(nunits):
            s0 = u * 256
            seq_lo, seq_hi = (0, TC) if u == 0 else (TC, self.T)
            xw, xwk = xwb.next()
            lo = max(s0 - 2, seq_lo)
            hi = min(s0 + NT + 1, seq_hi)
            if lo > s0 - 2:
                self.memset("pool", xw[:, :, 14:16], 0.0, [(xwk, "L")])
            if hi < s0 + NT + 1:
                self.memset("pool", xw[:, :, NT + 16:NT + 17], 0.0, [(xwk, "R")])
            self.dma("q_sp", xw[:, :, 14 + lo - (s0 - 2):14 + hi - (s0 - 2)], self.XM[:, :, lo:hi].rearrange("c p t -> p c t"), [],
                     [(xwk, "L"), (xwk, "M"), (xwk, "R")])
            xwks = [(xwk, "L"), (xwk, "M"), (xwk, "R")]
            xc, xck = xcb.next()
            for c in range(16):
                eng = "dve"
                ac, ack = accb.next()
                self.ts(eng, ac[:, :], xw[:, c, 14:14 + NT], cw[:, c, 0:1], None, ALU.mult, None, xwks + ["cw"], [ack])
                for j in range(1, 4):
                    self.stt(eng, ac[:, :], xw[:, c, 14 + j:14 + NT + j], cw[:, c, j:j + 1], ac[:, :], ALU.mult, ALU.add, xwks + ["cw", ack], [ack])
                self.act(xc[:, c, :], ac[:, :], AF.Silu, [ack, "cw"], [(xck, c)], bias=cbias[:, c:c + 1])
            xcks = [(xck, c) for c in range(16)]
            self.dma("q_pool", self.XC[:, :, s0:s0 + NT].rearrange("c p t -> p c t"), xc[:, :, :], xcks, [])
            qkv, qkvk = qkvb.next()
            qs, qsk = qsb.next()
            for m_ in range(3):
                for c in range(16):
                    pa, pk = pacc.next()
                    if m_ < 2:
                        self.mm(pa[:, :NT], bd[:, m_ * 16 + c, :], xc[:, c, :], True, True, ["bd", (xck, c)], [pk])
                    else:
                        self.mm(pa[:, :NT], bd[:, m_ * 16 + c, :], xw[:, c, 16:NT + 16], True, True, ["bd"] + xwks, [pk])
                    self.cp("act", qkv[:, m_ * 16 + c, :], pa[:, :NT], [pk], [(qkvk, m_ * 16 + c)])
                    if m_ == 0:
                        self.ts("pool", qs[:, c, :], qkv[:, c, :], s_q, None, ALU.mult, None, [(qkvk, c)], [(qsk, c)])
            self.dma("q_pool", self.KT[:, :, s0:s0 + NT].rearrange("c p t -> p c t"), qkv[:, 16:32, :], [(qkvk, 16 + c) for c in range(16)], [])
            for c in range(48):
                self.mm(pgI[0:64, :NT], wgI[:, c, :], qkv[:, c, :], c == 0, c == 47, ["wg", (qkvk, c)], ["pgI"])
            for c in range(48):
                self.mm(pgF[0:64, :NT], wgF[:, c, :], qkv[:, c, :], c == 0, c == 47, ["wg", (qkvk, c)], ["pgF"])
            g = gt_
            self.act(g["LI"][:], pgI[0:64, :NT], AF.Identity, ["pgI", "bI"], ["LI"], bias=bI[:, 0:1])
            self.act(g["E"][:], pgF[0:64, :NT], AF.Exp, ["pgF", "nbF"], ["E"], scale=-1.0, bias=nbF[:, 0:1])
            self.act(g["SP"][:], g["E"][:], AF.Ln, ["E"], ["SP"], bias=1.0)
            self.scan(g["CS"][:], gc[0:64, :NT], g["SP"][:], 0.0, ALU.mult, ALU.add, ["gc", "SP"], ["CS"])
            cs3 = g["CS"][:].rearrange("p (c t) -> p c t", t=64)
            self.cp("act", g["BN"][0:32, :], g["CS"][0:32, :], ["CS"], [("BN", 0)])
            self.tt("dve", g["BN"][32:64, :], g["SP"][32:64, :], g["CS"][32:64, :], ALU.subtract, ["SP", "CS"], [("BN", 1)])
            bn3 = g["BN"][:].rearrange("p (c t) -> p c t", t=64)
            self.tt("dve", bn3[32:64], bn3[32:64], cs3[32:64, :, 63:64].to_broadcast([32, nch, 64]), ALU.add, [("BN", 1), "CS"], [("BN", 1)])
            bnk = [("BN", 0), ("BN", 1)]
            self.act(g["EB"][:], g["BN"][:], AF.Exp, bnk, ["EB"], scale=-1.0)
            self.tt("dve", g["T1"][:], g["LI"][:], g["BN"][:], ALU.add, ["LI"] + bnk, ["T1"])
            self.act(g["COL"][:], g["T1"][:], AF.Exp, ["T1"], ["COL"])
            t13 = g["T1"][:].rearrange("p (c t) -> p c t", t=64)
            t23 = g["T2"][:].rearrange("p (c t) -> p c t", t=64)
            self.tt("dve", t23, t13, cs3[:, :, 63:64].to_broadcast([64, nch, 64]), ALU.subtract, ["T1", "CS"], ["T2"])
            self.act(g["CW"][:], g["T2"][:], AF.Exp, ["T2"], ["CW"])
            self.act(car[:, 0:nch], cs3[:, :, 63], AF.Exp, ["CS"], ["car"], scale=-1.0)
            for r8 in range(8):
                d, h = r8 // 4, r8 % 4
                pbt, pbk = pb.next()
                self.mm(pbt[:, :NT], sel[:, r8, :], g["EB"][:, :], True, True, ["sel", "EB"], [pbk])
                qb, qbk = qbb.next()
                self.tt("dve", qb[:, :, :], qs[:, h * 4:(h + 1) * 4, :], pbt[:, :NT].unsqueeze(1).to_broadcast([128, 4, NT]), ALU.mult,
                        [pbk] + [(qsk, h * 4 + i) for i in range(4)], [qbk])
                self.dma("q_pool", self.QB[d, h * 4:(h + 1) * 4, :, s0:s0 + NT].rearrange("c p t -> p c t"), qb[:, :, :], [qbk], [])
            for r8 in range(8):
                self.mm(pcx[:, 256 + r8 * nch:256 + (r8 + 1) * nch], sel[:, r8, :], car[:, 0:nch], r8 == 0, r8 == 7, ["sel", "car"], ["pcx"], sgc=True)
            cb_, cbk = carb.next()
            self.cp("dve", cb_[:, :, :], pcx[:, 256:256 + 8 * nch].rearrange("p (r c) -> p r c", c=nch), ["pcx"], [cbk])
            for d in range(2):
                self.dma("q_pool", self.CARB[d, :, :, s0 // 64:s0 // 64 + nch].rearrange("h p c -> p h c"), cb_[:, d * 4:(d + 1) * 4, :], [cbk], [])
            for jj in range(nj):
                tsl = slice(jj * 128, (jj + 1) * 128)
                self.tr(pcx[:, 0:64], g["COL"][:, tsl], identf[0:64, 0:64], ["COL", "identf"], ["pcx"])
                self.tr(pcx[:, 64:128], g["CW"][:, tsl], identf[0:64, 0:64], ["CW", "identf"], ["pcx"])
                ct, ctk = cwtb.next()
                self.cp("dve", ct[:, :], pcx[:, 0:128], ["pcx"], [ctk])
                self.dma("q_pool", self.CWT[s0 + jj * 128:s0 + (jj + 1) * 128, :], ct[:, :], [ctk], [])
                for c in range(16):
                    self.tr(ptkv[:, c * 128:(c + 1) * 128], qkv[:, 32 + c, tsl], self.ident[:], [(qkvk, 32 + c), "ident"], ["ptkv"])
                vx, vxk = vxb.next()
                self.cp("act", vx[:, :, 0:512], ptkv[:, :].rearrange("p (h v) -> p h v", h=4), ["ptkv"], [(vxk, "v")])
                self.dma("q_pool", self.VX[s0 + jj * 128:s0 + (jj + 1) * 128, :], vx[:, :, :].rearrange("p h v -> p (h v)"), [(vxk, "v"), (vxk, "ones")], [])
                for c in range(16):
                    self.tr(ptkv[:, c * 128:(c + 1) * 128], qkv[:, 16 + c, tsl], self.ident[:], [(qkvk, 16 + c), "ident"], ["ptkv"])
                for d in range(2):
                    kw, kwk = kwb.next()
                    for h in range(4):
                        col = ct[:, 64 + 32 * d + h:64 + 32 * d + h + 1]
                        if h < 2:
                            self.act(kw[:, h * 512:(h + 1) * 512], ptkv[:, h * 512:(h + 1) * 512], AF.Copy, ["ptkv", ctk], [(kwk, h)], scale=col)
                        else:
                            self.ts("dve", kw[:, h * 512:(h + 1) * 512], ptkv[:, h * 512:(h + 1) * 512], col, None, ALU.mult, None, ["ptkv", ctk], [(kwk, h)])
                    self.dma("q_pool", self.KWm[d, s0 + jj * 128:s0 + (jj + 1) * 128, :], kw[:, :], [(kwk, h) for h in range(4)], [])
        P.phase_end()

    def mlstm_phase_s(self, l, d):
        P = self.P
        P.phase_begin()
        NT, nj, nch = 256, 2, 4
        gc = P.sb([128, 768], F32, "gc")
        self.dma("q_sp", gc[:], self.glacf[:], (), ["gc"])
        mask = gc[:, 512 + d * 128:512 + (d + 1) * 128]
        qbb = Rot(P, 2, [128, 16, NT], BF16, "qbb")
        ktb = Rot(P, 2, [128, 16, NT], BF16, "ktb")
        vxb = Rot(P, 2, [128, nj, 2560], BF16, "vxb")
        kwb = Rot(P, 2, [128, nj, 2048], BF16, "kwb")
        cwb = Rot(P, 2, [128, nj, 128], F32, "cwb")
        crb = Rot(P, 2, [128, 4, nch], F32, "crb")
        C = P.sb([128, 16, 640], F32, "C")
        Cb = P.sb([128, 16, 640], BF16, "Cb")
        wtb = Rot(P, 3, [128, 128], BF16, "wtb")
        rdb = Rot(P, 2, [128, 128], F32, "rdb")
        htb = Rot(P, 2, [128, 16, NT], F32, "htb")
        pqk = Rot(P, 2, [128, 512], F32, "pqk", psum=True)
        pn = Rot(P, 2, [128, 1024], F32, "pn", psum=True)
        pst = Rot(P, 1, [128, 1024], F32, "pst", psum=True)
        for i in range(16):
            self.memset("dve", C[:, i, :], 0.0, [("C", i)])
            self.memset("pool", Cb[:, i, :], 0.0, [("Cb", i)])
        nunits = self.T // 256
        order = list(range(nunits)) if d == 0 else [0] + list(range(nunits - 1, 0, -1))
        for u in order:
            s0 = u * 256
            qb, qbk = qbb.next()
            kt, ktk = ktb.next()
            vx, vxk = vxb.next()
            kw, kwk = kwb.next()
            cw, cwk = cwb.next()
            cr, crk = crb.next()
            ht, htk = htb.next()
            self.dma("q_sp", qb[:, :, :], self.QB[d, :, :, s0:s0 + NT].rearrange("c p t -> p c t"), [], [qbk])
            self.dma("q_sp", kt[:, :, :], self.KT[:, :, s0:s0 + NT].rearrange("c p t -> p c t"), [], [ktk])
            self.dma("q_sp", vx[:, :, :], self.VX[s0:s0 + NT, :].rearrange("(j p) f -> p j f", p=128), [], [vxk])
            self.dma("q_sp", kw[:, :, :], self.KWm[d, s0:s0 + NT, :].rearrange("(j p) f -> p j f", p=128), [], [kwk])
            self.dma("q_sp", cw[:, :, :], self.CWT[s0:s0 + NT, :].rearrange("(j p) f -> p j f", p=128), [], [cwk])
            self.dma("q_sp", cr[:, :, :], self.CARB[d, :, :, s0 // 64:s0 // 64 + nch].rearrange("h p c -> p h c"), [], [crk])
            jjs = list(range(nj)) if d == 0 else list(range(nj - 1, -1, -1))
            cs_ = (0, 1) if d == 0 else (1, 0)
            for jj in jjs:
                tsl = slice(jj * 128, (jj + 1) * 128)
                pns = {}

                def step_A(h):
                    pq, pqk_ = pqk.next()
                    for dc in range(4):
                        self.mm(pq[:, 0:128], kt[:, h * 4 + dc, tsl], qb[:, h * 4 + dc, tsl], dc == 0, dc == 3, [ktk, qbk], [pqk_])
                    wt, wtk = wtb.next()
                    self.stt("dve", wt[:, :], pq[:, 0:128], cw[:, jj, 32 * d + h:32 * d + h + 1], mask, ALU.mult, ALU.mult, [pqk_, cwk, "gc"], [wtk])
                    pnt, pnk = pn.next()
                    pns[h] = (pnt, pnk)
                    for vc in range(4):
                        self.mm(pnt[:, vc * 128:(vc + 1) * 128], vx[:, jj, h * 640 + vc * 128:h * 640 + (vc + 1) * 128], wt[:, :], vc == 0, False,
                                [vxk, wtk], [pnk], sgc=True)
                    self.mm(pnt[:, 512:640], vx[:, jj, h * 640 + 512:h * 640 + 640], wt[:, :], True, False, [vxk, wtk], [pnk], sgc=True)

                def step_c(h, ci):
                    c = cs_[ci]
                    pnt, pnk = pns[h]
                    csl = slice(jj * 128 + c * 64, jj * 128 + (c + 1) * 64)
                    rows = slice(c * 64, (c + 1) * 64)
                    for vc in range(5):
                        dst = pnt[:, vc * 128 + c * 64:vc * 128 + (c + 1) * 64] if vc < 4 else pnt[:, 512 + c * 64:512 + (c + 1) * 64]
                        for dc in range(4):
                            self.mm(dst, Cb[:, h * 4 + dc, vc * 128:(vc + 1) * 128], qb[:, h * 4 + dc, csl], False, (ci == 1 and dc == 3),
                                    [("Cb", h * 4 + dc), qbk], [pnk], sgc=True)
                    ch = jj * 2 + c
                    for dc in range(4):
                        ps, psk = pst.next()
                        lhs = kw[rows, jj, h * 512 + dc * 128:h * 512 + (dc + 1) * 128]
                        self.mm(ps[:, 0:512], lhs, vx[rows, jj, h * 640:h * 640 + 512], True, True, [kwk, vxk], [psk])
                        self.mm(ps[:, 512:640], lhs, vx[rows, jj, h * 640 + 512:h * 640 + 640], True, True, [kwk, vxk], [psk])
                        i = h * 4 + dc
                        self.stt("dve", C[:, i, :], C[:, i, :], cr[:, h, ch:ch + 1], ps[:, 0:640], ALU.mult, ALU.add, [("C", i), crk, psk], [("C", i)])
                        self.cp("act", Cb[:, i, :], C[:, i, :], [("C", i)], [("Cb", i)])

                def step_E(h):
                    pnt, pnk = pns[h]
                    rd, rdk = rdb.next()
                    self.act(rd[:, :], pnt[:, 512:640], AF.Abs, [pnk], [rdk])
                    self.ts("dve", rd[:, :], rd[:, :], 1.0, None, ALU.max, None, [rdk], [rdk])
                    self.P.op("dve", (lambda rd=rd: (lambda e: e.reciprocal(rd[:, :], rd[:, :])))(), [rdk], [rdk])
                    self.tt("dve", ht[:, h * 4:(h + 1) * 4, tsl], pnt[:, 0:512].rearrange("p (v t) -> p v t", v=4),
                            rd[:, :].unsqueeze(1).to_broadcast([128, 4, 128]), ALU.mult, [pnk, rdk], [(htk, jj, h)])

                for h in range(4):
                    step_A(h)
                    step_c(h, 0)
                    if h >= 1:
                        step_c(h - 1, 1)
                        step_E(h - 1)
                step_c(3, 1)
                step_E(3)
            self.dma("q_pool", self.HT[d, :, :, s0:s0 + NT].rearrange("c p t -> p c t"), ht[:, :, :],
                     [(htk, jj, h) for jj in range(nj) for h in range(4)], [])
        P.phase_end()

    def mlstm_phase_o(self, l, need_ctx):
        P = self.P
        jm = self.kinds[:l + 1].count(1) - 1
        P.phase_begin()
        NT, nj = 256, 2
        wb = P.sb([128, 16 * D], BF16, "wdn")
        wd = wb[:, :].rearrange("p (k n) -> p k n", k=16)
        self.load_w(wd, self.ml_w_down[jm].rearrange("(k p) n -> p k n", p=128), "wdn", ncols_piece=1024)
        gn = P.sb([128, 16], F32, "gn")
        sk = P.sb([128, 16], F32, "sk")
        self.dma("q_sp", gn[:], self.ml_gn[jm], (), ["gn"])
        self.dma("q_sp", sk[:], self.ml_skip[jm], (), ["sk"])
        ones = P.sb([128, 128], BF16, "ones")
        self.dma("q_sp", ones[:], self.onesb[:], (), ["ones"])
        self.alloc_norm(need_aT=False)
        m = self.modt
        pacc = Rot(P, 4, [128, 512], F32, "pacc", psum=True)
        pm_ = Rot(P, 2, [128, 512], F32, "pm", psum=True)
        pq_ = Rot(P, 2, [128, 512], F32, "pq", psum=True)
        h0b = Rot(P, 1, [128, 16, NT], F32, "h0b")
        h1b = Rot(P, 1, [128, 16, NT], F32, "h1b")
        xcb = Rot(P, 1, [128, 16, NT], BF16, "xcb")
        szb = Rot(P, 1, [128, 16, NT], BF16, "szb")
        hbb = Rot(P, 1, [128, 16, NT], BF16, "hbb")
        sqb = Rot(P, 1, [128, 16, NT], BF16, "sqb")
        yTb = Rot(P, 2, [128, 16, NT], BF16, "yTb")
        mnb = Rot(P, 2, [128, 4, NT], F32, "mnb")
        rsb = Rot(P, 2, [128, 4, NT], F32, "rsb")
        tma = Rot(P, 3, [128, NT], F32, "tma")
        cur_row = None
        nunits = self.T // 256
        for u in range(nunits):
            if u == 0 and not need_ctx:
                continue
            row = 1 if u == 0 else 0
            if row != cur_row:
                self.load_mod(l, 0, None, row)
                cur_row = row
            s0 = u * 256
            h0, h0k = h0b.next()
            h1, h1k = h1b.next()
            xc, xck = xcb.next()
            sz, szk = szb.next()
            self.dma("q_sp", h0[:, :, :], self.HT[0, :, :, s0:s0 + NT].rearrange("c p t -> p c t"), [], [h0k])
            self.dma("q_sp", h1[:, :, :], self.HT[1, :, :, s0:s0 + NT].rearrange("c p t -> p c t"), [], [h1k])
            self.dma("q_sp", xc[:, :, :], self.XC[:, :, s0:s0 + NT].rearrange("c p t -> p c t"), [], [xck])
            self.dma("q_sp", sz[:, :, :], self.SZ[:, :, s0:s0 + NT].rearrange("c p t -> p c t"), [], [szk])
            self.tt("pool", h0[:, :, :], h0[:, :, :], h1[:, :, :], ALU.add, [h0k, h1k], [h0k])
            hb, hbk = hbb.next()
            sq, sqk = sqb.next()
            self.cp("act", hb[:, :, :], h0[:, :, :], [h0k], [hbk])
            self.act(sq[:, :, :], h0[:, :, :], AF.Square, [h0k], [sqk])
            mn, mnk = mnb.next()
            rs, rsk = rsb.next()
            for h in range(4):
                p1, p1k = pm_.next()
                p2, p2k = pq_.next()
                for vc in range(4):
                    self.mm(p1[:, :NT], ones[:], hb[:, 4 * h + vc, :], vc == 0, vc == 3, ["ones", hbk], [p1k])
                for vc in range(4):
                    self.mm(p2[:, :NT], ones[:], sq[:, 4 * h + vc, :], vc == 0, vc == 3, ["ones", sqk], [p2k])
                self.act(mn[:, h, :], p1[:, :NT], AF.Copy, [p1k], [(mnk, h)], scale=1.0 / 512)
                tq, tqk = tma.next()
                self.tt("dve", tq[:, :], mn[:, h, :], mn[:, h, :], ALU.mult, [(mnk, h)], [tqk])
                self.stt("dve", tq[:, :], p2[:, :NT], 1.0 / 512, tq[:, :], ALU.mult, ALU.subtract, [p2k, tqk], [tqk])
                self.act(rs[:, h, :], tq[:, :], AF.Sqrt, [tqk], [(rsk, h)], bias=EPS)
                self.P.op("dve", (lambda rs=rs, h=h: (lambda e: e.reciprocal(rs[:, h, :], rs[:, h, :])))(), [(rsk, h)], [(rsk, h)])
            yT, yTk = yTb.next()
            for i in range(16):
                h = i // 4
                eng = "dve" if i % 2 == 0 else "pool"
                ta, tak = tma.next()
                self.tt("pool", ta[:, :], h0[:, i, :], mn[:, h, :], ALU.subtract, [h0k, (mnk, h)], [tak])
                self.stt("dve", ta[:, :], ta[:, :], gn[:, i:i + 1], rs[:, h, :], ALU.mult, ALU.mult, [tak, "gn", (rsk, h)], [tak])
                self.stt("dve", ta[:, :], xc[:, i, :], sk[:, i:i + 1], ta[:, :], ALU.mult, ALU.add, [xck, "sk", tak], [tak])
                self.tt("pool", yT[:, i, :], ta[:, :], sz[:, i, :], ALU.mult, [tak, szk], [(yTk, i)])
            for jj in range(nj):
                j = s0 // 128 + jj
                h_, hk = self.hbuf.next()
                self.load_h(h_, hk, j)
                t1, t1k = self.t1buf.next()
                for nh in range(2):
                    pa, pk = pacc.next()
                    th, thk = self.thalf.next()
                    for k in range(16):
                        self.mm(pa[:, :], yT[:, k, jj * 128:(jj + 1) * 128], wd[:, k, nh * 512:(nh + 1) * 512], k == 0, k == 15,
                                [("wdn", k, 0), (yTk, k)], [pk])
                    self.tt("dve", th[:, :], pa[:, :], m["gt"][:, nh * 512:(nh + 1) * 512], ALU.mult, [pk, "gt"], [thk])
                    self.tt("pool", h_[:, nh * 512:(nh + 1) * 512], th[:, :], h_[:, nh * 512:(nh + 1) * 512], ALU.add,
                            [thk, hk], [hk])
                self.dma("q_pool", self.hrow(j), h_[:], [hk], [])
        P.phase_end()
        self.h_in_src = False

    def pool_phase_p(self, l):
        P = self.P
        P.phase_begin()
        self.alloc_norm(need_aT=False)
        cb = P.sb([128, 36, 128], BF16, "cb")
        cf = P.sb([128, 16, 128], F32, "cf")
        self.dma("q_sp", cb[:], self.poolcb[:], (), ["cb"])
        self.dma("q_sp", cf[:], self.poolcf[:], (), ["cf"])
        pp = Rot(P, 2, [128, 1024], F32, "pp", psum=True)
        cpb = Rot(P, 2, [128, D], BF16, "cpb")
        cur_row = None
        for j in range(self.T // 128):
            row = 1 if j < 2 else 0
            if row != cur_row:
                self.load_mod(l, 0, self.norm1_g, row)
                cur_row = row
            a, ak = self.norm_tile(j)
            self.dma("q_pool", self.A_tm[j * 128:(j + 1) * 128, :], a[:], [ak], [("A", j)])
            if j >= 2:
                pt, ptk = pp.next()
                cp_, cpk = cpb.next()
                for g in range(4):
                    self.mm(pt[:, g * 256:(g + 1) * 256], cb[:, g, :], a[:, g * 256:(g + 1) * 256], True, True, ["cb", ak], [(ptk, g // 2)])
                for g in range(4):
                    self.act(cp_[:, g * 256:(g + 1) * 256], pt[:, g * 256:(g + 1) * 256], AF.Copy, [(ptk, g // 2), "cf"], [(cpk, g)], scale=cf[:, 12 + g, 0:1])
                self.dma("q_pool", self.CP_tm[j * 128:(j + 1) * 128, :], cp_[:], [(cpk, g) for g in range(4)], [("CP", j)])
        P.phase_end()

    def pool_phase_q(self, l, need_ctx):
        P = self.P
        jp = self.kinds[:l + 1].count(2) - 1
        P.phase_begin()
        self.load_ident()
        R = self.TL // 64
        cpt = 128 // R
        cb = P.sb([128, 36, 128], BF16, "cb")
        cf = P.sb([128, 16, 128], F32, "cf")
        self.dma("q_sp", cb[:], self.poolcb[:], (), ["cb"])
        self.dma("q_sp", cf[:], self.poolcf[:], (), ["cf"])
        wp = P.sb([128, 4, 2, 256], BF16, "wp")
        for g in range(4):
            self.dma("q_pool", wp[:, g, :, :], self.pool_w[jp, g].rearrange("(k p) n -> p k n", p=128), (), [("wp", g)])
        gt = P.sb([128, D], F32, "gt")
        sg = P.sb([128, D], F32, "sg")
        bsg = P.sb([128, D], F32, "bsg")
        tmpa = P.sb([128, D], F32, "tmpa")
        ppt = Rot(P, 2, [128, 8, 128], F32, "ppt", psum=True)
        ppo = Rot(P, 2, [128, 1024], F32, "ppo", psum=True)
        cpb = Rot(P, 2, [128, D], BF16, "cpb")
        ab = Rot(P, 3, [128, D], BF16, "ab")
        hb = Rot(P, 3, [128, D], F32, "hb")
        tb = Rot(P, 2, [128, D], F32, "tb")
        plT = Rot(P, 2, [128, 8, 128], BF16, "plT")

        def load_gate(row):
            mv = self.modv[l, row:row + 1, :]
            self.dma("q_sp", gt[:], mv[:, 2 * D:3 * D].partition_broadcast(128), [], ["gt"])
            self.dma("q_sp", tmpa[:], self.pool_scale[jp:jp + 1, :].partition_broadcast(128), [], ["tmpa"])
            self.tt("dve", sg[:], gt[:], tmpa[:], ALU.mult, ["gt", "tmpa"], ["sg"])
            self.dma("q_sp", tmpa[:], self.pool_b[jp:jp + 1, :].partition_broadcast(128), [], ["tmpa"])
            self.tt("dve", bsg[:], sg[:], tmpa[:], ALU.mult, ["sg", "tmpa"], ["bsg"])

        def finish(pt, ptk, rr_idx, h, hks, store):
            pl, plk = plT.next()
            for g in range(4):
                self.tt("dve", pl[:, 2 * g:2 * g + 2, :], pt[:, 2 * g:2 * g + 2, :],
                        cf[:, rr_idx(g):rr_idx(g) + 1, :].to_broadcast([128, 2, 128]), ALU.mult, [ptk, "cf"], [(plk, g)])
            po, pok = ppo.next()
            for g in range(4):
                for kc in range(2):
                    self.mm(po[:, g * 256:(g + 1) * 256], pl[:, 2 * g + kc, :], wp[:, g, kc, :], kc == 0, kc == 1, [(plk, g), ("wp", g)], [pok])
            t, tk = tb.next()
            self.tt("dve", t[:], po[:], sg[:], ALU.mult, [pok, "sg"], [tk])
            self.tt("pool", t[:], t[:], bsg[:], ALU.add, [tk, "bsg"], [tk])
            self.tt("pool", h[:], t[:], h[:], ALU.add, [tk] + hks, hks)
            store(h, hks)

        if need_ctx:
            load_gate(1)
            a2 = []
            for j in range(2):
                a, ak = ab.next()
                aks_ = [(ak, cc) for cc in range(cpt)]
                self.dma("q_sp", a[:], self.A_tm[j * 128:(j + 1) * 128, :], [], aks_)
                a2.append((a, aks_))
            for j2 in range(2):
                h, hk = hb.next()
                hks_ = [(hk, cc) for cc in range(cpt)]
                self.dma("q_sp", h[:], self.src_row(j2) if self.h_in_src else self.hrow(j2), [], hks_)
                pt, ptk = ppt.next()
                for fc in range(8):
                    g = fc // 2
                    for j in range(2):
                        self.mm(pt[:, fc, :], a2[j][0][:, fc * 128:(fc + 1) * 128], cb[:, 12 + g * 4 + j * 2 + j2, :], j == 0, False,
                                a2[j][1] + ["cb"], [ptk])
                    self.mm(pt[:, fc, :], a2[j2][0][:, fc * 128:(fc + 1) * 128], cb[:, 28 + g * 2 + j2, :], False, True, a2[j2][1] + ["cb"], [ptk])

                def store_c(h, hks, j2=j2):
                    self.dma("q_pool", self.hrow(j2), h[:], hks, [])
                finish(pt, ptk, lambda g, j2=j2: 4 + g * 2 + j2, h, hks_, store_c)
        load_gate(0)
        cp_v = self.CP_tm[TC:, :].rearrange("(r c) f -> c r f", c=64)
        a_v = self.A_tm[TC:, :].rearrange("(r c) f -> c r f", c=64)
        hsrc = self.x[:, :] if self.h_in_src else self.hres[TC:, :]
        hs_v = hsrc.rearrange("(r c) f -> c r f", c=64)
        hd_v = self.hres[TC:, :].rearrange("(r c) f -> c r f", c=64)
        for m_ in range(64 // cpt):
            c0 = m_ * cpt
            cp_, cpk = cpb.next()
            a, ak = ab.next()
            h, hk = hb.next()
            for cc in range(cpt):
                rows = slice(cc * R, (cc + 1) * R)
                self.dma("q_sp", cp_[rows, :], cp_v[c0 + cc], [], [(cpk, cc)])
                self.dma("q_sp", a[rows, :], a_v[c0 + cc], [], [(ak, cc)])
                self.dma("q_sp", h[rows, :], hs_v[c0 + cc], [], [(hk, cc)])
            pt, ptk = ppt.next()
            cpks = [(cpk, cc) for cc in range(cpt)]
            aks = [(ak, cc) for cc in range(cpt)]
            hks = [(hk, cc) for cc in range(cpt)]
            for fc in range(8):
                g = fc // 2
                self.mm(pt[:, fc, :], cp_[:, fc * 128:(fc + 1) * 128], cb[:, 4 + g, :], True, False, cpks + ["cb"], [ptk])
                self.mm(pt[:, fc, :], a[:, fc * 128:(fc + 1) * 128], cb[:, 8 + g, :], False, True, aks + ["cb"], [ptk])

            def store_l(h, hks_, c0=c0):
                for cc in range(cpt):
                    self.dma("q_pool", hd_v[c0 + cc], h[cc * R:(cc + 1) * R, :], hks_, [])
            finish(pt, ptk, lambda g: g, h, hks, store_l)
        P.phase_end()
        self.h_in_src = False

    def build(self):
        self.setup()
        self.ada_phase()
        for l in range(self.depth):
            kind = self.kinds[l]
            last = l == self.depth - 1
            if kind == 0:
                self.gla_phase_p(l)
                self.gla_phase_s(l, 0)
                self.gla_phase_s(l, 1)
                self.gla_phase_o(l, need_ctx=not last)
            elif kind == 1:
                self.mlstm_phase_p1(l)
                self.mlstm_phase_p2(l)
                self.mlstm_phase_s(l, 0)
                self.mlstm_phase_s(l, 1)
                self.mlstm_phase_o(l, need_ctx=not last)
            elif kind == 2:
                self.pool_phase_p(l)
                self.pool_phase_q(l, need_ctx=not last)
            elif kind is not None:
                raise NotImplementedError
            self.mlp1_phase(l, skip_ctx=last)
            self.mlp2_phase(l, skip_ctx=last, final=last)
        self.P.emit()
        self.P.close()
        return self.nc


def _box(L, w):
    pos = np.arange(L)
    lo = np.maximum(pos - w // 2, 0)
    hi = np.minimum(pos + (w - w // 2), L)
    M = ((pos[:, None] >= lo[None, :]) & (pos[:, None] < hi[None, :])).astype(np.float32)
    return M, (hi - lo).astype(np.float32)


def _consts(T_lat):
    R = T_lat // 64
    cpt = 128 // R
    cb = np.zeros((128, 36, 128), np.float32)
    cf = np.zeros((128, 16, 128), np.float32)
    for g, w in enumerate((2, 4, 8, 16)):
        Mc, cc_ = _box(64, w)
        cb[:, g, :] = np.kron(np.eye(2, dtype=np.float32), Mc)
        cf[:, 12 + g, 0] = 1.0 / np.tile(cc_, 2)
        Mr, cr = _box(R, w)
        cb[:, 4 + g, :] = np.kron(np.eye(cpt, dtype=np.float32), Mr)
        cb[:, 8 + g, :] = -np.diag(np.tile(cr, cpt))
        cf[:, g, :] = (1.0 / np.tile(cr, cpt))[None, :]
        Mx, cx = _box(TC, w)
        for j in range(2):
            for j2 in range(2):
                cb[:, 12 + g * 4 + j * 2 + j2, :] = Mx[j * 128:(j + 1) * 128, j2 * 128:(j2 + 1) * 128]
        for j2 in range(2):
            cb[:, 28 + g * 2 + j2, :] = -np.diag(cx[j2 * 128:(j2 + 1) * 128])
            cf[:, 4 + g * 2 + j2, :] = (1.0 / cx[j2 * 128:(j2 + 1) * 128])[None, :]
    glacf = np.ones((128, 768), np.float32)
    glacf[:, 0:512:64] = 0.0
    si_, ti_ = np.meshgrid(np.arange(128), np.arange(128), indexing="ij")
    same = (si_ // 64) == (ti_ // 64)
    glacf[:, 512:640] = (same & (ti_ >= si_)).astype(np.float32)
    glacf[:, 640:768] = (same & (ti_ <= si_)).astype(np.float32)
    sel = np.zeros((64, 8, 128), np.float32)
    for r8 in range(8):
        sel[32 * (r8 // 4) + r8 % 4, r8, :] = 1.0
    return {"ml_sel": sel, "identf": np.eye(128, dtype=np.float32), "glacf": glacf, "onesb": np.ones((128, 128), np.float32).astype(ml_dtypes.bfloat16),
            "identb": np.eye(128, dtype=np.float32).astype(ml_dtypes.bfloat16),
            "poolcb": cb.astype(ml_dtypes.bfloat16), "poolcf": cf}


def _bd(w_qkv):
    n = w_qkv.shape[0]
    o = np.zeros((n, 3, 16, 128, 128), np.float32)
    w = w_qkv.reshape(n, 3, 16, 32, 4, 4)
    for b in range(32):
        o[:, :, :, 4 * b:4 * b + 4, 4 * b:4 * b + 4] = w[:, :, :, b]
    return o


def _wg(w_gate, off):
    n = w_gate.shape[0]
    o = np.zeros((n, 128, 48, 64), np.float32)
    w = w_gate.reshape(n, 2, 48, 128, 8)
    for d in range(2):
        o[:, :, :, 32 * d:32 * d + 4] = w[:, d, :, :, off:off + 4].transpose(0, 2, 1, 3)
    return o


def _bg(b_gate, off):
    n = b_gate.shape[0]
    o = np.zeros((n, 64, 1), np.float32)
    for d in range(2):
        o[:, 32 * d:32 * d + 4, 0] = b_gate[:, d, off:off + 4]
    return o


def _wa2blk(w_a2):
    n = w_a2.shape[0]
    o = np.zeros((n, 32, 1024), np.float32)
    o[:, 0:16, 0:512] = w_a2[:, 0]
    o[:, 16:32, 512:1024] = w_a2[:, 1]
    return o


def make_in_maps(inputs, T_lat, depth):
    B = inputs["x"].shape[0]
    consts = _consts(T_lat)
    maps = []
    for b in range(B):
        cT = np.stack([inputs["c"][b], inputs["c_ctx"]], axis=1)
        cT = np.ascontiguousarray(cT.reshape(KC, 128, 2).transpose(1, 0, 2))
        m = {
            "x": np.ascontiguousarray(inputs["x"][b]),
            "ctx": np.ascontiguousarray(inputs["ctx"][b]),
            "cT": cT.astype(np.float32),
            "ada_w": inputs["ada_w"], "ada_b": inputs["ada_b"],
            "norm1_g": inputs["norm1_g"], "norm2_g": inputs["norm2_g"],
            "mlp_w1": inputs["mlp_w1"], "mlp_w2": inputs["mlp_w2"],
            "final_g": inputs["final_g"].reshape(1, D),
            "gla_w_in": inputs["gla_w_in"],
            "gla_wa1": np.ascontiguousarray(np.concatenate([inputs["gla_w_a1"][:, 0], inputs["gla_w_a1"][:, 1]], axis=-1)),
            "gla_wa2": _wa2blk(inputs["gla_w_a2"]),
            "gla_ba": np.ascontiguousarray(inputs["gla_b_a"].reshape(-1, 8, 128).transpose(0, 2, 1)),
            "gla_gh": np.ascontiguousarray(inputs["gla_g_head"].reshape(-1, 8, 128).transpose(0, 2, 1)),
            "gla_w_o": inputs["gla_w_o"],
            "ml_w_up": inputs["mlstm_w_up"], "ml_w_down": inputs["mlstm_w_down"],
            "ml_bd": _bd(inputs["mlstm_w_qkv"]),
            "ml_wgI": _wg(inputs["mlstm_w_gate"], 0), "ml_wgF": _wg(inputs["mlstm_w_gate"], 4),
            "ml_convw": np.ascontiguousarray(inputs["mlstm_conv_w"].reshape(-1, 4, 16, 128).transpose(0, 3, 2, 1)),
            "ml_convb": np.ascontiguousarray(inputs["mlstm_conv_b"].reshape(-1, 16, 128).transpose(0, 2, 1)),
            "ml_bgI": _bg(inputs["mlstm_b_gate"], 0), "ml_bgF": _bg(inputs["mlstm_b_gate"], 4),
            "ml_gn": np.ascontiguousarray(inputs["mlstm_g_norm"].reshape(-1, 16, 128).transpose(0, 2, 1)),
            "ml_skip": np.ascontiguousarray(inputs["mlstm_skip"].reshape(-1, 16, 128).transpose(0, 2, 1)),
            "pool_w": inputs["pool_w"], "pool_b": inputs["pool_b"].reshape(-1, D),
            "pool_scale": inputs["pool_scale"],
        }
        m.update(consts)
        maps.append(m)
    return maps


def run(inputs, T_lat, depth, kinds=None, trace=False):
    inputs = {k: np.asarray(v) for k, v in inputs.items()}
    bld = Builder(T_lat, depth, kinds)
    nc = bld.build()
    maps = make_in_maps(inputs, T_lat, depth)
    maps = [{k: v for k, v in m.items() if k in bld.din} for m in maps]
    res = run_bass_kernel_spmd(nc, maps, core_ids=list(range(len(maps))), trace=trace)
    out = np.stack([r["out"] for r in res.results], axis=0)
    return out.astype(np.float32), res, bld


def kernel(**inputs):
    out, _, _ = run(inputs, 4096, 4)
    return out
```

```python
from contextlib import ExitStack
import numpy as np
import ml_dtypes
import concourse.bass as bass
import concourse.mybir as mybir
from concourse.bass_utils import run_bass_kernel_spmd

F32 = mybir.dt.float32
BF16 = mybir.dt.bfloat16
AF = mybir.ActivationFunctionType
ALU = mybir.AluOpType
AX = mybir.AxisListType

COMPUTE = ("pe", "act", "dve", "pool")
EPOCH = 20000
NDMASEM = 64

D = 1024
KC = 8
TC = 256
DFF = 4096
EPS = 1e-6


class Prog:
    def __init__(self, nc):
        self.nc = nc
        self.es = ExitStack()
        self.ops = []
        self.nname = 0
        self.nphase = 0
        self.phase_limit = 10 ** 9

    def sb(self, shape, dtype, name=None):
        self.nname += 1
        name = (name or "sb") + f"_{self.nname}"
        return self.es.enter_context(self.nc.sbuf_tensor(name, list(shape), dtype))

    def ps(self, shape, dtype, name=None):
        self.nname += 1
        name = (name or "ps") + f"_{self.nname}"
        return self.es.enter_context(self.nc.psum_tensor(name, list(shape), dtype))

    def op(self, eng, fn, reads=(), writes=()):
        if self.nphase >= self.phase_limit:
            return
        self.ops.append((eng, fn, tuple(reads), tuple(writes)))

    def barrier(self):
        if self.nphase > self.phase_limit:
            return
        self.ops.append(("barrier", None, (), ()))

    def phase_begin(self):
        self._saved_es = self.es
        self.es = ExitStack()

    def phase_end(self):
        self.nphase += 1
        self.barrier()
        self.es.close()
        self.es = self._saved_es

    def _engine(self, eng):
        nc = self.nc
        return {"pe": nc.tensor, "act": nc.scalar, "dve": nc.vector, "pool": nc.gpsimd,
                "q_sp": nc.sync, "q_act": nc.scalar, "q_pool": nc.gpsimd}[eng]

    def _seq(self, stream):
        nc = self.nc
        return {"pe": nc.tensor, "act": nc.scalar, "dve": nc.vector, "pool": nc.gpsimd,
                "sp": nc.sync}[stream]

    @staticmethod
    def _stream(eng):
        return {"q_sp": "sp", "q_act": "act", "q_pool": "pool"}.get(eng, eng)

    def emit(self, final_keys=()):
        nc = self.nc
        ops = self.ops
        n = len(ops)
        last_w = {}
        rd_eng = {}
        rd_dma = {}
        deps = [None] * n
        needed = [False] * n
        bar_deps = {}
        last_on = {}
        dma_since = []
        for i, (eng, fn, reads, writes) in enumerate(ops):
            if eng == "barrier":
                bd = list(last_on.values()) + dma_since
                bar_deps[i] = bd
                for j in bd:
                    needed[j] = True
                deps[i] = []
                last_w, rd_eng, rd_dma = {}, {}, {}
                dma_since = []
                continue
            if eng.startswith("q_"):
                dma_since.append(i)
            else:
                last_on[eng] = i
            isdma_i = eng.startswith("q_")
            d = set()
            raw = set()
            for k in reads:
                w = last_w.get(k)
                if w is not None:
                    d.add(w)
                    raw.add(w)
            for k in writes:
                w = last_w.get(k)
                if w is not None:
                    d.add(w)
                for r in rd_eng.get(k, {}).values():
                    d.add(r)
                for r in rd_dma.get(k, ()):
                    d.add(r)
            d.discard(i)
            dd = []
            for j in d:
                ej = ops[j][0]
                if (not isdma_i) and ej == eng and eng == "pe":
                    continue
                dd.append(j)
            deps[i] = dd
            for j in dd:
                needed[j] = True
            for k in reads:
                if isdma_i:
                    rd_dma.setdefault(k, []).append(i)
                else:
                    rd_eng.setdefault(k, {})[eng] = i
            for k in writes:
                last_w[k] = i
                rd_eng[k] = {}
                rd_dma[k] = []
        fin = [last_w[k] for k in final_keys if k in last_w]
        for j in fin:
            needed[j] = True

        cnt = {e: 0 for e in COMPUTE}
        sig = [None] * n
        dma_cnt = [0] * NDMASEM
        ndma = 0
        nsw = 0
        NHW = 48
        for i, (eng, fn, reads, writes) in enumerate(ops):
            if not needed[i] or eng == "barrier":
                continue
            if eng.startswith("q_"):
                if eng == "q_pool":
                    s = NHW + nsw % (NDMASEM - NHW)
                    nsw += 1
                else:
                    s = ndma % NHW
                    ndma += 1
                dma_cnt[s] += 1
                sig[i] = ("dma", s, dma_cnt[s] * 16)
            else:
                cnt[eng] += 1
                sig[i] = ("eng", eng, cnt[eng])
        sems = {}
        for e in COMPUTE:
            ne = max(1, (cnt[e] + EPOCH - 1) // EPOCH)
            sems[e] = [self.es.enter_context(nc.semaphore(f"s_{e}{k}")) for k in range(ne)]
        dsems = [self.es.enter_context(nc.semaphore(f"s_dma{k}")) for k in range(NDMASEM)]
        assert max(dma_cnt + [0]) * 16 < 60000, dma_cnt
        self.stats = dict(cnt=dict(cnt), ndma=ndma, nops=n)
        known = {}

        def do_wait(stream, s):
            if s[0] == "dma":
                key = ("dma", s[1])
                val = s[2]
                if known.get((stream, key), 0) >= val:
                    return
                known[(stream, key)] = val
                self._seq(stream).wait_ge(dsems[s[1]], val)
            else:
                e, idx = s[1], s[2]
                key = ("eng", e)
                if known.get((stream, key), 0) >= idx:
                    return
                known[(stream, key)] = idx
                ep = (idx - 1) // EPOCH
                self._seq(stream).wait_ge(sems[e][ep], idx - ep * EPOCH)

        for i, (eng, fn, reads, writes) in enumerate(ops):
            if eng == "barrier":
                for stream in ("pe", "act", "dve", "pool", "sp"):
                    for j in bar_deps[i]:
                        if sig[j][0] == "eng" and sig[j][1] == stream:
                            continue
                        do_wait(stream, sig[j])
                continue
            stream = self._stream(eng)
            for j in sorted(deps[i]):
                do_wait(stream, sig[j])
            if needed[i] and sig[i][0] == "dma" and sig[i][2] > 16:
                do_wait(stream, ("dma", sig[i][1], sig[i][2] - 16))
            ins = fn(self._engine(eng))
            if needed[i]:
                s = sig[i]
                if s[0] == "dma":
                    ins.then_inc(dsems[s[1]], 16)
                else:
                    ep = (s[2] - 1) // EPOCH
                    ins.then_inc(sems[s[1]][ep], 1)
        for j in fin:
            do_wait("sp", sig[j])

    def close(self):
        self.es.close()


class Rot:
    def __init__(self, P, n, shape, dtype, name, psum=False):
        self.bufs = [(P.ps if psum else P.sb)(shape, dtype, f"{name}{i}") for i in range(n)]
        self.keys = [(name, i) for i in range(n)]
        self.i = -1

    def next(self):
        self.i = (self.i + 1) % len(self.bufs)
        return self.bufs[self.i], self.keys[self.i]


class Builder:
    def __init__(self, T_lat, depth, kinds=None):
        self.TL = T_lat
        self.T = TC + T_lat
        self.depth = depth
        self.kinds = kinds if kinds is not None else [i % 3 for i in range(depth)]
        self.nc = bass.Bass("TRN2", target_bir_lowering=False)
        self.P = Prog(self.nc)
        self.sts = [(0, TC)] + [(TC + 512 * i, 512) for i in range(T_lat // 512)]
        self.din = {}

    def mm(self, out, lhsT, rhs, start, stop, r, w, sgc=False):
        self.P.op("pe", lambda e: e.matmul(out, lhsT=lhsT, rhs=rhs, start=start, stop=stop, skip_group_check=sgc), r, w)

    def tr(self, out, in_, ident, r, w):
        self.P.op("pe", lambda e: e.transpose(out, in_, ident), r, w)

    def act(self, out, in_, func, r, w, bias=None, scale=None, accum=None):
        kw = {}
        if bias is not None:
            kw["bias"] = bias
        if scale is not None:
            kw["scale"] = scale
        if accum is not None:
            kw["accum_out"] = accum
        self.P.op("act", lambda e: e.activation(out=out, in_=in_, func=func, **kw), r, w)

    def tt(self, eng, out, a, b, op, r, w):
        self.P.op(eng, lambda e: e.tensor_tensor(out=out, in0=a, in1=b, op=op), r, w)

    def ts(self, eng, out, a, s1, s2, op0, op1, r, w):
        if s2 is None:
            self.P.op(eng, lambda e: e.tensor_scalar(out=out, in0=a, scalar1=s1, scalar2=None, op0=op0), r, w)
        else:
            self.P.op(eng, lambda e: e.tensor_scalar(out=out, in0=a, scalar1=s1, scalar2=s2, op0=op0, op1=op1), r, w)

    def stt(self, eng, out, in0, scalar, in1, op0, op1, r, w):
        self.P.op(eng, lambda e: e.scalar_tensor_tensor(out=out, in0=in0, scalar=scalar, in1=in1, op0=op0, op1=op1), r, w)

    def cp(self, eng, out, in_, r, w):
        if eng == "act":
            self.P.op("act", lambda e: e.copy(out, in_), r, w)
        else:
            self.P.op(eng, lambda e: e.tensor_copy(out, in_), r, w)

    def dma(self, q, out, in_, r, w, slow=False):
        if slow:
            self.P.op(q, lambda e: e.dma_start(out=out, in_=in_, allow_slow_non_contiguous=True), r, w)
        else:
            self.P.op(q, lambda e: e.dma_start(out=out, in_=in_), r, w)

    def scan(self, out, d0, d1, init, op0, op1, r, w):
        self.P.op("dve", lambda e: e.tensor_tensor_scan(out=out, data0=d0, data1=d1, initial=init, op0=op0, op1=op1), r, w)

    def memset(self, eng, ap, val, w):
        self.P.op(eng, lambda e: e.memset(ap, val), (), w)

    def inp(self, name, shape, dtype=F32):
        t = self.nc.dram_tensor(name, list(shape), dtype, kind="ExternalInput")
        self.din[name] = t
        return t

    def scratch(self, name, shape, dtype):
        return self.nc.dram_tensor(name, list(shape), dtype, kind="Internal")

    def setup(self):
        P = self.P
        TL, T = self.TL, self.T
        self.x = self.inp("x", [TL, D])
        self.ctx = self.inp("ctx", [TC, D])
        self.cT = self.inp("cT", [128, KC, 2])
        self.ada_w = self.inp("ada_w", [4, D, 6 * D])
        self.ada_b = self.inp("ada_b", [4, 6 * D])
        self.norm1_g = self.inp("norm1_g", [4, D])
        self.norm2_g = self.inp("norm2_g", [4, D])
        self.mlp_w1 = self.inp("mlp_w1", [4, D, DFF])
        self.mlp_w2 = self.inp("mlp_w2", [4, DFF, D])
        self.final_g = self.inp("final_g", [1, D])
        self.identb = self.inp("identb", [128, 128], BF16)
        self.out = self.nc.dram_tensor("out", [TL, D], F32, kind="ExternalOutput")
        self.hres = self.scratch("hres", [T, D], F32)
        self.modv = self.scratch("modv", [4, 2, 6 * D], F32)
        self.U = self.scratch("U", [32, 128, T], BF16)
        ng = max(1, self.kinds.count(0))
        self.gla_w_in = self.inp("gla_w_in", [ng, D, 3072])
        self.gla_wa1 = self.inp("gla_wa1", [ng, D, 32])
        self.gla_wa2 = self.inp("gla_wa2", [ng, 32, 1024])
        self.gla_ba = self.inp("gla_ba", [ng, 128, 8])
        self.gla_gh = self.inp("gla_gh", [ng, 128, 8])
        self.gla_w_o = self.inp("gla_w_o", [ng, D, D])
        self.glacf = self.inp("glacf", [128, 768])
        self.onesb = self.inp("onesb", [128, 128], BF16)
        self.QD = self.scratch("QD", [2, 4, 128, T], BF16)
        self.KD = self.scratch("KD", [2, 4, 128, T], BF16)
        self.KW_tm = self.scratch("KW_tm", [2, T, 512], BF16)
        self.V_tm = self.scratch("V_tm", [T, D], BF16)
        self.SR = self.scratch("SR", [8, 128, T], BF16)
        self.DEC = self.scratch("DEC", [2, 4, 128, T // 64], F32)
        self.OO = self.scratch("OO", [2, 8, 128, T], F32)
        nm_ = max(1, self.kinds.count(1))
        self.ml_w_up = self.inp("ml_w_up", [nm_, D, 4096])
        self.ml_bd = self.inp("ml_bd", [nm_, 3, 16, 128, 128])
        self.ml_wgI = self.inp("ml_wgI", [nm_, 128, 48, 64])
        self.ml_wgF = self.inp("ml_wgF", [nm_, 128, 48, 64])
        self.ml_convw = self.inp("ml_convw", [nm_, 128, 16, 4])
        self.ml_convb = self.inp("ml_convb", [nm_, 128, 16])
        self.ml_bgI = self.inp("ml_bgI", [nm_, 64, 1])
        self.ml_bgF = self.inp("ml_bgF", [nm_, 64, 1])
        self.ml_gn = self.inp("ml_gn", [nm_, 128, 16])
        self.ml_skip = self.inp("ml_skip", [nm_, 128, 16])
        self.ml_w_down = self.inp("ml_w_down", [nm_, 2048, D])
        self.ml_sel = self.inp("ml_sel", [64, 8, 128])
        self.identf_d = self.inp("identf", [128, 128])
        self.XM = self.scratch("XM", [16, 128, T], BF16)
        self.SZ = self.scratch("SZ", [16, 128, T], BF16)
        self.XC = self.scratch("XC", [16, 128, T], BF16)
        self.KT = self.scratch("KT", [16, 128, T], BF16)
        self.QB = self.scratch("QB", [2, 16, 128, T], BF16)
        self.VX = self.scratch("VX", [T, 2560], BF16)
        self.KWm = self.scratch("KWm", [2, T, 2048], BF16)
        self.CWT = self.scratch("CWT", [T, 128], F32)
        self.CARB = self.scratch("CARB", [2, 4, 128, T // 64], F32)
        self.HT = self.scratch("HT", [2, 16, 128, T], F32)
        self.A_tm = self.scratch("A_tm", [T, D], BF16)
        self.CP_tm = self.scratch("CP_tm", [T, D], BF16)
        npool = max(1, self.kinds.count(2))
        self.pool_w = self.inp("pool_w", [npool, 4, 256, 256])
        self.pool_b = self.inp("pool_b", [npool, D])
        self.pool_scale = self.inp("pool_scale", [npool, D])
        self.poolcb = self.inp("poolcb", [128, 36, 128], BF16)
        self.poolcf = self.inp("poolcf", [128, 16, 128])
        self.h_in_src = True

    def hrow(self, j):
        return self.hres[j * 128:(j + 1) * 128, :]

    def src_row(self, j):
        if j < 2:
            return self.ctx[j * 128:(j + 1) * 128, :]
        return self.x[(j - 2) * 128:(j - 1) * 128, :]

    def load_h(self, h, hk, j):
        if self.h_in_src:
            self.dma("q_sp", h[:], self.src_row(j), [], [hk])
        else:
            self.dma("q_sp", h[:], self.hrow(j), [("hres", j)], [hk])

    def load_ident(self):
        self.ident = self.P.sb([128, 128], BF16, "ident")
        self.dma("q_sp", self.ident[:], self.identb[:], (), ["ident"])

    def ada_phase(self):
        P = self.P
        P.phase_begin()
        cs = P.sb([128, KC, 2], F32, "cs")
        cin = P.sb([128, KC, 2], F32, "cin")
        psm = Rot(P, 2, [128, 512], F32, "psm", psum=True)
        self.dma("q_sp", cin[:], self.cT[:], (), ["cin"])
        self.act(cs[:], cin[:], AF.Silu, ["cin"], ["cs"])
        wst = Rot(P, 2, [128, KC, 512], F32, "adaw")
        bsb = P.sb([2, 6 * D], F32, "adab")
        msb = Rot(P, 1, [2, 6 * D], F32, "adam")
        for l in range(self.depth):
            self.dma("q_sp", bsb[:], self.ada_b[l:l + 1, :].partition_broadcast(2), (), ["adab"])
            mt, mk = msb.next()
            for n in range(12):
                wt, wk = wst.next()
                self.dma("q_sp", wt[:], self.ada_w[l, :, n * 512:(n + 1) * 512].rearrange("(k p) n -> p k n", p=128), (), [wk])
                pt, pk = psm.next()
                for k in range(KC):
                    self.mm(pt[0:2, :], cs[:, k, :], wt[:, k, :], k == 0, k == KC - 1, [wk, "cs"], [pk])
                self.tt("dve", mt[:, n * 512:(n + 1) * 512], pt[0:2, :], bsb[:, n * 512:(n + 1) * 512], ALU.add, [pk, "adab"], [(mk, n)])
            self.dma("q_pool", self.modv[l], mt[:], [(mk, n) for n in range(12)], [("modv", l)])
        P.phase_end()

    def alloc_norm(self, need_aT=True):
        P = self.P
        self.load_ident()
        self.hbuf = Rot(P, 4, [128, D], F32, "hbuf")
        self.t1buf = Rot(P, 2, [128, D], F32, "t1buf")
        self.ssb = Rot(P, 4, [128, 2], F32, "ssb")
        self.thalf = Rot(P, 3, [128, 512], F32, "thalf")
        self.modt = {nm: P.sb([128, D], F32, nm) for nm in ("gs", "sh", "gt")}
        self.abuf = Rot(P, 2, [128, D], BF16, "abuf")
        if need_aT:
            self.aT = Rot(P, 2, [128, KC, 512], BF16, "aT")
            self.ptr = Rot(P, 2, [128, 1024], BF16, "ptr", psum=True)

    def load_mod(self, l, which, norm_g, row):
        m = self.modt
        base = 3 * which
        mv = self.modv[l, row:row + 1, :]
        tmps, tmpsk = self.t1buf.next()
        tmpg, tmpgk = self.t1buf.next()
        self.dma("q_sp", m["sh"][:], mv[:, (base + 0) * D:(base + 1) * D].partition_broadcast(128), [], ["sh"])
        self.dma("q_sp", tmps[:], mv[:, (base + 1) * D:(base + 2) * D].partition_broadcast(128), [], [tmpsk])
        self.dma("q_sp", m["gt"][:], mv[:, (base + 2) * D:(base + 3) * D].partition_broadcast(128), [], ["gt"])
        if norm_g is not None:
            self.dma("q_sp", tmpg[:], norm_g[l:l + 1, :].partition_broadcast(128), (), [tmpgk])
            self.stt("dve", m["gs"][:], tmps[:], 1.0, tmpg[:], ALU.add, ALU.mult, [tmpsk, tmpgk], ["gs"])

    def rstd(self, ss, ssk, n=D):
        self.act(ss[:, 1:2], ss[:, 0:1], AF.Sqrt, [ssk], [ssk], scale=1.0 / n, bias=EPS)
        self.P.op("dve", lambda e: e.reciprocal(ss[:, 1:2], ss[:, 1:2]), [ssk], [ssk])

    def norm_tile(self, j):
        m = self.modt
        h, hk = self.hbuf.next()
        self.load_h(h, hk, j)
        t1, t1k = self.t1buf.next()
        ss, ssk = self.ssb.next()
        self.act(t1[:], h[:], AF.Square, [hk], [t1k, ssk], accum=ss[:, 0:1])
        self.rstd(ss, ssk)
        self.stt("dve", t1[:], h[:], ss[:, 1:2], m["gs"][:], ALU.mult, ALU.mult, [hk, ssk, "gs", t1k], [t1k])
        a, ak = self.abuf.next()
        self.tt("pool", a[:], t1[:], m["sh"][:], ALU.add, [t1k, "sh"], [ak])
        return a, ak

    def norm_stage(self, si):
        s0, NT = self.sts[si]
        aT, aTk = self.aT.next()
        for jj in range(NT // 128):
            j = s0 // 128 + jj
            a, ak = self.norm_tile(j)
            pt, ptk = self.ptr.next()
            for k in range(KC):
                self.tr(pt[:, k * 128:(k + 1) * 128], a[:, k * 128:(k + 1) * 128], self.ident[:], [ak, "ident"], [ptk])
            self.cp("act", aT[:, :, jj * 128:(jj + 1) * 128], pt[:].rearrange("p (k t) -> p k t", k=KC), [ptk], [(aTk, jj)])
        return aT, aTk

    def load_w(self, dst_view, src_view, key, ncols_piece=2048):
        K = dst_view.shape[1]
        N = dst_view.shape[2]
        for k in range(K):
            for c in range(0, N, ncols_piece):
                ce = min(N, c + ncols_piece)
                self.dma("q_pool", dst_view[:, k, c:ce], src_view[:, k, c:ce], (), [(key, k, c // ncols_piece)])

    def sis(self, skip_ctx):
        return [si for si in range(len(self.sts)) if not (skip_ctx and si == 0)]

    def pipelined(self, sis, stage_a, stage_b, l, which, norm_g):
        prev = None
        cur_row = None
        for si in sis:
            row = 1 if si == 0 else 0
            if row != cur_row:
                if prev is not None:
                    stage_b(*prev)
                    prev = None
                self.load_mod(l, which, norm_g, row)
                cur_row = row
            a = stage_a(si)
            if prev is not None:
                stage_b(*prev)
            prev = (si,) + tuple(a)
        if prev is not None:
            stage_b(*prev)

    def mlp1_phase(self, l, skip_ctx=False):
        P = self.P
        P.phase_begin()
        wb = P.sb([128, KC * DFF], BF16, "w1")
        wkey = "w1"
        w1 = wb[:, :].rearrange("p (k n) -> p k n", k=KC)
        self.load_w(w1, self.mlp_w1[l].rearrange("(k p) n -> p k n", p=128), wkey)
        self.alloc_norm()
        pacc = Rot(P, 4, [128, 512], F32, "pacc", psum=True)
        rbuf = Rot(P, 3, [128, 512], F32, "rbuf")
        ubuf = Rot(P, 2, [128, 4, 512], BF16, "ubuf")

        def stage_b(si, aT, aTk):
            s0, NT = self.sts[si]
            nj = NT // 128
            for fo4 in range(8):
                ut, uk = ubuf.next()
                for q in range(4):
                    fo = fo4 * 4 + q
                    pa, pk = pacc.next()
                    for k in range(KC):
                        self.mm(pa[:, :NT], w1[:, k, fo * 128:(fo + 1) * 128], aT[:, k, :NT], k == 0, k == KC - 1,
                                [(wkey, k, fo // 16)] + [(aTk, jj) for jj in range(nj)], [pk])
                    rt, rk = rbuf.next()
                    self.act(rt[:, :NT], pa[:, :NT], AF.Relu, [pk], [rk])
                    self.tt("dve", ut[:, q, :NT], rt[:, :NT], rt[:, :NT], ALU.mult, [rk], [(uk, q)])
                self.dma("q_pool", self.U[fo4 * 4:(fo4 + 1) * 4, :, s0:s0 + NT].rearrange("f p t -> p f t"), ut[:, :, :NT],
                         [(uk, q) for q in range(4)], [("U", si, fo4)])

        self.pipelined(self.sis(skip_ctx), self.norm_stage, stage_b, l, 1, self.norm2_g)
        P.phase_end()

    def mlp2_phase(self, l, skip_ctx=False, final=False):
        P = self.P
        P.phase_begin()
        wb = P.sb([128, 32 * D], BF16, "w2")
        wkey = "w2"
        w2 = wb[:, :].rearrange("p (k n) -> p k n", k=32)
        self.load_w(w2, self.mlp_w2[l].rearrange("(k p) n -> p k n", p=128), wkey, ncols_piece=1024)
        self.alloc_norm(need_aT=False)
        pacc = Rot(P, 4, [128, 512], F32, "pacc", psum=True)
        u2buf = Rot(P, 2, [128, 32, 512], BF16, "u2buf")
        m = self.modt
        fg = None
        if final:
            fg = P.sb([128, D], F32, "fg")
            self.dma("q_sp", fg[:], self.final_g[0:1, :].partition_broadcast(128), (), ["fg"])
        cur_row = None
        for si in self.sis(skip_ctx):
            row = 1 if si == 0 else 0
            if row != cur_row:
                self.load_mod(l, 1, None, row)
                cur_row = row
            s0, NT = self.sts[si]
            nj = NT // 128
            ut, uk = u2buf.next()
            for f8 in range(4):
                self.dma("q_sp", ut[:, f8 * 8:(f8 + 1) * 8, :NT], self.U[f8 * 8:(f8 + 1) * 8, :, s0:s0 + NT].rearrange("f p t -> p f t"),
                         [("U", si, f8 * 2), ("U", si, f8 * 2 + 1)], [(uk, f8)])
            for jj in range(nj):
                j = s0 // 128 + jj
                h, hk = self.hbuf.next()
                self.load_h(h, hk, j)
                t1, t1k = self.t1buf.next()
                for nh in range(2):
                    pa, pk = pacc.next()
                    th, thk = self.thalf.next()
                    for k in range(32):
                        self.mm(pa[:, :], ut[:, k, jj * 128:(jj + 1) * 128], w2[:, k, nh * 512:(nh + 1) * 512], k == 0, k == 31,
                                [(wkey, k, 0), (uk, k // 8)], [pk])
                    self.tt("dve", th[:, :], pa[:, :], m["gt"][:, nh * 512:(nh + 1) * 512], ALU.mult,
                            [pk, "gt"], [thk])
                    self.tt("pool", h[:, nh * 512:(nh + 1) * 512], th[:, :], h[:, nh * 512:(nh + 1) * 512], ALU.add,
                            [thk, hk], [hk])
                if not final:
                    self.dma("q_pool", self.hrow(j), h[:], [hk], [("hres", j)])
                else:
                    t2, t2k = self.t1buf.next()
                    ss, ssk = self.ssb.next()
                    self.act(t2[:], h[:], AF.Square, [hk], [t2k, ssk], accum=ss[:, 0:1])
                    self.rstd(ss, ssk)
                    self.stt("dve", t2[:], h[:], ss[:, 1:2], fg[:], ALU.mult, ALU.mult, [hk, ssk, "fg", t2k], [t2k])
                    self.dma("q_pool", self.out[(j - 2) * 128:(j - 1) * 128, :], t2[:], [t2k], [("out", j)])
        P.phase_end()
        self.h_in_src = False

    def gla_phase_p(self, l):
        P = self.P
        jg = self.kinds[:l + 1].count(0) - 1
        P.phase_begin()
        wb = P.sb([128, KC * 3072], BF16, "win")
        win = wb[:, :].rearrange("p (k n) -> p k n", k=KC)
        self.load_w(win, self.gla_w_in[jg].rearrange("(k p) n -> p k n", p=128), "win", ncols_piece=1024)
        wa1 = P.sb([128, KC, 32], BF16, "wa1")
        self.dma("q_pool", wa1[:], self.gla_wa1[jg].rearrange("(k p) n -> p k n", p=128), (), ["wa1"])
        wa2 = P.sb([32, 1024], BF16, "wa2")
        self.dma("q_pool", wa2[:], self.gla_wa2[jg], (), ["wa2"])
        nba = P.sb([128, 8], F32, "nba")
        self.dma("q_sp", nba[:], self.gla_ba[jg], (), ["nba"])
        self.ts("dve", nba[:], nba[:], -1.0, None, ALU.mult, None, ["nba"], ["nba"])
        gc = P.sb([128, 768], F32, "gc")
        self.dma("q_sp", gc[:], self.glacf[:], (), ["gc"])
        self.alloc_norm()
        pacc = Rot(P, 4, [128, 512], F32, "pacc", psum=True)
        pz = Rot(P, 2, [128, 512], F32, "pz", psum=True)
        qkb = Rot(P, 2, [128, 8, 512], F32, "qkb")
        srb = Rot(P, 2, [128, 8, 512], BF16, "srb")
        vtb = Rot(P, 2, [128, D], BF16, "vtb")
        utb = Rot(P, 2, [32, 512], BF16, "utb")
        tA = Rot(P, 3, [128, 512], F32, "tA")
        tB = Rot(P, 2, [128, 512], F32, "tB")
        tC = Rot(P, 2, [128, 512], F32, "tC")
        tD = Rot(P, 2, [128, 512], F32, "tD")
        ob = Rot(P, 4, [128, 512], BF16, "ob")
        kw4 = Rot(P, 2, [128, 4, 512], BF16, "kw4")
        kwt = Rot(P, 2, [128, 512], BF16, "kwt")
        decb = Rot(P, 2, [128, 32], F32, "decb")

        def stage_b(si, aT, aTk):
            s0, NT = self.sts[si]
            nj = NT // 128
            nch = NT // 64
            aks = [(aTk, jj) for jj in range(nj)]
            qk, qkk = qkb.next()
            for i in range(8):
                pa, pk = pacc.next()
                for k in range(KC):
                    self.mm(pa[:, :NT], win[:, k, i * 128:(i + 1) * 128], aT[:, k, :NT], k == 0, k == KC - 1, [("win", k, 0)] + aks, [pk])
                self.act(qk[:, i, :NT], pa[:, :NT], AF.Copy, [pk], [(qkk, i)], scale=(128 ** -0.5 if i < 4 else 1.0))
            sr, srk = srb.next()
            for i in range(8):
                pa, pk = pacc.next()
                for k in range(KC):
                    self.mm(pa[:, :NT], win[:, k, 2048 + i * 128:2048 + (i + 1) * 128], aT[:, k, :NT], k == 0, k == KC - 1, [("win", k, 2)] + aks, [pk])
                self.act(sr[:, i, :NT], pa[:, :NT], AF.Silu, [pk], [(srk, i)])
            self.dma("q_pool", self.SR[:, :, s0:s0 + NT].rearrange("f p t -> p f t"), sr[:, :, :NT], [(srk, i) for i in range(8)], [])
            for jj in range(nj):
                vt, vtk = vtb.next()
                for nh in range(2):
                    pa, pk = pacc.next()
                    th, thk = self.thalf.next()
                    for k in range(KC):
                        self.mm(pa[:, :], aT[:, k, jj * 128:(jj + 1) * 128], win[:, k, 1024 + nh * 512:1024 + (nh + 1) * 512], k == 0, k == KC - 1,
                                [("win", k, 1), (aTk, jj)], [pk])
                    self.cp("dve", vt[:, nh * 512:(nh + 1) * 512], pa[:, :], [pk], [(vtk, nh)])
                self.dma("q_pool", self.V_tm[s0 + jj * 128:s0 + (jj + 1) * 128, :], vt[:], [(vtk, 0), (vtk, 1)], [])
            ut, utk = utb.next()
            pu, puk = pz.next()
            for k in range(KC):
                self.mm(pu[0:32, :NT], wa1[:, k, :], aT[:, k, :NT], k == 0, k == KC - 1, ["wa1"] + aks, [puk])
            self.cp("dve", ut[:, :NT], pu[0:32, :NT], [puk], [utk])
            for d in range(2):
                kw, kwk = kw4.next()
                dec, deck = decb.next()
                for h in range(4):
                    q = qk[:, h, :NT]
                    kk = qk[:, 4 + h, :NT]
                    pzt, pzk = pz.next()
                    c0 = d * 512 + h * 128
                    self.mm(pzt[:, :NT], wa2[0:32, c0:c0 + 128], ut[0:32, :NT], True, True, ["wa2", utk], [pzk])
                    e, ek = tA.next()
                    self.act(e[:, :NT], pzt[:, :NT], AF.Exp, [pzk, "nba"], [ek], scale=-1.0, bias=nba[:, d * 4 + h:d * 4 + h + 1])
                    sp, spk = tB.next()
                    self.act(sp[:, :NT], e[:, :NT], AF.Ln, [ek], [spk], bias=1.0)
                    cs, csk = tC.next()
                    self.scan(cs[:, :NT], gc[:, :NT], sp[:, :NT], 0.0, ALU.mult, ALU.add, ["gc", spk], [csk])
                    cs3 = cs[:, :NT].rearrange("p (c t) -> p c t", t=64)
                    cl_b = cs3[:, :, 63:64].to_broadcast([128, nch, 64])
                    dd, ddk = tD.next()
                    dd3 = dd[:, :NT].rearrange("p (c t) -> p c t", t=64)
                    sp3 = sp[:, :NT].rearrange("p (c t) -> p c t", t=64)
                    if d == 0:
                        xq, xs = cs, -1.0 / 16
                        xk, xks = cs, 1.0 / 16
                        self.tt("dve", dd3, cs3, cl_b, ALU.subtract, [csk], [ddk])
                        xw, xws = dd, 1.0 / 16
                        xqk = xkk = csk
                        xwk = ddk
                    else:
                        t1, t1k_ = tD.next()
                        t13 = t1[:, :NT].rearrange("p (c t) -> p c t", t=64)
                        self.tt("dve", t13, sp3, cs3, ALU.subtract, [csk, spk], [t1k_])
                        self.tt("dve", dd3, t13, cl_b, ALU.add, [t1k_, csk], [ddk])
                        xq, xs, xqk = dd, -1.0 / 16, ddk
                        xk, xks, xkk = dd, 1.0 / 16, ddk
                        xw, xws, xwk = t1, 1.0 / 16, t1k_
                    e1, e1k = tA.next()
                    self.act(e1[:, :NT], xq[:, :NT], AF.Exp, [xqk], [e1k], scale=xs)
                    o1, o1k = ob.next()
                    self.tt("pool", o1[:, :NT], q, e1[:, :NT], ALU.mult, [(qkk, h), e1k], [o1k])
                    self.dma("q_pool", self.QD[d, h, :, s0:s0 + NT], o1[:, :NT], [o1k], [])
                    e2, e2k = tA.next()
                    self.act(e2[:, :NT], xk[:, :NT], AF.Exp, [xkk], [e2k], scale=xks)
                    o2, o2k = ob.next()
                    self.tt("pool", o2[:, :NT], kk, e2[:, :NT], ALU.mult, [(qkk, 4 + h), e2k], [o2k])
                    self.dma("q_pool", self.KD[d, h, :, s0:s0 + NT], o2[:, :NT], [o2k], [])
                    e3, e3k = tA.next()
                    self.act(e3[:, :NT], xw[:, :NT], AF.Exp, [xwk], [e3k], scale=xws)
                    self.tt("pool", kw[:, h, :NT], kk, e3[:, :NT], ALU.mult, [(qkk, 4 + h), e3k], [(kwk, h)])
                    self.act(self._decv(dec, h, nch), cs3[:, :, 63], AF.Exp, [csk], [(deck, h)], scale=-1.0 / 16)
                self.dma("q_pool", self.DEC[d, :, :, s0 // 64:s0 // 64 + nch].rearrange("h p c -> p h c"),
                         self._decall(dec, nch), [(deck, h) for h in range(4)], [])
                for jj in range(nj):
                    pt, ptk = self.ptr.next()
                    for h in range(4):
                        self.tr(pt[:, h * 128:(h + 1) * 128], kw[:, h, jj * 128:(jj + 1) * 128], self.ident[:], [(kwk, h), "ident"], [ptk])
                    kt, ktk = kwt.next()
                    self.cp("act", kt[:], pt[:, 0:512], [ptk], [ktk])
                    self.dma("q_pool", self.KW_tm[d, s0 + jj * 128:s0 + (jj + 1) * 128, :], kt[:], [ktk], [])

        self.pipelined(self.sis(False), self.norm_stage, stage_b, l, 0, self.norm1_g)
        P.phase_end()

    def _decv(self, dec, h, nch):
        return dec[:, h * 8:h * 8 + nch]

    def _decall(self, dec, nch):
        return dec[:, :].rearrange("p (h c) -> p h c", h=4)[:, :, :nch]

    def tile_order(self, d):
        sis = list(range(len(self.sts)))
        if d == 1:
            sis = [0] + sis[:0:-1]
        return sis

    def gla_phase_s(self, l, d):
        P = self.P
        P.phase_begin()
        gc = P.sb([128, 768], F32, "gc")
        self.dma("q_sp", gc[:], self.glacf[:], (), ["gc"])
        mask = gc[:, 512 + d * 128:512 + (d + 1) * 128]
        qdb = Rot(P, 2, [128, 4, 512], BF16, "qdb")
        kdb = Rot(P, 2, [128, 4, 512], BF16, "kdb")
        kwb = Rot(P, 2, [128, 4, 512], BF16, "kwb")
        vtb = Rot(P, 2, [128, 4, D], BF16, "vtb")
        decb = Rot(P, 2, [128, 4, 8], F32, "decb")
        S = P.sb([128, 4, 256], F32, "S")
        Sb = P.sb([128, 4, 256], BF16, "Sb")
        attb = Rot(P, 4, [128, 128], BF16, "attb")
        otb = Rot(P, 2, [128, 8, 512], F32, "otb")
        patt = Rot(P, 2, [128, 512], F32, "patt", psum=True)
        po = Rot(P, 3, [128, 512], F32, "po", psum=True)
        pst = Rot(P, 3, [128, 512], F32, "pst", psum=True)
        for h in range(4):
            self.memset("dve", S[:, h, :], 0.0, [("S", h)])
            self.memset("pool", Sb[:, h, :], 0.0, [("Sb", h)])
        for si in self.tile_order(d):
            s0, NT = self.sts[si]
            nj = NT // 128
            nch = NT // 64
            qd, qdk = qdb.next()
            kd, kdk = kdb.next()
            kw, kwk = kwb.next()
            vt, vtk = vtb.next()
            dec, deck = decb.next()
            ot, otk = otb.next()
            self.dma("q_sp", qd[:, :, :NT], self.QD[d, :, :, s0:s0 + NT].rearrange("h p t -> p h t"), [], [qdk])
            self.dma("q_sp", kd[:, :, :NT], self.KD[d, :, :, s0:s0 + NT].rearrange("h p t -> p h t"), [], [kdk])
            self.dma("q_sp", kw[:, :nj, :], self.KW_tm[d, s0:s0 + NT, :].rearrange("(j p) f -> p j f", p=128), [], [kwk])
            self.dma("q_sp", vt[:, :nj, :], self.V_tm[s0:s0 + NT, :].rearrange("(j p) f -> p j f", p=128), [], [vtk])
            self.dma("q_sp", dec[:, :, :nch], self.DEC[d, :, :, s0 // 64:s0 // 64 + nch].rearrange("h p c -> p h c"), [], [deck])
            jjs = list(range(nj)) if d == 0 else list(range(nj - 1, -1, -1))
            cs_ = (0, 1) if d == 0 else (1, 0)
            for jj in jjs:
                tsl = slice(jj * 128, (jj + 1) * 128)
                pos = {}

                def g_A(h):
                    pa, pak = patt.next()
                    self.mm(pa[:, 0:128], kd[:, h, tsl], qd[:, h, tsl], True, True, [kdk, qdk], [pak])
                    at, atk = attb.next()
                    self.tt("dve", at[:], pa[:, 0:128], mask, ALU.mult, [pak, "gc"], [atk])
                    p_ob, pok = po.next()
                    p_o = p_ob[:, 0:256].rearrange("p (v t) -> p v t", v=2)
                    for vc in range(2):
                        self.mm(p_o[:, vc, :], vt[:, jj, h * 256 + vc * 128:h * 256 + (vc + 1) * 128], at[:], vc == 0, False, [vtk, atk], [pok], sgc=True)
                    pos[h] = (p_o, pok)

                def g_c(h, ci):
                    c = cs_[ci]
                    p_o, pok = pos[h]
                    csl = slice(jj * 128 + c * 64, jj * 128 + (c + 1) * 64)
                    rows = slice(c * 64, (c + 1) * 64)
                    for vc in range(2):
                        self.mm(p_o[:, vc, c * 64:(c + 1) * 64], Sb[:, h, vc * 128:(vc + 1) * 128], qd[:, h, csl], False, ci == 1,
                                [("Sb", h), qdk], [pok], sgc=True)
                    ps, psk = pst.next()
                    self.mm(ps[:, 0:256], kw[rows, jj, h * 128:(h + 1) * 128], vt[rows, jj, h * 256:(h + 1) * 256], True, True, [kwk, vtk], [psk])
                    ch = jj * 2 + c
                    self.stt("dve", S[:, h, :], S[:, h, :], dec[:, h, ch:ch + 1], ps[:, 0:256], ALU.mult, ALU.add, [("S", h), deck, psk], [("S", h)])
                    self.cp("act", Sb[:, h, :], S[:, h, :], [("S", h)], [("Sb", h)])

                def g_E(h):
                    p_o, pok = pos[h]
                    self.cp("act", ot[:, 2 * h:2 * h + 2, tsl], p_o[:, :, :], [pok], [(otk, jj, h)])

                for h in range(4):
                    g_A(h)
                    g_c(h, 0)
                    if h >= 1:
                        g_c(h - 1, 1)
                        g_E(h - 1)
                g_c(3, 1)
                g_E(3)
            self.dma("q_pool", self.OO[d, :, :, s0:s0 + NT].rearrange("f p t -> p f t"), ot[:, :, :NT],
                     [(otk, jj, h) for jj in range(nj) for h in range(4)], [])
        P.phase_end()

    def gla_phase_o(self, l, need_ctx):
        P = self.P
        jg = self.kinds[:l + 1].count(0) - 1
        P.phase_begin()
        wb = P.sb([128, KC * D], BF16, "wo")
        wo = wb[:, :].rearrange("p (k n) -> p k n", k=KC)
        self.load_w(wo, self.gla_w_o[jg].rearrange("(k p) n -> p k n", p=128), "wo", ncols_piece=1024)
        gh = P.sb([128, 8], F32, "gh")
        self.dma("q_sp", gh[:], self.gla_gh[jg], (), ["gh"])
        ones = P.sb([128, 128], BF16, "ones")
        self.dma("q_sp", ones[:], self.onesb[:], (), ["ones"])
        self.alloc_norm(need_aT=False)
        m = self.modt
        pacc = Rot(P, 4, [128, 512], F32, "pacc", psum=True)
        o0b = Rot(P, 2, [128, 8, 512], F32, "o0b")
        o1b = Rot(P, 1, [128, 8, 512], F32, "o1b")
        srb = Rot(P, 2, [128, 8, 512], BF16, "srb")
        sqb = Rot(P, 1, [128, 8, 512], BF16, "sqb")
        yTb = Rot(P, 2, [128, 8, 512], BF16, "yTb")
        rsb = Rot(P, 2, [128, 4, 512], F32, "rsb")
        tmpb = Rot(P, 2, [128, 512], F32, "tmpb")
        cur_row = None
        for si in self.sis(not need_ctx):
            row = 1 if si == 0 else 0
            if row != cur_row:
                self.load_mod(l, 0, None, row)
                cur_row = row
            s0, NT = self.sts[si]
            nj = NT // 128
            o0, o0k = o0b.next()
            o1, o1k = o1b.next()
            sr, srk = srb.next()
            self.dma("q_sp", o0[:, :, :NT], self.OO[0, :, :, s0:s0 + NT].rearrange("f p t -> p f t"), [], [o0k])
            self.dma("q_sp", o1[:, :, :NT], self.OO[1, :, :, s0:s0 + NT].rearrange("f p t -> p f t"), [], [o1k])
            self.dma("q_sp", sr[:, :, :NT], self.SR[:, :, s0:s0 + NT].rearrange("f p t -> p f t"), [], [srk])
            self.tt("pool", o0[:, :, :NT], o0[:, :, :NT], o1[:, :, :NT], ALU.add, [o0k, o1k], [o0k])
            sq, sqk = sqb.next()
            self.act(sq[:, :, :NT], o0[:, :, :NT], AF.Square, [o0k], [sqk])
            rs, rsk = rsb.next()
            for h in range(4):
                pa, pk = pacc.next()
                for vc in range(2):
                    self.mm(pa[:, :NT], ones[:], sq[:, 2 * h + vc, :NT], vc == 0, vc == 1, ["ones", sqk], [pk])
                self.act(rs[:, h, :NT], pa[:, :NT], AF.Sqrt, [pk], [(rsk, h)], scale=1.0 / 256, bias=EPS)
                self.P.op("dve", (lambda rs=rs, h=h, NT=NT: (lambda e: e.reciprocal(rs[:, h, :NT], rs[:, h, :NT])))(), [(rsk, h)], [(rsk, h)])
            yT, yTk = yTb.next()
            for i in range(8):
                tm, tmk = tmpb.next()
                self.stt("dve", tm[:, :NT], o0[:, i, :NT], gh[:, i:i + 1], rs[:, i // 2, :NT], ALU.mult, ALU.mult, [o0k, "gh", (rsk, i // 2)], [tmk])
                self.tt("pool", yT[:, i, :NT], tm[:, :NT], sr[:, i, :NT], ALU.mult, [tmk, srk], [(yTk, i)])
            yks = [(yTk, i) for i in range(8)]
            for jj in range(nj):
                j = s0 // 128 + jj
                h_, hk = self.hbuf.next()
                self.load_h(h_, hk, j)
                t1, t1k = self.t1buf.next()
                for nh in range(2):
                    pa, pk = pacc.next()
                    th, thk = self.thalf.next()
                    for k in range(KC):
                        self.mm(pa[:, :], yT[:, k, jj * 128:(jj + 1) * 128], wo[:, k, nh * 512:(nh + 1) * 512], k == 0, k == KC - 1,
                                [("wo", k, 0), (yTk, k)], [pk])
                    self.tt("dve", th[:, :], pa[:, :], m["gt"][:, nh * 512:(nh + 1) * 512], ALU.mult, [pk, "gt"], [thk])
                    self.tt("pool", h_[:, nh * 512:(nh + 1) * 512], th[:, :], h_[:, nh * 512:(nh + 1) * 512], ALU.add,
                            [thk, hk], [hk])
                self.dma("q_pool", self.hrow(j), h_[:], [hk], [])
        P.phase_end()
        self.h_in_src = False

    def units256(self):
        return [(s0, 256) for s0 in range(0, self.T, 256)]

    def mlstm_phase_p1(self, l):
        P = self.P
        jm = self.kinds[:l + 1].count(1) - 1
        P.phase_begin()
        wb = P.sb([128, KC * 4096], BF16, "wup")
        wup = wb[:, :].rearrange("p (k n) -> p k n", k=KC)
        self.load_w(wup, self.ml_w_up[jm].rearrange("(k p) n -> p k n", p=128), "wup")
        self.alloc_norm()
        pacc = Rot(P, 4, [128, 512], F32, "pacc", psum=True)
        obuf = Rot(P, 3, [128, 4, 512], BF16, "obuf")

        def stage_b(si, aT, aTk):
            s0, NT = self.sts[si]
            nj = NT // 128
            aks = [(aTk, jj) for jj in range(nj)]
            for i4 in range(8):
                ot, otk = obuf.next()
                for q in range(4):
                    i = i4 * 4 + q
                    pa, pk = pacc.next()
                    for k in range(KC):
                        self.mm(pa[:, :NT], wup[:, k, i * 128:(i + 1) * 128], aT[:, k, :NT], k == 0, k == KC - 1, [("wup", k, i // 16)] + aks, [pk])
                    if i < 16:
                        self.cp("act", ot[:, q, :NT], pa[:, :NT], [pk], [(otk, q)])
                    else:
                        self.act(ot[:, q, :NT], pa[:, :NT], AF.Silu, [pk], [(otk, q)])
                dst = self.XM if i4 < 4 else self.SZ
                i0 = (i4 % 4) * 4
                self.dma("q_pool", dst[i0:i0 + 4, :, s0:s0 + NT].rearrange("f p t -> p f t"), ot[:, :, :NT], [(otk, q) for q in range(4)], [])

        self.pipelined(self.sis(False), self.norm_stage, stage_b, l, 0, self.norm1_g)
        P.phase_end()

    def mlstm_phase_p2(self, l):
        P = self.P
        jm = self.kinds[:l + 1].count(1) - 1
        P.phase_begin()
        self.load_ident()
        NT = 256
        nj, nch = 2, 4
        bd = P.sb([128, 48, 128], BF16, "bd")
        for m_ in range(3):
            self.dma("q_pool", bd[:, m_ * 16:(m_ + 1) * 16, :], self.ml_bd[jm, m_].rearrange("c p n -> p c n"), (), ["bd"])
        wgI = P.sb([128, 48, 64], BF16, "wgI")
        wgF = P.sb([128, 48, 64], BF16, "wgF")
        self.dma("q_pool", wgI[:], self.ml_wgI[jm], (), ["wg"])
        self.dma("q_pool", wgF[:], self.ml_wgF[jm], (), ["wg"])
        cw = P.sb([128, 16, 4], F32, "cw")
        cbias = P.sb([128, 16], F32, "cbias")
        self.dma("q_sp", cw[:], self.ml_convw[jm], (), ["cw"])
        self.dma("q_sp", cbias[:], self.ml_convb[jm], (), ["cw"])
        bI = P.sb([64, 1], F32, "bI")
        nbF = P.sb([64, 1], F32, "nbF")
        self.dma("q_sp", bI[:], self.ml_bgI[jm], (), ["bI"])
        self.dma("q_sp", nbF[:], self.ml_bgF[jm], (), ["nbF"])
        self.ts("dve", nbF[:], nbF[:], -1.0, None, ALU.mult, None, ["nbF"], ["nbF"])
        gc = P.sb([128, 768], F32, "gc")
        self.dma("q_sp", gc[:], self.glacf[:], (), ["gc"])
        sel = P.sb([64, 8, 128], F32, "sel")
        self.dma("q_sp", sel[:], self.ml_sel[:], (), ["sel"])
        identf = P.sb([128, 128], F32, "identf")
        self.dma("q_sp", identf[:], self.identf_d[:], (), ["identf"])
        pacc = Rot(P, 2, [128, 512], F32, "pacc", psum=True)
        pgI = P.ps([128, 512], F32, "pgI")
        pgF = P.ps([128, 512], F32, "pgF")
        pb = Rot(P, 1, [128, 512], F32, "pb", psum=True)
        pcx = P.ps([128, 512], F32, "pcx")
        ptkv = P.ps([128, 2048], BF16, "ptkv")
        xwb = Rot(P, 2, [128, 16, NT + 32], BF16, "xwb")
        xcb = Rot(P, 2, [128, 16, NT], BF16, "xcb")
        qkvb = Rot(P, 1, [128, 48, NT], BF16, "qkvb")
        qsb = Rot(P, 1, [128, 16, NT], BF16, "qsb")
        qbb = Rot(P, 3, [128, 4, NT], BF16, "qbb")
        accb = Rot(P, 4, [128, NT], F32, "accb")
        gt_ = {nm: P.sb([64, NT], F32, "g" + nm) for nm in ("LI", "E", "SP", "CS", "BN", "EB", "T1", "COL", "T2", "CW")}
        car = P.sb([64, 8], F32, "car")
        carb = Rot(P, 2, [128, 8, nch], F32, "carb")
        cwtb = Rot(P, 2, [128, 128], F32, "cwtb")
        vxb = Rot(P, 2, [128, 4, 640], BF16, "vxb")
        for i in range(2):
            self.memset("pool", vxb.bufs[i][:, :, 512:640], 1.0, [(vxb.keys[i], "ones")])
        kwb = Rot(P, 2, [128, 2048], BF16, "kwb")
        s_q = 512.0 ** -0.5
        nunits = self.T // 256
        for u in range(nunits):
            s0 = u * 256
            seq_lo, seq_hi = (0, TC) if u == 0 else (TC, self.T)
            xw, xwk = xwb.next()
            lo = max(s0 - 2, seq_lo)
            hi = min(s0 + NT + 1, seq_hi)
            if lo > s0 - 2:
                self.memset("pool", xw[:, :, 14:16], 0.0, [(xwk, "L")])
            if hi < s0 + NT + 1:
                self.memset("pool", xw[:, :, NT + 16:NT + 17], 0.0, [(xwk, "R")])
            self.dma("q_sp", xw[:, :, 14 + lo - (s0 - 2):14 + hi - (s0 - 2)], self.XM[:, :, lo:hi].rearrange("c p t -> p c t"), [],
                     [(xwk, "L"), (xwk, "M"), (xwk, "R")])
            xwks = [(xwk, "L"), (xwk, "M"), (xwk, "R")]
            xc, xck = xcb.next()
            for c in range(16):
                eng = "dve"
                ac, ack = accb.next()
                self.ts(eng, ac[:, :], xw[:, c, 14:14 + NT], cw[:, c, 0:1], None, ALU.mult, None, xwks + ["cw"], [ack])
                for j in range(1, 4):
                    self.stt(eng, ac[:, :], xw[:, c, 14 + j:14 + NT + j], cw[:, c, j:j + 1], ac[:, :], ALU.mult, ALU.add, xwks + ["cw", ack], [ack])
                self.act(xc[:, c, :], ac[:, :], AF.Silu, [ack, "cw"], [(xck, c)], bias=cbias[:, c:c + 1])
            xcks = [(xck, c) for c in range(16)]
            self.dma("q_pool", self.XC[:, :, s0:s0 + NT].rearrange("c p t -> p c t"), xc[:, :, :], xcks, [])
            qkv, qkvk = qkvb.next()
            qs, qsk = qsb.next()
            for m_ in range(3):
                for c in range(16):
                    pa, pk = pacc.next()
                    if m_ < 2:
                        self.mm(pa[:, :NT], bd[:, m_ * 16 + c, :], xc[:, c, :], True, True, ["bd", (xck, c)], [pk])
                    else:
                        self.mm(pa[:, :NT], bd[:, m_ * 16 + c, :], xw[:, c, 16:NT + 16], True, True, ["bd"] + xwks, [pk])
                    self.cp("act", qkv[:, m_ * 16 + c, :], pa[:, :NT], [pk], [(qkvk, m_ * 16 + c)])
                    if m_ == 0:
                        self.ts("pool", qs[:, c, :], qkv[:, c, :], s_q, None, ALU.mult, None, [(qkvk, c)], [(qsk, c)])
            self.dma("q_pool", self.KT[:, :, s0:s0 + NT].rearrange("c p t -> p c t"), qkv[:, 16:32, :], [(qkvk, 16 + c) for c in range(16)], [])
            for c in range(48):
                self.mm(pgI[0:64, :NT], wgI[:, c, :], qkv[:, c, :], c == 0, c == 47, ["wg", (qkvk, c)], ["pgI"])
            for c in range(48):
                self.mm(pgF[0:64, :NT], wgF[:, c, :], qkv[:, c, :], c == 0, c == 47, ["wg", (qkvk, c)], ["pgF"])
            g = gt_
            self.act(g["LI"][:], pgI[0:64, :NT], AF.Identity, ["pgI", "bI"], ["LI"], bias=bI[:, 0:1])
            self.act(g["E"][:], pgF[0:64, :NT], AF.Exp, ["pgF", "nbF"], ["E"], scale=-1.0, bias=nbF[:, 0:1])
            self.act(g["SP"][:], g["E"][:], AF.Ln, ["E"], ["SP"], bias=1.0)
            self.scan(g["CS"][:], gc[0:64, :NT], g["SP"][:], 0.0, ALU.mult, ALU.add, ["gc", "SP"], ["CS"])
            cs3 = g["CS"][:].rearrange("p (c t) -> p c t", t=64)
            self.cp("act", g["BN"][0:32, :], g["CS"][0:32, :], ["CS"], [("BN", 0)])
            self.tt("dve", g["BN"][32:64, :], g["SP"][32:64, :], g["CS"][32:64, :], ALU.subtract, ["SP", "CS"], [("BN", 1)])
            bn3 = g["BN"][:].rearrange("p (c t) -> p c t", t=64)
            self.tt("dve", bn3[32:64], bn3[32:64], cs3[32:64, :, 63:64].to_broadcast([32, nch, 64]), ALU.add, [("BN", 1), "CS"], [("BN", 1)])
            bnk = [("BN", 0), ("BN", 1)]
            self.act(g["EB"][:], g["BN"][:], AF.Exp, bnk, ["EB"], scale=-1.0)
            self.tt("dve", g["T1"][:], g["LI"][:], g["BN"][:], ALU.add, ["LI"] + bnk, ["T1"])
            self.act(g["COL"][:], g["T1"][:], AF.Exp, ["T1"], ["COL"])
            t13 = g["T1"][:].rearrange("p (c t) -> p c t", t=64)
            t23 = g["T2"][:].rearrange("p (c t) -> p c t", t=64)
            self.tt("dve", t23, t13, cs3[:, :, 63:64].to_broadcast([64, nch, 64]), ALU.subtract, ["T1", "CS"], ["T2"])
            self.act(g["CW"][:], g["T2"][:], AF.Exp, ["T2"], ["CW"])
            self.act(car[:, 0:nch], cs3[:, :, 63], AF.Exp, ["CS"], ["car"], scale=-1.0)
            for r8 in range(8):
                d, h = r8 // 4, r8 % 4
                pbt, pbk = pb.next()
                self.mm(pbt[:, :NT], sel[:, r8, :], g["EB"][:, :], True, True, ["sel", "EB"], [pbk])
                qb, qbk = qbb.next()
                self.tt("dve", qb[:, :, :], qs[:, h * 4:(h + 1) * 4, :], pbt[:, :NT].unsqueeze(1).to_broadcast([128, 4, NT]), ALU.mult,
                        [pbk] + [(qsk, h * 4 + i) for i in range(4)], [qbk])
                self.dma("q_pool", self.QB[d, h * 4:(h + 1) * 4, :, s0:s0 + NT].rearrange("c p t -> p c t"), qb[:, :, :], [qbk], [])
            for r8 in range(8):
                self.mm(pcx[:, 256 + r8 * nch:256 + (r8 + 1) * nch], sel[:, r8, :], car[:, 0:nch], r8 == 0, r8 == 7, ["sel", "car"], ["pcx"], sgc=True)
            cb_, cbk = carb.next()
            self.cp("dve", cb_[:, :, :], pcx[:, 256:256 + 8 * nch].rearrange("p (r c) -> p r c", c=nch), ["pcx"], [cbk])
            for d in range(2):
                self.dma("q_pool", self.CARB[d, :, :, s0 // 64:s0 // 64 + nch].rearrange("h p c -> p h c"), cb_[:, d * 4:(d + 1) * 4, :], [cbk], [])
            for jj in range(nj):
                tsl = slice(jj * 128, (jj + 1) * 128)
                self.tr(pcx[:, 0:64], g["COL"][:, tsl], identf[0:64, 0:64], ["COL", "identf"], ["pcx"])
                self.tr(pcx[:, 64:128], g["CW"][:, tsl], identf[0:64, 0:64], ["CW", "identf"], ["pcx"])
                ct, ctk = cwtb.next()
                self.cp("dve", ct[:, :], pcx[:, 0:128], ["pcx"], [ctk])
                self.dma("q_pool", self.CWT[s0 + jj * 128:s0 + (jj + 1) * 128, :], ct[:, :], [ctk], [])
                for c in range(16):
                    self.tr(ptkv[:, c * 128:(c + 1) * 128], qkv[:, 32 + c, tsl], self.ident[:], [(qkvk, 32 + c), "ident"], ["ptkv"])
                vx, vxk = vxb.next()
                self.cp("act", vx[:, :, 0:512], ptkv[:, :].rearrange("p (h v) -> p h v", h=4), ["ptkv"], [(vxk, "v")])
                self.dma("q_pool", self.VX[s0 + jj * 128:s0 + (jj + 1) * 128, :], vx[:, :, :].rearrange("p h v -> p (h v)"), [(vxk, "v"), (vxk, "ones")], [])
                for c in range(16):
                    self.tr(ptkv[:, c * 128:(c + 1) * 128], qkv[:, 16 + c, tsl], self.ident[:], [(qkvk, 16 + c), "ident"], ["ptkv"])
                for d in range(2):
                    kw, kwk = kwb.next()
                    for h in range(4):
                        col = ct[:, 64 + 32 * d + h:64 + 32 * d + h + 1]
                        if h < 2:
                            self.act(kw[:, h * 512:(h + 1) * 512], ptkv[:, h * 512:(h + 1) * 512], AF.Copy, ["ptkv", ctk], [(kwk, h)], scale=col)
                        else:
                            self.ts("dve", kw[:, h * 512:(h + 1) * 512], ptkv[:, h * 512:(h + 1) * 512], col, None, ALU.mult, None, ["ptkv", ctk], [(kwk, h)])
                    self.dma("q_pool", self.KWm[d, s0 + jj * 128:s0 + (jj + 1) * 128, :], kw[:, :], [(kwk, h) for h in range(4)], [])
        P.phase_end()

    def mlstm_phase_s(self, l, d):
        P = self.P
        P.phase_begin()
        NT, nj, nch = 256, 2, 4
        gc = P.sb([128, 768], F32, "gc")
        self.dma("q_sp", gc[:], self.glacf[:], (), ["gc"])
        mask = gc[:, 512 + d * 128:512 + (d + 1) * 128]
        qbb = Rot(P, 2, [128, 16, NT], BF16, "qbb")
        ktb = Rot(P, 2, [128, 16, NT], BF16, "ktb")
        vxb = Rot(P, 2, [128, nj, 2560], BF16, "vxb")
        kwb = Rot(P, 2, [128, nj, 2048], BF16, "kwb")
        cwb = Rot(P, 2, [128, nj, 128], F32, "cwb")
        crb = Rot(P, 2, [128, 4, nch], F32, "crb")
        C = P.sb([128, 16, 640], F32, "C")
        Cb = P.sb([128, 16, 640], BF16, "Cb")
        wtb = Rot(P, 3, [128, 128], BF16, "wtb")
        rdb = Rot(P, 2, [128, 128], F32, "rdb")
        htb = Rot(P, 2, [128, 16, NT], F32, "htb")
        pn = Rot(P, 2, [128, 1024], F32, "pn", psum=True)
        pst = Rot(P, 2, [128, 1024], F32, "pst", psum=True)
        for i in range(16):
            self.memset("dve", C[:, i, :], 0.0, [("C", i)])
            self.memset("pool", Cb[:, i, :], 0.0, [("Cb", i)])
        nunits = self.T // 256
        order = list(range(nunits)) if d == 0 else [0] + list(range(nunits - 1, 0, -1))
        for u in order:
            s0 = u * 256
            qb, qbk = qbb.next()
            kt, ktk = ktb.next()
            vx, vxk = vxb.next()
            kw, kwk = kwb.next()
            cw, cwk = cwb.next()
            cr, crk = crb.next()
            ht, htk = htb.next()
            self.dma("q_sp", qb[:, :, :], self.QB[d, :, :, s0:s0 + NT].rearrange("c p t -> p c t"), [], [qbk])
            self.dma("q_sp", kt[:, :, :], self.KT[:, :, s0:s0 + NT].rearrange("c p t -> p c t"), [], [ktk])
            self.dma("q_sp", vx[:, :, :], self.VX[s0:s0 + NT, :].rearrange("(j p) f -> p j f", p=128), [], [vxk])
            self.dma("q_sp", kw[:, :, :], self.KWm[d, s0:s0 + NT, :].rearrange("(j p) f -> p j f", p=128), [], [kwk])
            self.dma("q_sp", cw[:, :, :], self.CWT[s0:s0 + NT, :].rearrange("(j p) f -> p j f", p=128), [], [cwk])
            self.dma("q_sp", cr[:, :, :], self.CARB[d, :, :, s0 // 64:s0 // 64 + nch].rearrange("h p c -> p h c"), [], [crk])
            jjs = list(range(nj)) if d == 0 else list(range(nj - 1, -1, -1))
            cs_ = (0, 1) if d == 0 else (1, 0)
            for jj in jjs:
                tsl = slice(jj * 128, (jj + 1) * 128)
                pns = {}

                def step_A(h):
                    pnt, pnk = pn.next()
                    pns[h] = (pnt, pnk)
                    for dc in range(4):
                        self.mm(pnt[:, 640:768], kt[:, h * 4 + dc, tsl], qb[:, h * 4 + dc, tsl], dc == 0, dc == 3, [ktk, qbk], [pnk], sgc=True)
                    wt, wtk = wtb.next()
                    self.stt("dve", wt[:, :], pnt[:, 640:768], cw[:, jj, 32 * d + h:32 * d + h + 1], mask, ALU.mult, ALU.mult, [pnk, cwk, "gc"], [wtk])
                    for vc in range(4):
                        self.mm(pnt[:, vc * 128:(vc + 1) * 128], vx[:, jj, h * 640 + vc * 128:h * 640 + (vc + 1) * 128], wt[:, :], vc == 0, False,
                                [vxk, wtk], [pnk], sgc=True)
                    self.mm(pnt[:, 512:640], vx[:, jj, h * 640 + 512:h * 640 + 640], wt[:, :], True, False, [vxk, wtk], [pnk], sgc=True)

                def step_c(h, ci):
                    c = cs_[ci]
                    pnt, pnk = pns[h]
                    csl = slice(jj * 128 + c * 64, jj * 128 + (c + 1) * 64)
                    rows = slice(c * 64, (c + 1) * 64)
                    for vc in range(5):
                        dst = pnt[:, vc * 128 + c * 64:vc * 128 + (c + 1) * 64] if vc < 4 else pnt[:, 512 + c * 64:512 + (c + 1) * 64]
                        for dc in range(4):
                            self.mm(dst, Cb[:, h * 4 + dc, vc * 128:(vc + 1) * 128], qb[:, h * 4 + dc, csl], False, (ci == 1 and dc == 3),
                                    [("Cb", h * 4 + dc), qbk], [pnk], sgc=True)
                    ch = jj * 2 + c
                    for dc in range(4):
                        ps, psk = pst.next()
                        lhs = kw[rows, jj, h * 512 + dc * 128:h * 512 + (dc + 1) * 128]
                        self.mm(ps[:, 0:512], lhs, vx[rows, jj, h * 640:h * 640 + 512], True, True, [kwk, vxk], [psk])
                        self.mm(ps[:, 512:640], lhs, vx[rows, jj, h * 640 + 512:h * 640 + 640], True, True, [kwk, vxk], [psk])
                        i = h * 4 + dc
                        self.stt("dve", C[:, i, :], C[:, i, :], cr[:, h, ch:ch + 1], ps[:, 0:640], ALU.mult, ALU.add, [("C", i), crk, psk], [("C", i)])
                        self.cp("act", Cb[:, i, :], C[:, i, :], [("C", i)], [("Cb", i)])

                def step_E(h):
                    pnt, pnk = pns[h]
                    rd, rdk = rdb.next()
                    self.act(rd[:, :], pnt[:, 512:640], AF.Abs, [pnk], [rdk])
                    self.ts("dve", rd[:, :], rd[:, :], 1.0, None, ALU.max, None, [rdk], [rdk])
                    self.P.op("dve", (lambda rd=rd: (lambda e: e.reciprocal(rd[:, :], rd[:, :])))(), [rdk], [rdk])
                    self.tt("dve", ht[:, h * 4:(h + 1) * 4, tsl], pnt[:, 0:512].rearrange("p (v t) -> p v t", v=4),
                            rd[:, :].unsqueeze(1).to_broadcast([128, 4, 128]), ALU.mult, [pnk, rdk], [(htk, jj, h)])

                for h in range(4):
                    step_A(h)
                    step_c(h, 0)
                    if h >= 1:
                        step_c(h - 1, 1)
                        step_E(h - 1)
                step_c(3, 1)
                step_E(3)
            self.dma("q_pool", self.HT[d, :, :, s0:s0 + NT].rearrange("c p t -> p c t"), ht[:, :, :],
                     [(htk, jj, h) for jj in range(nj) for h in range(4)], [])
        P.phase_end()

    def mlstm_phase_o(self, l, need_ctx):
        P = self.P
        jm = self.kinds[:l + 1].count(1) - 1
        P.phase_begin()
        NT, nj = 256, 2
        wb = P.sb([128, 16 * D], BF16, "wdn")
        wd = wb[:, :].rearrange("p (k n) -> p k n", k=16)
        self.load_w(wd, self.ml_w_down[jm].rearrange("(k p) n -> p k n", p=128), "wdn", ncols_piece=1024)
        gn = P.sb([128, 16], F32, "gn")
        sk = P.sb([128, 16], F32, "sk")
        self.dma("q_sp", gn[:], self.ml_gn[jm], (), ["gn"])
        self.dma("q_sp", sk[:], self.ml_skip[jm], (), ["sk"])
        ones = P.sb([128, 128], BF16, "ones")
        self.dma("q_sp", ones[:], self.onesb[:], (), ["ones"])
        self.alloc_norm(need_aT=False)
        m = self.modt
        pacc = Rot(P, 4, [128, 512], F32, "pacc", psum=True)
        pm_ = Rot(P, 2, [128, 512], F32, "pm", psum=True)
        pq_ = Rot(P, 2, [128, 512], F32, "pq", psum=True)
        h0b = Rot(P, 1, [128, 16, NT], F32, "h0b")
        h1b = Rot(P, 1, [128, 16, NT], F32, "h1b")
        xcb = Rot(P, 1, [128, 16, NT], BF16, "xcb")
        szb = Rot(P, 1, [128, 16, NT], BF16, "szb")
        hbb = Rot(P, 1, [128, 16, NT], BF16, "hbb")
        sqb = Rot(P, 1, [128, 16, NT], BF16, "sqb")
        yTb = Rot(P, 2, [128, 16, NT], BF16, "yTb")
        mnb = Rot(P, 2, [128, 4, NT], F32, "mnb")
        rsb = Rot(P, 2, [128, 4, NT], F32, "rsb")
        tma = Rot(P, 3, [128, NT], F32, "tma")
        cur_row = None
        nunits = self.T // 256
        for u in range(nunits):
            if u == 0 and not need_ctx:
                continue
            row = 1 if u == 0 else 0
            if row != cur_row:
                self.load_mod(l, 0, None, row)
                cur_row = row
            s0 = u * 256
            h0, h0k = h0b.next()
            h1, h1k = h1b.next()
            xc, xck = xcb.next()
            sz, szk = szb.next()
            self.dma("q_sp", h0[:, :, :], self.HT[0, :, :, s0:s0 + NT].rearrange("c p t -> p c t"), [], [h0k])
            self.dma("q_sp", h1[:, :, :], self.HT[1, :, :, s0:s0 + NT].rearrange("c p t -> p c t"), [], [h1k])
            self.dma("q_sp", xc[:, :, :], self.XC[:, :, s0:s0 + NT].rearrange("c p t -> p c t"), [], [xck])
            self.dma("q_sp", sz[:, :, :], self.SZ[:, :, s0:s0 + NT].rearrange("c p t -> p c t"), [], [szk])
            self.tt("pool", h0[:, :, :], h0[:, :, :], h1[:, :, :], ALU.add, [h0k, h1k], [h0k])
            hb, hbk = hbb.next()
            sq, sqk = sqb.next()
            self.cp("act", hb[:, :, :], h0[:, :, :], [h0k], [hbk])
            self.act(sq[:, :, :], h0[:, :, :], AF.Square, [h0k], [sqk])
            mn, mnk = mnb.next()
            rs, rsk = rsb.next()
            for h in range(4):
                p1, p1k = pm_.next()
                p2, p2k = pq_.next()
                for vc in range(4):
                    self.mm(p1[:, :NT], ones[:], hb[:, 4 * h + vc, :], vc == 0, vc == 3, ["ones", hbk], [p1k])
                for vc in range(4):
                    self.mm(p2[:, :NT], ones[:], sq[:, 4 * h + vc, :], vc == 0, vc == 3, ["ones", sqk], [p2k])
                self.act(mn[:, h, :], p1[:, :NT], AF.Copy, [p1k], [(mnk, h)], scale=1.0 / 512)
                tq, tqk = tma.next()
                self.tt("dve", tq[:, :], mn[:, h, :], mn[:, h, :], ALU.mult, [(mnk, h)], [tqk])
                self.stt("dve", tq[:, :], p2[:, :NT], 1.0 / 512, tq[:, :], ALU.mult, ALU.subtract, [p2k, tqk], [tqk])
                self.act(rs[:, h, :], tq[:, :], AF.Sqrt, [tqk], [(rsk, h)], bias=EPS)
                self.P.op("dve", (lambda rs=rs, h=h: (lambda e: e.reciprocal(rs[:, h, :], rs[:, h, :])))(), [(rsk, h)], [(rsk, h)])
            yT, yTk = yTb.next()
            for i in range(16):
                h = i // 4
                eng = "dve" if i % 2 == 0 else "pool"
                ta, tak = tma.next()
                self.tt("pool", ta[:, :], h0[:, i, :], mn[:, h, :], ALU.subtract, [h0k, (mnk, h)], [tak])
                self.stt("dve", ta[:, :], ta[:, :], gn[:, i:i + 1], rs[:, h, :], ALU.mult, ALU.mult, [tak, "gn", (rsk, h)], [tak])
                self.stt("dve", ta[:, :], xc[:, i, :], sk[:, i:i + 1], ta[:, :], ALU.mult, ALU.add, [xck, "sk", tak], [tak])
                self.tt("pool", yT[:, i, :], ta[:, :], sz[:, i, :], ALU.mult, [tak, szk], [(yTk, i)])
            for jj in range(nj):
                j = s0 // 128 + jj
                h_, hk = self.hbuf.next()
                self.load_h(h_, hk, j)
                t1, t1k = self.t1buf.next()
                for nh in range(2):
                    pa, pk = pacc.next()
                    th, thk = self.thalf.next()
                    for k in range(16):
                        self.mm(pa[:, :], yT[:, k, jj * 128:(jj + 1) * 128], wd[:, k, nh * 512:(nh + 1) * 512], k == 0, k == 15,
                                [("wdn", k, 0), (yTk, k)], [pk])
                    self.tt("dve", th[:, :], pa[:, :], m["gt"][:, nh * 512:(nh + 1) * 512], ALU.mult, [pk, "gt"], [thk])
                    self.tt("pool", h_[:, nh * 512:(nh + 1) * 512], th[:, :], h_[:, nh * 512:(nh + 1) * 512], ALU.add,
                            [thk, hk], [hk])
                self.dma("q_pool", self.hrow(j), h_[:], [hk], [])
        P.phase_end()
        self.h_in_src = False

    def pool_phase_p(self, l):
        P = self.P
        P.phase_begin()
        self.alloc_norm(need_aT=False)
        cb = P.sb([128, 36, 128], BF16, "cb")
        cf = P.sb([128, 16, 128], F32, "cf")
        self.dma("q_sp", cb[:], self.poolcb[:], (), ["cb"])
        self.dma("q_sp", cf[:], self.poolcf[:], (), ["cf"])
        pp = Rot(P, 2, [128, 1024], F32, "pp", psum=True)
        cpb = Rot(P, 2, [128, D], BF16, "cpb")
        cur_row = None
        for j in range(self.T // 128):
            row = 1 if j < 2 else 0
            if row != cur_row:
                self.load_mod(l, 0, self.norm1_g, row)
                cur_row = row
            a, ak = self.norm_tile(j)
            self.dma("q_pool", self.A_tm[j * 128:(j + 1) * 128, :], a[:], [ak], [("A", j)])
            if j >= 2:
                pt, ptk = pp.next()
                cp_, cpk = cpb.next()
                for g in range(4):
                    self.mm(pt[:, g * 256:(g + 1) * 256], cb[:, g, :], a[:, g * 256:(g + 1) * 256], True, True, ["cb", ak], [(ptk, g // 2)])
                for g in range(4):
                    self.act(cp_[:, g * 256:(g + 1) * 256], pt[:, g * 256:(g + 1) * 256], AF.Copy, [(ptk, g // 2), "cf"], [(cpk, g)], scale=cf[:, 12 + g, 0:1])
                self.dma("q_pool", self.CP_tm[j * 128:(j + 1) * 128, :], cp_[:], [(cpk, g) for g in range(4)], [("CP", j)])
        P.phase_end()

    def pool_phase_q(self, l, need_ctx):
        P = self.P
        jp = self.kinds[:l + 1].count(2) - 1
        P.phase_begin()
        self.load_ident()
        R = self.TL // 64
        cpt = 128 // R
        cb = P.sb([128, 36, 128], BF16, "cb")
        cf = P.sb([128, 16, 128], F32, "cf")
        self.dma("q_sp", cb[:], self.poolcb[:], (), ["cb"])
        self.dma("q_sp", cf[:], self.poolcf[:], (), ["cf"])
        wp = P.sb([128, 4, 2, 256], BF16, "wp")
        for g in range(4):
            self.dma("q_pool", wp[:, g, :, :], self.pool_w[jp, g].rearrange("(k p) n -> p k n", p=128), (), [("wp", g)])
        gt = P.sb([128, D], F32, "gt")
        sg = P.sb([128, D], F32, "sg")
        bsg = P.sb([128, D], F32, "bsg")
        tmpa = P.sb([128, D], F32, "tmpa")
        ppt = Rot(P, 2, [128, 8, 128], F32, "ppt", psum=True)
        ppo = Rot(P, 2, [128, 1024], F32, "ppo", psum=True)
        cpb = Rot(P, 2, [128, D], BF16, "cpb")
        ab = Rot(P, 3, [128, D], BF16, "ab")
        hb = Rot(P, 3, [128, D], F32, "hb")
        tb = Rot(P, 2, [128, D], F32, "tb")
        plT = Rot(P, 2, [128, 8, 128], BF16, "plT")

        def load_gate(row):
            mv = self.modv[l, row:row + 1, :]
            self.dma("q_sp", gt[:], mv[:, 2 * D:3 * D].partition_broadcast(128), [], ["gt"])
            self.dma("q_sp", tmpa[:], self.pool_scale[jp:jp + 1, :].partition_broadcast(128), [], ["tmpa"])
            self.tt("dve", sg[:], gt[:], tmpa[:], ALU.mult, ["gt", "tmpa"], ["sg"])
            self.dma("q_sp", tmpa[:], self.pool_b[jp:jp + 1, :].partition_broadcast(128), [], ["tmpa"])
            self.tt("dve", bsg[:], sg[:], tmpa[:], ALU.mult, ["sg", "tmpa"], ["bsg"])

        def finish(pt, ptk, rr_idx, h, hks, store):
            pl, plk = plT.next()
            for g in range(4):
                self.tt("dve", pl[:, 2 * g:2 * g + 2, :], pt[:, 2 * g:2 * g + 2, :],
                        cf[:, rr_idx(g):rr_idx(g) + 1, :].to_broadcast([128, 2, 128]), ALU.mult, [ptk, "cf"], [(plk, g)])
            po, pok = ppo.next()
            for g in range(4):
                for kc in range(2):
                    self.mm(po[:, g * 256:(g + 1) * 256], pl[:, 2 * g + kc, :], wp[:, g, kc, :], kc == 0, kc == 1, [(plk, g), ("wp", g)], [pok])
            t, tk = tb.next()
            self.tt("dve", t[:], po[:], sg[:], ALU.mult, [pok, "sg"], [tk])
            self.tt("pool", t[:], t[:], bsg[:], ALU.add, [tk, "bsg"], [tk])
            self.tt("pool", h[:], t[:], h[:], ALU.add, [tk] + hks, hks)
            store(h, hks)

        if need_ctx:
            load_gate(1)
            a2 = []
            for j in range(2):
                a, ak = ab.next()
                aks_ = [(ak, cc) for cc in range(cpt)]
                self.dma("q_sp", a[:], self.A_tm[j * 128:(j + 1) * 128, :], [], aks_)
                a2.append((a, aks_))
            for j2 in range(2):
                h, hk = hb.next()
                hks_ = [(hk, cc) for cc in range(cpt)]
                self.dma("q_sp", h[:], self.src_row(j2) if self.h_in_src else self.hrow(j2), [], hks_)
                pt, ptk = ppt.next()
                for fc in range(8):
                    g = fc // 2
                    for j in range(2):
                        self.mm(pt[:, fc, :], a2[j][0][:, fc * 128:(fc + 1) * 128], cb[:, 12 + g * 4 + j * 2 + j2, :], j == 0, False,
                                a2[j][1] + ["cb"], [ptk])
                    self.mm(pt[:, fc, :], a2[j2][0][:, fc * 128:(fc + 1) * 128], cb[:, 28 + g * 2 + j2, :], False, True, a2[j2][1] + ["cb"], [ptk])

                def store_c(h, hks, j2=j2):
                    self.dma("q_pool", self.hrow(j2), h[:], hks, [])
                finish(pt, ptk, lambda g, j2=j2: 4 + g * 2 + j2, h, hks_, store_c)
        load_gate(0)
        cp_v = self.CP_tm[TC:, :].rearrange("(r c) f -> c r f", c=64)
        a_v = self.A_tm[TC:, :].rearrange("(r c) f -> c r f", c=64)
        hsrc = self.x[:, :] if self.h_in_src else self.hres[TC:, :]
        hs_v = hsrc.rearrange("(r c) f -> c r f", c=64)
        hd_v = self.hres[TC:, :].rearrange("(r c) f -> c r f", c=64)
        for m_ in range(64 // cpt):
            c0 = m_ * cpt
            cp_, cpk = cpb.next()
            a, ak = ab.next()
            h, hk = hb.next()
            for cc in range(cpt):
                rows = slice(cc * R, (cc + 1) * R)
                self.dma("q_sp", cp_[rows, :], cp_v[c0 + cc], [], [(cpk, cc)])
                self.dma("q_sp", a[rows, :], a_v[c0 + cc], [], [(ak, cc)])
                self.dma("q_sp", h[rows, :], hs_v[c0 + cc], [], [(hk, cc)])
            pt, ptk = ppt.next()
            cpks = [(cpk, cc) for cc in range(cpt)]
            aks = [(ak, cc) for cc in range(cpt)]
            hks = [(hk, cc) for cc in range(cpt)]
            for fc in range(8):
                g = fc // 2
                self.mm(pt[:, fc, :], cp_[:, fc * 128:(fc + 1) * 128], cb[:, 4 + g, :], True, False, cpks + ["cb"], [ptk])
                self.mm(pt[:, fc, :], a[:, fc * 128:(fc + 1) * 128], cb[:, 8 + g, :], False, True, aks + ["cb"], [ptk])

            def store_l(h, hks_, c0=c0):
                for cc in range(cpt):
                    self.dma("q_pool", hd_v[c0 + cc], h[cc * R:(cc + 1) * R, :], hks_, [])
            finish(pt, ptk, lambda g: g, h, hks, store_l)
        P.phase_end()
        self.h_in_src = False

    def build(self):
        self.setup()
        self.ada_phase()
        for l in range(self.depth):
            kind = self.kinds[l]
            last = l == self.depth - 1
            if kind == 0:
                self.gla_phase_p(l)
                self.gla_phase_s(l, 0)
                self.gla_phase_s(l, 1)
                self.gla_phase_o(l, need_ctx=not last)
            elif kind == 1:
                self.mlstm_phase_p1(l)
                self.mlstm_phase_p2(l)
                self.mlstm_phase_s(l, 0)
                self.mlstm_phase_s(l, 1)
                self.mlstm_phase_o(l, need_ctx=not last)
            elif kind == 2:
                self.pool_phase_p(l)
                self.pool_phase_q(l, need_ctx=not last)
            elif kind is not None:
                raise NotImplementedError
            self.mlp1_phase(l, skip_ctx=last)
            self.mlp2_phase(l, skip_ctx=last, final=last)
        self.P.emit()
        self.P.close()
        return self.nc


def _box(L, w):
    pos = np.arange(L)
    lo = np.maximum(pos - w // 2, 0)
    hi = np.minimum(pos + (w - w // 2), L)
    M = ((pos[:, None] >= lo[None, :]) & (pos[:, None] < hi[None, :])).astype(np.float32)
    return M, (hi - lo).astype(np.float32)


def _consts(T_lat):
    R = T_lat // 64
    cpt = 128 // R
    cb = np.zeros((128, 36, 128), np.float32)
    cf = np.zeros((128, 16, 128), np.float32)
    for g, w in enumerate((2, 4, 8, 16)):
        Mc, cc_ = _box(64, w)
        cb[:, g, :] = np.kron(np.eye(2, dtype=np.float32), Mc)
        cf[:, 12 + g, 0] = 1.0 / np.tile(cc_, 2)
        Mr, cr = _box(R, w)
        cb[:, 4 + g, :] = np.kron(np.eye(cpt, dtype=np.float32), Mr)
        cb[:, 8 + g, :] = -np.diag(np.tile(cr, cpt))
        cf[:, g, :] = (1.0 / np.tile(cr, cpt))[None, :]
        Mx, cx = _box(TC, w)
        for j in range(2):
            for j2 in range(2):
                cb[:, 12 + g * 4 + j * 2 + j2, :] = Mx[j * 128:(j + 1) * 128, j2 * 128:(j2 + 1) * 128]
        for j2 in range(2):
            cb[:, 28 + g * 2 + j2, :] = -np.diag(cx[j2 * 128:(j2 + 1) * 128])
            cf[:, 4 + g * 2 + j2, :] = (1.0 / cx[j2 * 128:(j2 + 1) * 128])[None, :]
    glacf = np.ones((128, 768), np.float32)
    glacf[:, 0:512:64] = 0.0
    si_, ti_ = np.meshgrid(np.arange(128), np.arange(128), indexing="ij")
    same = (si_ // 64) == (ti_ // 64)
    glacf[:, 512:640] = (same & (ti_ >= si_)).astype(np.float32)
    glacf[:, 640:768] = (same & (ti_ <= si_)).astype(np.float32)
    sel = np.zeros((64, 8, 128), np.float32)
    for r8 in range(8):
        sel[32 * (r8 // 4) + r8 % 4, r8, :] = 1.0
    return {"ml_sel": sel, "identf": np.eye(128, dtype=np.float32), "glacf": glacf, "onesb": np.ones((128, 128), np.float32).astype(ml_dtypes.bfloat16),
            "identb": np.eye(128, dtype=np.float32).astype(ml_dtypes.bfloat16),
            "poolcb": cb.astype(ml_dtypes.bfloat16), "poolcf": cf}


def _bd(w_qkv):
    n = w_qkv.shape[0]
    o = np.zeros((n, 3, 16, 128, 128), np.float32)
    w = w_qkv.reshape(n, 3, 16, 32, 4, 4)
    for b in range(32):
        o[:, :, :, 4 * b:4 * b + 4, 4 * b:4 * b + 4] = w[:, :, :, b]
    return o


def _wg(w_gate, off):
    n = w_gate.shape[0]
    o = np.zeros((n, 128, 48, 64), np.float32)
    w = w_gate.reshape(n, 2, 48, 128, 8)
    for d in range(2):
        o[:, :, :, 32 * d:32 * d + 4] = w[:, d, :, :, off:off + 4].transpose(0, 2, 1, 3)
    return o


def _bg(b_gate, off):
    n = b_gate.shape[0]
    o = np.zeros((n, 64, 1), np.float32)
    for d in range(2):
        o[:, 32 * d:32 * d + 4, 0] = b_gate[:, d, off:off + 4]
    return o


def _wa2blk(w_a2):
    n = w_a2.shape[0]
    o = np.zeros((n, 32, 1024), np.float32)
    o[:, 0:16, 0:512] = w_a2[:, 0]
    o[:, 16:32, 512:1024] = w_a2[:, 1]
    return o


def make_in_maps(inputs, T_lat, depth):
    B = inputs["x"].shape[0]
    consts = _consts(T_lat)
    maps = []
    for b in range(B):
        cT = np.stack([inputs["c"][b], inputs["c_ctx"]], axis=1)
        cT = np.ascontiguousarray(cT.reshape(KC, 128, 2).transpose(1, 0, 2))
        m = {
            "x": np.ascontiguousarray(inputs["x"][b]),
            "ctx": np.ascontiguousarray(inputs["ctx"][b]),
            "cT": cT.astype(np.float32),
            "ada_w": inputs["ada_w"], "ada_b": inputs["ada_b"],
            "norm1_g": inputs["norm1_g"], "norm2_g": inputs["norm2_g"],
            "mlp_w1": inputs["mlp_w1"], "mlp_w2": inputs["mlp_w2"],
            "final_g": inputs["final_g"].reshape(1, D),
            "gla_w_in": inputs["gla_w_in"],
            "gla_wa1": np.ascontiguousarray(np.concatenate([inputs["gla_w_a1"][:, 0], inputs["gla_w_a1"][:, 1]], axis=-1)),
            "gla_wa2": _wa2blk(inputs["gla_w_a2"]),
            "gla_ba": np.ascontiguousarray(inputs["gla_b_a"].reshape(-1, 8, 128).transpose(0, 2, 1)),
            "gla_gh": np.ascontiguousarray(inputs["gla_g_head"].reshape(-1, 8, 128).transpose(0, 2, 1)),
            "gla_w_o": inputs["gla_w_o"],
            "ml_w_up": inputs["mlstm_w_up"], "ml_w_down": inputs["mlstm_w_down"],
            "ml_bd": _bd(inputs["mlstm_w_qkv"]),
            "ml_wgI": _wg(inputs["mlstm_w_gate"], 0), "ml_wgF": _wg(inputs["mlstm_w_gate"], 4),
            "ml_convw": np.ascontiguousarray(inputs["mlstm_conv_w"].reshape(-1, 4, 16, 128).transpose(0, 3, 2, 1)),
            "ml_convb": np.ascontiguousarray(inputs["mlstm_conv_b"].reshape(-1, 16, 128).transpose(0, 2, 1)),
            "ml_bgI": _bg(inputs["mlstm_b_gate"], 0), "ml_bgF": _bg(inputs["mlstm_b_gate"], 4),
            "ml_gn": np.ascontiguousarray(inputs["mlstm_g_norm"].reshape(-1, 16, 128).transpose(0, 2, 1)),
            "ml_skip": np.ascontiguousarray(inputs["mlstm_skip"].reshape(-1, 16, 128).transpose(0, 2, 1)),
            "pool_w": inputs["pool_w"], "pool_b": inputs["pool_b"].reshape(-1, D),
            "pool_scale": inputs["pool_scale"],
        }
        m.update(consts)
        maps.append(m)
    return maps


def run(inputs, T_lat, depth, kinds=None, trace=False):
    inputs = {k: np.asarray(v) for k, v in inputs.items()}
    bld = Builder(T_lat, depth, kinds)
    nc = bld.build()
    maps = make_in_maps(inputs, T_lat, depth)
    maps = [{k: v for k, v in m.items() if k in bld.din} for m in maps]
    res = run_bass_kernel_spmd(nc, maps, core_ids=list(range(len(maps))), trace=trace)
    out = np.stack([r["out"] for r in res.results], axis=0)
    return out.astype(np.float32), res, bld


def kernel(**inputs):
    out, _, _ = run(inputs, 4096, 4)
    return out
```

```python
from contextlib import ExitStack
import numpy as np
import ml_dtypes
import concourse.bass as bass
import concourse.mybir as mybir
from concourse.bass_utils import run_bass_kernel_spmd

F32 = mybir.dt.float32
BF16 = mybir.dt.bfloat16
AF = mybir.ActivationFunctionType
ALU = mybir.AluOpType
AX = mybir.AxisListType

COMPUTE = ("pe", "act", "dve", "pool")
EPOCH = 20000
NDMASEM = 64

D = 1024
KC = 8
TC = 256
DFF = 4096
EPS = 1e-6
MCH = 128


class Prog:
    def __init__(self, nc):
        self.nc = nc
        self.es = ExitStack()
        self.ops = []
        self.nname = 0
        self.nphase = 0
        self.phase_limit = 10 ** 9

    def sb(self, shape, dtype, name=None):
        self.nname += 1
        name = (name or "sb") + f"_{self.nname}"
        return self.es.enter_context(self.nc.sbuf_tensor(name, list(shape), dtype))

    def ps(self, shape, dtype, name=None):
        self.nname += 1
        name = (name or "ps") + f"_{self.nname}"
        return self.es.enter_context(self.nc.psum_tensor(name, list(shape), dtype))

    def op(self, eng, fn, reads=(), writes=()):
        if self.nphase >= self.phase_limit:
            return
        self.ops.append((eng, fn, tuple(reads), tuple(writes)))

    def barrier(self):
        if self.nphase > self.phase_limit:
            return
        self.ops.append(("barrier", None, (), ()))

    def phase_begin(self):
        self._saved_es = self.es
        self.es = ExitStack()

    def phase_end(self):
        self.nphase += 1
        self.barrier()
        self.es.close()
        self.es = self._saved_es

    def _engine(self, eng):
        nc = self.nc
        return {"pe": nc.tensor, "act": nc.scalar, "dve": nc.vector, "pool": nc.gpsimd,
                "q_sp": nc.sync, "q_act": nc.scalar, "q_pool": nc.gpsimd}[eng]

    def _seq(self, stream):
        nc = self.nc
        return {"pe": nc.tensor, "act": nc.scalar, "dve": nc.vector, "pool": nc.gpsimd,
                "sp": nc.sync}[stream]

    @staticmethod
    def _stream(eng):
        return {"q_sp": "sp", "q_act": "act", "q_pool": "pool"}.get(eng, eng)

    def emit(self, final_keys=()):
        nc = self.nc
        ops = self.ops
        n = len(ops)
        last_w = {}
        rd_eng = {}
        rd_dma = {}
        deps = [None] * n
        needed = [False] * n
        bar_deps = {}
        last_on = {}
        dma_since = []
        for i, (eng, fn, reads, writes) in enumerate(ops):
            if eng == "barrier":
                bd = list(last_on.values()) + dma_since
                bar_deps[i] = bd
                for j in bd:
                    needed[j] = True
                deps[i] = []
                last_w, rd_eng, rd_dma = {}, {}, {}
                dma_since = []
                continue
            if eng.startswith("q_"):
                dma_since.append(i)
            else:
                last_on[eng] = i
            isdma_i = eng.startswith("q_")
            d = set()
            raw = set()
            for k in reads:
                w = last_w.get(k)
                if w is not None:
                    d.add(w)
                    raw.add(w)
            for k in writes:
                w = last_w.get(k)
                if w is not None:
                    d.add(w)
                for r in rd_eng.get(k, {}).values():
                    d.add(r)
                for r in rd_dma.get(k, ()):
                    d.add(r)
            d.discard(i)
            dd = []
            for j in d:
                ej = ops[j][0]
                if (not isdma_i) and ej == eng and eng == "pe":
                    continue
                dd.append(j)
            deps[i] = dd
            for j in dd:
                needed[j] = True
            for k in reads:
                if isdma_i:
                    rd_dma.setdefault(k, []).append(i)
                else:
                    rd_eng.setdefault(k, {})[eng] = i
            for k in writes:
                last_w[k] = i
                rd_eng[k] = {}
                rd_dma[k] = []
        fin = [last_w[k] for k in final_keys if k in last_w]
        for j in fin:
            needed[j] = True

        cnt = {e: 0 for e in COMPUTE}
        sig = [None] * n
        dma_cnt = [0] * NDMASEM
        ndma = 0
        nsw = 0
        NHW = 48
        for i, (eng, fn, reads, writes) in enumerate(ops):
            if not needed[i] or eng == "barrier":
                continue
            if eng.startswith("q_"):
                if eng == "q_pool":
                    s = NHW + nsw % (NDMASEM - NHW)
                    nsw += 1
                else:
                    s = ndma % NHW
                    ndma += 1
                dma_cnt[s] += 1
                sig[i] = ("dma", s, dma_cnt[s] * 16)
            else:
                cnt[eng] += 1
                sig[i] = ("eng", eng, cnt[eng])
        sems = {}
        for e in COMPUTE:
            ne = max(1, (cnt[e] + EPOCH - 1) // EPOCH)
            sems[e] = [self.es.enter_context(nc.semaphore(f"s_{e}{k}")) for k in range(ne)]
        dsems = [self.es.enter_context(nc.semaphore(f"s_dma{k}")) for k in range(NDMASEM)]
        assert max(dma_cnt + [0]) * 16 < 60000, dma_cnt
        self.stats = dict(cnt=dict(cnt), ndma=ndma, nops=n)
        known = {}

        def do_wait(stream, s):
            if s[0] == "dma":
                key = ("dma", s[1])
                val = s[2]
                if known.get((stream, key), 0) >= val:
                    return
                known[(stream, key)] = val
                self._seq(stream).wait_ge(dsems[s[1]], val)
            else:
                e, idx = s[1], s[2]
                key = ("eng", e)
                if known.get((stream, key), 0) >= idx:
                    return
                known[(stream, key)] = idx
                ep = (idx - 1) // EPOCH
                self._seq(stream).wait_ge(sems[e][ep], idx - ep * EPOCH)

        for i, (eng, fn, reads, writes) in enumerate(ops):
            if eng == "barrier":
                for stream in ("pe", "act", "dve", "pool", "sp"):
                    for j in bar_deps[i]:
                        if sig[j][0] == "eng" and sig[j][1] == stream:
                            continue
                        do_wait(stream, sig[j])
                continue
            stream = self._stream(eng)
            for j in sorted(deps[i]):
                do_wait(stream, sig[j])
            if needed[i] and sig[i][0] == "dma" and sig[i][2] > 16:
                do_wait(stream, ("dma", sig[i][1], sig[i][2] - 16))
            ins = fn(self._engine(eng))
            if needed[i]:
                s = sig[i]
                if s[0] == "dma":
                    ins.then_inc(dsems[s[1]], 16)
                else:
                    ep = (s[2] - 1) // EPOCH
                    ins.then_inc(sems[s[1]][ep], 1)
        for j in fin:
            do_wait("sp", sig[j])

    def close(self):
        self.es.close()


class Rot:
    def __init__(self, P, n, shape, dtype, name, psum=False):
        self.bufs = [(P.ps if psum else P.sb)(shape, dtype, f"{name}{i}") for i in range(n)]
        self.keys = [(name, i) for i in range(n)]
        self.i = -1

    def next(self):
        self.i = (self.i + 1) % len(self.bufs)
        return self.bufs[self.i], self.keys[self.i]


class Builder:
    def __init__(self, T_lat, depth, kinds=None):
        self.TL = T_lat
        self.T = TC + T_lat
        self.depth = depth
        self.kinds = kinds if kinds is not None else [i % 3 for i in range(depth)]
        self.nc = bass.Bass("TRN2", target_bir_lowering=False)
        self.P = Prog(self.nc)
        self.sts = [(0, TC)] + [(TC + 512 * i, 512) for i in range(T_lat // 512)]
        self.din = {}

    def mm(self, out, lhsT, rhs, start, stop, r, w, sgc=False):
        self.P.op("pe", lambda e: e.matmul(out, lhsT=lhsT, rhs=rhs, start=start, stop=stop, skip_group_check=sgc), r, w)

    def tr(self, out, in_, ident, r, w):
        self.P.op("pe", lambda e: e.transpose(out, in_, ident), r, w)

    def act(self, out, in_, func, r, w, bias=None, scale=None, accum=None):
        kw = {}
        if bias is not None:
            kw["bias"] = bias
        if scale is not None:
            kw["scale"] = scale
        if accum is not None:
            kw["accum_out"] = accum
        self.P.op("act", lambda e: e.activation(out=out, in_=in_, func=func, **kw), r, w)

    def tt(self, eng, out, a, b, op, r, w):
        self.P.op(eng, lambda e: e.tensor_tensor(out=out, in0=a, in1=b, op=op), r, w)

    def ts(self, eng, out, a, s1, s2, op0, op1, r, w):
        if s2 is None:
            self.P.op(eng, lambda e: e.tensor_scalar(out=out, in0=a, scalar1=s1, scalar2=None, op0=op0), r, w)
        else:
            self.P.op(eng, lambda e: e.tensor_scalar(out=out, in0=a, scalar1=s1, scalar2=s2, op0=op0, op1=op1), r, w)

    def stt(self, eng, out, in0, scalar, in1, op0, op1, r, w):
        self.P.op(eng, lambda e: e.scalar_tensor_tensor(out=out, in0=in0, scalar=scalar, in1=in1, op0=op0, op1=op1), r, w)

    def cp(self, eng, out, in_, r, w):
        if eng == "act":
            self.P.op("act", lambda e: e.copy(out, in_), r, w)
        else:
            self.P.op(eng, lambda e: e.tensor_copy(out, in_), r, w)

    def dma(self, q, out, in_, r, w, slow=False):
        if slow:
            self.P.op(q, lambda e: e.dma_start(out=out, in_=in_, allow_slow_non_contiguous=True), r, w)
        else:
            self.P.op(q, lambda e: e.dma_start(out=out, in_=in_), r, w)

    def scan(self, out, d0, d1, init, op0, op1, r, w):
        self.P.op("dve", lambda e: e.tensor_tensor_scan(out=out, data0=d0, data1=d1, initial=init, op0=op0, op1=op1), r, w)

    def memset(self, eng, ap, val, w):
        self.P.op(eng, lambda e: e.memset(ap, val), (), w)

    def inp(self, name, shape, dtype=F32):
        t = self.nc.dram_tensor(name, list(shape), dtype, kind="ExternalInput")
        self.din[name] = t
        return t

    def scratch(self, name, shape, dtype):
        return self.nc.dram_tensor(name, list(shape), dtype, kind="Internal")

    def setup(self):
        P = self.P
        TL, T = self.TL, self.T
        self.x = self.inp("x", [TL, D])
        self.ctx = self.inp("ctx", [TC, D])
        self.cT = self.inp("cT", [128, KC, 2])
        self.ada_w = self.inp("ada_w", [4, D, 6 * D])
        self.ada_b = self.inp("ada_b", [4, 6 * D])
        self.norm1_g = self.inp("norm1_g", [4, D])
        self.norm2_g = self.inp("norm2_g", [4, D])
        self.mlp_w1 = self.inp("mlp_w1", [4, D, DFF])
        self.mlp_w2 = self.inp("mlp_w2", [4, DFF, D])
        self.final_g = self.inp("final_g", [1, D])
        self.identb = self.inp("identb", [128, 128], BF16)
        self.out = self.nc.dram_tensor("out", [TL, D], F32, kind="ExternalOutput")
        self.hres = self.scratch("hres", [T, D], F32)
        self.modv = self.scratch("modv", [4, 2, 6 * D], F32)
        self.U = self.scratch("U", [32, 128, T], BF16)
        ng = max(1, self.kinds.count(0))
        self.gla_w_in = self.inp("gla_w_in", [ng, D, 3072])
        self.gla_wa1 = self.inp("gla_wa1", [ng, D, 32])
        self.gla_wa2 = self.inp("gla_wa2", [ng, 32, 1024])
        self.gla_ba = self.inp("gla_ba", [ng, 128, 8])
        self.gla_gh = self.inp("gla_gh", [ng, 128, 8])
        self.gla_w_o = self.inp("gla_w_o", [ng, D, D])
        self.glacf = self.inp("glacf", [128, 768])
        self.onesb = self.inp("onesb", [128, 128], BF16)
        self.QD = self.scratch("QD", [2, 4, 128, T], BF16)
        self.KD = self.scratch("KD", [2, 4, 128, T], BF16)
        self.KW_tm = self.scratch("KW_tm", [2, T, 512], BF16)
        self.V_tm = self.scratch("V_tm", [T, D], BF16)
        self.SR = self.scratch("SR", [8, 128, T], BF16)
        self.DEC = self.scratch("DEC", [2, 4, 128, T // 64], F32)
        self.OO = self.scratch("OO", [2, 8, 128, T], F32)
        nm_ = max(1, self.kinds.count(1))
        self.ml_w_up = self.inp("ml_w_up", [nm_, D, 4096])
        self.ml_bd = self.inp("ml_bd", [nm_, 3, 16, 128, 128])
        self.ml_wgI = self.inp("ml_wgI", [nm_, 128, 48, 64])
        self.ml_wgF = self.inp("ml_wgF", [nm_, 128, 48, 64])
        self.ml_convw = self.inp("ml_convw", [nm_, 128, 16, 4])
        self.ml_convb = self.inp("ml_convb", [nm_, 128, 16])
        self.ml_bgI = self.inp("ml_bgI", [nm_, 64, 1])
        self.ml_bgF = self.inp("ml_bgF", [nm_, 64, 1])
        self.ml_gn = self.inp("ml_gn", [nm_, 128, 16])
        self.ml_skip = self.inp("ml_skip", [nm_, 128, 16])
        self.ml_w_down = self.inp("ml_w_down", [nm_, 2048, D])
        self.ml_sel = self.inp("ml_sel", [64, 8, 128])
        self.mlmask = self.inp("mlmask", [128, 768])
        self.identf_d = self.inp("identf", [128, 128])
        self.XM = self.scratch("XM", [16, 128, T], BF16)
        self.SZ = self.scratch("SZ", [16, 128, T], BF16)
        self.XC = self.scratch("XC", [16, 128, T], BF16)
        self.KT = self.scratch("KT", [16, 128, T], BF16)
        self.QB = self.scratch("QB", [2, 16, 128, T], BF16)
        self.VX = self.scratch("VX", [T, 2560], BF16)
        self.KWm = self.scratch("KWm", [2, T, 2048], BF16)
        self.CWT = self.scratch("CWT", [T, 128], F32)
        self.CARB = self.scratch("CARB", [2, 4, 128, T // 64], F32)
        self.HT = self.scratch("HT", [2, 16, 128, T], F32)
        self.A_tm = self.scratch("A_tm", [T, D], BF16)
        self.CP_tm = self.scratch("CP_tm", [T, D], BF16)
        npool = max(1, self.kinds.count(2))
        self.pool_w = self.inp("pool_w", [npool, 4, 256, 256])
        self.pool_b = self.inp("pool_b", [npool, D])
        self.pool_scale = self.inp("pool_scale", [npool, D])
        self.poolcb = self.inp("poolcb", [128, 36, 128], BF16)
        self.poolcf = self.inp("poolcf", [128, 16, 128])
        self.h_in_src = True

    def hrow(self, j):
        return self.hres[j * 128:(j + 1) * 128, :]

    def src_row(self, j):
        if j < 2:
            return self.ctx[j * 128:(j + 1) * 128, :]
        return self.x[(j - 2) * 128:(j - 1) * 128, :]

    def load_h(self, h, hk, j):
        if self.h_in_src:
            self.dma("q_sp", h[:], self.src_row(j), [], [hk])
        else:
            self.dma("q_sp", h[:], self.hrow(j), [("hres", j)], [hk])

    def load_ident(self):
        self.ident = self.P.sb([128, 128], BF16, "ident")
        self.dma("q_sp", self.ident[:], self.identb[:], (), ["ident"])

    def ada_phase(self):
        P = self.P
        P.phase_begin()
        cs = P.sb([128, KC, 2], F32, "cs")
        cin = P.sb([128, KC, 2], F32, "cin")
        psm = Rot(P, 2, [128, 512], F32, "psm", psum=True)
        self.dma("q_sp", cin[:], self.cT[:], (), ["cin"])
        self.act(cs[:], cin[:], AF.Silu, ["cin"], ["cs"])
        wst = Rot(P, 2, [128, KC, 512], F32, "adaw")
        bsb = P.sb([2, 6 * D], F32, "adab")
        msb = Rot(P, 1, [2, 6 * D], F32, "adam")
        for l in range(self.depth):
            self.dma("q_sp", bsb[:], self.ada_b[l:l + 1, :].partition_broadcast(2), (), ["adab"])
            mt, mk = msb.next()
            for n in range(12):
                wt, wk = wst.next()
                self.dma("q_sp", wt[:], self.ada_w[l, :, n * 512:(n + 1) * 512].rearrange("(k p) n -> p k n", p=128), (), [wk])
                pt, pk = psm.next()
                for k in range(KC):
                    self.mm(pt[0:2, :], cs[:, k, :], wt[:, k, :], k == 0, k == KC - 1, [wk, "cs"], [pk])
                self.tt("dve", mt[:, n * 512:(n + 1) * 512], pt[0:2, :], bsb[:, n * 512:(n + 1) * 512], ALU.add, [pk, "adab"], [(mk, n)])
            self.dma("q_pool", self.modv[l], mt[:], [(mk, n) for n in range(12)], [("modv", l)])
        P.phase_end()

    def alloc_norm(self, need_aT=True):
        P = self.P
        self.load_ident()
        self.hbuf = Rot(P, 4, [128, D], F32, "hbuf")
        self.t1buf = Rot(P, 2, [128, D], F32, "t1buf")
        self.ssb = Rot(P, 4, [128, 2], F32, "ssb")
        self.thalf = Rot(P, 3, [128, 512], F32, "thalf")
        self.modt = {nm: P.sb([128, D], F32, nm) for nm in ("gs", "sh", "gt")}
        self.abuf = Rot(P, 2, [128, D], BF16, "abuf")
        if need_aT:
            self.aT = Rot(P, 2, [128, KC, 512], BF16, "aT")
            self.ptr = Rot(P, 2, [128, 1024], BF16, "ptr", psum=True)

    def load_mod(self, l, which, norm_g, row):
        m = self.modt
        base = 3 * which
        mv = self.modv[l, row:row + 1, :]
        tmps, tmpsk = self.t1buf.next()
        tmpg, tmpgk = self.t1buf.next()
        self.dma("q_sp", m["sh"][:], mv[:, (base + 0) * D:(base + 1) * D].partition_broadcast(128), [], ["sh"])
        self.dma("q_sp", tmps[:], mv[:, (base + 1) * D:(base + 2) * D].partition_broadcast(128), [], [tmpsk])
        self.dma("q_sp", m["gt"][:], mv[:, (base + 2) * D:(base + 3) * D].partition_broadcast(128), [], ["gt"])
        if norm_g is not None:
            self.dma("q_sp", tmpg[:], norm_g[l:l + 1, :].partition_broadcast(128), (), [tmpgk])
            self.stt("dve", m["gs"][:], tmps[:], 1.0, tmpg[:], ALU.add, ALU.mult, [tmpsk, tmpgk], ["gs"])

    def rstd(self, ss, ssk, n=D):
        self.act(ss[:, 1:2], ss[:, 0:1], AF.Sqrt, [ssk], [ssk], scale=1.0 / n, bias=EPS)
        self.P.op("dve", lambda e: e.reciprocal(ss[:, 1:2], ss[:, 1:2]), [ssk], [ssk])

    def norm_tile(self, j):
        m = self.modt
        h, hk = self.hbuf.next()
        self.load_h(h, hk, j)
        t1, t1k = self.t1buf.next()
        ss, ssk = self.ssb.next()
        self.act(t1[:], h[:], AF.Square, [hk], [t1k, ssk], accum=ss[:, 0:1])
        self.rstd(ss, ssk)
        self.stt("dve", t1[:], h[:], ss[:, 1:2], m["gs"][:], ALU.mult, ALU.mult, [hk, ssk, "gs", t1k], [t1k])
        a, ak = self.abuf.next()
        self.tt("pool", a[:], t1[:], m["sh"][:], ALU.add, [t1k, "sh"], [ak])
        return a, ak

    def norm_stage(self, si):
        s0, NT = self.sts[si]
        aT, aTk = self.aT.next()
        for jj in range(NT // 128):
            j = s0 // 128 + jj
            a, ak = self.norm_tile(j)
            pt, ptk = self.ptr.next()
            for k in range(KC):
                self.tr(pt[:, k * 128:(k + 1) * 128], a[:, k * 128:(k + 1) * 128], self.ident[:], [ak, "ident"], [ptk])
            self.cp("act", aT[:, :, jj * 128:(jj + 1) * 128], pt[:].rearrange("p (k t) -> p k t", k=KC), [ptk], [(aTk, jj)])
        return aT, aTk

    def load_w(self, dst_view, src_view, key, ncols_piece=2048):
        K = dst_view.shape[1]
        N = dst_view.shape[2]
        for k in range(K):
            for c in range(0, N, ncols_piece):
                ce = min(N, c + ncols_piece)
                self.dma("q_pool", dst_view[:, k, c:ce], src_view[:, k, c:ce], (), [(key, k, c // ncols_piece)])

    def sis(self, skip_ctx):
        return [si for si in range(len(self.sts)) if not (skip_ctx and si == 0)]

    def pipelined(self, sis, stage_a, stage_b, l, which, norm_g):
        prev = None
        cur_row = None
        for si in sis:
            row = 1 if si == 0 else 0
            if row != cur_row:
                if prev is not None:
                    stage_b(*prev)
                    prev = None
                self.load_mod(l, which, norm_g, row)
                cur_row = row
            a = stage_a(si)
            if prev is not None:
                stage_b(*prev)
            prev = (si,) + tuple(a)
        if prev is not None:
            stage_b(*prev)

    def mlp1_phase(self, l, skip_ctx=False):
        P = self.P
        P.phase_begin()
        wb = P.sb([128, KC * DFF], BF16, "w1")
        wkey = "w1"
        w1 = wb[:, :].rearrange("p (k n) -> p k n", k=KC)
        self.load_w(w1, self.mlp_w1[l].rearrange("(k p) n -> p k n", p=128), wkey)
        self.alloc_norm()
        pacc = Rot(P, 4, [128, 512], F32, "pacc", psum=True)
        rbuf = Rot(P, 3, [128, 512], F32, "rbuf")
        ubuf = Rot(P, 2, [128, 4, 512], BF16, "ubuf")

        def stage_b(si, aT, aTk):
            s0, NT = self.sts[si]
            nj = NT // 128
            for fo4 in range(8):
                ut, uk = ubuf.next()
                for q in range(4):
                    fo = fo4 * 4 + q
                    pa, pk = pacc.next()
                    for k in range(KC):
                        self.mm(pa[:, :NT], w1[:, k, fo * 128:(fo + 1) * 128], aT[:, k, :NT], k == 0, k == KC - 1,
                                [(wkey, k, fo // 16)] + [(aTk, jj) for jj in range(nj)], [pk])
                    rt, rk = rbuf.next()
                    self.act(rt[:, :NT], pa[:, :NT], AF.Relu, [pk], [rk])
                    self.tt("dve", ut[:, q, :NT], rt[:, :NT], rt[:, :NT], ALU.mult, [rk], [(uk, q)])
                self.dma("q_pool", self.U[fo4 * 4:(fo4 + 1) * 4, :, s0:s0 + NT].rearrange("f p t -> p f t"), ut[:, :, :NT],
                         [(uk, q) for q in range(4)], [("U", si, fo4)])

        self.pipelined(self.sis(skip_ctx), self.norm_stage, stage_b, l, 1, self.norm2_g)
        P.phase_end()

    def mlp2_phase(self, l, skip_ctx=False, final=False):
        P = self.P
        P.phase_begin()
        wb = P.sb([128, 32 * D], BF16, "w2")
        wkey = "w2"
        w2 = wb[:, :].rearrange("p (k n) -> p k n", k=32)
        self.load_w(w2, self.mlp_w2[l].rearrange("(k p) n -> p k n", p=128), wkey, ncols_piece=1024)
        self.alloc_norm(need_aT=False)
        pacc = Rot(P, 4, [128, 512], F32, "pacc", psum=True)
        u2buf = Rot(P, 2, [128, 32, 512], BF16, "u2buf")
        m = self.modt
        fg = None
        if final:
            fg = P.sb([128, D], F32, "fg")
            self.dma("q_sp", fg[:], self.final_g[0:1, :].partition_broadcast(128), (), ["fg"])
        cur_row = None
        for si in self.sis(skip_ctx):
            row = 1 if si == 0 else 0
            if row != cur_row:
                self.load_mod(l, 1, None, row)
                cur_row = row
            s0, NT = self.sts[si]
            nj = NT // 128
            ut, uk = u2buf.next()
            for f8 in range(4):
                self.dma("q_sp", ut[:, f8 * 8:(f8 + 1) * 8, :NT], self.U[f8 * 8:(f8 + 1) * 8, :, s0:s0 + NT].rearrange("f p t -> p f t"),
                         [("U", si, f8 * 2), ("U", si, f8 * 2 + 1)], [(uk, f8)])
            for jj in range(nj):
                j = s0 // 128 + jj
                h, hk = self.hbuf.next()
                self.load_h(h, hk, j)
                t1, t1k = self.t1buf.next()
                for nh in range(2):
                    pa, pk = pacc.next()
                    th, thk = self.thalf.next()
                    for k in range(32):
                        self.mm(pa[:, :], ut[:, k, jj * 128:(jj + 1) * 128], w2[:, k, nh * 512:(nh + 1) * 512], k == 0, k == 31,
                                [(wkey, k, 0), (uk, k // 8)], [pk])
                    self.tt("dve", th[:, :], pa[:, :], m["gt"][:, nh * 512:(nh + 1) * 512], ALU.mult,
                            [pk, "gt"], [thk])
                    self.tt("pool", h[:, nh * 512:(nh + 1) * 512], th[:, :], h[:, nh * 512:(nh + 1) * 512], ALU.add,
                            [thk, hk], [hk])
                if not final:
                    self.dma("q_pool", self.hrow(j), h[:], [hk], [("hres", j)])
                else:
                    t2, t2k = self.t1buf.next()
                    ss, ssk = self.ssb.next()
                    self.act(t2[:], h[:], AF.Square, [hk], [t2k, ssk], accum=ss[:, 0:1])
                    self.rstd(ss, ssk)
                    self.stt("dve", t2[:], h[:], ss[:, 1:2], fg[:], ALU.mult, ALU.mult, [hk, ssk, "fg", t2k], [t2k])
                    self.dma("q_pool", self.out[(j - 2) * 128:(j - 1) * 128, :], t2[:], [t2k], [("out", j)])
        P.phase_end()
        self.h_in_src = False

    def gla_phase_p(self, l):
        P = self.P
        jg = self.kinds[:l + 1].count(0) - 1
        P.phase_begin()
        wb = P.sb([128, KC * 3072], BF16, "win")
        win = wb[:, :].rearrange("p (k n) -> p k n", k=KC)
        self.load_w(win, self.gla_w_in[jg].rearrange("(k p) n -> p k n", p=128), "win", ncols_piece=1024)
        wa1 = P.sb([128, KC, 32], BF16, "wa1")
        self.dma("q_pool", wa1[:], self.gla_wa1[jg].rearrange("(k p) n -> p k n", p=128), (), ["wa1"])
        wa2 = P.sb([32, 1024], BF16, "wa2")
        self.dma("q_pool", wa2[:], self.gla_wa2[jg], (), ["wa2"])
        nba = P.sb([128, 8], F32, "nba")
        self.dma("q_sp", nba[:], self.gla_ba[jg], (), ["nba"])
        self.ts("dve", nba[:], nba[:], -1.0, None, ALU.mult, None, ["nba"], ["nba"])
        gc = P.sb([128, 768], F32, "gc")
        self.dma("q_sp", gc[:], self.glacf[:], (), ["gc"])
        self.alloc_norm()
        pacc = Rot(P, 4, [128, 512], F32, "pacc", psum=True)
        pz = Rot(P, 2, [128, 512], F32, "pz", psum=True)
        qkb = Rot(P, 2, [128, 8, 512], F32, "qkb")
        srb = Rot(P, 2, [128, 8, 512], BF16, "srb")
        vtb = Rot(P, 2, [128, D], BF16, "vtb")
        utb = Rot(P, 2, [32, 512], BF16, "utb")
        tA = Rot(P, 3, [128, 512], F32, "tA")
        tB = Rot(P, 2, [128, 512], F32, "tB")
        tC = Rot(P, 2, [128, 512], F32, "tC")
        tD = Rot(P, 2, [128, 512], F32, "tD")
        ob = Rot(P, 4, [128, 512], BF16, "ob")
        kw4 = Rot(P, 2, [128, 4, 512], BF16, "kw4")
        kwt = Rot(P, 2, [128, 512], BF16, "kwt")
        decb = Rot(P, 2, [128, 32], F32, "decb")

        def stage_b(si, aT, aTk):
            s0, NT = self.sts[si]
            nj = NT // 128
            nch = NT // 64
            aks = [(aTk, jj) for jj in range(nj)]
            qk, qkk = qkb.next()
            for i in range(8):
                pa, pk = pacc.next()
                for k in range(KC):
                    self.mm(pa[:, :NT], win[:, k, i * 128:(i + 1) * 128], aT[:, k, :NT], k == 0, k == KC - 1, [("win", k, 0)] + aks, [pk])
                self.act(qk[:, i, :NT], pa[:, :NT], AF.Copy, [pk], [(qkk, i)], scale=(128 ** -0.5 if i < 4 else 1.0))
            sr, srk = srb.next()
            for i in range(8):
                pa, pk = pacc.next()
                for k in range(KC):
                    self.mm(pa[:, :NT], win[:, k, 2048 + i * 128:2048 + (i + 1) * 128], aT[:, k, :NT], k == 0, k == KC - 1, [("win", k, 2)] + aks, [pk])
                self.act(sr[:, i, :NT], pa[:, :NT], AF.Silu, [pk], [(srk, i)])
            self.dma("q_pool", self.SR[:, :, s0:s0 + NT].rearrange("f p t -> p f t"), sr[:, :, :NT], [(srk, i) for i in range(8)], [])
            for jj in range(nj):
                vt, vtk = vtb.next()
                for nh in range(2):
                    pa, pk = pacc.next()
                    th, thk = self.thalf.next()
                    for k in range(KC):
                        self.mm(pa[:, :], aT[:, k, jj * 128:(jj + 1) * 128], win[:, k, 1024 + nh * 512:1024 + (nh + 1) * 512], k == 0, k == KC - 1,
                                [("win", k, 1), (aTk, jj)], [pk])
                    self.cp("dve", vt[:, nh * 512:(nh + 1) * 512], pa[:, :], [pk], [(vtk, nh)])
                self.dma("q_pool", self.V_tm[s0 + jj * 128:s0 + (jj + 1) * 128, :], vt[:], [(vtk, 0), (vtk, 1)], [])
            ut, utk = utb.next()
            pu, puk = pz.next()
            for k in range(KC):
                self.mm(pu[0:32, :NT], wa1[:, k, :], aT[:, k, :NT], k == 0, k == KC - 1, ["wa1"] + aks, [puk])
            self.cp("dve", ut[:, :NT], pu[0:32, :NT], [puk], [utk])
            for d in range(2):
                kw, kwk = kw4.next()
                dec, deck = decb.next()
                for h in range(4):
                    q = qk[:, h, :NT]
                    kk = qk[:, 4 + h, :NT]
                    pzt, pzk = pz.next()
                    c0 = d * 512 + h * 128
                    self.mm(pzt[:, :NT], wa2[0:32, c0:c0 + 128], ut[0:32, :NT], True, True, ["wa2", utk], [pzk])
                    e, ek = tA.next()
                    self.act(e[:, :NT], pzt[:, :NT], AF.Exp, [pzk, "nba"], [ek], scale=-1.0, bias=nba[:, d * 4 + h:d * 4 + h + 1])
                    sp, spk = tB.next()
                    self.act(sp[:, :NT], e[:, :NT], AF.Ln, [ek], [spk], bias=1.0)
                    cs, csk = tC.next()
                    self.scan(cs[:, :NT], gc[:, :NT], sp[:, :NT], 0.0, ALU.mult, ALU.add, ["gc", spk], [csk])
                    cs3 = cs[:, :NT].rearrange("p (c t) -> p c t", t=64)
                    cl_b = cs3[:, :, 63:64].to_broadcast([128, nch, 64])
                    dd, ddk = tD.next()
                    dd3 = dd[:, :NT].rearrange("p (c t) -> p c t", t=64)
                    sp3 = sp[:, :NT].rearrange("p (c t) -> p c t", t=64)
                    if d == 0:
                        xq, xs = cs, -1.0 / 16
                        xk, xks = cs, 1.0 / 16
                        self.tt("dve", dd3, cs3, cl_b, ALU.subtract, [csk], [ddk])
                        xw, xws = dd, 1.0 / 16
                        xqk = xkk = csk
                        xwk = ddk
                    else:
                        t1, t1k_ = tD.next()
                        t13 = t1[:, :NT].rearrange("p (c t) -> p c t", t=64)
                        self.tt("dve", t13, sp3, cs3, ALU.subtract, [csk, spk], [t1k_])
                        self.tt("dve", dd3, t13, cl_b, ALU.add, [t1k_, csk], [ddk])
                        xq, xs, xqk = dd, -1.0 / 16, ddk
                        xk, xks, xkk = dd, 1.0 / 16, ddk
                        xw, xws, xwk = t1, 1.0 / 16, t1k_
                    e1, e1k = tA.next()
                    self.act(e1[:, :NT], xq[:, :NT], AF.Exp, [xqk], [e1k], scale=xs)
                    o1, o1k = ob.next()
                    self.tt("pool", o1[:, :NT], q, e1[:, :NT], ALU.mult, [(qkk, h), e1k], [o1k])
                    self.dma("q_pool", self.QD[d, h, :, s0:s0 + NT], o1[:, :NT], [o1k], [])
                    e2, e2k = tA.next()
                    self.act(e2[:, :NT], xk[:, :NT], AF.Exp, [xkk], [e2k], scale=xks)
                    o2, o2k = ob.next()
                    self.tt("pool", o2[:, :NT], kk, e2[:, :NT], ALU.mult, [(qkk, 4 + h), e2k], [o2k])
                    self.dma("q_pool", self.KD[d, h, :, s0:s0 + NT], o2[:, :NT], [o2k], [])
                    e3, e3k = tA.next()
                    self.act(e3[:, :NT], xw[:, :NT], AF.Exp, [xwk], [e3k], scale=xws)
                    self.tt("pool", kw[:, h, :NT], kk, e3[:, :NT], ALU.mult, [(qkk, 4 + h), e3k], [(kwk, h)])
                    self.act(self._decv(dec, h, nch), cs3[:, :, 63], AF.Exp, [csk], [(deck, h)], scale=-1.0 / 16)
                self.dma("q_pool", self.DEC[d, :, :, s0 // 64:s0 // 64 + nch].rearrange("h p c -> p h c"),
                         self._decall(dec, nch), [(deck, h) for h in range(4)], [])
                for jj in range(nj):
                    pt, ptk = self.ptr.next()
                    for h in range(4):
                        self.tr(pt[:, h * 128:(h + 1) * 128], kw[:, h, jj * 128:(jj + 1) * 128], self.ident[:], [(kwk, h), "ident"], [ptk])
                    kt, ktk = kwt.next()
                    self.cp("act", kt[:], pt[:, 0:512], [ptk], [ktk])
                    self.dma("q_pool", self.KW_tm[d, s0 + jj * 128:s0 + (jj + 1) * 128, :], kt[:], [ktk], [])

        self.pipelined(self.sis(False), self.norm_stage, stage_b, l, 0, self.norm1_g)
        P.phase_end()

    def _decv(self, dec, h, nch):
        return dec[:, h * 8:h * 8 + nch]

    def _decall(self, dec, nch):
        return dec[:, :].rearrange("p (h c) -> p h c", h=4)[:, :, :nch]

    def tile_order(self, d):
        sis = list(range(len(self.sts)))
        if d == 1:
            sis = [0] + sis[:0:-1]
        return sis

    def gla_phase_s(self, l, d):
        P = self.P
        P.phase_begin()
        gc = P.sb([128, 768], F32, "gc")
        self.dma("q_sp", gc[:], self.glacf[:], (), ["gc"])
        mask = gc[:, 512 + d * 128:512 + (d + 1) * 128]
        qdb = Rot(P, 2, [128, 4, 512], BF16, "qdb")
        kdb = Rot(P, 2, [128, 4, 512], BF16, "kdb")
        kwb = Rot(P, 2, [128, 4, 512], BF16, "kwb")
        vtb = Rot(P, 2, [128, 4, D], BF16, "vtb")
        decb = Rot(P, 2, [128, 4, 8], F32, "decb")
        S = P.sb([128, 4, 256], F32, "S")
        Sb = P.sb([128, 4, 256], BF16, "Sb")
        attb = Rot(P, 4, [128, 128], BF16, "attb")
        otb = Rot(P, 2, [128, 8, 512], F32, "otb")
        patt = Rot(P, 2, [128, 512], F32, "patt", psum=True)
        po = Rot(P, 3, [128, 512], F32, "po", psum=True)
        pst = Rot(P, 3, [128, 512], F32, "pst", psum=True)
        for h in range(4):
            self.memset("dve", S[:, h, :], 0.0, [("S", h)])
            self.memset("pool", Sb[:, h, :], 0.0, [("Sb", h)])
        for si in self.tile_order(d):
            s0, NT = self.sts[si]
            nj = NT // 128
            nch = NT // 64
            qd, qdk = qdb.next()
            kd, kdk = kdb.next()
            kw, kwk = kwb.next()
            vt, vtk = vtb.next()
            dec, deck = decb.next()
            ot, otk = otb.next()
            self.dma("q_sp", qd[:, :, :NT], self.QD[d, :, :, s0:s0 + NT].rearrange("h p t -> p h t"), [], [qdk])
            self.dma("q_sp", kd[:, :, :NT], self.KD[d, :, :, s0:s0 + NT].rearrange("h p t -> p h t"), [], [kdk])
            self.dma("q_sp", kw[:, :nj, :], self.KW_tm[d, s0:s0 + NT, :].rearrange("(j p) f -> p j f", p=128), [], [kwk])
            self.dma("q_sp", vt[:, :nj, :], self.V_tm[s0:s0 + NT, :].rearrange("(j p) f -> p j f", p=128), [], [vtk])
            self.dma("q_sp", dec[:, :, :nch], self.DEC[d, :, :, s0 // 64:s0 // 64 + nch].rearrange("h p c -> p h c"), [], [deck])
            jjs = list(range(nj)) if d == 0 else list(range(nj - 1, -1, -1))
            cs_ = (0, 1) if d == 0 else (1, 0)
            for jj in jjs:
                tsl = slice(jj * 128, (jj + 1) * 128)
                pos = {}

                def g_A(h):
                    pa, pak = patt.next()
                    self.mm(pa[:, 0:128], kd[:, h, tsl], qd[:, h, tsl], True, True, [kdk, qdk], [pak])
                    at, atk = attb.next()
                    self.tt("dve", at[:], pa[:, 0:128], mask, ALU.mult, [pak, "gc"], [atk])
                    p_ob, pok = po.next()
                    p_o = p_ob[:, 0:256].rearrange("p (v t) -> p v t", v=2)
                    for vc in range(2):
                        self.mm(p_o[:, vc, :], vt[:, jj, h * 256 + vc * 128:h * 256 + (vc + 1) * 128], at[:], vc == 0, False, [vtk, atk], [pok], sgc=True)
                    pos[h] = (p_o, pok)

                def g_c(h, ci):
                    c = cs_[ci]
                    p_o, pok = pos[h]
                    csl = slice(jj * 128 + c * 64, jj * 128 + (c + 1) * 64)
                    rows = slice(c * 64, (c + 1) * 64)
                    for vc in range(2):
                        self.mm(p_o[:, vc, c * 64:(c + 1) * 64], Sb[:, h, vc * 128:(vc + 1) * 128], qd[:, h, csl], False, ci == 1,
                                [("Sb", h), qdk], [pok], sgc=True)
                    ps, psk = pst.next()
                    self.mm(ps[:, 0:256], kw[rows, jj, h * 128:(h + 1) * 128], vt[rows, jj, h * 256:(h + 1) * 256], True, True, [kwk, vtk], [psk])
                    ch = jj * 2 + c
                    self.stt("dve", S[:, h, :], S[:, h, :], dec[:, h, ch:ch + 1], ps[:, 0:256], ALU.mult, ALU.add, [("S", h), deck, psk], [("S", h)])
                    self.cp("act", Sb[:, h, :], S[:, h, :], [("S", h)], [("Sb", h)])

                def g_E(h):
                    p_o, pok = pos[h]
                    self.cp("act", ot[:, 2 * h:2 * h + 2, tsl], p_o[:, :, :], [pok], [(otk, jj, h)])

                for h in range(4):
                    g_A(h)
                    g_c(h, 0)
                    if h >= 1:
                        g_c(h - 1, 1)
                        g_E(h - 1)
                g_c(3, 1)
                g_E(3)
            self.dma("q_pool", self.OO[d, :, :, s0:s0 + NT].rearrange("f p t -> p f t"), ot[:, :, :NT],
                     [(otk, jj, h) for jj in range(nj) for h in range(4)], [])
        P.phase_end()

    def gla_phase_o(self, l, need_ctx):
        P = self.P
        jg = self.kinds[:l + 1].count(0) - 1
        P.phase_begin()
        wb = P.sb([128, KC * D], BF16, "wo")
        wo = wb[:, :].rearrange("p (k n) -> p k n", k=KC)
        self.load_w(wo, self.gla_w_o[jg].rearrange("(k p) n -> p k n", p=128), "wo", ncols_piece=1024)
        gh = P.sb([128, 8], F32, "gh")
        self.dma("q_sp", gh[:], self.gla_gh[jg], (), ["gh"])
        ones = P.sb([128, 128], BF16, "ones")
        self.dma("q_sp", ones[:], self.onesb[:], (), ["ones"])
        self.alloc_norm(need_aT=False)
        m = self.modt
        pacc = Rot(P, 4, [128, 512], F32, "pacc", psum=True)
        o0b = Rot(P, 2, [128, 8, 512], F32, "o0b")
        o1b = Rot(P, 1, [128, 8, 512], F32, "o1b")
        srb = Rot(P, 2, [128, 8, 512], BF16, "srb")
        sqb = Rot(P, 1, [128, 8, 512], BF16, "sqb")
        yTb = Rot(P, 2, [128, 8, 512], BF16, "yTb")
        rsb = Rot(P, 2, [128, 4, 512], F32, "rsb")
        tmpb = Rot(P, 2, [128, 512], F32, "tmpb")
        cur_row = None
        for si in self.sis(not need_ctx):
            row = 1 if si == 0 else 0
            if row != cur_row:
                self.load_mod(l, 0, None, row)
                cur_row = row
            s0, NT = self.sts[si]
            nj = NT // 128
            o0, o0k = o0b.next()
            o1, o1k = o1b.next()
            sr, srk = srb.next()
            self.dma("q_sp", o0[:, :, :NT], self.OO[0, :, :, s0:s0 + NT].rearrange("f p t -> p f t"), [], [o0k])
            self.dma("q_sp", o1[:, :, :NT], self.OO[1, :, :, s0:s0 + NT].rearrange("f p t -> p f t"), [], [o1k])
            self.dma("q_sp", sr[:, :, :NT], self.SR[:, :, s0:s0 + NT].rearrange("f p t -> p f t"), [], [srk])
            self.tt("pool", o0[:, :, :NT], o0[:, :, :NT], o1[:, :, :NT], ALU.add, [o0k, o1k], [o0k])
            sq, sqk = sqb.next()
            self.act(sq[:, :, :NT], o0[:, :, :NT], AF.Square, [o0k], [sqk])
            rs, rsk = rsb.next()
            for h in range(4):
                pa, pk = pacc.next()
                for vc in range(2):
                    self.mm(pa[:, :NT], ones[:], sq[:, 2 * h + vc, :NT], vc == 0, vc == 1, ["ones", sqk], [pk])
                self.act(rs[:, h, :NT], pa[:, :NT], AF.Sqrt, [pk], [(rsk, h)], scale=1.0 / 256, bias=EPS)
                self.P.op("dve", (lambda rs=rs, h=h, NT=NT: (lambda e: e.reciprocal(rs[:, h, :NT], rs[:, h, :NT])))(), [(rsk, h)], [(rsk, h)])
            yT, yTk = yTb.next()
            for i in range(8):
                tm, tmk = tmpb.next()
                self.stt("dve", tm[:, :NT], o0[:, i, :NT], gh[:, i:i + 1], rs[:, i // 2, :NT], ALU.mult, ALU.mult, [o0k, "gh", (rsk, i // 2)], [tmk])
                self.tt("pool", yT[:, i, :NT], tm[:, :NT], sr[:, i, :NT], ALU.mult, [tmk, srk], [(yTk, i)])
            yks = [(yTk, i) for i in range(8)]
            for jj in range(nj):
                j = s0 // 128 + jj
                h_, hk = self.hbuf.next()
                self.load_h(h_, hk, j)
                t1, t1k = self.t1buf.next()
                for nh in range(2):
                    pa, pk = pacc.next()
                    th, thk = self.thalf.next()
                    for k in range(KC):
                        self.mm(pa[:, :], yT[:, k, jj * 128:(jj + 1) * 128], wo[:, k, nh * 512:(nh + 1) * 512], k == 0, k == KC - 1,
                                [("wo", k, 0), (yTk, k)], [pk])
                    self.tt("dve", th[:, :], pa[:, :], m["gt"][:, nh * 512:(nh + 1) * 512], ALU.mult, [pk, "gt"], [thk])
                    self.tt("pool", h_[:, nh * 512:(nh + 1) * 512], th[:, :], h_[:, nh * 512:(nh + 1) * 512], ALU.add,
                            [thk, hk], [hk])
                self.dma("q_pool", self.hrow(j), h_[:], [hk], [])
        P.phase_end()
        self.h_in_src = False

    def units256(self):
        return [(s0, 256) for s0 in range(0, self.T, 256)]

    def mlstm_phase_p1(self, l):
        P = self.P
        jm = self.kinds[:l + 1].count(1) - 1
        P.phase_begin()
        wb = P.sb([128, KC * 4096], BF16, "wup")
        wup = wb[:, :].rearrange("p (k n) -> p k n", k=KC)
        self.load_w(wup, self.ml_w_up[jm].rearrange("(k p) n -> p k n", p=128), "wup")
        self.alloc_norm()
        pacc = Rot(P, 4, [128, 512], F32, "pacc", psum=True)
        obuf = Rot(P, 3, [128, 4, 512], BF16, "obuf")

        def stage_b(si, aT, aTk):
            s0, NT = self.sts[si]
            nj = NT // 128
            aks = [(aTk, jj) for jj in range(nj)]
            for i4 in range(8):
                ot, otk = obuf.next()
                for q in range(4):
                    i = i4 * 4 + q
                    pa, pk = pacc.next()
                    for k in range(KC):
                        self.mm(pa[:, :NT], wup[:, k, i * 128:(i + 1) * 128], aT[:, k, :NT], k == 0, k == KC - 1, [("wup", k, i // 16)] + aks, [pk])
                    if i < 16:
                        self.cp("act", ot[:, q, :NT], pa[:, :NT], [pk], [(otk, q)])
                    else:
                        self.act(ot[:, q, :NT], pa[:, :NT], AF.Silu, [pk], [(otk, q)])
                dst = self.XM if i4 < 4 else self.SZ
                i0 = (i4 % 4) * 4
                self.dma("q_pool", dst[i0:i0 + 4, :, s0:s0 + NT].rearrange("f p t -> p f t"), ot[:, :, :NT], [(otk, q) for q in range(4)], [])

        self.pipelined(self.sis(False), self.norm_stage, stage_b, l, 0, self.norm1_g)
        P.phase_end()

    def mlstm_phase_p2(self, l):
        P = self.P
        jm = self.kinds[:l + 1].count(1) - 1
        P.phase_begin()
        self.load_ident()
        NT = 256
        CH = MCH
        nj, nch = 2, NT // MCH
        bd = P.sb([128, 48, 128], BF16, "bd")
        for m_ in range(3):
            self.dma("q_pool", bd[:, m_ * 16:(m_ + 1) * 16, :], self.ml_bd[jm, m_].rearrange("c p n -> p c n"), (), ["bd"])
        wgI = P.sb([128, 48, 64], BF16, "wgI")
        wgF = P.sb([128, 48, 64], BF16, "wgF")
        self.dma("q_pool", wgI[:], self.ml_wgI[jm], (), ["wg"])
        self.dma("q_pool", wgF[:], self.ml_wgF[jm], (), ["wg"])
        cw = P.sb([128, 16, 4], F32, "cw")
        cbias = P.sb([128, 16], F32, "cbias")
        self.dma("q_sp", cw[:], self.ml_convw[jm], (), ["cw"])
        self.dma("q_sp", cbias[:], self.ml_convb[jm], (), ["cw"])
        bI = P.sb([64, 1], F32, "bI")
        nbF = P.sb([64, 1], F32, "nbF")
        self.dma("q_sp", bI[:], self.ml_bgI[jm], (), ["bI"])
        self.dma("q_sp", nbF[:], self.ml_bgF[jm], (), ["nbF"])
        self.ts("dve", nbF[:], nbF[:], -1.0, None, ALU.mult, None, ["nbF"], ["nbF"])
        gc = P.sb([128, 512], F32, "gc")
        self.dma("q_sp", gc[:], self.mlmask[:, 0:512], (), ["gc"])
        sel = P.sb([64, 8, 128], F32, "sel")
        self.dma("q_sp", sel[:], self.ml_sel[:], (), ["sel"])
        identf = P.sb([128, 128], F32, "identf")
        self.dma("q_sp", identf[:], self.identf_d[:], (), ["identf"])
        pacc = Rot(P, 2, [128, 512], F32, "pacc", psum=True)
        pgI = P.ps([128, 512], F32, "pgI")
        pgF = P.ps([128, 512], F32, "pgF")
        pb = Rot(P, 1, [128, 512], F32, "pb", psum=True)
        pcx = P.ps([128, 512], F32, "pcx")
        ptkv = P.ps([128, 2048], BF16, "ptkv")
        xwb = Rot(P, 2, [128, 16, NT + 32], BF16, "xwb")
        xcb = Rot(P, 2, [128, 16, NT], BF16, "xcb")
        qkvb = Rot(P, 1, [128, 48, NT], BF16, "qkvb")
        qsb = Rot(P, 1, [128, 16, NT], BF16, "qsb")
        qbb = Rot(P, 3, [128, 4, NT], BF16, "qbb")
        accb = Rot(P, 4, [128, NT], F32, "accb")
        gt_ = {nm: P.sb([64, NT], F32, "g" + nm) for nm in ("LI", "E", "SP", "CS", "BN", "EB", "T1", "COL", "T2", "CW")}
        car = P.sb([64, 8], F32, "car")
        carb = Rot(P, 2, [128, 8, nch], F32, "carb")
        cwtb = Rot(P, 2, [128, 128], F32, "cwtb")
        vxb = Rot(P, 2, [128, 4, 640], BF16, "vxb")
        for i in range(2):
            self.memset("pool", vxb.bufs[i][:, :, 512:640], 1.0, [(vxb.keys[i], "ones")])
        kwb = Rot(P, 2, [128, 2048], BF16, "kwb")
        s_q = 512.0 ** -0.5
        nunits = self.T // 256
        for u in range(nunits):
            s0 = u * 256
            seq_lo, seq_hi = (0, TC) if u == 0 else (TC, self.T)
            xw, xwk = xwb.next()
            lo = max(s0 - 2, seq_lo)
            hi = min(s0 + NT + 1, seq_hi)
            if lo > s0 - 2:
                self.memset("pool", xw[:, :, 14:16], 0.0, [(xwk, "L")])
            if hi < s0 + NT + 1:
                self.memset("pool", xw[:, :, NT + 16:NT + 17], 0.0, [(xwk, "R")])
            self.dma("q_sp", xw[:, :, 14 + lo - (s0 - 2):14 + hi - (s0 - 2)], self.XM[:, :, lo:hi].rearrange("c p t -> p c t"), [],
                     [(xwk, "L"), (xwk, "M"), (xwk, "R")])
            xwks = [(xwk, "L"), (xwk, "M"), (xwk, "R")]
            xc, xck = xcb.next()
            for c in range(16):
                eng = "dve"
                ac, ack = accb.next()
                self.ts(eng, ac[:, :], xw[:, c, 14:14 + NT], cw[:, c, 0:1], None, ALU.mult, None, xwks + ["cw"], [ack])
                for j in range(1, 4):
                    self.stt(eng, ac[:, :], xw[:, c, 14 + j:14 + NT + j], cw[:, c, j:j + 1], ac[:, :], ALU.mult, ALU.add, xwks + ["cw", ack], [ack])
                self.act(xc[:, c, :], ac[:, :], AF.Silu, [ack, "cw"], [(xck, c)], bias=cbias[:, c:c + 1])
            xcks = [(xck, c) for c in range(16)]
            self.dma("q_pool", self.XC[:, :, s0:s0 + NT].rearrange("c p t -> p c t"), xc[:, :, :], xcks, [])
            qkv, qkvk = qkvb.next()
            qs, qsk = qsb.next()
            for m_ in range(3):
                for c in range(16):
                    pa, pk = pacc.next()
                    if m_ < 2:
                        self.mm(pa[:, :NT], bd[:, m_ * 16 + c, :], xc[:, c, :], True, True, ["bd", (xck, c)], [pk])
                    else:
                        self.mm(pa[:, :NT], bd[:, m_ * 16 + c, :], xw[:, c, 16:NT + 16], True, True, ["bd"] + xwks, [pk])
                    self.cp("act", qkv[:, m_ * 16 + c, :], pa[:, :NT], [pk], [(qkvk, m_ * 16 + c)])
                    if m_ == 0:
                        self.ts("pool", qs[:, c, :], qkv[:, c, :], s_q, None, ALU.mult, None, [(qkvk, c)], [(qsk, c)])
            self.dma("q_pool", self.KT[:, :, s0:s0 + NT].rearrange("c p t -> p c t"), qkv[:, 16:32, :], [(qkvk, 16 + c) for c in range(16)], [])
            for c in range(48):
                self.mm(pgI[0:64, :NT], wgI[:, c, :], qkv[:, c, :], c == 0, c == 47, ["wg", (qkvk, c)], ["pgI"])
            for c in range(48):
                self.mm(pgF[0:64, :NT], wgF[:, c, :], qkv[:, c, :], c == 0, c == 47, ["wg", (qkvk, c)], ["pgF"])
            g = gt_
            self.act(g["LI"][:], pgI[0:64, :NT], AF.Identity, ["pgI", "bI"], ["LI"], bias=bI[:, 0:1])
            self.act(g["E"][:], pgF[0:64, :NT], AF.Exp, ["pgF", "nbF"], ["E"], scale=-1.0, bias=nbF[:, 0:1])
            self.act(g["SP"][:], g["E"][:], AF.Ln, ["E"], ["SP"], bias=1.0)
            self.scan(g["CS"][:], gc[0:64, :NT], g["SP"][:], 0.0, ALU.mult, ALU.add, ["gc", "SP"], ["CS"])
            cs3 = g["CS"][:].rearrange("p (c t) -> p c t", t=CH)
            self.cp("act", g["BN"][0:32, :], g["CS"][0:32, :], ["CS"], [("BN", 0)])
            self.tt("dve", g["BN"][32:64, :], g["SP"][32:64, :], g["CS"][32:64, :], ALU.subtract, ["SP", "CS"], [("BN", 1)])
            bn3 = g["BN"][:].rearrange("p (c t) -> p c t", t=CH)
            self.tt("dve", bn3[32:64], bn3[32:64], cs3[32:64, :, CH - 1:CH].to_broadcast([32, nch, CH]), ALU.add, [("BN", 1), "CS"], [("BN", 1)])
            bnk = [("BN", 0), ("BN", 1)]
            self.act(g["EB"][:], g["BN"][:], AF.Exp, bnk, ["EB"], scale=-1.0)
            self.tt("dve", g["T1"][:], g["LI"][:], g["BN"][:], ALU.add, ["LI"] + bnk, ["T1"])
            self.act(g["COL"][:], g["T1"][:], AF.Exp, ["T1"], ["COL"])
            t13 = g["T1"][:].rearrange("p (c t) -> p c t", t=CH)
            t23 = g["T2"][:].rearrange("p (c t) -> p c t", t=CH)
            self.tt("dve", t23, t13, cs3[:, :, CH - 1:CH].to_broadcast([64, nch, CH]), ALU.subtract, ["T1", "CS"], ["T2"])
            self.act(g["CW"][:], g["T2"][:], AF.Exp, ["T2"], ["CW"])
            self.act(car[:, 0:nch], cs3[:, :, CH - 1], AF.Exp, ["CS"], ["car"], scale=-1.0)
            for r8 in range(8):
                d, h = r8 // 4, r8 % 4
                pbt, pbk = pb.next()
                self.mm(pbt[:, :NT], sel[:, r8, :], g["EB"][:, :], True, True, ["sel", "EB"], [pbk])
                qb, qbk = qbb.next()
                self.tt("dve", qb[:, :, :], qs[:, h * 4:(h + 1) * 4, :], pbt[:, :NT].unsqueeze(1).to_broadcast([128, 4, NT]), ALU.mult,
                        [pbk] + [(qsk, h * 4 + i) for i in range(4)], [qbk])
                self.dma("q_pool", self.QB[d, h * 4:(h + 1) * 4, :, s0:s0 + NT].rearrange("c p t -> p c t"), qb[:, :, :], [qbk], [])
            for r8 in range(8):
                self.mm(pcx[:, 256 + r8 * nch:256 + (r8 + 1) * nch], sel[:, r8, :], car[:, 0:nch], r8 == 0, r8 == 7, ["sel", "car"], ["pcx"], sgc=True)
            cb_, cbk = carb.next()
            self.cp("dve", cb_[:, :, :], pcx[:, 256:256 + 8 * nch].rearrange("p (r c) -> p r c", c=nch), ["pcx"], [cbk])
            for d in range(2):
                self.dma("q_pool", self.CARB[d, :, :, s0 // CH:s0 // CH + nch].rearrange("h p c -> p h c"), cb_[:, d * 4:(d + 1) * 4, :], [cbk], [])
            for jj in range(nj):
                tsl = slice(jj * 128, (jj + 1) * 128)
                self.tr(pcx[:, 0:64], g["COL"][:, tsl], identf[0:64, 0:64], ["COL", "identf"], ["pcx"])
                self.tr(pcx[:, 64:128], g["CW"][:, tsl], identf[0:64, 0:64], ["CW", "identf"], ["pcx"])
                ct, ctk = cwtb.next()
                self.cp("dve", ct[:, :], pcx[:, 0:128], ["pcx"], [ctk])
                self.dma("q_pool", self.CWT[s0 + jj * 128:s0 + (jj + 1) * 128, :], ct[:, :], [ctk], [])
                for c in range(16):
                    self.tr(ptkv[:, c * 128:(c + 1) * 128], qkv[:, 32 + c, tsl], self.ident[:], [(qkvk, 32 + c), "ident"], ["ptkv"])
                vx, vxk = vxb.next()
                self.cp("act", vx[:, :, 0:512], ptkv[:, :].rearrange("p (h v) -> p h v", h=4), ["ptkv"], [(vxk, "v")])
                self.dma("q_pool", self.VX[s0 + jj * 128:s0 + (jj + 1) * 128, :], vx[:, :, :].rearrange("p h v -> p (h v)"), [(vxk, "v"), (vxk, "ones")], [])
                for c in range(16):
                    self.tr(ptkv[:, c * 128:(c + 1) * 128], qkv[:, 16 + c, tsl], self.ident[:], [(qkvk, 16 + c), "ident"], ["ptkv"])
                for d in range(2):
                    kw, kwk = kwb.next()
                    for h in range(4):
                        col = ct[:, 64 + 32 * d + h:64 + 32 * d + h + 1]
                        if h < 2:
                            self.act(kw[:, h * 512:(h + 1) * 512], ptkv[:, h * 512:(h + 1) * 512], AF.Copy, ["ptkv", ctk], [(kwk, h)], scale=col)
                        else:
                            self.ts("dve", kw[:, h * 512:(h + 1) * 512], ptkv[:, h * 512:(h + 1) * 512], col, None, ALU.mult, None, ["ptkv", ctk], [(kwk, h)])
                    self.dma("q_pool", self.KWm[d, s0 + jj * 128:s0 + (jj + 1) * 128, :], kw[:, :], [(kwk, h) for h in range(4)], [])
        P.phase_end()

    def mlstm_phase_s(self, l, d):
        P = self.P
        P.phase_begin()
        CH = MCH
        NT, nj, nch = 256, 2, 256 // MCH
        gc = P.sb([128, 256], F32, "gc")
        self.dma("q_sp", gc[:], self.mlmask[:, 512:768], (), ["gc"])
        mask = gc[:, d * 128:(d + 1) * 128]
        qbb = Rot(P, 2, [128, 16, NT], BF16, "qbb")
        ktb = Rot(P, 2, [128, 16, NT], BF16, "ktb")
        vxb = Rot(P, 2, [128, nj, 2560], BF16, "vxb")
        kwb = Rot(P, 2, [128, nj, 2048], BF16, "kwb")
        cwb = Rot(P, 2, [128, nj, 128], F32, "cwb")
        crb = Rot(P, 2, [128, 4, nch], F32, "crb")
        C = P.sb([128, 16, 640], F32, "C")
        Cb = P.sb([128, 16, 640], BF16, "Cb")
        wtb = Rot(P, 3, [128, 128], BF16, "wtb")
        rdb = Rot(P, 2, [128, 128], F32, "rdb")
        htb = Rot(P, 2, [128, 16, NT], F32, "htb")
        pn = Rot(P, 2, [128, 1024], F32, "pn", psum=True)
        pst = Rot(P, 2, [128, 1024], F32, "pst", psum=True)
        for i in range(16):
            self.memset("dve", C[:, i, :], 0.0, [("C", i)])
            self.memset("pool", Cb[:, i, :], 0.0, [("Cb", i)])
        nunits = self.T // 256
        order = list(range(nunits)) if d == 0 else [0] + list(range(nunits - 1, 0, -1))
        for u in order:
            s0 = u * 256
            qb, qbk = qbb.next()
            kt, ktk = ktb.next()
            vx, vxk = vxb.next()
            kw, kwk = kwb.next()
            cw, cwk = cwb.next()
            cr, crk = crb.next()
            ht, htk = htb.next()
            self.dma("q_sp", qb[:, :, :], self.QB[d, :, :, s0:s0 + NT].rearrange("c p t -> p c t"), [], [qbk])
            self.dma("q_sp", kt[:, :, :], self.KT[:, :, s0:s0 + NT].rearrange("c p t -> p c t"), [], [ktk])
            self.dma("q_sp", vx[:, :, :], self.VX[s0:s0 + NT, :].rearrange("(j p) f -> p j f", p=128), [], [vxk])
            self.dma("q_sp", kw[:, :, :], self.KWm[d, s0:s0 + NT, :].rearrange("(j p) f -> p j f", p=128), [], [kwk])
            self.dma("q_sp", cw[:, :, :], self.CWT[s0:s0 + NT, :].rearrange("(j p) f -> p j f", p=128), [], [cwk])
            self.dma("q_sp", cr[:, :, :], self.CARB[d, :, :, s0 // CH:s0 // CH + nch].rearrange("h p c -> p h c"), [], [crk])
            jjs = list(range(nj)) if d == 0 else list(range(nj - 1, -1, -1))
            for jj in jjs:
                tsl = slice(jj * 128, (jj + 1) * 128)
                pns = {}

                def step_A(h):
                    pnt, pnk = pn.next()
                    pns[h] = (pnt, pnk)
                    for dc in range(4):
                        self.mm(pnt[:, 640:768], kt[:, h * 4 + dc, tsl], qb[:, h * 4 + dc, tsl], dc == 0, dc == 3, [ktk, qbk], [pnk], sgc=True)
                    wt, wtk = wtb.next()
                    self.stt("dve", wt[:, :], pnt[:, 640:768], cw[:, jj, 32 * d + h:32 * d + h + 1], mask, ALU.mult, ALU.mult, [pnk, cwk, "gc"], [wtk])
                    for vc in range(4):
                        self.mm(pnt[:, vc * 128:(vc + 1) * 128], vx[:, jj, h * 640 + vc * 128:h * 640 + (vc + 1) * 128], wt[:, :], vc == 0, False,
                                [vxk, wtk], [pnk], sgc=True)
                    self.mm(pnt[:, 512:640], vx[:, jj, h * 640 + 512:h * 640 + 640], wt[:, :], True, False, [vxk, wtk], [pnk], sgc=True)

                def step_c(h):
                    pnt, pnk = pns[h]
                    for vc in range(5):
                        dst = pnt[:, vc * 128:(vc + 1) * 128] if vc < 4 else pnt[:, 512:640]
                        for dc in range(4):
                            self.mm(dst, Cb[:, h * 4 + dc, vc * 128:(vc + 1) * 128], qb[:, h * 4 + dc, tsl], False, dc == 3,
                                    [("Cb", h * 4 + dc), qbk], [pnk], sgc=True)
                    for dc in range(4):
                        ps, psk = pst.next()
                        lhs = kw[:, jj, h * 512 + dc * 128:h * 512 + (dc + 1) * 128]
                        self.mm(ps[:, 0:512], lhs, vx[:, jj, h * 640:h * 640 + 512], True, True, [kwk, vxk], [psk])
                        self.mm(ps[:, 512:640], lhs, vx[:, jj, h * 640 + 512:h * 640 + 640], True, True, [kwk, vxk], [psk])
                        i = h * 4 + dc
                        self.stt("dve", C[:, i, :], C[:, i, :], cr[:, h, jj:jj + 1], ps[:, 0:640], ALU.mult, ALU.add, [("C", i), crk, psk], [("C", i)])
                        self.cp("act", Cb[:, i, :], C[:, i, :], [("C", i)], [("Cb", i)])

                def step_E(h):
                    pnt, pnk = pns[h]
                    rd, rdk = rdb.next()
                    self.act(rd[:, :], pnt[:, 512:640], AF.Abs, [pnk], [rdk])
                    self.ts("dve", rd[:, :], rd[:, :], 1.0, None, ALU.max, None, [rdk], [rdk])
                    self.P.op("dve", (lambda rd=rd: (lambda e: e.reciprocal(rd[:, :], rd[:, :])))(), [rdk], [rdk])
                    self.tt("dve", ht[:, h * 4:(h + 1) * 4, tsl], pnt[:, 0:512].rearrange("p (v t) -> p v t", v=4),
                            rd[:, :].unsqueeze(1).to_broadcast([128, 4, 128]), ALU.mult, [pnk, rdk], [(htk, jj, h)])

                for h in range(4):
                    step_A(h)
                    step_c(h)
                    step_E(h)
            self.dma("q_pool", self.HT[d, :, :, s0:s0 + NT].rearrange("c p t -> p c t"), ht[:, :, :],
                     [(htk, jj, h) for jj in range(nj) for h in range(4)], [])
        P.phase_end()

    def mlstm_phase_o(self, l, need_ctx):
        P = self.P
        jm = self.kinds[:l + 1].count(1) - 1
        P.phase_begin()
        NT, nj = 256, 2
        wb = P.sb([128, 16 * D], BF16, "wdn")
        wd = wb[:, :].rearrange("p (k n) -> p k n", k=16)
        self.load_w(wd, self.ml_w_down[jm].rearrange("(k p) n -> p k n", p=128), "wdn", ncols_piece=1024)
        gn = P.sb([128, 16], F32, "gn")
        sk = P.sb([128, 16], F32, "sk")
        self.dma("q_sp", gn[:], self.ml_gn[jm], (), ["gn"])
        self.dma("q_sp", sk[:], self.ml_skip[jm], (), ["sk"])
        ones = P.sb([128, 128], BF16, "ones")
        self.dma("q_sp", ones[:], self.onesb[:], (), ["ones"])
        self.alloc_norm(need_aT=False)
        m = self.modt
        pacc = Rot(P, 4, [128, 512], F32, "pacc", psum=True)
        pm_ = Rot(P, 2, [128, 512], F32, "pm", psum=True)
        pq_ = Rot(P, 2, [128, 512], F32, "pq", psum=True)
        h0b = Rot(P, 1, [128, 16, NT], F32, "h0b")
        h1b = Rot(P, 1, [128, 16, NT], F32, "h1b")
        xcb = Rot(P, 1, [128, 16, NT], BF16, "xcb")
        szb = Rot(P, 1, [128, 16, NT], BF16, "szb")
        hbb = Rot(P, 1, [128, 16, NT], BF16, "hbb")
        sqb = Rot(P, 1, [128, 16, NT], BF16, "sqb")
        yTb = Rot(P, 2, [128, 16, NT], BF16, "yTb")
        mnb = Rot(P, 2, [128, 4, NT], F32, "mnb")
        rsb = Rot(P, 2, [128, 4, NT], F32, "rsb")
        tma = Rot(P, 3, [128, NT], F32, "tma")
        cur_row = None
        nunits = self.T // 256
        for u in range(nunits):
            if u == 0 and not need_ctx:
                continue
            row = 1 if u == 0 else 0
            if row != cur_row:
                self.load_mod(l, 0, None, row)
                cur_row = row
            s0 = u * 256
            h0, h0k = h0b.next()
            h1, h1k = h1b.next()
            xc, xck = xcb.next()
            sz, szk = szb.next()
            self.dma("q_sp", h0[:, :, :], self.HT[0, :, :, s0:s0 + NT].rearrange("c p t -> p c t"), [], [h0k])
            self.dma("q_sp", h1[:, :, :], self.HT[1, :, :, s0:s0 + NT].rearrange("c p t -> p c t"), [], [h1k])
            self.dma("q_sp", xc[:, :, :], self.XC[:, :, s0:s0 + NT].rearrange("c p t -> p c t"), [], [xck])
            self.dma("q_sp", sz[:, :, :], self.SZ[:, :, s0:s0 + NT].rearrange("c p t -> p c t"), [], [szk])
            self.tt("pool", h0[:, :, :], h0[:, :, :], h1[:, :, :], ALU.add, [h0k, h1k], [h0k])
            hb, hbk = hbb.next()
            sq, sqk = sqb.next()
            self.cp("act", hb[:, :, :], h0[:, :, :], [h0k], [hbk])
            self.act(sq[:, :, :], h0[:, :, :], AF.Square, [h0k], [sqk])
            mn, mnk = mnb.next()
            rs, rsk = rsb.next()
            for h in range(4):
                p1, p1k = pm_.next()
                p2, p2k = pq_.next()
                for vc in range(4):
                    self.mm(p1[:, :NT], ones[:], hb[:, 4 * h + vc, :], vc == 0, vc == 3, ["ones", hbk], [p1k])
                for vc in range(4):
                    self.mm(p2[:, :NT], ones[:], sq[:, 4 * h + vc, :], vc == 0, vc == 3, ["ones", sqk], [p2k])
                self.act(mn[:, h, :], p1[:, :NT], AF.Copy, [p1k], [(mnk, h)], scale=1.0 / 512)
                tq, tqk = tma.next()
                self.tt("dve", tq[:, :], mn[:, h, :], mn[:, h, :], ALU.mult, [(mnk, h)], [tqk])
                self.stt("dve", tq[:, :], p2[:, :NT], 1.0 / 512, tq[:, :], ALU.mult, ALU.subtract, [p2k, tqk], [tqk])
                self.act(rs[:, h, :], tq[:, :], AF.Sqrt, [tqk], [(rsk, h)], bias=EPS)
                self.P.op("dve", (lambda rs=rs, h=h: (lambda e: e.reciprocal(rs[:, h, :], rs[:, h, :])))(), [(rsk, h)], [(rsk, h)])
            yT, yTk = yTb.next()
            for i in range(16):
                h = i // 4
                eng = "dve" if i % 2 == 0 else "pool"
                ta, tak = tma.next()
                self.tt("pool", ta[:, :], h0[:, i, :], mn[:, h, :], ALU.subtract, [h0k, (mnk, h)], [tak])
                self.stt("dve", ta[:, :], ta[:, :], gn[:, i:i + 1], rs[:, h, :], ALU.mult, ALU.mult, [tak, "gn", (rsk, h)], [tak])
                self.stt("dve", ta[:, :], xc[:, i, :], sk[:, i:i + 1], ta[:, :], ALU.mult, ALU.add, [xck, "sk", tak], [tak])
                self.tt("pool", yT[:, i, :], ta[:, :], sz[:, i, :], ALU.mult, [tak, szk], [(yTk, i)])
            for jj in range(nj):
                j = s0 // 128 + jj
                h_, hk = self.hbuf.next()
                self.load_h(h_, hk, j)
                t1, t1k = self.t1buf.next()
                for nh in range(2):
                    pa, pk = pacc.next()
                    th, thk = self.thalf.next()
                    for k in range(16):
                        self.mm(pa[:, :], yT[:, k, jj * 128:(jj + 1) * 128], wd[:, k, nh * 512:(nh + 1) * 512], k == 0, k == 15,
                                [("wdn", k, 0), (yTk, k)], [pk])
                    self.tt("dve", th[:, :], pa[:, :], m["gt"][:, nh * 512:(nh + 1) * 512], ALU.mult, [pk, "gt"], [thk])
                    self.tt("pool", h_[:, nh * 512:(nh + 1) * 512], th[:, :], h_[:, nh * 512:(nh + 1) * 512], ALU.add,
                            [thk, hk], [hk])
                self.dma("q_pool", self.hrow(j), h_[:], [hk], [])
        P.phase_end()
        self.h_in_src = False

    def pool_phase_p(self, l):
        P = self.P
        P.phase_begin()
        self.alloc_norm(need_aT=False)
        cb = P.sb([128, 36, 128], BF16, "cb")
        cf = P.sb([128, 16, 128], F32, "cf")
        self.dma("q_sp", cb[:], self.poolcb[:], (), ["cb"])
        self.dma("q_sp", cf[:], self.poolcf[:], (), ["cf"])
        pp = Rot(P, 2, [128, 1024], F32, "pp", psum=True)
        cpb = Rot(P, 2, [128, D], BF16, "cpb")
        cur_row = None
        for j in range(self.T // 128):
            row = 1 if j < 2 else 0
            if row != cur_row:
                self.load_mod(l, 0, self.norm1_g, row)
                cur_row = row
            a, ak = self.norm_tile(j)
            self.dma("q_pool", self.A_tm[j * 128:(j + 1) * 128, :], a[:], [ak], [("A", j)])
            if j >= 2:
                pt, ptk = pp.next()
                cp_, cpk = cpb.next()
                for g in range(4):
                    self.mm(pt[:, g * 256:(g + 1) * 256], cb[:, g, :], a[:, g * 256:(g + 1) * 256], True, True, ["cb", ak], [(ptk, g // 2)])
                for g in range(4):
                    self.act(cp_[:, g * 256:(g + 1) * 256], pt[:, g * 256:(g + 1) * 256], AF.Copy, [(ptk, g // 2), "cf"], [(cpk, g)], scale=cf[:, 12 + g, 0:1])
                self.dma("q_pool", self.CP_tm[j * 128:(j + 1) * 128, :], cp_[:], [(cpk, g) for g in range(4)], [("CP", j)])
        P.phase_end()

    def pool_phase_q(self, l, need_ctx):
        P = self.P
        jp = self.kinds[:l + 1].count(2) - 1
        P.phase_begin()
        self.load_ident()
        R = self.TL // 64
        cpt = 128 // R
        cb = P.sb([128, 36, 128], BF16, "cb")
        cf = P.sb([128, 16, 128], F32, "cf")
        self.dma("q_sp", cb[:], self.poolcb[:], (), ["cb"])
        self.dma("q_sp", cf[:], self.poolcf[:], (), ["cf"])
        wp = P.sb([128, 4, 2, 256], BF16, "wp")
        for g in range(4):
            self.dma("q_pool", wp[:, g, :, :], self.pool_w[jp, g].rearrange("(k p) n -> p k n", p=128), (), [("wp", g)])
        gt = P.sb([128, D], F32, "gt")
        sg = P.sb([128, D], F32, "sg")
        bsg = P.sb([128, D], F32, "bsg")
        tmpa = P.sb([128, D], F32, "tmpa")
        ppt = Rot(P, 2, [128, 8, 128], F32, "ppt", psum=True)
        ppo = Rot(P, 2, [128, 1024], F32, "ppo", psum=True)
        cpb = Rot(P, 2, [128, D], BF16, "cpb")
        ab = Rot(P, 3, [128, D], BF16, "ab")
        hb = Rot(P, 3, [128, D], F32, "hb")
        tb = Rot(P, 2, [128, D], F32, "tb")
        plT = Rot(P, 2, [128, 8, 128], BF16, "plT")

        def load_gate(row):
            mv = self.modv[l, row:row + 1, :]
            self.dma("q_sp", gt[:], mv[:, 2 * D:3 * D].partition_broadcast(128), [], ["gt"])
            self.dma("q_sp", tmpa[:], self.pool_scale[jp:jp + 1, :].partition_broadcast(128), [], ["tmpa"])
            self.tt("dve", sg[:], gt[:], tmpa[:], ALU.mult, ["gt", "tmpa"], ["sg"])
            self.dma("q_sp", tmpa[:], self.pool_b[jp:jp + 1, :].partition_broadcast(128), [], ["tmpa"])
            self.tt("dve", bsg[:], sg[:], tmpa[:], ALU.mult, ["sg", "tmpa"], ["bsg"])

        def finish(pt, ptk, rr_idx, h, hks, store):
            pl, plk = plT.next()
            for g in range(4):
                self.tt("dve", pl[:, 2 * g:2 * g + 2, :], pt[:, 2 * g:2 * g + 2, :],
                        cf[:, rr_idx(g):rr_idx(g) + 1, :].to_broadcast([128, 2, 128]), ALU.mult, [ptk, "cf"], [(plk, g)])
            po, pok = ppo.next()
            for g in range(4):
                for kc in range(2):
                    self.mm(po[:, g * 256:(g + 1) * 256], pl[:, 2 * g + kc, :], wp[:, g, kc, :], kc == 0, kc == 1, [(plk, g), ("wp", g)], [pok])
            t, tk = tb.next()
            self.tt("dve", t[:], po[:], sg[:], ALU.mult, [pok, "sg"], [tk])
            self.tt("pool", t[:], t[:], bsg[:], ALU.add, [tk, "bsg"], [tk])
            self.tt("pool", h[:], t[:], h[:], ALU.add, [tk] + hks, hks)
            store(h, hks)

        if need_ctx:
            load_gate(1)
            a2 = []
            for j in range(2):
                a, ak = ab.next()
                aks_ = [(ak, cc) for cc in range(cpt)]
                self.dma("q_sp", a[:], self.A_tm[j * 128:(j + 1) * 128, :], [], aks_)
                a2.append((a, aks_))
            for j2 in range(2):
                h, hk = hb.next()
                hks_ = [(hk, cc) for cc in range(cpt)]
                self.dma("q_sp", h[:], self.src_row(j2) if self.h_in_src else self.hrow(j2), [], hks_)
                pt, ptk = ppt.next()
                for fc in range(8):
                    g = fc // 2
                    for j in range(2):
                        self.mm(pt[:, fc, :], a2[j][0][:, fc * 128:(fc + 1) * 128], cb[:, 12 + g * 4 + j * 2 + j2, :], j == 0, False,
                                a2[j][1] + ["cb"], [ptk])
                    self.mm(pt[:, fc, :], a2[j2][0][:, fc * 128:(fc + 1) * 128], cb[:, 28 + g * 2 + j2, :], False, True, a2[j2][1] + ["cb"], [ptk])

                def store_c(h, hks, j2=j2):
                    self.dma("q_pool", self.hrow(j2), h[:], hks, [])
                finish(pt, ptk, lambda g, j2=j2: 4 + g * 2 + j2, h, hks_, store_c)
        load_gate(0)
        cp_v = self.CP_tm[TC:, :].rearrange("(r c) f -> c r f", c=64)
        a_v = self.A_tm[TC:, :].rearrange("(r c) f -> c r f", c=64)
        hsrc = self.x[:, :] if self.h_in_src else self.hres[TC:, :]
        hs_v = hsrc.rearrange("(r c) f -> c r f", c=64)
        hd_v = self.hres[TC:, :].rearrange("(r c) f -> c r f", c=64)
        for m_ in range(64 // cpt):
            c0 = m_ * cpt
            cp_, cpk = cpb.next()
            a, ak = ab.next()
            h, hk = hb.next()
            for cc in range(cpt):
                rows = slice(cc * R, (cc + 1) * R)
                self.dma("q_sp", cp_[rows, :], cp_v[c0 + cc], [], [(cpk, cc)])
                self.dma("q_sp", a[rows, :], a_v[c0 + cc], [], [(ak, cc)])
                self.dma("q_sp", h[rows, :], hs_v[c0 + cc], [], [(hk, cc)])
            pt, ptk = ppt.next()
            cpks = [(cpk, cc) for cc in range(cpt)]
            aks = [(ak, cc) for cc in range(cpt)]
            hks = [(hk, cc) for cc in range(cpt)]
            for fc in range(8):
                g = fc // 2
                self.mm(pt[:, fc, :], cp_[:, fc * 128:(fc + 1) * 128], cb[:, 4 + g, :], True, False, cpks + ["cb"], [ptk])
                self.mm(pt[:, fc, :], a[:, fc * 128:(fc + 1) * 128], cb[:, 8 + g, :], False, True, aks + ["cb"], [ptk])

            def store_l(h, hks_, c0=c0):
                for cc in range(cpt):
                    self.dma("q_pool", hd_v[c0 + cc], h[cc * R:(cc + 1) * R, :], hks_, [])
            finish(pt, ptk, lambda g: g, h, hks, store_l)
        P.phase_end()
        self.h_in_src = False

    def build(self):
        self.setup()
        self.ada_phase()
        for l in range(self.depth):
            kind = self.kinds[l]
            last = l == self.depth - 1
            if kind == 0:
                self.gla_phase_p(l)
                self.gla_phase_s(l, 0)
                self.gla_phase_s(l, 1)
                self.gla_phase_o(l, need_ctx=not last)
            elif kind == 1:
                self.mlstm_phase_p1(l)
                self.mlstm_phase_p2(l)
                self.mlstm_phase_s(l, 0)
                self.mlstm_phase_s(l, 1)
                self.mlstm_phase_o(l, need_ctx=not last)
            elif kind == 2:
                self.pool_phase_p(l)
                self.pool_phase_q(l, need_ctx=not last)
            elif kind is not None:
                raise NotImplementedError
            self.mlp1_phase(l, skip_ctx=last)
            self.mlp2_phase(l, skip_ctx=last, final=last)
        self.P.emit()
        self.P.close()
        return self.nc


def _box(L, w):
    pos = np.arange(L)
    lo = np.maximum(pos - w // 2, 0)
    hi = np.minimum(pos + (w - w // 2), L)
    M = ((pos[:, None] >= lo[None, :]) & (pos[:, None] < hi[None, :])).astype(np.float32)
    return M, (hi - lo).astype(np.float32)


def _consts(T_lat):
    R = T_lat // 64
    cpt = 128 // R
    cb = np.zeros((128, 36, 128), np.float32)
    cf = np.zeros((128, 16, 128), np.float32)
    for g, w in enumerate((2, 4, 8, 16)):
        Mc, cc_ = _box(64, w)
        cb[:, g, :] = np.kron(np.eye(2, dtype=np.float32), Mc)
        cf[:, 12 + g, 0] = 1.0 / np.tile(cc_, 2)
        Mr, cr = _box(R, w)
        cb[:, 4 + g, :] = np.kron(np.eye(cpt, dtype=np.float32), Mr)
        cb[:, 8 + g, :] = -np.diag(np.tile(cr, cpt))
        cf[:, g, :] = (1.0 / np.tile(cr, cpt))[None, :]
        Mx, cx = _box(TC, w)
        for j in range(2):
            for j2 in range(2):
                cb[:, 12 + g * 4 + j * 2 + j2, :] = Mx[j * 128:(j + 1) * 128, j2 * 128:(j2 + 1) * 128]
        for j2 in range(2):
            cb[:, 28 + g * 2 + j2, :] = -np.diag(cx[j2 * 128:(j2 + 1) * 128])
            cf[:, 4 + g * 2 + j2, :] = (1.0 / cx[j2 * 128:(j2 + 1) * 128])[None, :]
    glacf = np.ones((128, 768), np.float32)
    glacf[:, 0:512:64] = 0.0
    si_, ti_ = np.meshgrid(np.arange(128), np.arange(128), indexing="ij")
    same = (si_ // 64) == (ti_ // 64)
    glacf[:, 512:640] = (same & (ti_ >= si_)).astype(np.float32)
    glacf[:, 640:768] = (same & (ti_ <= si_)).astype(np.float32)
    sel = np.zeros((64, 8, 128), np.float32)
    for r8 in range(8):
        sel[32 * (r8 // 4) + r8 % 4, r8, :] = 1.0
    mlmask = np.ones((128, 768), np.float32)
    mlmask[:, 0:512:MCH] = 0.0
    mlmask[:, 512:640] = (ti_ >= si_).astype(np.float32)
    mlmask[:, 640:768] = (ti_ <= si_).astype(np.float32)
    return {"mlmask": mlmask, "ml_sel": sel, "identf": np.eye(128, dtype=np.float32), "glacf": glacf, "onesb": np.ones((128, 128), np.float32).astype(ml_dtypes.bfloat16),
            "identb": np.eye(128, dtype=np.float32).astype(ml_dtypes.bfloat16),
            "poolcb": cb.astype(ml_dtypes.bfloat16), "poolcf": cf}


def _bd(w_qkv):
    n = w_qkv.shape[0]
    o = np.zeros((n, 3, 16, 128, 128), np.float32)
    w = w_qkv.reshape(n, 3, 16, 32, 4, 4)
    for b in range(32):
        o[:, :, :, 4 * b:4 * b + 4, 4 * b:4 * b + 4] = w[:, :, :, b]
    return o


def _wg(w_gate, off):
    n = w_gate.shape[0]
    o = np.zeros((n, 128, 48, 64), np.float32)
    w = w_gate.reshape(n, 2, 48, 128, 8)
    for d in range(2):
        o[:, :, :, 32 * d:32 * d + 4] = w[:, d, :, :, off:off + 4].transpose(0, 2, 1, 3)
    return o


def _bg(b_gate, off):
    n = b_gate.shape[0]
    o = np.zeros((n, 64, 1), np.float32)
    for d in range(2):
        o[:, 32 * d:32 * d + 4, 0] = b_gate[:, d, off:off + 4]
    return o


def _wa2blk(w_a2):
    n = w_a2.shape[0]
    o = np.zeros((n, 32, 1024), np.float32)
    o[:, 0:16, 0:512] = w_a2[:, 0]
    o[:, 16:32, 512:1024] = w_a2[:, 1]
    return o


def make_in_maps(inputs, T_lat, depth):
    B = inputs["x"].shape[0]
    consts = _consts(T_lat)
    maps = []
    for b in range(B):
        cT = np.stack([inputs["c"][b], inputs["c_ctx"]], axis=1)
        cT = np.ascontiguousarray(cT.reshape(KC, 128, 2).transpose(1, 0, 2))
        m = {
            "x": np.ascontiguousarray(inputs["x"][b]),
            "ctx": np.ascontiguousarray(inputs["ctx"][b]),
            "cT": cT.astype(np.float32),
            "ada_w": inputs["ada_w"], "ada_b": inputs["ada_b"],
            "norm1_g": inputs["norm1_g"], "norm2_g": inputs["norm2_g"],
            "mlp_w1": inputs["mlp_w1"], "mlp_w2": inputs["mlp_w2"],
            "final_g": inputs["final_g"].reshape(1, D),
            "gla_w_in": inputs["gla_w_in"],
            "gla_wa1": np.ascontiguousarray(np.concatenate([inputs["gla_w_a1"][:, 0], inputs["gla_w_a1"][:, 1]], axis=-1)),
            "gla_wa2": _wa2blk(inputs["gla_w_a2"]),
            "gla_ba": np.ascontiguousarray(inputs["gla_b_a"].reshape(-1, 8, 128).transpose(0, 2, 1)),
            "gla_gh": np.ascontiguousarray(inputs["gla_g_head"].reshape(-1, 8, 128).transpose(0, 2, 1)),
            "gla_w_o": inputs["gla_w_o"],
            "ml_w_up": inputs["mlstm_w_up"], "ml_w_down": inputs["mlstm_w_down"],
            "ml_bd": _bd(inputs["mlstm_w_qkv"]),
            "ml_wgI": _wg(inputs["mlstm_w_gate"], 0), "ml_wgF": _wg(inputs["mlstm_w_gate"], 4),
            "ml_convw": np.ascontiguousarray(inputs["mlstm_conv_w"].reshape(-1, 4, 16, 128).transpose(0, 3, 2, 1)),
            "ml_convb": np.ascontiguousarray(inputs["mlstm_conv_b"].reshape(-1, 16, 128).transpose(0, 2, 1)),
            "ml_bgI": _bg(inputs["mlstm_b_gate"], 0), "ml_bgF": _bg(inputs["mlstm_b_gate"], 4),
            "ml_gn": np.ascontiguousarray(inputs["mlstm_g_norm"].reshape(-1, 16, 128).transpose(0, 2, 1)),
            "ml_skip": np.ascontiguousarray(inputs["mlstm_skip"].reshape(-1, 16, 128).transpose(0, 2, 1)),
            "pool_w": inputs["pool_w"], "pool_b": inputs["pool_b"].reshape(-1, D),
            "pool_scale": inputs["pool_scale"],
        }
        m.update(consts)
        maps.append(m)
    return maps


def run(inputs, T_lat, depth, kinds=None, trace=False):
    inputs = {k: np.asarray(v) for k, v in inputs.items()}
    bld = Builder(T_lat, depth, kinds)
    nc = bld.build()
    maps = make_in_maps(inputs, T_lat, depth)
    maps = [{k: v for k, v in m.items() if k in bld.din} for m in maps]
    res = run_bass_kernel_spmd(nc, maps, core_ids=list(range(len(maps))), trace=trace)
    out = np.stack([r["out"] for r in res.results], axis=0)
    return out.astype(np.float32), res, bld


def kernel(**inputs):
    out, _, _ = run(inputs, 4096, 4)
    return out
```

```python
from contextlib import ExitStack
import numpy as np
import ml_dtypes
import concourse.bass as bass
import concourse.mybir as mybir
from concourse.bass_utils import run_bass_kernel_spmd

F32 = mybir.dt.float32
BF16 = mybir.dt.bfloat16
AF = mybir.ActivationFunctionType
ALU = mybir.AluOpType
AX = mybir.AxisListType

COMPUTE = ("pe", "act", "dve", "pool")
EPOCH = 20000
NDMASEM = 64

D = 1024
KC = 8
TC = 256
DFF = 4096
EPS = 1e-6
MCH = 128


class Prog:
    def __init__(self, nc):
        self.nc = nc
        self.es = ExitStack()
        self.ops = []
        self.nname = 0
        self.nphase = 0
        self.phase_limit = 10 ** 9

    def sb(self, shape, dtype, name=None):
        self.nname += 1
        name = (name or "sb") + f"_{self.nname}"
        return self.es.enter_context(self.nc.sbuf_tensor(name, list(shape), dtype))

    def ps(self, shape, dtype, name=None):
        self.nname += 1
        name = (name or "ps") + f"_{self.nname}"
        return self.es.enter_context(self.nc.psum_tensor(name, list(shape), dtype))

    def op(self, eng, fn, reads=(), writes=()):
        if self.nphase >= self.phase_limit:
            return
        self.ops.append((eng, fn, tuple(reads), tuple(writes)))

    def barrier(self):
        if self.nphase > self.phase_limit:
            return
        self.ops.append(("barrier", None, (), ()))

    def phase_begin(self):
        self._saved_es = self.es
        self.es = ExitStack()

    def phase_end(self):
        self.nphase += 1
        self.barrier()
        self.es.close()
        self.es = self._saved_es

    def _engine(self, eng):
        nc = self.nc
        return {"pe": nc.tensor, "act": nc.scalar, "dve": nc.vector, "pool": nc.gpsimd,
                "q_sp": nc.sync, "q_act": nc.scalar, "q_pool": nc.gpsimd}[eng]

    def _seq(self, stream):
        nc = self.nc
        return {"pe": nc.tensor, "act": nc.scalar, "dve": nc.vector, "pool": nc.gpsimd,
                "sp": nc.sync}[stream]

    @staticmethod
    def _stream(eng):
        return {"q_sp": "sp", "q_act": "act", "q_pool": "pool"}.get(eng, eng)

    def emit(self, final_keys=()):
        nc = self.nc
        ops = self.ops
        n = len(ops)
        last_w = {}
        rd_eng = {}
        rd_dma = {}
        deps = [None] * n
        needed = [False] * n
        bar_deps = {}
        last_on = {}
        dma_since = []
        for i, (eng, fn, reads, writes) in enumerate(ops):
            if eng == "barrier":
                bd = list(last_on.values()) + dma_since
                bar_deps[i] = bd
                for j in bd:
                    needed[j] = True
                deps[i] = []
                last_w, rd_eng, rd_dma = {}, {}, {}
                dma_since = []
                continue
            if eng.startswith("q_"):
                dma_since.append(i)
            else:
                last_on[eng] = i
            isdma_i = eng.startswith("q_")
            d = set()
            raw = set()
            for k in reads:
                w = last_w.get(k)
                if w is not None:
                    d.add(w)
                    raw.add(w)
            for k in writes:
                w = last_w.get(k)
                if w is not None:
                    d.add(w)
                for r in rd_eng.get(k, {}).values():
                    d.add(r)
                for r in rd_dma.get(k, ()):
                    d.add(r)
            d.discard(i)
            dd = []
            for j in d:
                ej = ops[j][0]
                if (not isdma_i) and ej == eng and eng == "pe":
                    continue
                dd.append(j)
            deps[i] = dd
            for j in dd:
                needed[j] = True
            for k in reads:
                if isdma_i:
                    rd_dma.setdefault(k, []).append(i)
                else:
                    rd_eng.setdefault(k, {})[eng] = i
            for k in writes:
                last_w[k] = i
                rd_eng[k] = {}
                rd_dma[k] = []
        fin = [last_w[k] for k in final_keys if k in last_w]
        for j in fin:
            needed[j] = True

        cnt = {e: 0 for e in COMPUTE}
        sig = [None] * n
        dma_cnt = [0] * NDMASEM
        ndma = 0
        nsw = 0
        NHW = 48
        for i, (eng, fn, reads, writes) in enumerate(ops):
            if not needed[i] or eng == "barrier":
                continue
            if eng.startswith("q_"):
                if eng == "q_pool":
                    s = NHW + nsw % (NDMASEM - NHW)
                    nsw += 1
                else:
                    s = ndma % NHW
                    ndma += 1
                dma_cnt[s] += 1
                sig[i] = ("dma", s, dma_cnt[s] * 16)
            else:
                cnt[eng] += 1
                sig[i] = ("eng", eng, cnt[eng])
        sems = {}
        for e in COMPUTE:
            ne = max(1, (cnt[e] + EPOCH - 1) // EPOCH)
            sems[e] = [self.es.enter_context(nc.semaphore(f"s_{e}{k}")) for k in range(ne)]
        dsems = [self.es.enter_context(nc.semaphore(f"s_dma{k}")) for k in range(NDMASEM)]
        assert max(dma_cnt + [0]) * 16 < 60000, dma_cnt
        self.stats = dict(cnt=dict(cnt), ndma=ndma, nops=n)
        known = {}

        def do_wait(stream, s):
            if s[0] == "dma":
                key = ("dma", s[1])
                val = s[2]
                if known.get((stream, key), 0) >= val:
                    return
                known[(stream, key)] = val
                self._seq(stream).wait_ge(dsems[s[1]], val)
            else:
                e, idx = s[1], s[2]
                key = ("eng", e)
                if known.get((stream, key), 0) >= idx:
                    return
                known[(stream, key)] = idx
                ep = (idx - 1) // EPOCH
                self._seq(stream).wait_ge(sems[e][ep], idx - ep * EPOCH)

        for i, (eng, fn, reads, writes) in enumerate(ops):
            if eng == "barrier":
                for stream in ("pe", "act", "dve", "pool", "sp"):
                    for j in bar_deps[i]:
                        if sig[j][0] == "eng" and sig[j][1] == stream:
                            continue
                        do_wait(stream, sig[j])
                continue
            stream = self._stream(eng)
            for j in sorted(deps[i]):
                do_wait(stream, sig[j])
            if needed[i] and sig[i][0] == "dma" and sig[i][2] > 16:
                do_wait(stream, ("dma", sig[i][1], sig[i][2] - 16))
            ins = fn(self._engine(eng))
            if needed[i]:
                s = sig[i]
                if s[0] == "dma":
                    ins.then_inc(dsems[s[1]], 16)
                else:
                    ep = (s[2] - 1) // EPOCH
                    ins.then_inc(sems[s[1]][ep], 1)
        for j in fin:
            do_wait("sp", sig[j])

    def close(self):
        self.es.close()


class Rot:
    def __init__(self, P, n, shape, dtype, name, psum=False):
        self.bufs = [(P.ps if psum else P.sb)(shape, dtype, f"{name}{i}") for i in range(n)]
        self.keys = [(name, i) for i in range(n)]
        self.i = -1

    def next(self):
        self.i = (self.i + 1) % len(self.bufs)
        return self.bufs[self.i], self.keys[self.i]


class Builder:
    def __init__(self, T_lat, depth, kinds=None):
        self.TL = T_lat
        self.T = TC + T_lat
        self.depth = depth
        self.kinds = kinds if kinds is not None else [i % 3 for i in range(depth)]
        self.nc = bass.Bass("TRN2", target_bir_lowering=False)
        self.P = Prog(self.nc)
        self.sts = [(0, TC)] + [(TC + 512 * i, 512) for i in range(T_lat // 512)]
        self.din = {}

    def mm(self, out, lhsT, rhs, start, stop, r, w, sgc=False):
        self.P.op("pe", lambda e: e.matmul(out, lhsT=lhsT, rhs=rhs, start=start, stop=stop, skip_group_check=sgc), r, w)

    def tr(self, out, in_, ident, r, w):
        self.P.op("pe", lambda e: e.transpose(out, in_, ident), r, w)

    def act(self, out, in_, func, r, w, bias=None, scale=None, accum=None):
        kw = {}
        if bias is not None:
            kw["bias"] = bias
        if scale is not None:
            kw["scale"] = scale
        if accum is not None:
            kw["accum_out"] = accum
        self.P.op("act", lambda e: e.activation(out=out, in_=in_, func=func, **kw), r, w)

    def tt(self, eng, out, a, b, op, r, w):
        self.P.op(eng, lambda e: e.tensor_tensor(out=out, in0=a, in1=b, op=op), r, w)

    def ts(self, eng, out, a, s1, s2, op0, op1, r, w):
        if s2 is None:
            self.P.op(eng, lambda e: e.tensor_scalar(out=out, in0=a, scalar1=s1, scalar2=None, op0=op0), r, w)
        else:
            self.P.op(eng, lambda e: e.tensor_scalar(out=out, in0=a, scalar1=s1, scalar2=s2, op0=op0, op1=op1), r, w)

    def stt(self, eng, out, in0, scalar, in1, op0, op1, r, w):
        self.P.op(eng, lambda e: e.scalar_tensor_tensor(out=out, in0=in0, scalar=scalar, in1=in1, op0=op0, op1=op1), r, w)

    def cp(self, eng, out, in_, r, w):
        if eng == "act":
            self.P.op("act", lambda e: e.copy(out, in_), r, w)
        else:
            self.P.op(eng, lambda e: e.tensor_copy(out, in_), r, w)

    def dma(self, q, out, in_, r, w, slow=False):
        if slow:
            self.P.op(q, lambda e: e.dma_start(out=out, in_=in_, allow_slow_non_contiguous=True), r, w)
        else:
            self.P.op(q, lambda e: e.dma_start(out=out, in_=in_), r, w)

    def scan(self, out, d0, d1, init, op0, op1, r, w):
        self.P.op("dve", lambda e: e.tensor_tensor_scan(out=out, data0=d0, data1=d1, initial=init, op0=op0, op1=op1), r, w)

    def memset(self, eng, ap, val, w):
        self.P.op(eng, lambda e: e.memset(ap, val), (), w)

    def inp(self, name, shape, dtype=F32):
        t = self.nc.dram_tensor(name, list(shape), dtype, kind="ExternalInput")
        self.din[name] = t
        return t

    def scratch(self, name, shape, dtype):
        return self.nc.dram_tensor(name, list(shape), dtype, kind="Internal")

    def setup(self):
        P = self.P
        TL, T = self.TL, self.T
        self.x = self.inp("x", [TL, D])
        self.ctx = self.inp("ctx", [TC, D])
        self.cT = self.inp("cT", [128, KC, 2])
        self.ada_w = self.inp("ada_w", [4, D, 6 * D])
        self.ada_b = self.inp("ada_b", [4, 6 * D])
        self.norm1_g = self.inp("norm1_g", [4, D])
        self.norm2_g = self.inp("norm2_g", [4, D])
        self.mlp_w1 = self.inp("mlp_w1", [4, D, DFF])
        self.mlp_w2 = self.inp("mlp_w2", [4, DFF, D])
        self.final_g = self.inp("final_g", [1, D])
        self.identb = self.inp("identb", [128, 128], BF16)
        self.out = self.nc.dram_tensor("out", [TL, D], F32, kind="ExternalOutput")
        self.hres = self.scratch("hres", [T, D], F32)
        self.modv = self.scratch("modv", [4, 2, 6 * D], F32)
        self.U = self.scratch("U", [32, 128, T], BF16)
        ng = max(1, self.kinds.count(0))
        self.gla_w_in = self.inp("gla_w_in", [ng, D, 3072])
        self.gla_wa1 = self.inp("gla_wa1", [ng, D, 32])
        self.gla_wa2 = self.inp("gla_wa2", [ng, 32, 1024])
        self.gla_ba = self.inp("gla_ba", [ng, 128, 8])
        self.gla_gh = self.inp("gla_gh", [ng, 128, 8])
        self.gla_w_o = self.inp("gla_w_o", [ng, D, D])
        self.glacf = self.inp("glacf", [128, 768])
        self.onesb = self.inp("onesb", [128, 128], BF16)
        self.QD = self.scratch("QD", [2, 4, 128, T], BF16)
        self.KD = self.scratch("KD", [2, 4, 128, T], BF16)
        self.KW_tm = self.scratch("KW_tm", [2, T, 512], BF16)
        self.V_tm = self.scratch("V_tm", [T, D], BF16)
        self.SR = self.scratch("SR", [8, 128, T], BF16)
        self.DEC = self.scratch("DEC", [2, 4, 128, T // 64], F32)
        self.OO = self.scratch("OO", [2, 8, 128, T], F32)
        nm_ = max(1, self.kinds.count(1))
        self.ml_w_up = self.inp("ml_w_up", [nm_, D, 4096])
        self.ml_bd = self.inp("ml_bd", [nm_, 3, 16, 128, 128])
        self.ml_wgI = self.inp("ml_wgI", [nm_, 128, 48, 64])
        self.ml_wgF = self.inp("ml_wgF", [nm_, 128, 48, 64])
        self.ml_convw = self.inp("ml_convw", [nm_, 128, 16, 4])
        self.ml_convb = self.inp("ml_convb", [nm_, 128, 16])
        self.ml_bgI = self.inp("ml_bgI", [nm_, 64, 1])
        self.ml_bgF = self.inp("ml_bgF", [nm_, 64, 1])
        self.ml_gn = self.inp("ml_gn", [nm_, 128, 16])
        self.ml_skip = self.inp("ml_skip", [nm_, 128, 16])
        self.ml_w_down = self.inp("ml_w_down", [nm_, 2048, D])
        self.ml_sel = self.inp("ml_sel", [64, 8, 128])
        self.mlmask = self.inp("mlmask", [128, 768])
        self.identf_d = self.inp("identf", [128, 128])
        self.XM = self.scratch("XM", [16, 128, T], BF16)
        self.SZ = self.scratch("SZ", [16, 128, T], BF16)
        self.XC = self.scratch("XC", [16, 128, T], BF16)
        self.KT = self.scratch("KT", [16, 128, T], BF16)
        self.QB = self.scratch("QB", [2, 16, 128, T], BF16)
        self.VX = self.scratch("VX", [T, 2560], BF16)
        self.KWm = self.scratch("KWm", [2, T, 2048], BF16)
        self.CWT = self.scratch("CWT", [T, 128], F32)
        self.CARB = self.scratch("CARB", [2, 4, 128, T // 64], F32)
        self.HT = self.scratch("HT", [2, 16, 128, T], F32)
        self.A_tm = self.scratch("A_tm", [T, D], BF16)
        self.CP_tm = self.scratch("CP_tm", [T, D], BF16)
        npool = max(1, self.kinds.count(2))
        self.pool_w = self.inp("pool_w", [npool, 4, 256, 256])
        self.pool_b = self.inp("pool_b", [npool, D])
        self.pool_scale = self.inp("pool_scale", [npool, D])
        self.poolcb = self.inp("poolcb", [128, 36, 128], BF16)
        self.poolcf = self.inp("poolcf", [128, 16, 128])
        self.h_in_src = True

    def hrow(self, j):
        return self.hres[j * 128:(j + 1) * 128, :]

    def src_row(self, j):
        if j < 2:
            return self.ctx[j * 128:(j + 1) * 128, :]
        return self.x[(j - 2) * 128:(j - 1) * 128, :]

    def load_h(self, h, hk, j):
        if self.h_in_src:
            self.dma("q_sp", h[:], self.src_row(j), [], [hk])
        else:
            self.dma("q_sp", h[:], self.hrow(j), [("hres", j)], [hk])

    def load_ident(self):
        self.ident = self.P.sb([128, 128], BF16, "ident")
        self.dma("q_sp", self.ident[:], self.identb[:], (), ["ident"])

    def ada_phase(self):
        P = self.P
        P.phase_begin()
        cs = P.sb([128, KC, 2], F32, "cs")
        cin = P.sb([128, KC, 2], F32, "cin")
        psm = Rot(P, 2, [128, 512], F32, "psm", psum=True)
        self.dma("q_sp", cin[:], self.cT[:], (), ["cin"])
        self.act(cs[:], cin[:], AF.Silu, ["cin"], ["cs"])
        wst = Rot(P, 2, [128, KC, 512], F32, "adaw")
        bsb = P.sb([2, 6 * D], F32, "adab")
        msb = Rot(P, 1, [2, 6 * D], F32, "adam")
        for l in range(self.depth):
            self.dma("q_sp", bsb[:], self.ada_b[l:l + 1, :].partition_broadcast(2), (), ["adab"])
            mt, mk = msb.next()
            for n in range(12):
                wt, wk = wst.next()
                self.dma("q_sp", wt[:], self.ada_w[l, :, n * 512:(n + 1) * 512].rearrange("(k p) n -> p k n", p=128), (), [wk])
                pt, pk = psm.next()
                for k in range(KC):
                    self.mm(pt[0:2, :], cs[:, k, :], wt[:, k, :], k == 0, k == KC - 1, [wk, "cs"], [pk])
                self.tt("dve", mt[:, n * 512:(n + 1) * 512], pt[0:2, :], bsb[:, n * 512:(n + 1) * 512], ALU.add, [pk, "adab"], [(mk, n)])
            self.dma("q_pool", self.modv[l], mt[:], [(mk, n) for n in range(12)], [("modv", l)])
        P.phase_end()

    def alloc_norm(self, need_aT=True):
        P = self.P
        self.load_ident()
        self.hbuf = Rot(P, 4, [128, D], F32, "hbuf")
        self.t1buf = Rot(P, 2, [128, D], F32, "t1buf")
        self.ssb = Rot(P, 4, [128, 2], F32, "ssb")
        self.thalf = Rot(P, 3, [128, 512], F32, "thalf")
        self.modt = {nm: P.sb([128, D], F32, nm) for nm in ("gs", "sh", "gt")}
        self.abuf = Rot(P, 2, [128, D], BF16, "abuf")
        if need_aT:
            self.aT = Rot(P, 2, [128, KC, 512], BF16, "aT")
            self.ptr = Rot(P, 2, [128, 1024], BF16, "ptr", psum=True)

    def load_mod(self, l, which, norm_g, row):
        m = self.modt
        base = 3 * which
        mv = self.modv[l, row:row + 1, :]
        tmps, tmpsk = self.t1buf.next()
        tmpg, tmpgk = self.t1buf.next()
        self.dma("q_sp", m["sh"][:], mv[:, (base + 0) * D:(base + 1) * D].partition_broadcast(128), [], ["sh"])
        self.dma("q_sp", tmps[:], mv[:, (base + 1) * D:(base + 2) * D].partition_broadcast(128), [], [tmpsk])
        self.dma("q_sp", m["gt"][:], mv[:, (base + 2) * D:(base + 3) * D].partition_broadcast(128), [], ["gt"])
        if norm_g is not None:
            self.dma("q_sp", tmpg[:], norm_g[l:l + 1, :].partition_broadcast(128), (), [tmpgk])
            self.stt("dve", m["gs"][:], tmps[:], 1.0, tmpg[:], ALU.add, ALU.mult, [tmpsk, tmpgk], ["gs"])

    def rstd(self, ss, ssk, n=D):
        self.act(ss[:, 1:2], ss[:, 0:1], AF.Sqrt, [ssk], [ssk], scale=1.0 / n, bias=EPS)
        self.P.op("dve", lambda e: e.reciprocal(ss[:, 1:2], ss[:, 1:2]), [ssk], [ssk])

    def norm_tile(self, j):
        m = self.modt
        h, hk = self.hbuf.next()
        self.load_h(h, hk, j)
        t1, t1k = self.t1buf.next()
        ss, ssk = self.ssb.next()
        self.act(t1[:], h[:], AF.Square, [hk], [t1k, ssk], accum=ss[:, 0:1])
        self.rstd(ss, ssk)
        self.stt("dve", t1[:], h[:], ss[:, 1:2], m["gs"][:], ALU.mult, ALU.mult, [hk, ssk, "gs", t1k], [t1k])
        a, ak = self.abuf.next()
        self.tt("pool", a[:], t1[:], m["sh"][:], ALU.add, [t1k, "sh"], [ak])
        return a, ak

    def norm_stage(self, si):
        s0, NT = self.sts[si]
        aT, aTk = self.aT.next()
        for jj in range(NT // 128):
            j = s0 // 128 + jj
            a, ak = self.norm_tile(j)
            pt, ptk = self.ptr.next()
            for k in range(KC):
                self.tr(pt[:, k * 128:(k + 1) * 128], a[:, k * 128:(k + 1) * 128], self.ident[:], [ak, "ident"], [ptk])
            self.cp("act", aT[:, :, jj * 128:(jj + 1) * 128], pt[:].rearrange("p (k t) -> p k t", k=KC), [ptk], [(aTk, jj)])
        return aT, aTk

    def load_w(self, dst_view, src_view, key, ncols_piece=2048):
        K = dst_view.shape[1]
        N = dst_view.shape[2]
        for k in range(K):
            for c in range(0, N, ncols_piece):
                ce = min(N, c + ncols_piece)
                self.dma("q_pool", dst_view[:, k, c:ce], src_view[:, k, c:ce], (), [(key, k, c // ncols_piece)])

    def sis(self, skip_ctx):
        return [si for si in range(len(self.sts)) if not (skip_ctx and si == 0)]

    def pipelined(self, sis, stage_a, stage_b, l, which, norm_g):
        prev = None
        cur_row = None
        for si in sis:
            row = 1 if si == 0 else 0
            if row != cur_row:
                if prev is not None:
                    stage_b(*prev)
                    prev = None
                self.load_mod(l, which, norm_g, row)
                cur_row = row
            a = stage_a(si)
            if prev is not None:
                stage_b(*prev)
            prev = (si,) + tuple(a)
        if prev is not None:
            stage_b(*prev)

    def mlp1_phase(self, l, skip_ctx=False):
        P = self.P
        P.phase_begin()
        wb = P.sb([128, KC * DFF], BF16, "w1")
        wkey = "w1"
        w1 = wb[:, :].rearrange("p (k n) -> p k n", k=KC)
        self.load_w(w1, self.mlp_w1[l].rearrange("(k p) n -> p k n", p=128), wkey)
        self.alloc_norm()
        pacc = Rot(P, 4, [128, 512], F32, "pacc", psum=True)
        rbuf = Rot(P, 3, [128, 512], F32, "rbuf")
        ubuf = Rot(P, 2, [128, 4, 512], BF16, "ubuf")

        def stage_b(si, aT, aTk):
            s0, NT = self.sts[si]
            nj = NT // 128
            for fo4 in range(8):
                ut, uk = ubuf.next()
                for q in range(4):
                    fo = fo4 * 4 + q
                    pa, pk = pacc.next()
                    for k in range(KC):
                        self.mm(pa[:, :NT], w1[:, k, fo * 128:(fo + 1) * 128], aT[:, k, :NT], k == 0, k == KC - 1,
                                [(wkey, k, fo // 16)] + [(aTk, jj) for jj in range(nj)], [pk])
                    rt, rk = rbuf.next()
                    self.act(rt[:, :NT], pa[:, :NT], AF.Relu, [pk], [rk])
                    self.tt("dve", ut[:, q, :NT], rt[:, :NT], rt[:, :NT], ALU.mult, [rk], [(uk, q)])
                self.dma("q_pool", self.U[fo4 * 4:(fo4 + 1) * 4, :, s0:s0 + NT].rearrange("f p t -> p f t"), ut[:, :, :NT],
                         [(uk, q) for q in range(4)], [("U", si, fo4)])

        self.pipelined(self.sis(skip_ctx), self.norm_stage, stage_b, l, 1, self.norm2_g)
        P.phase_end()

    def mlp2_phase(self, l, skip_ctx=False, final=False):
        P = self.P
        P.phase_begin()
        wb = P.sb([128, 32 * D], BF16, "w2")
        wkey = "w2"
        w2 = wb[:, :].rearrange("p (k n) -> p k n", k=32)
        self.load_w(w2, self.mlp_w2[l].rearrange("(k p) n -> p k n", p=128), wkey, ncols_piece=1024)
        self.alloc_norm(need_aT=False)
        pacc = Rot(P, 4, [128, 512], F32, "pacc", psum=True)
        u2buf = Rot(P, 2, [128, 32, 512], BF16, "u2buf")
        m = self.modt
        fg = None
        if final:
            fg = P.sb([128, D], F32, "fg")
            self.dma("q_sp", fg[:], self.final_g[0:1, :].partition_broadcast(128), (), ["fg"])
        cur_row = None
        for si in self.sis(skip_ctx):
            row = 1 if si == 0 else 0
            if row != cur_row:
                self.load_mod(l, 1, None, row)
                cur_row = row
            s0, NT = self.sts[si]
            nj = NT // 128
            ut, uk = u2buf.next()
            for f8 in range(4):
                self.dma("q_sp", ut[:, f8 * 8:(f8 + 1) * 8, :NT], self.U[f8 * 8:(f8 + 1) * 8, :, s0:s0 + NT].rearrange("f p t -> p f t"),
                         [("U", si, f8 * 2), ("U", si, f8 * 2 + 1)], [(uk, f8)])
            for jj in range(nj):
                j = s0 // 128 + jj
                h, hk = self.hbuf.next()
                self.load_h(h, hk, j)
                t1, t1k = self.t1buf.next()
                for nh in range(2):
                    pa, pk = pacc.next()
                    th, thk = self.thalf.next()
                    for k in range(32):
                        self.mm(pa[:, :], ut[:, k, jj * 128:(jj + 1) * 128], w2[:, k, nh * 512:(nh + 1) * 512], k == 0, k == 31,
                                [(wkey, k, 0), (uk, k // 8)], [pk])
                    self.tt("dve", th[:, :], pa[:, :], m["gt"][:, nh * 512:(nh + 1) * 512], ALU.mult,
                            [pk, "gt"], [thk])
                    self.tt("pool", h[:, nh * 512:(nh + 1) * 512], th[:, :], h[:, nh * 512:(nh + 1) * 512], ALU.add,
                            [thk, hk], [hk])
                if not final:
                    self.dma("q_pool", self.hrow(j), h[:], [hk], [("hres", j)])
                else:
                    t2, t2k = self.t1buf.next()
                    ss, ssk = self.ssb.next()
                    self.act(t2[:], h[:], AF.Square, [hk], [t2k, ssk], accum=ss[:, 0:1])
                    self.rstd(ss, ssk)
                    self.stt("dve", t2[:], h[:], ss[:, 1:2], fg[:], ALU.mult, ALU.mult, [hk, ssk, "fg", t2k], [t2k])
                    self.dma("q_pool", self.out[(j - 2) * 128:(j - 1) * 128, :], t2[:], [t2k], [("out", j)])
        P.phase_end()
        self.h_in_src = False

    def gla_phase_p(self, l):
        P = self.P
        jg = self.kinds[:l + 1].count(0) - 1
        P.phase_begin()
        wb = P.sb([128, KC * 3072], BF16, "win")
        win = wb[:, :].rearrange("p (k n) -> p k n", k=KC)
        self.load_w(win, self.gla_w_in[jg].rearrange("(k p) n -> p k n", p=128), "win", ncols_piece=1024)
        wa1 = P.sb([128, KC, 32], BF16, "wa1")
        self.dma("q_pool", wa1[:], self.gla_wa1[jg].rearrange("(k p) n -> p k n", p=128), (), ["wa1"])
        wa2 = P.sb([32, 1024], BF16, "wa2")
        self.dma("q_pool", wa2[:], self.gla_wa2[jg], (), ["wa2"])
        nba = P.sb([128, 8], F32, "nba")
        self.dma("q_sp", nba[:], self.gla_ba[jg], (), ["nba"])
        self.ts("dve", nba[:], nba[:], -1.0, None, ALU.mult, None, ["nba"], ["nba"])
        gc = P.sb([128, 512], F32, "gc")
        self.dma("q_sp", gc[:], self.mlmask[:, 0:512], (), ["gc"])
        self.alloc_norm()
        pacc = Rot(P, 4, [128, 512], F32, "pacc", psum=True)
        pz = Rot(P, 2, [128, 512], F32, "pz", psum=True)
        qkb = Rot(P, 2, [128, 8, 512], F32, "qkb")
        srb = Rot(P, 2, [128, 8, 512], BF16, "srb")
        vtb = Rot(P, 2, [128, D], BF16, "vtb")
        utb = Rot(P, 2, [32, 512], BF16, "utb")
        tA = Rot(P, 3, [128, 512], F32, "tA")
        tB = Rot(P, 2, [128, 512], F32, "tB")
        tC = Rot(P, 2, [128, 512], F32, "tC")
        tD = Rot(P, 2, [128, 512], F32, "tD")
        ob = Rot(P, 4, [128, 512], BF16, "ob")
        kw4 = Rot(P, 2, [128, 4, 512], BF16, "kw4")
        kwt = Rot(P, 2, [128, 512], BF16, "kwt")
        decb = Rot(P, 2, [128, 32], F32, "decb")

        def stage_b(si, aT, aTk):
            s0, NT = self.sts[si]
            nj = NT // 128
            nch = NT // MCH
            aks = [(aTk, jj) for jj in range(nj)]
            qk, qkk = qkb.next()
            for i in range(8):
                pa, pk = pacc.next()
                for k in range(KC):
                    self.mm(pa[:, :NT], win[:, k, i * 128:(i + 1) * 128], aT[:, k, :NT], k == 0, k == KC - 1, [("win", k, 0)] + aks, [pk])
                self.act(qk[:, i, :NT], pa[:, :NT], AF.Copy, [pk], [(qkk, i)], scale=(128 ** -0.5 if i < 4 else 1.0))
            sr, srk = srb.next()
            for i in range(8):
                pa, pk = pacc.next()
                for k in range(KC):
                    self.mm(pa[:, :NT], win[:, k, 2048 + i * 128:2048 + (i + 1) * 128], aT[:, k, :NT], k == 0, k == KC - 1, [("win", k, 2)] + aks, [pk])
                self.act(sr[:, i, :NT], pa[:, :NT], AF.Silu, [pk], [(srk, i)])
            self.dma("q_pool", self.SR[:, :, s0:s0 + NT].rearrange("f p t -> p f t"), sr[:, :, :NT], [(srk, i) for i in range(8)], [])
            for jj in range(nj):
                vt, vtk = vtb.next()
                for nh in range(2):
                    pa, pk = pacc.next()
                    th, thk = self.thalf.next()
                    for k in range(KC):
                        self.mm(pa[:, :], aT[:, k, jj * 128:(jj + 1) * 128], win[:, k, 1024 + nh * 512:1024 + (nh + 1) * 512], k == 0, k == KC - 1,
                                [("win", k, 1), (aTk, jj)], [pk])
                    self.cp("dve", vt[:, nh * 512:(nh + 1) * 512], pa[:, :], [pk], [(vtk, nh)])
                self.dma("q_pool", self.V_tm[s0 + jj * 128:s0 + (jj + 1) * 128, :], vt[:], [(vtk, 0), (vtk, 1)], [])
            ut, utk = utb.next()
            pu, puk = pz.next()
            for k in range(KC):
                self.mm(pu[0:32, :NT], wa1[:, k, :], aT[:, k, :NT], k == 0, k == KC - 1, ["wa1"] + aks, [puk])
            self.cp("dve", ut[:, :NT], pu[0:32, :NT], [puk], [utk])
            for d in range(2):
                kw, kwk = kw4.next()
                dec, deck = decb.next()
                for h in range(4):
                    q = qk[:, h, :NT]
                    kk = qk[:, 4 + h, :NT]
                    pzt, pzk = pz.next()
                    c0 = d * 512 + h * 128
                    self.mm(pzt[:, :NT], wa2[0:32, c0:c0 + 128], ut[0:32, :NT], True, True, ["wa2", utk], [pzk])
                    e, ek = tA.next()
                    self.act(e[:, :NT], pzt[:, :NT], AF.Exp, [pzk, "nba"], [ek], scale=-1.0, bias=nba[:, d * 4 + h:d * 4 + h + 1])
                    sp, spk = tB.next()
                    self.act(sp[:, :NT], e[:, :NT], AF.Ln, [ek], [spk], bias=1.0)
                    cs, csk = tC.next()
                    self.scan(cs[:, :NT], gc[:, :NT], sp[:, :NT], 0.0, ALU.mult, ALU.add, ["gc", spk], [csk])
                    cs3 = cs[:, :NT].rearrange("p (c t) -> p c t", t=MCH)
                    cl_b = cs3[:, :, MCH - 1:MCH].to_broadcast([128, nch, MCH])
                    dd, ddk = tD.next()
                    dd3 = dd[:, :NT].rearrange("p (c t) -> p c t", t=MCH)
                    sp3 = sp[:, :NT].rearrange("p (c t) -> p c t", t=MCH)
                    if d == 0:
                        xq, xs = cs, -1.0 / 16
                        xk, xks = cs, 1.0 / 16
                        self.tt("dve", dd3, cs3, cl_b, ALU.subtract, [csk], [ddk])
                        xw, xws = dd, 1.0 / 16
                        xqk = xkk = csk
                        xwk = ddk
                    else:
                        t1, t1k_ = tD.next()
                        t13 = t1[:, :NT].rearrange("p (c t) -> p c t", t=MCH)
                        self.tt("dve", t13, sp3, cs3, ALU.subtract, [csk, spk], [t1k_])
                        self.tt("dve", dd3, t13, cl_b, ALU.add, [t1k_, csk], [ddk])
                        xq, xs, xqk = dd, -1.0 / 16, ddk
                        xk, xks, xkk = dd, 1.0 / 16, ddk
                        xw, xws, xwk = t1, 1.0 / 16, t1k_
                    e1, e1k = tA.next()
                    self.act(e1[:, :NT], xq[:, :NT], AF.Exp, [xqk], [e1k], scale=xs)
                    o1, o1k = ob.next()
                    self.tt("pool", o1[:, :NT], q, e1[:, :NT], ALU.mult, [(qkk, h), e1k], [o1k])
                    self.dma("q_pool", self.QD[d, h, :, s0:s0 + NT], o1[:, :NT], [o1k], [])
                    e2, e2k = tA.next()
                    self.act(e2[:, :NT], xk[:, :NT], AF.Exp, [xkk], [e2k], scale=xks)
                    o2, o2k = ob.next()
                    self.tt("pool", o2[:, :NT], kk, e2[:, :NT], ALU.mult, [(qkk, 4 + h), e2k], [o2k])
                    self.dma("q_pool", self.KD[d, h, :, s0:s0 + NT], o2[:, :NT], [o2k], [])
                    e3, e3k = tA.next()
                    self.act(e3[:, :NT], xw[:, :NT], AF.Exp, [xwk], [e3k], scale=xws)
                    self.tt("pool", kw[:, h, :NT], kk, e3[:, :NT], ALU.mult, [(qkk, 4 + h), e3k], [(kwk, h)])
                    self.act(self._decv(dec, h, nch), cs3[:, :, MCH - 1], AF.Exp, [csk], [(deck, h)], scale=-1.0 / 16)
                self.dma("q_pool", self.DEC[d, :, :, s0 // MCH:s0 // MCH + nch].rearrange("h p c -> p h c"),
                         self._decall(dec, nch), [(deck, h) for h in range(4)], [])
                for jj in range(nj):
                    pt, ptk = self.ptr.next()
                    for h in range(4):
                        self.tr(pt[:, h * 128:(h + 1) * 128], kw[:, h, jj * 128:(jj + 1) * 128], self.ident[:], [(kwk, h), "ident"], [ptk])
                    kt, ktk = kwt.next()
                    self.cp("act", kt[:], pt[:, 0:512], [ptk], [ktk])
                    self.dma("q_pool", self.KW_tm[d, s0 + jj * 128:s0 + (jj + 1) * 128, :], kt[:], [ktk], [])

        self.pipelined(self.sis(False), self.norm_stage, stage_b, l, 0, self.norm1_g)
        P.phase_end()

    def _decv(self, dec, h, nch):
        return dec[:, h * 8:h * 8 + nch]

    def _decall(self, dec, nch):
        return dec[:, :].rearrange("p (h c) -> p h c", h=4)[:, :, :nch]

    def tile_order(self, d):
        sis = list(range(len(self.sts)))
        if d == 1:
            sis = [0] + sis[:0:-1]
        return sis

    def gla_phase_s(self, l, d):
        P = self.P
        P.phase_begin()
        gc = P.sb([128, 256], F32, "gc")
        self.dma("q_sp", gc[:], self.mlmask[:, 512:768], (), ["gc"])
        mask = gc[:, d * 128:(d + 1) * 128]
        qdb = Rot(P, 2, [128, 4, 512], BF16, "qdb")
        kdb = Rot(P, 2, [128, 4, 512], BF16, "kdb")
        kwb = Rot(P, 2, [128, 4, 512], BF16, "kwb")
        vtb = Rot(P, 2, [128, 4, D], BF16, "vtb")
        decb = Rot(P, 2, [128, 4, 8], F32, "decb")
        S = P.sb([128, 4, 256], F32, "S")
        Sb = P.sb([128, 4, 256], BF16, "Sb")
        attb = Rot(P, 4, [128, 128], BF16, "attb")
        otb = Rot(P, 2, [128, 8, 512], F32, "otb")
        patt = Rot(P, 2, [128, 512], F32, "patt", psum=True)
        po = Rot(P, 3, [128, 512], F32, "po", psum=True)
        pst = Rot(P, 3, [128, 512], F32, "pst", psum=True)
        for h in range(4):
            self.memset("dve", S[:, h, :], 0.0, [("S", h)])
            self.memset("pool", Sb[:, h, :], 0.0, [("Sb", h)])
        for si in self.tile_order(d):
            s0, NT = self.sts[si]
            nj = NT // 128
            nch = NT // MCH
            qd, qdk = qdb.next()
            kd, kdk = kdb.next()
            kw, kwk = kwb.next()
            vt, vtk = vtb.next()
            dec, deck = decb.next()
            ot, otk = otb.next()
            self.dma("q_sp", qd[:, :, :NT], self.QD[d, :, :, s0:s0 + NT].rearrange("h p t -> p h t"), [], [qdk])
            self.dma("q_sp", kd[:, :, :NT], self.KD[d, :, :, s0:s0 + NT].rearrange("h p t -> p h t"), [], [kdk])
            self.dma("q_sp", kw[:, :nj, :], self.KW_tm[d, s0:s0 + NT, :].rearrange("(j p) f -> p j f", p=128), [], [kwk])
            self.dma("q_sp", vt[:, :nj, :], self.V_tm[s0:s0 + NT, :].rearrange("(j p) f -> p j f", p=128), [], [vtk])
            self.dma("q_sp", dec[:, :, :nch], self.DEC[d, :, :, s0 // MCH:s0 // MCH + nch].rearrange("h p c -> p h c"), [], [deck])
            jjs = list(range(nj)) if d == 0 else list(range(nj - 1, -1, -1))
            cs_ = (0, 1) if d == 0 else (1, 0)
            for jj in jjs:
                tsl = slice(jj * 128, (jj + 1) * 128)
                pos = {}

                def g_A(h):
                    pa, pak = patt.next()
                    self.mm(pa[:, 0:128], kd[:, h, tsl], qd[:, h, tsl], True, True, [kdk, qdk], [pak])
                    at, atk = attb.next()
                    self.tt("dve", at[:], pa[:, 0:128], mask, ALU.mult, [pak, "gc"], [atk])
                    p_ob, pok = po.next()
                    p_o = p_ob[:, 0:256].rearrange("p (v t) -> p v t", v=2)
                    for vc in range(2):
                        self.mm(p_o[:, vc, :], vt[:, jj, h * 256 + vc * 128:h * 256 + (vc + 1) * 128], at[:], vc == 0, False, [vtk, atk], [pok], sgc=True)
                    pos[h] = (p_o, pok)

                def g_c(h):
                    p_o, pok = pos[h]
                    for vc in range(2):
                        self.mm(p_o[:, vc, :], Sb[:, h, vc * 128:(vc + 1) * 128], qd[:, h, tsl], False, True,
                                [("Sb", h), qdk], [pok], sgc=True)
                    ps, psk = pst.next()
                    self.mm(ps[:, 0:256], kw[:, jj, h * 128:(h + 1) * 128], vt[:, jj, h * 256:(h + 1) * 256], True, True, [kwk, vtk], [psk])
                    self.stt("dve", S[:, h, :], S[:, h, :], dec[:, h, jj:jj + 1], ps[:, 0:256], ALU.mult, ALU.add, [("S", h), deck, psk], [("S", h)])
                    self.cp("act", Sb[:, h, :], S[:, h, :], [("S", h)], [("Sb", h)])

                def g_E(h):
                    p_o, pok = pos[h]
                    self.cp("act", ot[:, 2 * h:2 * h + 2, tsl], p_o[:, :, :], [pok], [(otk, jj, h)])

                for h in range(4):
                    g_A(h)
                    g_c(h)
                    g_E(h)
            self.dma("q_pool", self.OO[d, :, :, s0:s0 + NT].rearrange("f p t -> p f t"), ot[:, :, :NT],
                     [(otk, jj, h) for jj in range(nj) for h in range(4)], [])
        P.phase_end()

    def gla_phase_o(self, l, need_ctx):
        P = self.P
        jg = self.kinds[:l + 1].count(0) - 1
        P.phase_begin()
        wb = P.sb([128, KC * D], BF16, "wo")
        wo = wb[:, :].rearrange("p (k n) -> p k n", k=KC)
        self.load_w(wo, self.gla_w_o[jg].rearrange("(k p) n -> p k n", p=128), "wo", ncols_piece=1024)
        gh = P.sb([128, 8], F32, "gh")
        self.dma("q_sp", gh[:], self.gla_gh[jg], (), ["gh"])
        ones = P.sb([128, 128], BF16, "ones")
        self.dma("q_sp", ones[:], self.onesb[:], (), ["ones"])
        self.alloc_norm(need_aT=False)
        m = self.modt
        pacc = Rot(P, 4, [128, 512], F32, "pacc", psum=True)
        o0b = Rot(P, 2, [128, 8, 512], F32, "o0b")
        o1b = Rot(P, 1, [128, 8, 512], F32, "o1b")
        srb = Rot(P, 2, [128, 8, 512], BF16, "srb")
        sqb = Rot(P, 1, [128, 8, 512], BF16, "sqb")
        yTb = Rot(P, 2, [128, 8, 512], BF16, "yTb")
        rsb = Rot(P, 2, [128, 4, 512], F32, "rsb")
        tmpb = Rot(P, 2, [128, 512], F32, "tmpb")
        cur_row = None
        for si in self.sis(not need_ctx):
            row = 1 if si == 0 else 0
            if row != cur_row:
                self.load_mod(l, 0, None, row)
                cur_row = row
            s0, NT = self.sts[si]
            nj = NT // 128
            o0, o0k = o0b.next()
            o1, o1k = o1b.next()
            sr, srk = srb.next()
            self.dma("q_sp", o0[:, :, :NT], self.OO[0, :, :, s0:s0 + NT].rearrange("f p t -> p f t"), [], [o0k])
            self.dma("q_sp", o1[:, :, :NT], self.OO[1, :, :, s0:s0 + NT].rearrange("f p t -> p f t"), [], [o1k])
            self.dma("q_sp", sr[:, :, :NT], self.SR[:, :, s0:s0 + NT].rearrange("f p t -> p f t"), [], [srk])
            self.tt("pool", o0[:, :, :NT], o0[:, :, :NT], o1[:, :, :NT], ALU.add, [o0k, o1k], [o0k])
            sq, sqk = sqb.next()
            self.act(sq[:, :, :NT], o0[:, :, :NT], AF.Square, [o0k], [sqk])
            rs, rsk = rsb.next()
            for h in range(4):
                pa, pk = pacc.next()
                for vc in range(2):
                    self.mm(pa[:, :NT], ones[:], sq[:, 2 * h + vc, :NT], vc == 0, vc == 1, ["ones", sqk], [pk])
                self.act(rs[:, h, :NT], pa[:, :NT], AF.Sqrt, [pk], [(rsk, h)], scale=1.0 / 256, bias=EPS)
                self.P.op("dve", (lambda rs=rs, h=h, NT=NT: (lambda e: e.reciprocal(rs[:, h, :NT], rs[:, h, :NT])))(), [(rsk, h)], [(rsk, h)])
            yT, yTk = yTb.next()
            for i in range(8):
                tm, tmk = tmpb.next()
                self.stt("dve", tm[:, :NT], o0[:, i, :NT], gh[:, i:i + 1], rs[:, i // 2, :NT], ALU.mult, ALU.mult, [o0k, "gh", (rsk, i // 2)], [tmk])
                self.tt("pool", yT[:, i, :NT], tm[:, :NT], sr[:, i, :NT], ALU.mult, [tmk, srk], [(yTk, i)])
            yks = [(yTk, i) for i in range(8)]
            for jj in range(nj):
                j = s0 // 128 + jj
                h_, hk = self.hbuf.next()
                self.load_h(h_, hk, j)
                t1, t1k = self.t1buf.next()
                for nh in range(2):
                    pa, pk = pacc.next()
                    th, thk = self.thalf.next()
                    for k in range(KC):
                        self.mm(pa[:, :], yT[:, k, jj * 128:(jj + 1) * 128], wo[:, k, nh * 512:(nh + 1) * 512], k == 0, k == KC - 1,
                                [("wo", k, 0), (yTk, k)], [pk])
                    self.tt("dve", th[:, :], pa[:, :], m["gt"][:, nh * 512:(nh + 1) * 512], ALU.mult, [pk, "gt"], [thk])
                    self.tt("pool", h_[:, nh * 512:(nh + 1) * 512], th[:, :], h_[:, nh * 512:(nh + 1) * 512], ALU.add,
                            [thk, hk], [hk])
                self.dma("q_pool", self.hrow(j), h_[:], [hk], [])
        P.phase_end()
        self.h_in_src = False

    def units256(self):
        return [(s0, 256) for s0 in range(0, self.T, 256)]

    def mlstm_phase_p1(self, l):
        P = self.P
        jm = self.kinds[:l + 1].count(1) - 1
        P.phase_begin()
        wb = P.sb([128, KC * 4096], BF16, "wup")
        wup = wb[:, :].rearrange("p (k n) -> p k n", k=KC)
        self.load_w(wup, self.ml_w_up[jm].rearrange("(k p) n -> p k n", p=128), "wup")
        self.alloc_norm()
        pacc = Rot(P, 4, [128, 512], F32, "pacc", psum=True)
        obuf = Rot(P, 3, [128, 4, 512], BF16, "obuf")

        def stage_b(si, aT, aTk):
            s0, NT = self.sts[si]
            nj = NT // 128
            aks = [(aTk, jj) for jj in range(nj)]
            for i4 in range(8):
                ot, otk = obuf.next()
                for q in range(4):
                    i = i4 * 4 + q
                    pa, pk = pacc.next()
                    for k in range(KC):
                        self.mm(pa[:, :NT], wup[:, k, i * 128:(i + 1) * 128], aT[:, k, :NT], k == 0, k == KC - 1, [("wup", k, i // 16)] + aks, [pk])
                    if i < 16:
                        self.cp("act", ot[:, q, :NT], pa[:, :NT], [pk], [(otk, q)])
                    else:
                        self.act(ot[:, q, :NT], pa[:, :NT], AF.Silu, [pk], [(otk, q)])
                dst = self.XM if i4 < 4 else self.SZ
                i0 = (i4 % 4) * 4
                self.dma("q_pool", dst[i0:i0 + 4, :, s0:s0 + NT].rearrange("f p t -> p f t"), ot[:, :, :NT], [(otk, q) for q in range(4)], [])

        self.pipelined(self.sis(False), self.norm_stage, stage_b, l, 0, self.norm1_g)
        P.phase_end()

    def mlstm_phase_p2(self, l):
        P = self.P
        jm = self.kinds[:l + 1].count(1) - 1
        P.phase_begin()
        self.load_ident()
        NT = 256
        CH = MCH
        nj, nch = 2, NT // MCH
        bd = P.sb([128, 48, 128], BF16, "bd")
        for m_ in range(3):
            self.dma("q_pool", bd[:, m_ * 16:(m_ + 1) * 16, :], self.ml_bd[jm, m_].rearrange("c p n -> p c n"), (), ["bd"])
        wgI = P.sb([128, 48, 64], BF16, "wgI")
        wgF = P.sb([128, 48, 64], BF16, "wgF")
        self.dma("q_pool", wgI[:], self.ml_wgI[jm], (), ["wg"])
        self.dma("q_pool", wgF[:], self.ml_wgF[jm], (), ["wg"])
        cw = P.sb([128, 16, 4], F32, "cw")
        cbias = P.sb([128, 16], F32, "cbias")
        self.dma("q_sp", cw[:], self.ml_convw[jm], (), ["cw"])
        self.dma("q_sp", cbias[:], self.ml_convb[jm], (), ["cw"])
        bI = P.sb([64, 1], F32, "bI")
        nbF = P.sb([64, 1], F32, "nbF")
        self.dma("q_sp", bI[:], self.ml_bgI[jm], (), ["bI"])
        self.dma("q_sp", nbF[:], self.ml_bgF[jm], (), ["nbF"])
        self.ts("dve", nbF[:], nbF[:], -1.0, None, ALU.mult, None, ["nbF"], ["nbF"])
        gc = P.sb([128, 512], F32, "gc")
        self.dma("q_sp", gc[:], self.mlmask[:, 0:512], (), ["gc"])
        sel = P.sb([64, 8, 128], F32, "sel")
        self.dma("q_sp", sel[:], self.ml_sel[:], (), ["sel"])
        identf = P.sb([128, 128], F32, "identf")
        self.dma("q_sp", identf[:], self.identf_d[:], (), ["identf"])
        pacc = Rot(P, 2, [128, 512], F32, "pacc", psum=True)
        pgI = P.ps([128, 512], F32, "pgI")
        pgF = P.ps([128, 512], F32, "pgF")
        pb = Rot(P, 1, [128, 512], F32, "pb", psum=True)
        pcx = P.ps([128, 512], F32, "pcx")
        ptkv = P.ps([128, 2048], BF16, "ptkv")
        xwb = Rot(P, 2, [128, 16, NT + 32], BF16, "xwb")
        xcb = Rot(P, 2, [128, 16, NT], BF16, "xcb")
        qkvb = Rot(P, 1, [128, 48, NT], BF16, "qkvb")
        qsb = Rot(P, 1, [128, 16, NT], BF16, "qsb")
        qbb = Rot(P, 3, [128, 4, NT], BF16, "qbb")
        accb = Rot(P, 4, [128, NT], F32, "accb")
        gt_ = {nm: P.sb([64, NT], F32, "g" + nm) for nm in ("LI", "E", "SP", "CS", "BN", "EB", "T1", "COL", "T2", "CW")}
        car = P.sb([64, 8], F32, "car")
        carb = Rot(P, 2, [128, 8, nch], F32, "carb")
        cwtb = Rot(P, 2, [128, 128], F32, "cwtb")
        vxb = Rot(P, 2, [128, 4, 640], BF16, "vxb")
        for i in range(2):
            self.memset("pool", vxb.bufs[i][:, :, 512:640], 1.0, [(vxb.keys[i], "ones")])
        kwb = Rot(P, 2, [128, 2048], BF16, "kwb")
        s_q = 512.0 ** -0.5
        nunits = self.T // 256
        for u in range(nunits):
            s0 = u * 256
            seq_lo, seq_hi = (0, TC) if u == 0 else (TC, self.T)
            xw, xwk = xwb.next()
            lo = max(s0 - 2, seq_lo)
            hi = min(s0 + NT + 1, seq_hi)
            if lo > s0 - 2:
                self.memset("pool", xw[:, :, 14:16], 0.0, [(xwk, "L")])
            if hi < s0 + NT + 1:
                self.memset("pool", xw[:, :, NT + 16:NT + 17], 0.0, [(xwk, "R")])
            self.dma("q_sp", xw[:, :, 14 + lo - (s0 - 2):14 + hi - (s0 - 2)], self.XM[:, :, lo:hi].rearrange("c p t -> p c t"), [],
                     [(xwk, "L"), (xwk, "M"), (xwk, "R")])
            xwks = [(xwk, "L"), (xwk, "M"), (xwk, "R")]
            xc, xck = xcb.next()
            for c in range(16):
                eng = "dve"
                ac, ack = accb.next()
                self.ts(eng, ac[:, :], xw[:, c, 14:14 + NT], cw[:, c, 0:1], None, ALU.mult, None, xwks + ["cw"], [ack])
                for j in range(1, 4):
                    self.stt(eng, ac[:, :], xw[:, c, 14 + j:14 + NT + j], cw[:, c, j:j + 1], ac[:, :], ALU.mult, ALU.add, xwks + ["cw", ack], [ack])
                self.act(xc[:, c, :], ac[:, :], AF.Silu, [ack, "cw"], [(xck, c)], bias=cbias[:, c:c + 1])
            xcks = [(xck, c) for c in range(16)]
            self.dma("q_pool", self.XC[:, :, s0:s0 + NT].rearrange("c p t -> p c t"), xc[:, :, :], xcks, [])
            qkv, qkvk = qkvb.next()
            qs, qsk = qsb.next()
            for m_ in range(3):
                for c in range(16):
                    pa, pk = pacc.next()
                    if m_ < 2:
                        self.mm(pa[:, :NT], bd[:, m_ * 16 + c, :], xc[:, c, :], True, True, ["bd", (xck, c)], [pk])
                    else:
                        self.mm(pa[:, :NT], bd[:, m_ * 16 + c, :], xw[:, c, 16:NT + 16], True, True, ["bd"] + xwks, [pk])
                    self.cp("act", qkv[:, m_ * 16 + c, :], pa[:, :NT], [pk], [(qkvk, m_ * 16 + c)])
                    if m_ == 0:
                        self.ts("pool", qs[:, c, :], qkv[:, c, :], s_q, None, ALU.mult, None, [(qkvk, c)], [(qsk, c)])
            self.dma("q_pool", self.KT[:, :, s0:s0 + NT].rearrange("c p t -> p c t"), qkv[:, 16:32, :], [(qkvk, 16 + c) for c in range(16)], [])
            for c in range(48):
                self.mm(pgI[0:64, :NT], wgI[:, c, :], qkv[:, c, :], c == 0, c == 47, ["wg", (qkvk, c)], ["pgI"])
            for c in range(48):
                self.mm(pgF[0:64, :NT], wgF[:, c, :], qkv[:, c, :], c == 0, c == 47, ["wg", (qkvk, c)], ["pgF"])
            g = gt_
            self.act(g["LI"][:], pgI[0:64, :NT], AF.Identity, ["pgI", "bI"], ["LI"], bias=bI[:, 0:1])
            self.act(g["E"][:], pgF[0:64, :NT], AF.Exp, ["pgF", "nbF"], ["E"], scale=-1.0, bias=nbF[:, 0:1])
            self.act(g["SP"][:], g["E"][:], AF.Ln, ["E"], ["SP"], bias=1.0)
            self.scan(g["CS"][:], gc[0:64, :NT], g["SP"][:], 0.0, ALU.mult, ALU.add, ["gc", "SP"], ["CS"])
            cs3 = g["CS"][:].rearrange("p (c t) -> p c t", t=CH)
            self.cp("act", g["BN"][0:32, :], g["CS"][0:32, :], ["CS"], [("BN", 0)])
            self.tt("dve", g["BN"][32:64, :], g["SP"][32:64, :], g["CS"][32:64, :], ALU.subtract, ["SP", "CS"], [("BN", 1)])
            bn3 = g["BN"][:].rearrange("p (c t) -> p c t", t=CH)
            self.tt("dve", bn3[32:64], bn3[32:64], cs3[32:64, :, CH - 1:CH].to_broadcast([32, nch, CH]), ALU.add, [("BN", 1), "CS"], [("BN", 1)])
            bnk = [("BN", 0), ("BN", 1)]
            self.act(g["EB"][:], g["BN"][:], AF.Exp, bnk, ["EB"], scale=-1.0)
            self.tt("dve", g["T1"][:], g["LI"][:], g["BN"][:], ALU.add, ["LI"] + bnk, ["T1"])
            self.act(g["COL"][:], g["T1"][:], AF.Exp, ["T1"], ["COL"])
            t13 = g["T1"][:].rearrange("p (c t) -> p c t", t=CH)
            t23 = g["T2"][:].rearrange("p (c t) -> p c t", t=CH)
            self.tt("dve", t23, t13, cs3[:, :, CH - 1:CH].to_broadcast([64, nch, CH]), ALU.subtract, ["T1", "CS"], ["T2"])
            self.act(g["CW"][:], g["T2"][:], AF.Exp, ["T2"], ["CW"])
            self.act(car[:, 0:nch], cs3[:, :, CH - 1], AF.Exp, ["CS"], ["car"], scale=-1.0)
            for r8 in range(8):
                d, h = r8 // 4, r8 % 4
                pbt, pbk = pb.next()
                self.mm(pbt[:, :NT], sel[:, r8, :], g["EB"][:, :], True, True, ["sel", "EB"], [pbk])
                qb, qbk = qbb.next()
                self.tt("dve", qb[:, :, :], qs[:, h * 4:(h + 1) * 4, :], pbt[:, :NT].unsqueeze(1).to_broadcast([128, 4, NT]), ALU.mult,
                        [pbk] + [(qsk, h * 4 + i) for i in range(4)], [qbk])
                self.dma("q_pool", self.QB[d, h * 4:(h + 1) * 4, :, s0:s0 + NT].rearrange("c p t -> p c t"), qb[:, :, :], [qbk], [])
            for r8 in range(8):
                self.mm(pcx[:, 256 + r8 * nch:256 + (r8 + 1) * nch], sel[:, r8, :], car[:, 0:nch], r8 == 0, r8 == 7, ["sel", "car"], ["pcx"], sgc=True)
            cb_, cbk = carb.next()
            self.cp("dve", cb_[:, :, :], pcx[:, 256:256 + 8 * nch].rearrange("p (r c) -> p r c", c=nch), ["pcx"], [cbk])
            for d in range(2):
                self.dma("q_pool", self.CARB[d, :, :, s0 // CH:s0 // CH + nch].rearrange("h p c -> p h c"), cb_[:, d * 4:(d + 1) * 4, :], [cbk], [])
            for jj in range(nj):
                tsl = slice(jj * 128, (jj + 1) * 128)
                self.tr(pcx[:, 0:64], g["COL"][:, tsl], identf[0:64, 0:64], ["COL", "identf"], ["pcx"])
                self.tr(pcx[:, 64:128], g["CW"][:, tsl], identf[0:64, 0:64], ["CW", "identf"], ["pcx"])
                ct, ctk = cwtb.next()
                self.cp("dve", ct[:, :], pcx[:, 0:128], ["pcx"], [ctk])
                self.dma("q_pool", self.CWT[s0 + jj * 128:s0 + (jj + 1) * 128, :], ct[:, :], [ctk], [])
                for c in range(16):
                    self.tr(ptkv[:, c * 128:(c + 1) * 128], qkv[:, 32 + c, tsl], self.ident[:], [(qkvk, 32 + c), "ident"], ["ptkv"])
                vx, vxk = vxb.next()
                self.cp("act", vx[:, :, 0:512], ptkv[:, :].rearrange("p (h v) -> p h v", h=4), ["ptkv"], [(vxk, "v")])
                self.dma("q_pool", self.VX[s0 + jj * 128:s0 + (jj + 1) * 128, :], vx[:, :, :].rearrange("p h v -> p (h v)"), [(vxk, "v"), (vxk, "ones")], [])
                for c in range(16):
                    self.tr(ptkv[:, c * 128:(c + 1) * 128], qkv[:, 16 + c, tsl], self.ident[:], [(qkvk, 16 + c), "ident"], ["ptkv"])
                for d in range(2):
                    kw, kwk = kwb.next()
                    for h in range(4):
                        col = ct[:, 64 + 32 * d + h:64 + 32 * d + h + 1]
                        if h < 2:
                            self.act(kw[:, h * 512:(h + 1) * 512], ptkv[:, h * 512:(h + 1) * 512], AF.Copy, ["ptkv", ctk], [(kwk, h)], scale=col)
                        else:
                            self.ts("dve", kw[:, h * 512:(h + 1) * 512], ptkv[:, h * 512:(h + 1) * 512], col, None, ALU.mult, None, ["ptkv", ctk], [(kwk, h)])
                    self.dma("q_pool", self.KWm[d, s0 + jj * 128:s0 + (jj + 1) * 128, :], kw[:, :], [(kwk, h) for h in range(4)], [])
        P.phase_end()

    def mlstm_phase_s(self, l, d):
        P = self.P
        P.phase_begin()
        CH = MCH
        NT, nj, nch = 256, 2, 256 // MCH
        gc = P.sb([128, 256], F32, "gc")
        self.dma("q_sp", gc[:], self.mlmask[:, 512:768], (), ["gc"])
        mask = gc[:, d * 128:(d + 1) * 128]
        qbb = Rot(P, 2, [128, 16, NT], BF16, "qbb")
        ktb = Rot(P, 2, [128, 16, NT], BF16, "ktb")
        vxb = Rot(P, 2, [128, nj, 2560], BF16, "vxb")
        kwb = Rot(P, 2, [128, nj, 2048], BF16, "kwb")
        cwb = Rot(P, 2, [128, nj, 128], F32, "cwb")
        crb = Rot(P, 2, [128, 4, nch], F32, "crb")
        C = P.sb([128, 16, 640], F32, "C")
        Cb = P.sb([128, 16, 640], BF16, "Cb")
        wtb = Rot(P, 3, [128, 128], BF16, "wtb")
        rdb = Rot(P, 2, [128, 128], F32, "rdb")
        htb = Rot(P, 2, [128, 16, NT], F32, "htb")
        pn = Rot(P, 2, [128, 1024], F32, "pn", psum=True)
        pst = Rot(P, 2, [128, 1024], F32, "pst", psum=True)
        for i in range(16):
            self.memset("dve", C[:, i, :], 0.0, [("C", i)])
            self.memset("pool", Cb[:, i, :], 0.0, [("Cb", i)])
        nunits = self.T // 256
        order = list(range(nunits)) if d == 0 else [0] + list(range(nunits - 1, 0, -1))
        for u in order:
            s0 = u * 256
            qb, qbk = qbb.next()
            kt, ktk = ktb.next()
            vx, vxk = vxb.next()
            kw, kwk = kwb.next()
            cw, cwk = cwb.next()
            cr, crk = crb.next()
            ht, htk = htb.next()
            self.dma("q_sp", qb[:, :, :], self.QB[d, :, :, s0:s0 + NT].rearrange("c p t -> p c t"), [], [qbk])
            self.dma("q_sp", kt[:, :, :], self.KT[:, :, s0:s0 + NT].rearrange("c p t -> p c t"), [], [ktk])
            self.dma("q_sp", vx[:, :, :], self.VX[s0:s0 + NT, :].rearrange("(j p) f -> p j f", p=128), [], [vxk])
            self.dma("q_sp", kw[:, :, :], self.KWm[d, s0:s0 + NT, :].rearrange("(j p) f -> p j f", p=128), [], [kwk])
            self.dma("q_sp", cw[:, :, :], self.CWT[s0:s0 + NT, :].rearrange("(j p) f -> p j f", p=128), [], [cwk])
            self.dma("q_sp", cr[:, :, :], self.CARB[d, :, :, s0 // CH:s0 // CH + nch].rearrange("h p c -> p h c"), [], [crk])
            jjs = list(range(nj)) if d == 0 else list(range(nj - 1, -1, -1))
            for jj in jjs:
                tsl = slice(jj * 128, (jj + 1) * 128)
                pns = {}

                def step_A(h):
                    pnt, pnk = pn.next()
                    pns[h] = (pnt, pnk)
                    for dc in range(4):
                        self.mm(pnt[:, 640:768], kt[:, h * 4 + dc, tsl], qb[:, h * 4 + dc, tsl], dc == 0, dc == 3, [ktk, qbk], [pnk], sgc=True)
                    wt, wtk = wtb.next()
                    self.stt("dve", wt[:, :], pnt[:, 640:768], cw[:, jj, 32 * d + h:32 * d + h + 1], mask, ALU.mult, ALU.mult, [pnk, cwk, "gc"], [wtk])
                    for vc in range(4):
                        self.mm(pnt[:, vc * 128:(vc + 1) * 128], vx[:, jj, h * 640 + vc * 128:h * 640 + (vc + 1) * 128], wt[:, :], vc == 0, False,
                                [vxk, wtk], [pnk], sgc=True)
                    self.mm(pnt[:, 512:640], vx[:, jj, h * 640 + 512:h * 640 + 640], wt[:, :], True, False, [vxk, wtk], [pnk], sgc=True)

                def step_c(h):
                    pnt, pnk = pns[h]
                    for vc in range(5):
                        dst = pnt[:, vc * 128:(vc + 1) * 128] if vc < 4 else pnt[:, 512:640]
                        for dc in range(4):
                            self.mm(dst, Cb[:, h * 4 + dc, vc * 128:(vc + 1) * 128], qb[:, h * 4 + dc, tsl], False, dc == 3,
                                    [("Cb", h * 4 + dc), qbk], [pnk], sgc=True)
                    for dc in range(4):
                        ps, psk = pst.next()
                        lhs = kw[:, jj, h * 512 + dc * 128:h * 512 + (dc + 1) * 128]
                        self.mm(ps[:, 0:512], lhs, vx[:, jj, h * 640:h * 640 + 512], True, True, [kwk, vxk], [psk])
                        self.mm(ps[:, 512:640], lhs, vx[:, jj, h * 640 + 512:h * 640 + 640], True, True, [kwk, vxk], [psk])
                        i = h * 4 + dc
                        self.stt("dve", C[:, i, :], C[:, i, :], cr[:, h, jj:jj + 1], ps[:, 0:640], ALU.mult, ALU.add, [("C", i), crk, psk], [("C", i)])
                        self.cp("act", Cb[:, i, :], C[:, i, :], [("C", i)], [("Cb", i)])

                def step_E(h):
                    pnt, pnk = pns[h]
                    rd, rdk = rdb.next()
                    self.act(rd[:, :], pnt[:, 512:640], AF.Abs, [pnk], [rdk])
                    self.ts("dve", rd[:, :], rd[:, :], 1.0, None, ALU.max, None, [rdk], [rdk])
                    self.P.op("dve", (lambda rd=rd: (lambda e: e.reciprocal(rd[:, :], rd[:, :])))(), [rdk], [rdk])
                    self.tt("dve", ht[:, h * 4:(h + 1) * 4, tsl], pnt[:, 0:512].rearrange("p (v t) -> p v t", v=4),
                            rd[:, :].unsqueeze(1).to_broadcast([128, 4, 128]), ALU.mult, [pnk, rdk], [(htk, jj, h)])

                for h in range(4):
                    step_A(h)
                    step_c(h)
                    step_E(h)
            self.dma("q_pool", self.HT[d, :, :, s0:s0 + NT].rearrange("c p t -> p c t"), ht[:, :, :],
                     [(htk, jj, h) for jj in range(nj) for h in range(4)], [])
        P.phase_end()

    def mlstm_phase_o(self, l, need_ctx):
        P = self.P
        jm = self.kinds[:l + 1].count(1) - 1
        P.phase_begin()
        NT, nj = 256, 2
        wb = P.sb([128, 16 * D], BF16, "wdn")
        wd = wb[:, :].rearrange("p (k n) -> p k n", k=16)
        self.load_w(wd, self.ml_w_down[jm].rearrange("(k p) n -> p k n", p=128), "wdn", ncols_piece=1024)
        gn = P.sb([128, 16], F32, "gn")
        sk = P.sb([128, 16], F32, "sk")
        self.dma("q_sp", gn[:], self.ml_gn[jm], (), ["gn"])
        self.dma("q_sp", sk[:], self.ml_skip[jm], (), ["sk"])
        ones = P.sb([128, 128], BF16, "ones")
        self.dma("q_sp", ones[:], self.onesb[:], (), ["ones"])
        self.alloc_norm(need_aT=False)
        m = self.modt
        pacc = Rot(P, 4, [128, 512], F32, "pacc", psum=True)
        pm_ = Rot(P, 2, [128, 512], F32, "pm", psum=True)
        pq_ = Rot(P, 2, [128, 512], F32, "pq", psum=True)
        h0b = Rot(P, 1, [128, 16, NT], F32, "h0b")
        h1b = Rot(P, 1, [128, 16, NT], F32, "h1b")
        xcb = Rot(P, 1, [128, 16, NT], BF16, "xcb")
        szb = Rot(P, 1, [128, 16, NT], BF16, "szb")
        hbb = Rot(P, 1, [128, 16, NT], BF16, "hbb")
        sqb = Rot(P, 1, [128, 16, NT], BF16, "sqb")
        yTb = Rot(P, 2, [128, 16, NT], BF16, "yTb")
        mnb = Rot(P, 2, [128, 4, NT], F32, "mnb")
        rsb = Rot(P, 2, [128, 4, NT], F32, "rsb")
        tma = Rot(P, 3, [128, NT], F32, "tma")
        cur_row = None
        nunits = self.T // 256
        for u in range(nunits):
            if u == 0 and not need_ctx:
                continue
            row = 1 if u == 0 else 0
            if row != cur_row:
                self.load_mod(l, 0, None, row)
                cur_row = row
            s0 = u * 256
            h0, h0k = h0b.next()
            h1, h1k = h1b.next()
            xc, xck = xcb.next()
            sz, szk = szb.next()
            self.dma("q_sp", h0[:, :, :], self.HT[0, :, :, s0:s0 + NT].rearrange("c p t -> p c t"), [], [h0k])
            self.dma("q_sp", h1[:, :, :], self.HT[1, :, :, s0:s0 + NT].rearrange("c p t -> p c t"), [], [h1k])
            self.dma("q_sp", xc[:, :, :], self.XC[:, :, s0:s0 + NT].rearrange("c p t -> p c t"), [], [xck])
            self.dma("q_sp", sz[:, :, :], self.SZ[:, :, s0:s0 + NT].rearrange("c p t -> p c t"), [], [szk])
            self.tt("pool", h0[:, :, :], h0[:, :, :], h1[:, :, :], ALU.add, [h0k, h1k], [h0k])
            hb, hbk = hbb.next()
            sq, sqk = sqb.next()
            self.cp("act", hb[:, :, :], h0[:, :, :], [h0k], [hbk])
            self.act(sq[:, :, :], h0[:, :, :], AF.Square, [h0k], [sqk])
            mn, mnk = mnb.next()
            rs, rsk = rsb.next()
            for h in range(4):
                p1, p1k = pm_.next()
                p2, p2k = pq_.next()
                for vc in range(4):
                    self.mm(p1[:, :NT], ones[:], hb[:, 4 * h + vc, :], vc == 0, vc == 3, ["ones", hbk], [p1k])
                for vc in range(4):
                    self.mm(p2[:, :NT], ones[:], sq[:, 4 * h + vc, :], vc == 0, vc == 3, ["ones", sqk], [p2k])
                self.act(mn[:, h, :], p1[:, :NT], AF.Copy, [p1k], [(mnk, h)], scale=1.0 / 512)
                tq, tqk = tma.next()
                self.tt("dve", tq[:, :], mn[:, h, :], mn[:, h, :], ALU.mult, [(mnk, h)], [tqk])
                self.stt("dve", tq[:, :], p2[:, :NT], 1.0 / 512, tq[:, :], ALU.mult, ALU.subtract, [p2k, tqk], [tqk])
                self.act(rs[:, h, :], tq[:, :], AF.Sqrt, [tqk], [(rsk, h)], bias=EPS)
                self.P.op("dve", (lambda rs=rs, h=h: (lambda e: e.reciprocal(rs[:, h, :], rs[:, h, :])))(), [(rsk, h)], [(rsk, h)])
            yT, yTk = yTb.next()
            for i in range(16):
                h = i // 4
                eng = "dve" if i % 2 == 0 else "pool"
                ta, tak = tma.next()
                self.tt("pool", ta[:, :], h0[:, i, :], mn[:, h, :], ALU.subtract, [h0k, (mnk, h)], [tak])
                self.stt("dve", ta[:, :], ta[:, :], gn[:, i:i + 1], rs[:, h, :], ALU.mult, ALU.mult, [tak, "gn", (rsk, h)], [tak])
                self.stt("dve", ta[:, :], xc[:, i, :], sk[:, i:i + 1], ta[:, :], ALU.mult, ALU.add, [xck, "sk", tak], [tak])
                self.tt("pool", yT[:, i, :], ta[:, :], sz[:, i, :], ALU.mult, [tak, szk], [(yTk, i)])
            for jj in range(nj):
                j = s0 // 128 + jj
                h_, hk = self.hbuf.next()
                self.load_h(h_, hk, j)
                t1, t1k = self.t1buf.next()
                for nh in range(2):
                    pa, pk = pacc.next()
                    th, thk = self.thalf.next()
                    for k in range(16):
                        self.mm(pa[:, :], yT[:, k, jj * 128:(jj + 1) * 128], wd[:, k, nh * 512:(nh + 1) * 512], k == 0, k == 15,
                                [("wdn", k, 0), (yTk, k)], [pk])
                    self.tt("dve", th[:, :], pa[:, :], m["gt"][:, nh * 512:(nh + 1) * 512], ALU.mult, [pk, "gt"], [thk])
                    self.tt("pool", h_[:, nh * 512:(nh + 1) * 512], th[:, :], h_[:, nh * 512:(nh + 1) * 512], ALU.add,
                            [thk, hk], [hk])
                self.dma("q_pool", self.hrow(j), h_[:], [hk], [])
        P.phase_end()
        self.h_in_src = False

    def pool_phase_p(self, l):
        P = self.P
        P.phase_begin()
        self.alloc_norm(need_aT=False)
        cb = P.sb([128, 36, 128], BF16, "cb")
        cf = P.sb([128, 16, 128], F32, "cf")
        self.dma("q_sp", cb[:], self.poolcb[:], (), ["cb"])
        self.dma("q_sp", cf[:], self.poolcf[:], (), ["cf"])
        pp = Rot(P, 2, [128, 1024], F32, "pp", psum=True)
        cpb = Rot(P, 2, [128, D], BF16, "cpb")
        cur_row = None
        for j in range(self.T // 128):
            row = 1 if j < 2 else 0
            if row != cur_row:
                self.load_mod(l, 0, self.norm1_g, row)
                cur_row = row
            a, ak = self.norm_tile(j)
            self.dma("q_pool", self.A_tm[j * 128:(j + 1) * 128, :], a[:], [ak], [("A", j)])
            if j >= 2:
                pt, ptk = pp.next()
                cp_, cpk = cpb.next()
                for g in range(4):
                    self.mm(pt[:, g * 256:(g + 1) * 256], cb[:, g, :], a[:, g * 256:(g + 1) * 256], True, True, ["cb", ak], [(ptk, g // 2)])
                for g in range(4):
                    self.act(cp_[:, g * 256:(g + 1) * 256], pt[:, g * 256:(g + 1) * 256], AF.Copy, [(ptk, g // 2), "cf"], [(cpk, g)], scale=cf[:, 12 + g, 0:1])
                self.dma("q_pool", self.CP_tm[j * 128:(j + 1) * 128, :], cp_[:], [(cpk, g) for g in range(4)], [("CP", j)])
        P.phase_end()

    def pool_phase_q(self, l, need_ctx):
        P = self.P
        jp = self.kinds[:l + 1].count(2) - 1
        P.phase_begin()
        self.load_ident()
        R = self.TL // 64
        cpt = 128 // R
        cb = P.sb([128, 36, 128], BF16, "cb")
        cf = P.sb([128, 16, 128], F32, "cf")
        self.dma("q_sp", cb[:], self.poolcb[:], (), ["cb"])
        self.dma("q_sp", cf[:], self.poolcf[:], (), ["cf"])
        wp = P.sb([128, 4, 2, 256], BF16, "wp")
        for g in range(4):
            self.dma("q_pool", wp[:, g, :, :], self.pool_w[jp, g].rearrange("(k p) n -> p k n", p=128), (), [("wp", g)])
        gt = P.sb([128, D], F32, "gt")
        sg = P.sb([128, D], F32, "sg")
        bsg = P.sb([128, D], F32, "bsg")
        tmpa = P.sb([128, D], F32, "tmpa")
        ppt = Rot(P, 2, [128, 8, 128], F32, "ppt", psum=True)
        ppo = Rot(P, 2, [128, 1024], F32, "ppo", psum=True)
        cpb = Rot(P, 2, [128, D], BF16, "cpb")
        ab = Rot(P, 3, [128, D], BF16, "ab")
        hb = Rot(P, 3, [128, D], F32, "hb")
        tb = Rot(P, 2, [128, D], F32, "tb")
        plT = Rot(P, 2, [128, 8, 128], BF16, "plT")

        def load_gate(row):
            mv = self.modv[l, row:row + 1, :]
            self.dma("q_sp", gt[:], mv[:, 2 * D:3 * D].partition_broadcast(128), [], ["gt"])
            self.dma("q_sp", tmpa[:], self.pool_scale[jp:jp + 1, :].partition_broadcast(128), [], ["tmpa"])
            self.tt("dve", sg[:], gt[:], tmpa[:], ALU.mult, ["gt", "tmpa"], ["sg"])
            self.dma("q_sp", tmpa[:], self.pool_b[jp:jp + 1, :].partition_broadcast(128), [], ["tmpa"])
            self.tt("dve", bsg[:], sg[:], tmpa[:], ALU.mult, ["sg", "tmpa"], ["bsg"])

        def finish(pt, ptk, rr_idx, h, hks, store):
            pl, plk = plT.next()
            for g in range(4):
                self.tt("dve", pl[:, 2 * g:2 * g + 2, :], pt[:, 2 * g:2 * g + 2, :],
                        cf[:, rr_idx(g):rr_idx(g) + 1, :].to_broadcast([128, 2, 128]), ALU.mult, [ptk, "cf"], [(plk, g)])
            po, pok = ppo.next()
            for g in range(4):
                for kc in range(2):
                    self.mm(po[:, g * 256:(g + 1) * 256], pl[:, 2 * g + kc, :], wp[:, g, kc, :], kc == 0, kc == 1, [(plk, g), ("wp", g)], [pok])
            t, tk = tb.next()
            self.tt("dve", t[:], po[:], sg[:], ALU.mult, [pok, "sg"], [tk])
            self.tt("pool", t[:], t[:], bsg[:], ALU.add, [tk, "bsg"], [tk])
            self.tt("pool", h[:], t[:], h[:], ALU.add, [tk] + hks, hks)
            store(h, hks)

        if need_ctx:
            load_gate(1)
            a2 = []
            for j in range(2):
                a, ak = ab.next()
                aks_ = [(ak, cc) for cc in range(cpt)]
                self.dma("q_sp", a[:], self.A_tm[j * 128:(j + 1) * 128, :], [], aks_)
                a2.append((a, aks_))
            for j2 in range(2):
                h, hk = hb.next()
                hks_ = [(hk, cc) for cc in range(cpt)]
                self.dma("q_sp", h[:], self.src_row(j2) if self.h_in_src else self.hrow(j2), [], hks_)
                pt, ptk = ppt.next()
                for fc in range(8):
                    g = fc // 2
                    for j in range(2):
                        self.mm(pt[:, fc, :], a2[j][0][:, fc * 128:(fc + 1) * 128], cb[:, 12 + g * 4 + j * 2 + j2, :], j == 0, False,
                                a2[j][1] + ["cb"], [ptk])
                    self.mm(pt[:, fc, :], a2[j2][0][:, fc * 128:(fc + 1) * 128], cb[:, 28 + g * 2 + j2, :], False, True, a2[j2][1] + ["cb"], [ptk])

                def store_c(h, hks, j2=j2):
                    self.dma("q_pool", self.hrow(j2), h[:], hks, [])
                finish(pt, ptk, lambda g, j2=j2: 4 + g * 2 + j2, h, hks_, store_c)
        load_gate(0)
        cp_v = self.CP_tm[TC:, :].rearrange("(r c) f -> c r f", c=64)
        a_v = self.A_tm[TC:, :].rearrange("(r c) f -> c r f", c=64)
        hsrc = self.x[:, :] if self.h_in_src else self.hres[TC:, :]
        hs_v = hsrc.rearrange("(r c) f -> c r f", c=64)
        hd_v = self.hres[TC:, :].rearrange("(r c) f -> c r f", c=64)
        for m_ in range(64 // cpt):
            c0 = m_ * cpt
            cp_, cpk = cpb.next()
            a, ak = ab.next()
            h, hk = hb.next()
            for cc in range(cpt):
                rows = slice(cc * R, (cc + 1) * R)
                self.dma("q_sp", cp_[rows, :], cp_v[c0 + cc], [], [(cpk, cc)])
                self.dma("q_sp", a[rows, :], a_v[c0 + cc], [], [(ak, cc)])
                self.dma("q_sp", h[rows, :], hs_v[c0 + cc], [], [(hk, cc)])
            pt, ptk = ppt.next()
            cpks = [(cpk, cc) for cc in range(cpt)]
            aks = [(ak, cc) for cc in range(cpt)]
            hks = [(hk, cc) for cc in range(cpt)]
            for fc in range(8):
                g = fc // 2
                self.mm(pt[:, fc, :], cp_[:, fc * 128:(fc + 1) * 128], cb[:, 4 + g, :], True, False, cpks + ["cb"], [ptk])
                self.mm(pt[:, fc, :], a[:, fc * 128:(fc + 1) * 128], cb[:, 8 + g, :], False, True, aks + ["cb"], [ptk])

            def store_l(h, hks_, c0=c0):
                for cc in range(cpt):
                    self.dma("q_pool", hd_v[c0 + cc], h[cc * R:(cc + 1) * R, :], hks_, [])
            finish(pt, ptk, lambda g: g, h, hks, store_l)
        P.phase_end()
        self.h_in_src = False

    def build(self):
        self.setup()
        self.ada_phase()
        for l in range(self.depth):
            kind = self.kinds[l]
            last = l == self.depth - 1
            if kind == 0:
                self.gla_phase_p(l)
                self.gla_phase_s(l, 0)
                self.gla_phase_s(l, 1)
                self.gla_phase_o(l, need_ctx=not last)
            elif kind == 1:
                self.mlstm_phase_p1(l)
                self.mlstm_phase_p2(l)
                self.mlstm_phase_s(l, 0)
                self.mlstm_phase_s(l, 1)
                self.mlstm_phase_o(l, need_ctx=not last)
            elif kind == 2:
                self.pool_phase_p(l)
                self.pool_phase_q(l, need_ctx=not last)
            elif kind is not None:
                raise NotImplementedError
            self.mlp1_phase(l, skip_ctx=last)
            self.mlp2_phase(l, skip_ctx=last, final=last)
        self.P.emit()
        self.P.close()
        return self.nc


def _box(L, w):
    pos = np.arange(L)
    lo = np.maximum(pos - w // 2, 0)
    hi = np.minimum(pos + (w - w // 2), L)
    M = ((pos[:, None] >= lo[None, :]) & (pos[:, None] < hi[None, :])).astype(np.float32)
    return M, (hi - lo).astype(np.float32)


def _consts(T_lat):
    R = T_lat // 64
    cpt = 128 // R
    cb = np.zeros((128, 36, 128), np.float32)
    cf = np.zeros((128, 16, 128), np.float32)
    for g, w in enumerate((2, 4, 8, 16)):
        Mc, cc_ = _box(64, w)
        cb[:, g, :] = np.kron(np.eye(2, dtype=np.float32), Mc)
        cf[:, 12 + g, 0] = 1.0 / np.tile(cc_, 2)
        Mr, cr = _box(R, w)
        cb[:, 4 + g, :] = np.kron(np.eye(cpt, dtype=np.float32), Mr)
        cb[:, 8 + g, :] = -np.diag(np.tile(cr, cpt))
        cf[:, g, :] = (1.0 / np.tile(cr, cpt))[None, :]
        Mx, cx = _box(TC, w)
        for j in range(2):
            for j2 in range(2):
                cb[:, 12 + g * 4 + j * 2 + j2, :] = Mx[j * 128:(j + 1) * 128, j2 * 128:(j2 + 1) * 128]
        for j2 in range(2):
            cb[:, 28 + g * 2 + j2, :] = -np.diag(cx[j2 * 128:(j2 + 1) * 128])
            cf[:, 4 + g * 2 + j2, :] = (1.0 / cx[j2 * 128:(j2 + 1) * 128])[None, :]
    glacf = np.ones((128, 768), np.float32)
    glacf[:, 0:512:64] = 0.0
    si_, ti_ = np.meshgrid(np.arange(128), np.arange(128), indexing="ij")
    same = (si_ // 64) == (ti_ // 64)
    glacf[:, 512:640] = (same & (ti_ >= si_)).astype(np.float32)
    glacf[:, 640:768] = (same & (ti_ <= si_)).astype(np.float32)
    sel = np.zeros((64, 8, 128), np.float32)
    for r8 in range(8):
        sel[32 * (r8 // 4) + r8 % 4, r8, :] = 1.0
    mlmask = np.ones((128, 768), np.float32)
    mlmask[:, 0:512:MCH] = 0.0
    mlmask[:, 512:640] = (ti_ >= si_).astype(np.float32)
    mlmask[:, 640:768] = (ti_ <= si_).astype(np.float32)
    return {"mlmask": mlmask, "ml_sel": sel, "identf": np.eye(128, dtype=np.float32), "glacf": glacf, "onesb": np.ones((128, 128), np.float32).astype(ml_dtypes.bfloat16),
            "identb": np.eye(128, dtype=np.float32).astype(ml_dtypes.bfloat16),
            "poolcb": cb.astype(ml_dtypes.bfloat16), "poolcf": cf}


def _bd(w_qkv):
    n = w_qkv.shape[0]
    o = np.zeros((n, 3, 16, 128, 128), np.float32)
    w = w_qkv.reshape(n, 3, 16, 32, 4, 4)
    for b in range(32):
        o[:, :, :, 4 * b:4 * b + 4, 4 * b:4 * b + 4] = w[:, :, :, b]
    return o


def _wg(w_gate, off):
    n = w_gate.shape[0]
    o = np.zeros((n, 128, 48, 64), np.float32)
    w = w_gate.reshape(n, 2, 48, 128, 8)
    for d in range(2):
        o[:, :, :, 32 * d:32 * d + 4] = w[:, d, :, :, off:off + 4].transpose(0, 2, 1, 3)
    return o


def _bg(b_gate, off):
    n = b_gate.shape[0]
    o = np.zeros((n, 64, 1), np.float32)
    for d in range(2):
        o[:, 32 * d:32 * d + 4, 0] = b_gate[:, d, off:off + 4]
    return o


def _wa2blk(w_a2):
    n = w_a2.shape[0]
    o = np.zeros((n, 32, 1024), np.float32)
    o[:, 0:16, 0:512] = w_a2[:, 0]
    o[:, 16:32, 512:1024] = w_a2[:, 1]
    return o


def make_in_maps(inputs, T_lat, depth):
    B = inputs["x"].shape[0]
    consts = _consts(T_lat)
    maps = []
    for b in range(B):
        cT = np.stack([inputs["c"][b], inputs["c_ctx"]], axis=1)
        cT = np.ascontiguousarray(cT.reshape(KC, 128, 2).transpose(1, 0, 2))
        m = {
            "x": np.ascontiguousarray(inputs["x"][b]),
            "ctx": np.ascontiguousarray(inputs["ctx"][b]),
            "cT": cT.astype(np.float32),
            "ada_w": inputs["ada_w"], "ada_b": inputs["ada_b"],
            "norm1_g": inputs["norm1_g"], "norm2_g": inputs["norm2_g"],
            "mlp_w1": inputs["mlp_w1"], "mlp_w2": inputs["mlp_w2"],
            "final_g": inputs["final_g"].reshape(1, D),
            "gla_w_in": inputs["gla_w_in"],
            "gla_wa1": np.ascontiguousarray(np.concatenate([inputs["gla_w_a1"][:, 0], inputs["gla_w_a1"][:, 1]], axis=-1)),
            "gla_wa2": _wa2blk(inputs["gla_w_a2"]),
            "gla_ba": np.ascontiguousarray(inputs["gla_b_a"].reshape(-1, 8, 128).transpose(0, 2, 1)),
            "gla_gh": np.ascontiguousarray(inputs["gla_g_head"].reshape(-1, 8, 128).transpose(0, 2, 1)),
            "gla_w_o": inputs["gla_w_o"],
            "ml_w_up": inputs["mlstm_w_up"], "ml_w_down": inputs["mlstm_w_down"],
            "ml_bd": _bd(inputs["mlstm_w_qkv"]),
            "ml_wgI": _wg(inputs["mlstm_w_gate"], 0), "ml_wgF": _wg(inputs["mlstm_w_gate"], 4),
            "ml_convw": np.ascontiguousarray(inputs["mlstm_conv_w"].reshape(-1, 4, 16, 128).transpose(0, 3, 2, 1)),
            "ml_convb": np.ascontiguousarray(inputs["mlstm_conv_b"].reshape(-1, 16, 128).transpose(0, 2, 1)),
            "ml_bgI": _bg(inputs["mlstm_b_gate"], 0), "ml_bgF": _bg(inputs["mlstm_b_gate"], 4),
            "ml_gn": np.ascontiguousarray(inputs["mlstm_g_norm"].reshape(-1, 16, 128).transpose(0, 2, 1)),
            "ml_skip": np.ascontiguousarray(inputs["mlstm_skip"].reshape(-1, 16, 128).transpose(0, 2, 1)),
            "pool_w": inputs["pool_w"], "pool_b": inputs["pool_b"].reshape(-1, D),
            "pool_scale": inputs["pool_scale"],
        }
        m.update(consts)
        maps.append(m)
    return maps


def run(inputs, T_lat, depth, kinds=None, trace=False):
    inputs = {k: np.asarray(v) for k, v in inputs.items()}
    bld = Builder(T_lat, depth, kinds)
    nc = bld.build()
    maps = make_in_maps(inputs, T_lat, depth)
    maps = [{k: v for k, v in m.items() if k in bld.din} for m in maps]
    res = run_bass_kernel_spmd(nc, maps, core_ids=list(range(len(maps))), trace=trace)
    out = np.stack([r["out"] for r in res.results], axis=0)
    return out.astype(np.float32), res, bld


def kernel(**inputs):
    out, _, _ = run(inputs, 4096, 4)
    return out
```

```python
from contextlib import ExitStack
import numpy as np
import ml_dtypes
import concourse.bass as bass
import concourse.mybir as mybir
from concourse.bass_utils import run_bass_kernel_spmd

F32 = mybir.dt.float32
BF16 = mybir.dt.bfloat16
AF = mybir.ActivationFunctionType
ALU = mybir.AluOpType
AX = mybir.AxisListType

COMPUTE = ("pe", "act", "dve", "pool")
EPOCH = 20000
NDMASEM = 64

D = 1024
KC = 8
TC = 256
DFF = 4096
EPS = 1e-6
MCH = 128


class Prog:
    def __init__(self, nc):
        self.nc = nc
        self.es = ExitStack()
        self.ops = []
        self.nname = 0
        self.nphase = 0
        self.phase_limit = 10 ** 9

    def sb(self, shape, dtype, name=None):
        self.nname += 1
        name = (name or "sb") + f"_{self.nname}"
        return self.es.enter_context(self.nc.sbuf_tensor(name, list(shape), dtype))

    def ps(self, shape, dtype, name=None):
        self.nname += 1
        name = (name or "ps") + f"_{self.nname}"
        return self.es.enter_context(self.nc.psum_tensor(name, list(shape), dtype))

    def op(self, eng, fn, reads=(), writes=()):
        if self.nphase >= self.phase_limit:
            return
        self.ops.append((eng, fn, tuple(reads), tuple(writes)))

    def barrier(self):
        if self.nphase > self.phase_limit:
            return
        self.ops.append(("barrier", None, (), ()))

    def phase_begin(self):
        self._saved_es = self.es
        self.es = ExitStack()

    def phase_end(self):
        self.nphase += 1
        self.barrier()
        self.es.close()
        self.es = self._saved_es

    def _engine(self, eng):
        nc = self.nc
        return {"pe": nc.tensor, "act": nc.scalar, "dve": nc.vector, "pool": nc.gpsimd,
                "q_sp": nc.sync, "q_act": nc.scalar, "q_pool": nc.gpsimd}[eng]

    def _seq(self, stream):
        nc = self.nc
        return {"pe": nc.tensor, "act": nc.scalar, "dve": nc.vector, "pool": nc.gpsimd,
                "sp": nc.sync}[stream]

    @staticmethod
    def _stream(eng):
        return {"q_sp": "sp", "q_act": "act", "q_pool": "pool"}.get(eng, eng)

    def emit(self, final_keys=()):
        nc = self.nc
        ops = self.ops
        n = len(ops)
        last_w = {}
        rd_eng = {}
        rd_dma = {}
        deps = [None] * n
        needed = [False] * n
        bar_deps = {}
        last_on = {}
        dma_since = []
        for i, (eng, fn, reads, writes) in enumerate(ops):
            if eng == "barrier":
                bd = list(last_on.values()) + dma_since
                bar_deps[i] = bd
                for j in bd:
                    needed[j] = True
                deps[i] = []
                last_w, rd_eng, rd_dma = {}, {}, {}
                dma_since = []
                continue
            if eng.startswith("q_"):
                dma_since.append(i)
            else:
                last_on[eng] = i
            isdma_i = eng.startswith("q_")
            d = set()
            raw = set()
            for k in reads:
                w = last_w.get(k)
                if w is not None:
                    d.add(w)
                    raw.add(w)
            for k in writes:
                w = last_w.get(k)
                if w is not None:
                    d.add(w)
                for r in rd_eng.get(k, {}).values():
                    d.add(r)
                for r in rd_dma.get(k, ()):
                    d.add(r)
            d.discard(i)
            dd = []
            for j in d:
                ej = ops[j][0]
                if (not isdma_i) and ej == eng and eng == "pe":
                    continue
                dd.append(j)
            deps[i] = dd
            for j in dd:
                needed[j] = True
            for k in reads:
                if isdma_i:
                    rd_dma.setdefault(k, []).append(i)
                else:
                    rd_eng.setdefault(k, {})[eng] = i
            for k in writes:
                last_w[k] = i
                rd_eng[k] = {}
                rd_dma[k] = []
        fin = [last_w[k] for k in final_keys if k in last_w]
        for j in fin:
            needed[j] = True

        cnt = {e: 0 for e in COMPUTE}
        sig = [None] * n
        dma_cnt = [0] * NDMASEM
        ndma = 0
        nsw = 0
        NHW = 48
        for i, (eng, fn, reads, writes) in enumerate(ops):
            if not needed[i] or eng == "barrier":
                continue
            if eng.startswith("q_"):
                if eng == "q_pool":
                    s = NHW + nsw % (NDMASEM - NHW)
                    nsw += 1
                else:
                    s = ndma % NHW
                    ndma += 1
                dma_cnt[s] += 1
                sig[i] = ("dma", s, dma_cnt[s] * 16)
            else:
                cnt[eng] += 1
                sig[i] = ("eng", eng, cnt[eng])
        sems = {}
        for e in COMPUTE:
            ne = max(1, (cnt[e] + EPOCH - 1) // EPOCH)
            sems[e] = [self.es.enter_context(nc.semaphore(f"s_{e}{k}")) for k in range(ne)]
        dsems = [self.es.enter_context(nc.semaphore(f"s_dma{k}")) for k in range(NDMASEM)]
        assert max(dma_cnt + [0]) * 16 < 60000, dma_cnt
        self.stats = dict(cnt=dict(cnt), ndma=ndma, nops=n)
        known = {}

        def do_wait(stream, s):
            if s[0] == "dma":
                key = ("dma", s[1])
                val = s[2]
                if known.get((stream, key), 0) >= val:
                    return
                known[(stream, key)] = val
                self._seq(stream).wait_ge(dsems[s[1]], val)
            else:
                e, idx = s[1], s[2]
                key = ("eng", e)
                if known.get((stream, key), 0) >= idx:
                    return
                known[(stream, key)] = idx
                ep = (idx - 1) // EPOCH
                self._seq(stream).wait_ge(sems[e][ep], idx - ep * EPOCH)

        for i, (eng, fn, reads, writes) in enumerate(ops):
            if eng == "barrier":
                for stream in ("pe", "act", "dve", "pool", "sp"):
                    for j in bar_deps[i]:
                        if sig[j][0] == "eng" and sig[j][1] == stream:
                            continue
                        do_wait(stream, sig[j])
                continue
            stream = self._stream(eng)
            for j in sorted(deps[i]):
                do_wait(stream, sig[j])
            if needed[i] and sig[i][0] == "dma" and sig[i][2] > 16:
                do_wait(stream, ("dma", sig[i][1], sig[i][2] - 16))
            ins = fn(self._engine(eng))
            if needed[i]:
                s = sig[i]
                if s[0] == "dma":
                    ins.then_inc(dsems[s[1]], 16)
                else:
                    ep = (s[2] - 1) // EPOCH
                    ins.then_inc(sems[s[1]][ep], 1)
        for j in fin:
            do_wait("sp", sig[j])

    def close(self):
        self.es.close()


class Rot:
    def __init__(self, P, n, shape, dtype, name, psum=False):
        self.bufs = [(P.ps if psum else P.sb)(shape, dtype, f"{name}{i}") for i in range(n)]
        self.keys = [(name, i) for i in range(n)]
        self.i = -1

    def next(self):
        self.i = (self.i + 1) % len(self.bufs)
        return self.bufs[self.i], self.keys[self.i]


class Builder:
    def __init__(self, T_lat, depth, kinds=None):
        self.TL = T_lat
        self.T = TC + T_lat
        self.depth = depth
        self.kinds = kinds if kinds is not None else [i % 3 for i in range(depth)]
        self.nc = bass.Bass("TRN2", target_bir_lowering=False)
        self.P = Prog(self.nc)
        self.sts = [(0, TC)] + [(TC + 512 * i, 512) for i in range(T_lat // 512)]
        self.din = {}

    def mm(self, out, lhsT, rhs, start, stop, r, w, sgc=False):
        self.P.op("pe", lambda e: e.matmul(out, lhsT=lhsT, rhs=rhs, start=start, stop=stop, skip_group_check=sgc), r, w)

    def tr(self, out, in_, ident, r, w):
        self.P.op("pe", lambda e: e.transpose(out, in_, ident), r, w)

    def act(self, out, in_, func, r, w, bias=None, scale=None, accum=None):
        kw = {}
        if bias is not None:
            kw["bias"] = bias
        if scale is not None:
            kw["scale"] = scale
        if accum is not None:
            kw["accum_out"] = accum
        self.P.op("act", lambda e: e.activation(out=out, in_=in_, func=func, **kw), r, w)

    def tt(self, eng, out, a, b, op, r, w):
        self.P.op(eng, lambda e: e.tensor_tensor(out=out, in0=a, in1=b, op=op), r, w)

    def ts(self, eng, out, a, s1, s2, op0, op1, r, w):
        if s2 is None:
            self.P.op(eng, lambda e: e.tensor_scalar(out=out, in0=a, scalar1=s1, scalar2=None, op0=op0), r, w)
        else:
            self.P.op(eng, lambda e: e.tensor_scalar(out=out, in0=a, scalar1=s1, scalar2=s2, op0=op0, op1=op1), r, w)

    def stt(self, eng, out, in0, scalar, in1, op0, op1, r, w):
        self.P.op(eng, lambda e: e.scalar_tensor_tensor(out=out, in0=in0, scalar=scalar, in1=in1, op0=op0, op1=op1), r, w)

    def cp(self, eng, out, in_, r, w):
        if eng == "act":
            self.P.op("act", lambda e: e.copy(out, in_), r, w)
        else:
            self.P.op(eng, lambda e: e.tensor_copy(out, in_), r, w)

    def dma(self, q, out, in_, r, w, slow=False):
        if slow:
            self.P.op(q, lambda e: e.dma_start(out=out, in_=in_, allow_slow_non_contiguous=True), r, w)
        else:
            self.P.op(q, lambda e: e.dma_start(out=out, in_=in_), r, w)

    def scan(self, out, d0, d1, init, op0, op1, r, w):
        self.P.op("dve", lambda e: e.tensor_tensor_scan(out=out, data0=d0, data1=d1, initial=init, op0=op0, op1=op1), r, w)

    def memset(self, eng, ap, val, w):
        self.P.op(eng, lambda e: e.memset(ap, val), (), w)

    def inp(self, name, shape, dtype=F32):
        t = self.nc.dram_tensor(name, list(shape), dtype, kind="ExternalInput")
        self.din[name] = t
        return t

    def scratch(self, name, shape, dtype):
        return self.nc.dram_tensor(name, list(shape), dtype, kind="Internal")

    def setup(self):
        P = self.P
        TL, T = self.TL, self.T
        self.x = self.inp("x", [TL, D])
        self.ctx = self.inp("ctx", [TC, D])
        self.cT = self.inp("cT", [128, KC, 2])
        self.ada_w = self.inp("ada_w", [4, D, 6 * D])
        self.ada_b = self.inp("ada_b", [4, 6 * D])
        self.norm1_g = self.inp("norm1_g", [4, D])
        self.norm2_g = self.inp("norm2_g", [4, D])
        self.mlp_w1 = self.inp("mlp_w1", [4, D, DFF])
        self.mlp_w2 = self.inp("mlp_w2", [4, DFF, D])
        self.final_g = self.inp("final_g", [1, D])
        self.identb = self.inp("identb", [128, 128], BF16)
        self.out = self.nc.dram_tensor("out", [TL, D], F32, kind="ExternalOutput")
        self.hres = self.scratch("hres", [T, D], F32)
        self.modv = self.scratch("modv", [4, 2, 6 * D], F32)
        self.U = self.scratch("U", [32, 128, T], BF16)
        ng = max(1, self.kinds.count(0))
        self.gla_w_in = self.inp("gla_w_in", [ng, D, 3072])
        self.gla_wa1 = self.inp("gla_wa1", [ng, D, 32])
        self.gla_wa2 = self.inp("gla_wa2", [ng, 32, 1024])
        self.gla_ba = self.inp("gla_ba", [ng, 128, 8])
        self.gla_gh = self.inp("gla_gh", [ng, 128, 8])
        self.gla_w_o = self.inp("gla_w_o", [ng, D, D])
        self.glacf = self.inp("glacf", [128, 768])
        self.onesb = self.inp("onesb", [128, 128], BF16)
        self.QD = self.scratch("QD", [2, 4, 128, T], BF16)
        self.KD = self.scratch("KD", [2, 4, 128, T], BF16)
        self.KW_tm = self.scratch("KW_tm", [2, T, 512], BF16)
        self.V_tm = self.scratch("V_tm", [T, D], BF16)
        self.SR = self.scratch("SR", [8, 128, T], BF16)
        self.DEC = self.scratch("DEC", [2, 4, 128, T // 64], F32)
        self.OO = self.scratch("OO", [2, 8, 128, T], F32)
        nm_ = max(1, self.kinds.count(1))
        self.ml_w_up = self.inp("ml_w_up", [nm_, D, 4096])
        self.ml_bd = self.inp("ml_bd", [nm_, 3, 16, 128, 128])
        self.ml_wgI = self.inp("ml_wgI", [nm_, 128, 48, 64])
        self.ml_wgF = self.inp("ml_wgF", [nm_, 128, 48, 64])
        self.ml_convw = self.inp("ml_convw", [nm_, 128, 16, 4])
        self.ml_convb = self.inp("ml_convb", [nm_, 128, 16])
        self.ml_bgI = self.inp("ml_bgI", [nm_, 64, 1])
        self.ml_bgF = self.inp("ml_bgF", [nm_, 64, 1])
        self.ml_gn = self.inp("ml_gn", [nm_, 128, 16])
        self.ml_skip = self.inp("ml_skip", [nm_, 128, 16])
        self.ml_w_down = self.inp("ml_w_down", [nm_, 2048, D])
        self.ml_sel = self.inp("ml_sel", [64, 8, 128])
        self.mlmask = self.inp("mlmask", [128, 768])
        self.identf_d = self.inp("identf", [128, 128])
        self.XM = self.scratch("XM", [16, 128, T], BF16)
        self.SZ = self.scratch("SZ", [16, 128, T], BF16)
        self.XC = self.scratch("XC", [16, 128, T], BF16)
        self.KT = self.scratch("KT", [16, 128, T], BF16)
        self.QB = self.scratch("QB", [2, 16, 128, T], BF16)
        self.VX = self.scratch("VX", [T, 2560], BF16)
        self.KWm = self.scratch("KWm", [2, T, 2048], BF16)
        self.CWT = self.scratch("CWT", [T, 128], F32)
        self.CARB = self.scratch("CARB", [2, 4, 128, T // 64], F32)
        self.HT = self.scratch("HT", [2, 16, 128, T], F32)
        self.A_tm = self.scratch("A_tm", [T, D], BF16)
        self.CP_tm = self.scratch("CP_tm", [T, D], BF16)
        npool = max(1, self.kinds.count(2))
        self.pool_w = self.inp("pool_w", [npool, 4, 256, 256])
        self.pool_b = self.inp("pool_b", [npool, D])
        self.pool_scale = self.inp("pool_scale", [npool, D])
        self.poolcb = self.inp("poolcb", [128, 36, 128], BF16)
        self.poolcf = self.inp("poolcf", [128, 16, 128])
        self.h_in_src = True

    def hrow(self, j):
        return self.hres[j * 128:(j + 1) * 128, :]

    def src_row(self, j):
        if j < 2:
            return self.ctx[j * 128:(j + 1) * 128, :]
        return self.x[(j - 2) * 128:(j - 1) * 128, :]

    def load_h(self, h, hk, j):
        if self.h_in_src:
            self.dma("q_sp", h[:], self.src_row(j), [], [hk])
        else:
            self.dma("q_sp", h[:], self.hrow(j), [("hres", j)], [hk])

    def load_ident(self):
        self.ident = self.P.sb([128, 128], BF16, "ident")
        self.dma("q_sp", self.ident[:], self.identb[:], (), ["ident"])

    def ada_phase(self):
        P = self.P
        P.phase_begin()
        cs = P.sb([128, KC, 2], F32, "cs")
        cin = P.sb([128, KC, 2], F32, "cin")
        psm = Rot(P, 2, [128, 512], F32, "psm", psum=True)
        self.dma("q_sp", cin[:], self.cT[:], (), ["cin"])
        self.act(cs[:], cin[:], AF.Silu, ["cin"], ["cs"])
        wst = Rot(P, 2, [128, KC, 512], F32, "adaw")
        bsb = P.sb([2, 6 * D], F32, "adab")
        msb = Rot(P, 1, [2, 6 * D], F32, "adam")
        for l in range(self.depth):
            self.dma("q_sp", bsb[:], self.ada_b[l:l + 1, :].partition_broadcast(2), (), ["adab"])
            mt, mk = msb.next()
            for n in range(12):
                wt, wk = wst.next()
                self.dma("q_sp", wt[:], self.ada_w[l, :, n * 512:(n + 1) * 512].rearrange("(k p) n -> p k n", p=128), (), [wk])
                pt, pk = psm.next()
                for k in range(KC):
                    self.mm(pt[0:2, :], cs[:, k, :], wt[:, k, :], k == 0, k == KC - 1, [wk, "cs"], [pk])
                self.tt("dve", mt[:, n * 512:(n + 1) * 512], pt[0:2, :], bsb[:, n * 512:(n + 1) * 512], ALU.add, [pk, "adab"], [(mk, n)])
            self.dma("q_pool", self.modv[l], mt[:], [(mk, n) for n in range(12)], [("modv", l)])
        P.phase_end()

    def alloc_norm(self, need_aT=True):
        P = self.P
        self.load_ident()
        self.hbuf = Rot(P, 4, [128, D], F32, "hbuf")
        self.t1buf = Rot(P, 2, [128, D], F32, "t1buf")
        self.ssb = Rot(P, 4, [128, 2], F32, "ssb")
        self.thalf = Rot(P, 3, [128, 512], F32, "thalf")
        self.modt = {nm: P.sb([128, D], F32, nm) for nm in ("gs", "sh", "gt")}
        self.abuf = Rot(P, 2, [128, D], BF16, "abuf")
        if need_aT:
            self.aT = Rot(P, 2, [128, KC, 512], BF16, "aT")
            self.ptr = Rot(P, 2, [128, 1024], BF16, "ptr", psum=True)

    def load_mod(self, l, which, norm_g, row):
        m = self.modt
        base = 3 * which
        mv = self.modv[l, row:row + 1, :]
        tmps, tmpsk = self.t1buf.next()
        tmpg, tmpgk = self.t1buf.next()
        self.dma("q_sp", m["sh"][:], mv[:, (base + 0) * D:(base + 1) * D].partition_broadcast(128), [], ["sh"])
        self.dma("q_sp", tmps[:], mv[:, (base + 1) * D:(base + 2) * D].partition_broadcast(128), [], [tmpsk])
        self.dma("q_sp", m["gt"][:], mv[:, (base + 2) * D:(base + 3) * D].partition_broadcast(128), [], ["gt"])
        if norm_g is not None:
            self.dma("q_sp", tmpg[:], norm_g[l:l + 1, :].partition_broadcast(128), (), [tmpgk])
            self.stt("dve", m["gs"][:], tmps[:], 1.0, tmpg[:], ALU.add, ALU.mult, [tmpsk, tmpgk], ["gs"])

    def rstd(self, ss, ssk, n=D):
        self.act(ss[:, 1:2], ss[:, 0:1], AF.Sqrt, [ssk], [ssk], scale=1.0 / n, bias=EPS)
        self.P.op("dve", lambda e: e.reciprocal(ss[:, 1:2], ss[:, 1:2]), [ssk], [ssk])

    def norm_tile(self, j):
        m = self.modt
        h, hk = self.hbuf.next()
        self.load_h(h, hk, j)
        t1, t1k = self.t1buf.next()
        ss, ssk = self.ssb.next()
        self.act(t1[:], h[:], AF.Square, [hk], [t1k, ssk], accum=ss[:, 0:1])
        self.rstd(ss, ssk)
        self.stt("dve", t1[:], h[:], ss[:, 1:2], m["gs"][:], ALU.mult, ALU.mult, [hk, ssk, "gs", t1k], [t1k])
        a, ak = self.abuf.next()
        self.tt("pool", a[:], t1[:], m["sh"][:], ALU.add, [t1k, "sh"], [ak])
        return a, ak

    def norm_stage(self, si):
        s0, NT = self.sts[si]
        aT, aTk = self.aT.next()
        for jj in range(NT // 128):
            j = s0 // 128 + jj
            a, ak = self.norm_tile(j)
            pt, ptk = self.ptr.next()
            for k in range(KC):
                self.tr(pt[:, k * 128:(k + 1) * 128], a[:, k * 128:(k + 1) * 128], self.ident[:], [ak, "ident"], [ptk])
            self.cp("act", aT[:, :, jj * 128:(jj + 1) * 128], pt[:].rearrange("p (k t) -> p k t", k=KC), [ptk], [(aTk, jj)])
        return aT, aTk

    def load_w(self, dst_view, src_view, key, ncols_piece=2048):
        K = dst_view.shape[1]
        N = dst_view.shape[2]
        for c in range(0, N, ncols_piece):
            for k in range(K):
                ce = min(N, c + ncols_piece)
                self.dma("q_pool", dst_view[:, k, c:ce], src_view[:, k, c:ce], (), [(key, k, c // ncols_piece)])

    def sis(self, skip_ctx):
        return [si for si in range(len(self.sts)) if not (skip_ctx and si == 0)]

    def pipelined(self, sis, stage_a, stage_b, l, which, norm_g):
        prev = None
        cur_row = None
        for si in sis:
            row = 1 if si == 0 else 0
            if row != cur_row:
                if prev is not None:
                    stage_b(*prev)
                    prev = None
                self.load_mod(l, which, norm_g, row)
                cur_row = row
            a = stage_a(si)
            if prev is not None:
                stage_b(*prev)
            prev = (si,) + tuple(a)
        if prev is not None:
            stage_b(*prev)

    def mlp1_phase(self, l, skip_ctx=False):
        P = self.P
        P.phase_begin()
        wb = P.sb([128, KC * DFF], BF16, "w1")
        wkey = "w1"
        w1 = wb[:, :].rearrange("p (k n) -> p k n", k=KC)
        self.load_w(w1, self.mlp_w1[l].rearrange("(k p) n -> p k n", p=128), wkey)
        self.alloc_norm()
        pacc = Rot(P, 4, [128, 512], F32, "pacc", psum=True)
        rbuf = Rot(P, 3, [128, 512], F32, "rbuf")
        ubuf = Rot(P, 2, [128, 4, 512], BF16, "ubuf")

        def stage_b(si, aT, aTk):
            s0, NT = self.sts[si]
            nj = NT // 128
            for fo4 in range(8):
                ut, uk = ubuf.next()
                for q in range(4):
                    fo = fo4 * 4 + q
                    pa, pk = pacc.next()
                    for k in range(KC):
                        self.mm(pa[:, :NT], w1[:, k, fo * 128:(fo + 1) * 128], aT[:, k, :NT], k == 0, k == KC - 1,
                                [(wkey, k, fo // 16)] + [(aTk, jj) for jj in range(nj)], [pk])
                    rt, rk = rbuf.next()
                    self.act(rt[:, :NT], pa[:, :NT], AF.Relu, [pk], [rk])
                    self.tt("dve", ut[:, q, :NT], rt[:, :NT], rt[:, :NT], ALU.mult, [rk], [(uk, q)])
                self.dma("q_pool", self.U[fo4 * 4:(fo4 + 1) * 4, :, s0:s0 + NT].rearrange("f p t -> p f t"), ut[:, :, :NT],
                         [(uk, q) for q in range(4)], [("U", si, fo4)])

        self.pipelined(self.sis(skip_ctx), self.norm_stage, stage_b, l, 1, self.norm2_g)
        P.phase_end()

    def mlp2_phase(self, l, skip_ctx=False, final=False):
        P = self.P
        P.phase_begin()
        wb = P.sb([128, 32 * D], BF16, "w2")
        wkey = "w2"
        w2 = wb[:, :].rearrange("p (k n) -> p k n", k=32)
        self.load_w(w2, self.mlp_w2[l].rearrange("(k p) n -> p k n", p=128), wkey, ncols_piece=1024)
        self.alloc_norm(need_aT=False)
        pacc = Rot(P, 4, [128, 512], F32, "pacc", psum=True)
        u2buf = Rot(P, 2, [128, 32, 512], BF16, "u2buf")
        m = self.modt
        fg = None
        if final:
            fg = P.sb([128, D], F32, "fg")
            self.dma("q_sp", fg[:], self.final_g[0:1, :].partition_broadcast(128), (), ["fg"])
        cur_row = None
        for si in self.sis(skip_ctx):
            row = 1 if si == 0 else 0
            if row != cur_row:
                self.load_mod(l, 1, None, row)
                cur_row = row
            s0, NT = self.sts[si]
            nj = NT // 128
            ut, uk = u2buf.next()
            for f8 in range(4):
                self.dma("q_sp", ut[:, f8 * 8:(f8 + 1) * 8, :NT], self.U[f8 * 8:(f8 + 1) * 8, :, s0:s0 + NT].rearrange("f p t -> p f t"),
                         [("U", si, f8 * 2), ("U", si, f8 * 2 + 1)], [(uk, f8)])
            for jj in range(nj):
                j = s0 // 128 + jj
                h, hk = self.hbuf.next()
                self.load_h(h, hk, j)
                t1, t1k = self.t1buf.next()
                for nh in range(2):
                    pa, pk = pacc.next()
                    th, thk = self.thalf.next()
                    for k in range(32):
                        self.mm(pa[:, :], ut[:, k, jj * 128:(jj + 1) * 128], w2[:, k, nh * 512:(nh + 1) * 512], k == 0, k == 31,
                                [(wkey, k, 0), (uk, k // 8)], [pk])
                    self.tt("dve", th[:, :], pa[:, :], m["gt"][:, nh * 512:(nh + 1) * 512], ALU.mult,
                            [pk, "gt"], [thk])
                    self.tt("pool", h[:, nh * 512:(nh + 1) * 512], th[:, :], h[:, nh * 512:(nh + 1) * 512], ALU.add,
                            [thk, hk], [hk])
                if not final:
                    self.dma("q_pool", self.hrow(j), h[:], [hk], [("hres", j)])
                else:
                    t2, t2k = self.t1buf.next()
                    ss, ssk = self.ssb.next()
                    self.act(t2[:], h[:], AF.Square, [hk], [t2k, ssk], accum=ss[:, 0:1])
                    self.rstd(ss, ssk)
                    self.stt("dve", t2[:], h[:], ss[:, 1:2], fg[:], ALU.mult, ALU.mult, [hk, ssk, "fg", t2k], [t2k])
                    self.dma("q_pool", self.out[(j - 2) * 128:(j - 1) * 128, :], t2[:], [t2k], [("out", j)])
        P.phase_end()
        self.h_in_src = False

    def gla_phase_p(self, l):
        P = self.P
        jg = self.kinds[:l + 1].count(0) - 1
        P.phase_begin()
        wb = P.sb([128, KC * 3072], BF16, "win")
        win = wb[:, :].rearrange("p (k n) -> p k n", k=KC)
        self.load_w(win, self.gla_w_in[jg].rearrange("(k p) n -> p k n", p=128), "win", ncols_piece=1024)
        wa1 = P.sb([128, KC, 32], BF16, "wa1")
        self.dma("q_pool", wa1[:], self.gla_wa1[jg].rearrange("(k p) n -> p k n", p=128), (), ["wa1"])
        wa2 = P.sb([32, 1024], BF16, "wa2")
        self.dma("q_pool", wa2[:], self.gla_wa2[jg], (), ["wa2"])
        nba = P.sb([128, 8], F32, "nba")
        self.dma("q_sp", nba[:], self.gla_ba[jg], (), ["nba"])
        self.ts("dve", nba[:], nba[:], -1.0, None, ALU.mult, None, ["nba"], ["nba"])
        gc = P.sb([128, 512], F32, "gc")
        self.dma("q_sp", gc[:], self.mlmask[:, 0:512], (), ["gc"])
        self.alloc_norm()
        pacc = Rot(P, 4, [128, 512], F32, "pacc", psum=True)
        pz = Rot(P, 2, [128, 512], F32, "pz", psum=True)
        qkb = Rot(P, 2, [128, 8, 512], F32, "qkb")
        srb = Rot(P, 2, [128, 8, 512], BF16, "srb")
        vtb = Rot(P, 2, [128, D], BF16, "vtb")
        utb = Rot(P, 2, [32, 512], BF16, "utb")
        tA = Rot(P, 3, [128, 512], F32, "tA")
        tB = Rot(P, 2, [128, 512], F32, "tB")
        tC = Rot(P, 2, [128, 512], F32, "tC")
        tD = Rot(P, 2, [128, 512], F32, "tD")
        ob = Rot(P, 4, [128, 512], BF16, "ob")
        kw4 = Rot(P, 2, [128, 4, 512], BF16, "kw4")
        kwt = Rot(P, 2, [128, 512], BF16, "kwt")
        decb = Rot(P, 2, [128, 32], F32, "decb")

        def stage_b(si, aT, aTk):
            s0, NT = self.sts[si]
            nj = NT // 128
            nch = NT // MCH
            aks = [(aTk, jj) for jj in range(nj)]
            qk, qkk = qkb.next()
            for i in range(8):
                pa, pk = pacc.next()
                for k in range(KC):
                    self.mm(pa[:, :NT], win[:, k, i * 128:(i + 1) * 128], aT[:, k, :NT], k == 0, k == KC - 1, [("win", k, 0)] + aks, [pk])
                self.act(qk[:, i, :NT], pa[:, :NT], AF.Copy, [pk], [(qkk, i)], scale=(128 ** -0.5 if i < 4 else 1.0))
            sr, srk = srb.next()
            for i in range(8):
                pa, pk = pacc.next()
                for k in range(KC):
                    self.mm(pa[:, :NT], win[:, k, 2048 + i * 128:2048 + (i + 1) * 128], aT[:, k, :NT], k == 0, k == KC - 1, [("win", k, 2)] + aks, [pk])
                self.act(sr[:, i, :NT], pa[:, :NT], AF.Silu, [pk], [(srk, i)])
            self.dma("q_pool", self.SR[:, :, s0:s0 + NT].rearrange("f p t -> p f t"), sr[:, :, :NT], [(srk, i) for i in range(8)], [])
            for jj in range(nj):
                vt, vtk = vtb.next()
                for nh in range(2):
                    pa, pk = pacc.next()
                    th, thk = self.thalf.next()
                    for k in range(KC):
                        self.mm(pa[:, :], aT[:, k, jj * 128:(jj + 1) * 128], win[:, k, 1024 + nh * 512:1024 + (nh + 1) * 512], k == 0, k == KC - 1,
                                [("win", k, 1), (aTk, jj)], [pk])
                    self.cp("dve", vt[:, nh * 512:(nh + 1) * 512], pa[:, :], [pk], [(vtk, nh)])
                self.dma("q_pool", self.V_tm[s0 + jj * 128:s0 + (jj + 1) * 128, :], vt[:], [(vtk, 0), (vtk, 1)], [])
            ut, utk = utb.next()
            pu, puk = pz.next()
            for k in range(KC):
                self.mm(pu[0:32, :NT], wa1[:, k, :], aT[:, k, :NT], k == 0, k == KC - 1, ["wa1"] + aks, [puk])
            self.cp("dve", ut[:, :NT], pu[0:32, :NT], [puk], [utk])
            for d in range(2):
                kw, kwk = kw4.next()
                dec, deck = decb.next()
                for h in range(4):
                    q = qk[:, h, :NT]
                    kk = qk[:, 4 + h, :NT]
                    pzt, pzk = pz.next()
                    c0 = d * 512 + h * 128
                    self.mm(pzt[:, :NT], wa2[0:32, c0:c0 + 128], ut[0:32, :NT], True, True, ["wa2", utk], [pzk])
                    e, ek = tA.next()
                    self.act(e[:, :NT], pzt[:, :NT], AF.Exp, [pzk, "nba"], [ek], scale=-1.0, bias=nba[:, d * 4 + h:d * 4 + h + 1])
                    sp, spk = tB.next()
                    self.act(sp[:, :NT], e[:, :NT], AF.Ln, [ek], [spk], bias=1.0)
                    cs, csk = tC.next()
                    self.scan(cs[:, :NT], gc[:, :NT], sp[:, :NT], 0.0, ALU.mult, ALU.add, ["gc", spk], [csk])
                    cs3 = cs[:, :NT].rearrange("p (c t) -> p c t", t=MCH)
                    cl_b = cs3[:, :, MCH - 1:MCH].to_broadcast([128, nch, MCH])
                    dd, ddk = tD.next()
                    dd3 = dd[:, :NT].rearrange("p (c t) -> p c t", t=MCH)
                    sp3 = sp[:, :NT].rearrange("p (c t) -> p c t", t=MCH)
                    if d == 0:
                        xq, xs = cs, -1.0 / 16
                        xk, xks = cs, 1.0 / 16
                        self.tt("dve", dd3, cs3, cl_b, ALU.subtract, [csk], [ddk])
                        xw, xws = dd, 1.0 / 16
                        xqk = xkk = csk
                        xwk = ddk
                    else:
                        t1, t1k_ = tD.next()
                        t13 = t1[:, :NT].rearrange("p (c t) -> p c t", t=MCH)
                        self.tt("dve", t13, sp3, cs3, ALU.subtract, [csk, spk], [t1k_])
                        self.tt("dve", dd3, t13, cl_b, ALU.add, [t1k_, csk], [ddk])
                        xq, xs, xqk = dd, -1.0 / 16, ddk
                        xk, xks, xkk = dd, 1.0 / 16, ddk
                        xw, xws, xwk = t1, 1.0 / 16, t1k_
                    e1, e1k = tA.next()
                    self.act(e1[:, :NT], xq[:, :NT], AF.Exp, [xqk], [e1k], scale=xs)
                    o1, o1k = ob.next()
                    self.tt("pool", o1[:, :NT], q, e1[:, :NT], ALU.mult, [(qkk, h), e1k], [o1k])
                    self.dma("q_pool", self.QD[d, h, :, s0:s0 + NT], o1[:, :NT], [o1k], [])
                    e2, e2k = tA.next()
                    self.act(e2[:, :NT], xk[:, :NT], AF.Exp, [xkk], [e2k], scale=xks)
                    o2, o2k = ob.next()
                    self.tt("pool", o2[:, :NT], kk, e2[:, :NT], ALU.mult, [(qkk, 4 + h), e2k], [o2k])
                    self.dma("q_pool", self.KD[d, h, :, s0:s0 + NT], o2[:, :NT], [o2k], [])
                    e3, e3k = tA.next()
                    self.act(e3[:, :NT], xw[:, :NT], AF.Exp, [xwk], [e3k], scale=xws)
                    self.tt("pool", kw[:, h, :NT], kk, e3[:, :NT], ALU.mult, [(qkk, 4 + h), e3k], [(kwk, h)])
                    self.act(self._decv(dec, h, nch), cs3[:, :, MCH - 1], AF.Exp, [csk], [(deck, h)], scale=-1.0 / 16)
                self.dma("q_pool", self.DEC[d, :, :, s0 // MCH:s0 // MCH + nch].rearrange("h p c -> p h c"),
                         self._decall(dec, nch), [(deck, h) for h in range(4)], [])
                for jj in range(nj):
                    pt, ptk = self.ptr.next()
                    for h in range(4):
                        self.tr(pt[:, h * 128:(h + 1) * 128], kw[:, h, jj * 128:(jj + 1) * 128], self.ident[:], [(kwk, h), "ident"], [ptk])
                    kt, ktk = kwt.next()
                    self.cp("act", kt[:], pt[:, 0:512], [ptk], [ktk])
                    self.dma("q_pool", self.KW_tm[d, s0 + jj * 128:s0 + (jj + 1) * 128, :], kt[:], [ktk], [])

        self.pipelined(self.sis(False), self.norm_stage, stage_b, l, 0, self.norm1_g)
        P.phase_end()

    def _decv(self, dec, h, nch):
        return dec[:, h * 8:h * 8 + nch]

    def _decall(self, dec, nch):
        return dec[:, :].rearrange("p (h c) -> p h c", h=4)[:, :, :nch]

    def tile_order(self, d):
        sis = list(range(len(self.sts)))
        if d == 1:
            sis = [0] + sis[:0:-1]
        return sis

    def gla_phase_s(self, l, d):
        P = self.P
        P.phase_begin()
        gc = P.sb([128, 256], F32, "gc")
        self.dma("q_sp", gc[:], self.mlmask[:, 512:768], (), ["gc"])
        mask = gc[:, d * 128:(d + 1) * 128]
        qdb = Rot(P, 2, [128, 4, 512], BF16, "qdb")
        kdb = Rot(P, 2, [128, 4, 512], BF16, "kdb")
        kwb = Rot(P, 2, [128, 4, 512], BF16, "kwb")
        vtb = Rot(P, 2, [128, 4, D], BF16, "vtb")
        decb = Rot(P, 2, [128, 4, 8], F32, "decb")
        S = P.sb([128, 4, 256], F32, "S")
        Sb = P.sb([128, 4, 256], BF16, "Sb")
        attb = Rot(P, 4, [128, 128], BF16, "attb")
        otb = Rot(P, 2, [128, 8, 512], F32, "otb")
        patt = Rot(P, 2, [128, 512], F32, "patt", psum=True)
        po = Rot(P, 3, [128, 512], F32, "po", psum=True)
        pst = Rot(P, 3, [128, 512], F32, "pst", psum=True)
        for h in range(4):
            self.memset("dve", S[:, h, :], 0.0, [("S", h)])
            self.memset("pool", Sb[:, h, :], 0.0, [("Sb", h)])
        for si in self.tile_order(d):
            s0, NT = self.sts[si]
            nj = NT // 128
            nch = NT // MCH
            qd, qdk = qdb.next()
            kd, kdk = kdb.next()
            kw, kwk = kwb.next()
            vt, vtk = vtb.next()
            dec, deck = decb.next()
            ot, otk = otb.next()
            self.dma("q_sp", qd[:, :, :NT], self.QD[d, :, :, s0:s0 + NT].rearrange("h p t -> p h t"), [], [qdk])
            self.dma("q_sp", kd[:, :, :NT], self.KD[d, :, :, s0:s0 + NT].rearrange("h p t -> p h t"), [], [kdk])
            self.dma("q_sp", kw[:, :nj, :], self.KW_tm[d, s0:s0 + NT, :].rearrange("(j p) f -> p j f", p=128), [], [kwk])
            self.dma("q_sp", vt[:, :nj, :], self.V_tm[s0:s0 + NT, :].rearrange("(j p) f -> p j f", p=128), [], [vtk])
            self.dma("q_sp", dec[:, :, :nch], self.DEC[d, :, :, s0 // MCH:s0 // MCH + nch].rearrange("h p c -> p h c"), [], [deck])
            jjs = list(range(nj)) if d == 0 else list(range(nj - 1, -1, -1))
            cs_ = (0, 1) if d == 0 else (1, 0)
            for jj in jjs:
                tsl = slice(jj * 128, (jj + 1) * 128)
                pos = {}

                def g_A(h):
                    pa, pak = patt.next()
                    self.mm(pa[:, 0:128], kd[:, h, tsl], qd[:, h, tsl], True, True, [kdk, qdk], [pak])
                    at, atk = attb.next()
                    self.tt("dve", at[:], pa[:, 0:128], mask, ALU.mult, [pak, "gc"], [atk])
                    p_ob, pok = po.next()
                    p_o = p_ob[:, 0:256].rearrange("p (v t) -> p v t", v=2)
                    for vc in range(2):
                        self.mm(p_o[:, vc, :], vt[:, jj, h * 256 + vc * 128:h * 256 + (vc + 1) * 128], at[:], vc == 0, False, [vtk, atk], [pok], sgc=True)
                    pos[h] = (p_o, pok)

                def g_c(h):
                    p_o, pok = pos[h]
                    for vc in range(2):
                        self.mm(p_o[:, vc, :], Sb[:, h, vc * 128:(vc + 1) * 128], qd[:, h, tsl], False, True,
                                [("Sb", h), qdk], [pok], sgc=True)
                    ps, psk = pst.next()
                    self.mm(ps[:, 0:256], kw[:, jj, h * 128:(h + 1) * 128], vt[:, jj, h * 256:(h + 1) * 256], True, True, [kwk, vtk], [psk])
                    self.stt("dve", S[:, h, :], S[:, h, :], dec[:, h, jj:jj + 1], ps[:, 0:256], ALU.mult, ALU.add, [("S", h), deck, psk], [("S", h)])
                    self.cp("act", Sb[:, h, :], S[:, h, :], [("S", h)], [("Sb", h)])

                def g_E(h):
                    p_o, pok = pos[h]
                    self.cp("act", ot[:, 2 * h:2 * h + 2, tsl], p_o[:, :, :], [pok], [(otk, jj, h)])

                for h in range(4):
                    g_A(h)
                    g_c(h)
                    g_E(h)
            self.dma("q_pool", self.OO[d, :, :, s0:s0 + NT].rearrange("f p t -> p f t"), ot[:, :, :NT],
                     [(otk, jj, h) for jj in range(nj) for h in range(4)], [])
        P.phase_end()

    def gla_phase_o(self, l, need_ctx):
        P = self.P
        jg = self.kinds[:l + 1].count(0) - 1
        P.phase_begin()
        wb = P.sb([128, KC * D], BF16, "wo")
        wo = wb[:, :].rearrange("p (k n) -> p k n", k=KC)
        self.load_w(wo, self.gla_w_o[jg].rearrange("(k p) n -> p k n", p=128), "wo", ncols_piece=1024)
        gh = P.sb([128, 8], F32, "gh")
        self.dma("q_sp", gh[:], self.gla_gh[jg], (), ["gh"])
        ones = P.sb([128, 128], BF16, "ones")
        self.dma("q_sp", ones[:], self.onesb[:], (), ["ones"])
        self.alloc_norm(need_aT=False)
        m = self.modt
        pacc = Rot(P, 4, [128, 512], F32, "pacc", psum=True)
        o0b = Rot(P, 2, [128, 8, 512], F32, "o0b")
        o1b = Rot(P, 1, [128, 8, 512], F32, "o1b")
        srb = Rot(P, 2, [128, 8, 512], BF16, "srb")
        sqb = Rot(P, 1, [128, 8, 512], BF16, "sqb")
        yTb = Rot(P, 2, [128, 8, 512], BF16, "yTb")
        rsb = Rot(P, 2, [128, 4, 512], F32, "rsb")
        tmpb = Rot(P, 2, [128, 512], F32, "tmpb")
        cur_row = None
        for si in self.sis(not need_ctx):
            row = 1 if si == 0 else 0
            if row != cur_row:
                self.load_mod(l, 0, None, row)
                cur_row = row
            s0, NT = self.sts[si]
            nj = NT // 128
            o0, o0k = o0b.next()
            o1, o1k = o1b.next()
            sr, srk = srb.next()
            self.dma("q_sp", o0[:, :, :NT], self.OO[0, :, :, s0:s0 + NT].rearrange("f p t -> p f t"), [], [o0k])
            self.dma("q_sp", o1[:, :, :NT], self.OO[1, :, :, s0:s0 + NT].rearrange("f p t -> p f t"), [], [o1k])
            self.dma("q_sp", sr[:, :, :NT], self.SR[:, :, s0:s0 + NT].rearrange("f p t -> p f t"), [], [srk])
            self.tt("pool", o0[:, :, :NT], o0[:, :, :NT], o1[:, :, :NT], ALU.add, [o0k, o1k], [o0k])
            sq, sqk = sqb.next()
            self.act(sq[:, :, :NT], o0[:, :, :NT], AF.Square, [o0k], [sqk])
            rs, rsk = rsb.next()
            for h in range(4):
                pa, pk = pacc.next()
                for vc in range(2):
                    self.mm(pa[:, :NT], ones[:], sq[:, 2 * h + vc, :NT], vc == 0, vc == 1, ["ones", sqk], [pk])
                self.act(rs[:, h, :NT], pa[:, :NT], AF.Sqrt, [pk], [(rsk, h)], scale=1.0 / 256, bias=EPS)
                self.P.op("dve", (lambda rs=rs, h=h, NT=NT: (lambda e: e.reciprocal(rs[:, h, :NT], rs[:, h, :NT])))(), [(rsk, h)], [(rsk, h)])
            yT, yTk = yTb.next()
            for i in range(8):
                tm, tmk = tmpb.next()
                self.stt("dve", tm[:, :NT], o0[:, i, :NT], gh[:, i:i + 1], rs[:, i // 2, :NT], ALU.mult, ALU.mult, [o0k, "gh", (rsk, i // 2)], [tmk])
                self.tt("pool", yT[:, i, :NT], tm[:, :NT], sr[:, i, :NT], ALU.mult, [tmk, srk], [(yTk, i)])
            yks = [(yTk, i) for i in range(8)]
            for jj in range(nj):
                j = s0 // 128 + jj
                h_, hk = self.hbuf.next()
                self.load_h(h_, hk, j)
                t1, t1k = self.t1buf.next()
                for nh in range(2):
                    pa, pk = pacc.next()
                    th, thk = self.thalf.next()
                    for k in range(KC):
                        self.mm(pa[:, :], yT[:, k, jj * 128:(jj + 1) * 128], wo[:, k, nh * 512:(nh + 1) * 512], k == 0, k == KC - 1,
                                [("wo", k, 0), (yTk, k)], [pk])
                    self.tt("dve", th[:, :], pa[:, :], m["gt"][:, nh * 512:(nh + 1) * 512], ALU.mult, [pk, "gt"], [thk])
                    self.tt("pool", h_[:, nh * 512:(nh + 1) * 512], th[:, :], h_[:, nh * 512:(nh + 1) * 512], ALU.add,
                            [thk, hk], [hk])
                self.dma("q_pool", self.hrow(j), h_[:], [hk], [])
        P.phase_end()
        self.h_in_src = False

    def units256(self):
        return [(s0, 256) for s0 in range(0, self.T, 256)]

    def mlstm_phase_p1(self, l):
        P = self.P
        jm = self.kinds[:l + 1].count(1) - 1
        P.phase_begin()
        wb = P.sb([128, KC * 4096], BF16, "wup")
        wup = wb[:, :].rearrange("p (k n) -> p k n", k=KC)
        self.load_w(wup, self.ml_w_up[jm].rearrange("(k p) n -> p k n", p=128), "wup")
        self.alloc_norm()
        pacc = Rot(P, 4, [128, 512], F32, "pacc", psum=True)
        obuf = Rot(P, 3, [128, 4, 512], BF16, "obuf")

        def stage_b(si, aT, aTk):
            s0, NT = self.sts[si]
            nj = NT // 128
            aks = [(aTk, jj) for jj in range(nj)]
            for i4 in range(8):
                ot, otk = obuf.next()
                for q in range(4):
                    i = i4 * 4 + q
                    pa, pk = pacc.next()
                    for k in range(KC):
                        self.mm(pa[:, :NT], wup[:, k, i * 128:(i + 1) * 128], aT[:, k, :NT], k == 0, k == KC - 1, [("wup", k, i // 16)] + aks, [pk])
                    if i < 16:
                        self.cp("act", ot[:, q, :NT], pa[:, :NT], [pk], [(otk, q)])
                    else:
                        self.act(ot[:, q, :NT], pa[:, :NT], AF.Silu, [pk], [(otk, q)])
                dst = self.XM if i4 < 4 else self.SZ
                i0 = (i4 % 4) * 4
                self.dma("q_pool", dst[i0:i0 + 4, :, s0:s0 + NT].rearrange("f p t -> p f t"), ot[:, :, :NT], [(otk, q) for q in range(4)], [])

        self.pipelined(self.sis(False), self.norm_stage, stage_b, l, 0, self.norm1_g)
        P.phase_end()

    def mlstm_phase_p2(self, l):
        P = self.P
        jm = self.kinds[:l + 1].count(1) - 1
        P.phase_begin()
        self.load_ident()
        NT = 256
        CH = MCH
        nj, nch = 2, NT // MCH
        bd = P.sb([128, 48, 128], BF16, "bd")
        for m_ in range(3):
            self.dma("q_pool", bd[:, m_ * 16:(m_ + 1) * 16, :], self.ml_bd[jm, m_].rearrange("c p n -> p c n"), (), ["bd"])
        wgI = P.sb([128, 48, 64], BF16, "wgI")
        wgF = P.sb([128, 48, 64], BF16, "wgF")
        self.dma("q_pool", wgI[:], self.ml_wgI[jm], (), ["wg"])
        self.dma("q_pool", wgF[:], self.ml_wgF[jm], (), ["wg"])
        cw = P.sb([128, 16, 4], F32, "cw")
        cbias = P.sb([128, 16], F32, "cbias")
        self.dma("q_sp", cw[:], self.ml_convw[jm], (), ["cw"])
        self.dma("q_sp", cbias[:], self.ml_convb[jm], (), ["cw"])
        bI = P.sb([64, 1], F32, "bI")
        nbF = P.sb([64, 1], F32, "nbF")
        self.dma("q_sp", bI[:], self.ml_bgI[jm], (), ["bI"])
        self.dma("q_sp", nbF[:], self.ml_bgF[jm], (), ["nbF"])
        self.ts("dve", nbF[:], nbF[:], -1.0, None, ALU.mult, None, ["nbF"], ["nbF"])
        gc = P.sb([128, 512], F32, "gc")
        self.dma("q_sp", gc[:], self.mlmask[:, 0:512], (), ["gc"])
        sel = P.sb([64, 8, 128], F32, "sel")
        self.dma("q_sp", sel[:], self.ml_sel[:], (), ["sel"])
        identf = P.sb([128, 128], F32, "identf")
        self.dma("q_sp", identf[:], self.identf_d[:], (), ["identf"])
        pacc = Rot(P, 2, [128, 512], F32, "pacc", psum=True)
        pgI = P.ps([128, 512], F32, "pgI")
        pgF = P.ps([128, 512], F32, "pgF")
        pb = Rot(P, 1, [128, 512], F32, "pb", psum=True)
        pcx = P.ps([128, 512], F32, "pcx")
        ptkv = P.ps([128, 2048], BF16, "ptkv")
        xwb = Rot(P, 2, [128, 16, NT + 32], BF16, "xwb")
        xcb = Rot(P, 2, [128, 16, NT], BF16, "xcb")
        qkvb = Rot(P, 1, [128, 48, NT], BF16, "qkvb")
        qsb = Rot(P, 1, [128, 16, NT], BF16, "qsb")
        qbb = Rot(P, 3, [128, 4, NT], BF16, "qbb")
        accb = Rot(P, 4, [128, NT], F32, "accb")
        gt_ = {nm: P.sb([64, NT], F32, "g" + nm) for nm in ("LI", "E", "SP", "CS", "BN", "EB", "T1", "COL", "T2", "CW")}
        car = P.sb([64, 8], F32, "car")
        carb = Rot(P, 2, [128, 8, nch], F32, "carb")
        cwtb = Rot(P, 2, [128, 128], F32, "cwtb")
        vxb = Rot(P, 2, [128, 4, 640], BF16, "vxb")
        for i in range(2):
            self.memset("pool", vxb.bufs[i][:, :, 512:640], 1.0, [(vxb.keys[i], "ones")])
        kwb = Rot(P, 2, [128, 2048], BF16, "kwb")
        s_q = 512.0 ** -0.5
        nunits = self.T // 256
        for u in range(nunits):
            s0 = u * 256
            seq_lo, seq_hi = (0, TC) if u == 0 else (TC, self.T)
            xw, xwk = xwb.next()
            lo = max(s0 - 2, seq_lo)
            hi = min(s0 + NT + 1, seq_hi)
            if lo > s0 - 2:
                self.memset("pool", xw[:, :, 14:16], 0.0, [(xwk, "L")])
            if hi < s0 + NT + 1:
                self.memset("pool", xw[:, :, NT + 16:NT + 17], 0.0, [(xwk, "R")])
            self.dma("q_sp", xw[:, :, 14 + lo - (s0 - 2):14 + hi - (s0 - 2)], self.XM[:, :, lo:hi].rearrange("c p t -> p c t"), [],
                     [(xwk, "L"), (xwk, "M"), (xwk, "R")])
            xwks = [(xwk, "L"), (xwk, "M"), (xwk, "R")]
            xc, xck = xcb.next()
            for c in range(16):
                eng = "dve"
                ac, ack = accb.next()
                self.ts(eng, ac[:, :], xw[:, c, 14:14 + NT], cw[:, c, 0:1], None, ALU.mult, None, xwks + ["cw"], [ack])
                for j in range(1, 4):
                    self.stt(eng, ac[:, :], xw[:, c, 14 + j:14 + NT + j], cw[:, c, j:j + 1], ac[:, :], ALU.mult, ALU.add, xwks + ["cw", ack], [ack])
                self.act(xc[:, c, :], ac[:, :], AF.Silu, [ack, "cw"], [(xck, c)], bias=cbias[:, c:c + 1])
            xcks = [(xck, c) for c in range(16)]
            self.dma("q_pool", self.XC[:, :, s0:s0 + NT].rearrange("c p t -> p c t"), xc[:, :, :], xcks, [])
            qkv, qkvk = qkvb.next()
            qs, qsk = qsb.next()
            for m_ in range(3):
                for c in range(16):
                    pa, pk = pacc.next()
                    if m_ < 2:
                        self.mm(pa[:, :NT], bd[:, m_ * 16 + c, :], xc[:, c, :], True, True, ["bd", (xck, c)], [pk])
                    else:
                        self.mm(pa[:, :NT], bd[:, m_ * 16 + c, :], xw[:, c, 16:NT + 16], True, True, ["bd"] + xwks, [pk])
                    self.cp("act", qkv[:, m_ * 16 + c, :], pa[:, :NT], [pk], [(qkvk, m_ * 16 + c)])
                    if m_ == 0:
                        self.ts("pool", qs[:, c, :], qkv[:, c, :], s_q, None, ALU.mult, None, [(qkvk, c)], [(qsk, c)])
            self.dma("q_pool", self.KT[:, :, s0:s0 + NT].rearrange("c p t -> p c t"), qkv[:, 16:32, :], [(qkvk, 16 + c) for c in range(16)], [])
            for c in range(48):
                self.mm(pgI[0:64, :NT], wgI[:, c, :], qkv[:, c, :], c == 0, c == 47, ["wg", (qkvk, c)], ["pgI"])
            for c in range(48):
                self.mm(pgF[0:64, :NT], wgF[:, c, :], qkv[:, c, :], c == 0, c == 47, ["wg", (qkvk, c)], ["pgF"])
            g = gt_
            self.act(g["LI"][:], pgI[0:64, :NT], AF.Identity, ["pgI", "bI"], ["LI"], bias=bI[:, 0:1])
            self.act(g["E"][:], pgF[0:64, :NT], AF.Exp, ["pgF", "nbF"], ["E"], scale=-1.0, bias=nbF[:, 0:1])
            self.act(g["SP"][:], g["E"][:], AF.Ln, ["E"], ["SP"], bias=1.0)
            self.scan(g["CS"][:], gc[0:64, :NT], g["SP"][:], 0.0, ALU.mult, ALU.add, ["gc", "SP"], ["CS"])
            cs3 = g["CS"][:].rearrange("p (c t) -> p c t", t=CH)
            self.cp("act", g["BN"][0:32, :], g["CS"][0:32, :], ["CS"], [("BN", 0)])
            self.tt("dve", g["BN"][32:64, :], g["SP"][32:64, :], g["CS"][32:64, :], ALU.subtract, ["SP", "CS"], [("BN", 1)])
            bn3 = g["BN"][:].rearrange("p (c t) -> p c t", t=CH)
            self.tt("dve", bn3[32:64], bn3[32:64], cs3[32:64, :, CH - 1:CH].to_broadcast([32, nch, CH]), ALU.add, [("BN", 1), "CS"], [("BN", 1)])
            bnk = [("BN", 0), ("BN", 1)]
            self.act(g["EB"][:], g["BN"][:], AF.Exp, bnk, ["EB"], scale=-1.0)
            self.tt("dve", g["T1"][:], g["LI"][:], g["BN"][:], ALU.add, ["LI"] + bnk, ["T1"])
            self.act(g["COL"][:], g["T1"][:], AF.Exp, ["T1"], ["COL"])
            t13 = g["T1"][:].rearrange("p (c t) -> p c t", t=CH)
            t23 = g["T2"][:].rearrange("p (c t) -> p c t", t=CH)
            self.tt("dve", t23, t13, cs3[:, :, CH - 1:CH].to_broadcast([64, nch, CH]), ALU.subtract, ["T1", "CS"], ["T2"])
            self.act(g["CW"][:], g["T2"][:], AF.Exp, ["T2"], ["CW"])
            self.act(car[:, 0:nch], cs3[:, :, CH - 1], AF.Exp, ["CS"], ["car"], scale=-1.0)
            for r8 in range(8):
                d, h = r8 // 4, r8 % 4
                pbt, pbk = pb.next()
                self.mm(pbt[:, :NT], sel[:, r8, :], g["EB"][:, :], True, True, ["sel", "EB"], [pbk])
                qb, qbk = qbb.next()
                self.tt("dve", qb[:, :, :], qs[:, h * 4:(h + 1) * 4, :], pbt[:, :NT].unsqueeze(1).to_broadcast([128, 4, NT]), ALU.mult,
                        [pbk] + [(qsk, h * 4 + i) for i in range(4)], [qbk])
                self.dma("q_pool", self.QB[d, h * 4:(h + 1) * 4, :, s0:s0 + NT].rearrange("c p t -> p c t"), qb[:, :, :], [qbk], [])
            for r8 in range(8):
                self.mm(pcx[:, 256 + r8 * nch:256 + (r8 + 1) * nch], sel[:, r8, :], car[:, 0:nch], r8 == 0, r8 == 7, ["sel", "car"], ["pcx"], sgc=True)
            cb_, cbk = carb.next()
            self.cp("dve", cb_[:, :, :], pcx[:, 256:256 + 8 * nch].rearrange("p (r c) -> p r c", c=nch), ["pcx"], [cbk])
            for d in range(2):
                self.dma("q_pool", self.CARB[d, :, :, s0 // CH:s0 // CH + nch].rearrange("h p c -> p h c"), cb_[:, d * 4:(d + 1) * 4, :], [cbk], [])
            for jj in range(nj):
                tsl = slice(jj * 128, (jj + 1) * 128)
                self.tr(pcx[:, 0:64], g["COL"][:, tsl], identf[0:64, 0:64], ["COL", "identf"], ["pcx"])
                self.tr(pcx[:, 64:128], g["CW"][:, tsl], identf[0:64, 0:64], ["CW", "identf"], ["pcx"])
                ct, ctk = cwtb.next()
                self.cp("dve", ct[:, :], pcx[:, 0:128], ["pcx"], [ctk])
                self.dma("q_pool", self.CWT[s0 + jj * 128:s0 + (jj + 1) * 128, :], ct[:, :], [ctk], [])
                for c in range(16):
                    self.tr(ptkv[:, c * 128:(c + 1) * 128], qkv[:, 32 + c, tsl], self.ident[:], [(qkvk, 32 + c), "ident"], ["ptkv"])
                vx, vxk = vxb.next()
                self.cp("act", vx[:, :, 0:512], ptkv[:, :].rearrange("p (h v) -> p h v", h=4), ["ptkv"], [(vxk, "v")])
                self.dma("q_pool", self.VX[s0 + jj * 128:s0 + (jj + 1) * 128, :], vx[:, :, :].rearrange("p h v -> p (h v)"), [(vxk, "v"), (vxk, "ones")], [])
                for c in range(16):
                    self.tr(ptkv[:, c * 128:(c + 1) * 128], qkv[:, 16 + c, tsl], self.ident[:], [(qkvk, 16 + c), "ident"], ["ptkv"])
                for d in range(2):
                    kw, kwk = kwb.next()
                    for h in range(4):
                        col = ct[:, 64 + 32 * d + h:64 + 32 * d + h + 1]
                        if h < 2:
                            self.act(kw[:, h * 512:(h + 1) * 512], ptkv[:, h * 512:(h + 1) * 512], AF.Copy, ["ptkv", ctk], [(kwk, h)], scale=col)
                        else:
                            self.ts("dve", kw[:, h * 512:(h + 1) * 512], ptkv[:, h * 512:(h + 1) * 512], col, None, ALU.mult, None, ["ptkv", ctk], [(kwk, h)])
                    self.dma("q_pool", self.KWm[d, s0 + jj * 128:s0 + (jj + 1) * 128, :], kw[:, :], [(kwk, h) for h in range(4)], [])
        P.phase_end()

    def mlstm_phase_s(self, l, d):
        P = self.P
        P.phase_begin()
        CH = MCH
        NT, nj, nch = 256, 2, 256 // MCH
        gc = P.sb([128, 256], F32, "gc")
        self.dma("q_sp", gc[:], self.mlmask[:, 512:768], (), ["gc"])
        mask = gc[:, d * 128:(d + 1) * 128]
        qbb = Rot(P, 2, [128, 16, NT], BF16, "qbb")
        ktb = Rot(P, 2, [128, 16, NT], BF16, "ktb")
        vxb = Rot(P, 2, [128, nj, 2560], BF16, "vxb")
        kwb = Rot(P, 2, [128, nj, 2048], BF16, "kwb")
        cwb = Rot(P, 2, [128, nj, 128], F32, "cwb")
        crb = Rot(P, 2, [128, 4, nch], F32, "crb")
        C = P.sb([128, 16, 640], F32, "C")
        Cb = P.sb([128, 16, 640], BF16, "Cb")
        wtb = Rot(P, 3, [128, 128], BF16, "wtb")
        rdb = Rot(P, 2, [128, 128], F32, "rdb")
        htb = Rot(P, 2, [128, 16, NT], F32, "htb")
        pn = Rot(P, 2, [128, 1024], F32, "pn", psum=True)
        pst = Rot(P, 2, [128, 1024], F32, "pst", psum=True)
        for i in range(16):
            self.memset("dve", C[:, i, :], 0.0, [("C", i)])
            self.memset("pool", Cb[:, i, :], 0.0, [("Cb", i)])
        nunits = self.T // 256
        order = list(range(nunits)) if d == 0 else [0] + list(range(nunits - 1, 0, -1))
        for u in order:
            s0 = u * 256
            qb, qbk = qbb.next()
            kt, ktk = ktb.next()
            vx, vxk = vxb.next()
            kw, kwk = kwb.next()
            cw, cwk = cwb.next()
            cr, crk = crb.next()
            ht, htk = htb.next()
            self.dma("q_sp", qb[:, :, :], self.QB[d, :, :, s0:s0 + NT].rearrange("c p t -> p c t"), [], [qbk])
            self.dma("q_sp", kt[:, :, :], self.KT[:, :, s0:s0 + NT].rearrange("c p t -> p c t"), [], [ktk])
            self.dma("q_sp", vx[:, :, :], self.VX[s0:s0 + NT, :].rearrange("(j p) f -> p j f", p=128), [], [vxk])
            self.dma("q_sp", kw[:, :, :], self.KWm[d, s0:s0 + NT, :].rearrange("(j p) f -> p j f", p=128), [], [kwk])
            self.dma("q_sp", cw[:, :, :], self.CWT[s0:s0 + NT, :].rearrange("(j p) f -> p j f", p=128), [], [cwk])
            self.dma("q_sp", cr[:, :, :], self.CARB[d, :, :, s0 // CH:s0 // CH + nch].rearrange("h p c -> p h c"), [], [crk])
            jjs = list(range(nj)) if d == 0 else list(range(nj - 1, -1, -1))
            for jj in jjs:
                tsl = slice(jj * 128, (jj + 1) * 128)
                pns = {}

                def step_A(h):
                    pnt, pnk = pn.next()
                    pns[h] = (pnt, pnk)
                    for dc in range(4):
                        self.mm(pnt[:, 640:768], kt[:, h * 4 + dc, tsl], qb[:, h * 4 + dc, tsl], dc == 0, dc == 3, [ktk, qbk], [pnk], sgc=True)
                    wt, wtk = wtb.next()
                    self.stt("dve", wt[:, :], pnt[:, 640:768], cw[:, jj, 32 * d + h:32 * d + h + 1], mask, ALU.mult, ALU.mult, [pnk, cwk, "gc"], [wtk])
                    for vc in range(4):
                        self.mm(pnt[:, vc * 128:(vc + 1) * 128], vx[:, jj, h * 640 + vc * 128:h * 640 + (vc + 1) * 128], wt[:, :], vc == 0, False,
                                [vxk, wtk], [pnk], sgc=True)
                    self.mm(pnt[:, 512:640], vx[:, jj, h * 640 + 512:h * 640 + 640], wt[:, :], True, False, [vxk, wtk], [pnk], sgc=True)

                def step_c(h):
                    pnt, pnk = pns[h]
                    for vc in range(5):
                        dst = pnt[:, vc * 128:(vc + 1) * 128] if vc < 4 else pnt[:, 512:640]
                        for dc in range(4):
                            self.mm(dst, Cb[:, h * 4 + dc, vc * 128:(vc + 1) * 128], qb[:, h * 4 + dc, tsl], False, dc == 3,
                                    [("Cb", h * 4 + dc), qbk], [pnk], sgc=True)
                    for dc in range(4):
                        ps, psk = pst.next()
                        lhs = kw[:, jj, h * 512 + dc * 128:h * 512 + (dc + 1) * 128]
                        self.mm(ps[:, 0:512], lhs, vx[:, jj, h * 640:h * 640 + 512], True, True, [kwk, vxk], [psk])
                        self.mm(ps[:, 512:640], lhs, vx[:, jj, h * 640 + 512:h * 640 + 640], True, True, [kwk, vxk], [psk])
                        i = h * 4 + dc
                        self.stt("dve", C[:, i, :], C[:, i, :], cr[:, h, jj:jj + 1], ps[:, 0:640], ALU.mult, ALU.add, [("C", i), crk, psk], [("C", i)])
                        self.cp("act", Cb[:, i, :], C[:, i, :], [("C", i)], [("Cb", i)])

                def step_E(h):
                    pnt, pnk = pns[h]
                    rd, rdk = rdb.next()
                    self.act(rd[:, :], pnt[:, 512:640], AF.Abs, [pnk], [rdk])
                    self.ts("dve", rd[:, :], rd[:, :], 1.0, None, ALU.max, None, [rdk], [rdk])
                    self.P.op("dve", (lambda rd=rd: (lambda e: e.reciprocal(rd[:, :], rd[:, :])))(), [rdk], [rdk])
                    self.tt("dve", ht[:, h * 4:(h + 1) * 4, tsl], pnt[:, 0:512].rearrange("p (v t) -> p v t", v=4),
                            rd[:, :].unsqueeze(1).to_broadcast([128, 4, 128]), ALU.mult, [pnk, rdk], [(htk, jj, h)])

                for h in range(4):
                    step_A(h)
                    step_c(h)
                    step_E(h)
            self.dma("q_pool", self.HT[d, :, :, s0:s0 + NT].rearrange("c p t -> p c t"), ht[:, :, :],
                     [(htk, jj, h) for jj in range(nj) for h in range(4)], [])
        P.phase_end()

    def mlstm_phase_o(self, l, need_ctx):
        P = self.P
        jm = self.kinds[:l + 1].count(1) - 1
        P.phase_begin()
        NT, nj = 256, 2
        wb = P.sb([128, 16 * D], BF16, "wdn")
        wd = wb[:, :].rearrange("p (k n) -> p k n", k=16)
        self.load_w(wd, self.ml_w_down[jm].rearrange("(k p) n -> p k n", p=128), "wdn", ncols_piece=1024)
        gn = P.sb([128, 16], F32, "gn")
        sk = P.sb([128, 16], F32, "sk")
        self.dma("q_sp", gn[:], self.ml_gn[jm], (), ["gn"])
        self.dma("q_sp", sk[:], self.ml_skip[jm], (), ["sk"])
        ones = P.sb([128, 128], BF16, "ones")
        self.dma("q_sp", ones[:], self.onesb[:], (), ["ones"])
        self.alloc_norm(need_aT=False)
        m = self.modt
        pacc = Rot(P, 4, [128, 512], F32, "pacc", psum=True)
        pm_ = Rot(P, 2, [128, 512], F32, "pm", psum=True)
        pq_ = Rot(P, 2, [128, 512], F32, "pq", psum=True)
        h0b = Rot(P, 1, [128, 16, NT], F32, "h0b")
        h1b = Rot(P, 1, [128, 16, NT], F32, "h1b")
        xcb = Rot(P, 1, [128, 16, NT], BF16, "xcb")
        szb = Rot(P, 1, [128, 16, NT], BF16, "szb")
        hbb = Rot(P, 1, [128, 16, NT], BF16, "hbb")
        sqb = Rot(P, 1, [128, 16, NT], BF16, "sqb")
        yTb = Rot(P, 2, [128, 16, NT], BF16, "yTb")
        mnb = Rot(P, 2, [128, 4, NT], F32, "mnb")
        rsb = Rot(P, 2, [128, 4, NT], F32, "rsb")
        tma = Rot(P, 3, [128, NT], F32, "tma")
        cur_row = None
        nunits = self.T // 256
        for u in range(nunits):
            if u == 0 and not need_ctx:
                continue
            row = 1 if u == 0 else 0
            if row != cur_row:
                self.load_mod(l, 0, None, row)
                cur_row = row
            s0 = u * 256
            h0, h0k = h0b.next()
            h1, h1k = h1b.next()
            xc, xck = xcb.next()
            sz, szk = szb.next()
            self.dma("q_sp", h0[:, :, :], self.HT[0, :, :, s0:s0 + NT].rearrange("c p t -> p c t"), [], [h0k])
            self.dma("q_sp", h1[:, :, :], self.HT[1, :, :, s0:s0 + NT].rearrange("c p t -> p c t"), [], [h1k])
            self.dma("q_sp", xc[:, :, :], self.XC[:, :, s0:s0 + NT].rearrange("c p t -> p c t"), [], [xck])
            self.dma("q_sp", sz[:, :, :], self.SZ[:, :, s0:s0 + NT].rearrange("c p t -> p c t"), [], [szk])
            self.tt("pool", h0[:, :, :], h0[:, :, :], h1[:, :, :], ALU.add, [h0k, h1k], [h0k])
            hb, hbk = hbb.next()
            sq, sqk = sqb.next()
            self.cp("act", hb[:, :, :], h0[:, :, :], [h0k], [hbk])
            self.act(sq[:, :, :], h0[:, :, :], AF.Square, [h0k], [sqk])
            mn, mnk = mnb.next()
            rs, rsk = rsb.next()
            for h in range(4):
                p1, p1k = pm_.next()
                p2, p2k = pq_.next()
                for vc in range(4):
                    self.mm(p1[:, :NT], ones[:], hb[:, 4 * h + vc, :], vc == 0, vc == 3, ["ones", hbk], [p1k])
                for vc in range(4):
                    self.mm(p2[:, :NT], ones[:], sq[:, 4 * h + vc, :], vc == 0, vc == 3, ["ones", sqk], [p2k])
                self.act(mn[:, h, :], p1[:, :NT], AF.Copy, [p1k], [(mnk, h)], scale=1.0 / 512)
                tq, tqk = tma.next()
                self.tt("dve", tq[:, :], mn[:, h, :], mn[:, h, :], ALU.mult, [(mnk, h)], [tqk])
                self.stt("dve", tq[:, :], p2[:, :NT], 1.0 / 512, tq[:, :], ALU.mult, ALU.subtract, [p2k, tqk], [tqk])
                self.act(rs[:, h, :], tq[:, :], AF.Sqrt, [tqk], [(rsk, h)], bias=EPS)
                self.P.op("dve", (lambda rs=rs, h=h: (lambda e: e.reciprocal(rs[:, h, :], rs[:, h, :])))(), [(rsk, h)], [(rsk, h)])
            yT, yTk = yTb.next()
            for i in range(16):
                h = i // 4
                eng = "dve" if i % 2 == 0 else "pool"
                ta, tak = tma.next()
                self.tt("pool", ta[:, :], h0[:, i, :], mn[:, h, :], ALU.subtract, [h0k, (mnk, h)], [tak])
                self.stt("dve", ta[:, :], ta[:, :], gn[:, i:i + 1], rs[:, h, :], ALU.mult, ALU.mult, [tak, "gn", (rsk, h)], [tak])
                self.stt("dve", ta[:, :], xc[:, i, :], sk[:, i:i + 1], ta[:, :], ALU.mult, ALU.add, [xck, "sk", tak], [tak])
                self.tt("pool", yT[:, i, :], ta[:, :], sz[:, i, :], ALU.mult, [tak, szk], [(yTk, i)])
            for jj in range(nj):
                j = s0 // 128 + jj
                h_, hk = self.hbuf.next()
                self.load_h(h_, hk, j)
                t1, t1k = self.t1buf.next()
                for nh in range(2):
                    pa, pk = pacc.next()
                    th, thk = self.thalf.next()
                    for k in range(16):
                        self.mm(pa[:, :], yT[:, k, jj * 128:(jj + 1) * 128], wd[:, k, nh * 512:(nh + 1) * 512], k == 0, k == 15,
                                [("wdn", k, 0), (yTk, k)], [pk])
                    self.tt("dve", th[:, :], pa[:, :], m["gt"][:, nh * 512:(nh + 1) * 512], ALU.mult, [pk, "gt"], [thk])
                    self.tt("pool", h_[:, nh * 512:(nh + 1) * 512], th[:, :], h_[:, nh * 512:(nh + 1) * 512], ALU.add,
                            [thk, hk], [hk])
                self.dma("q_pool", self.hrow(j), h_[:], [hk], [])
        P.phase_end()
        self.h_in_src = False

    def pool_phase_p(self, l):
        P = self.P
        P.phase_begin()
        self.alloc_norm(need_aT=False)
        cb = P.sb([128, 36, 128], BF16, "cb")
        cf = P.sb([128, 16, 128], F32, "cf")
        self.dma("q_sp", cb[:], self.poolcb[:], (), ["cb"])
        self.dma("q_sp", cf[:], self.poolcf[:], (), ["cf"])
        pp = Rot(P, 2, [128, 1024], F32, "pp", psum=True)
        cpb = Rot(P, 2, [128, D], BF16, "cpb")
        cur_row = None
        for j in range(self.T // 128):
            row = 1 if j < 2 else 0
            if row != cur_row:
                self.load_mod(l, 0, self.norm1_g, row)
                cur_row = row
            a, ak = self.norm_tile(j)
            self.dma("q_pool", self.A_tm[j * 128:(j + 1) * 128, :], a[:], [ak], [("A", j)])
            if j >= 2:
                pt, ptk = pp.next()
                cp_, cpk = cpb.next()
                for g in range(4):
                    self.mm(pt[:, g * 256:(g + 1) * 256], cb[:, g, :], a[:, g * 256:(g + 1) * 256], True, True, ["cb", ak], [(ptk, g // 2)])
                for g in range(4):
                    self.act(cp_[:, g * 256:(g + 1) * 256], pt[:, g * 256:(g + 1) * 256], AF.Copy, [(ptk, g // 2), "cf"], [(cpk, g)], scale=cf[:, 12 + g, 0:1])
                self.dma("q_pool", self.CP_tm[j * 128:(j + 1) * 128, :], cp_[:], [(cpk, g) for g in range(4)], [("CP", j)])
        P.phase_end()

    def pool_phase_q(self, l, need_ctx):
        P = self.P
        jp = self.kinds[:l + 1].count(2) - 1
        P.phase_begin()
        self.load_ident()
        R = self.TL // 64
        cpt = 128 // R
        cb = P.sb([128, 36, 128], BF16, "cb")
        cf = P.sb([128, 16, 128], F32, "cf")
        self.dma("q_sp", cb[:], self.poolcb[:], (), ["cb"])
        self.dma("q_sp", cf[:], self.poolcf[:], (), ["cf"])
        wp = P.sb([128, 4, 2, 256], BF16, "wp")
        for g in range(4):
            self.dma("q_pool", wp[:, g, :, :], self.pool_w[jp, g].rearrange("(k p) n -> p k n", p=128), (), [("wp", g)])
        gt = P.sb([128, D], F32, "gt")
        sg = P.sb([128, D], F32, "sg")
        bsg = P.sb([128, D], F32, "bsg")
        tmpa = P.sb([128, D], F32, "tmpa")
        ppt = Rot(P, 2, [128, 8, 128], F32, "ppt", psum=True)
        ppo = Rot(P, 2, [128, 1024], F32, "ppo", psum=True)
        cpb = Rot(P, 2, [128, D], BF16, "cpb")
        ab = Rot(P, 3, [128, D], BF16, "ab")
        hb = Rot(P, 3, [128, D], F32, "hb")
        tb = Rot(P, 2, [128, D], F32, "tb")
        plT = Rot(P, 2, [128, 8, 128], BF16, "plT")

        def load_gate(row):
            mv = self.modv[l, row:row + 1, :]
            self.dma("q_sp", gt[:], mv[:, 2 * D:3 * D].partition_broadcast(128), [], ["gt"])
            self.dma("q_sp", tmpa[:], self.pool_scale[jp:jp + 1, :].partition_broadcast(128), [], ["tmpa"])
            self.tt("dve", sg[:], gt[:], tmpa[:], ALU.mult, ["gt", "tmpa"], ["sg"])
            self.dma("q_sp", tmpa[:], self.pool_b[jp:jp + 1, :].partition_broadcast(128), [], ["tmpa"])
            self.tt("dve", bsg[:], sg[:], tmpa[:], ALU.mult, ["sg", "tmpa"], ["bsg"])

        def finish(pt, ptk, rr_idx, h, hks, store):
            pl, plk = plT.next()
            for g in range(4):
                self.tt("dve", pl[:, 2 * g:2 * g + 2, :], pt[:, 2 * g:2 * g + 2, :],
                        cf[:, rr_idx(g):rr_idx(g) + 1, :].to_broadcast([128, 2, 128]), ALU.mult, [ptk, "cf"], [(plk, g)])
            po, pok = ppo.next()
            for g in range(4):
                for kc in range(2):
                    self.mm(po[:, g * 256:(g + 1) * 256], pl[:, 2 * g + kc, :], wp[:, g, kc, :], kc == 0, kc == 1, [(plk, g), ("wp", g)], [pok])
            t, tk = tb.next()
            self.tt("dve", t[:], po[:], sg[:], ALU.mult, [pok, "sg"], [tk])
            self.tt("pool", t[:], t[:], bsg[:], ALU.add, [tk, "bsg"], [tk])
            self.tt("pool", h[:], t[:], h[:], ALU.add, [tk] + hks, hks)
            store(h, hks)

        if need_ctx:
            load_gate(1)
            a2 = []
            for j in range(2):
                a, ak = ab.next()
                aks_ = [(ak, cc) for cc in range(cpt)]
                self.dma("q_sp", a[:], self.A_tm[j * 128:(j + 1) * 128, :], [], aks_)
                a2.append((a, aks_))
            for j2 in range(2):
                h, hk = hb.next()
                hks_ = [(hk, cc) for cc in range(cpt)]
                self.dma("q_sp", h[:], self.src_row(j2) if self.h_in_src else self.hrow(j2), [], hks_)
                pt, ptk = ppt.next()
                for fc in range(8):
                    g = fc // 2
                    for j in range(2):
                        self.mm(pt[:, fc, :], a2[j][0][:, fc * 128:(fc + 1) * 128], cb[:, 12 + g * 4 + j * 2 + j2, :], j == 0, False,
                                a2[j][1] + ["cb"], [ptk])
                    self.mm(pt[:, fc, :], a2[j2][0][:, fc * 128:(fc + 1) * 128], cb[:, 28 + g * 2 + j2, :], False, True, a2[j2][1] + ["cb"], [ptk])

                def store_c(h, hks, j2=j2):
                    self.dma("q_pool", self.hrow(j2), h[:], hks, [])
                finish(pt, ptk, lambda g, j2=j2: 4 + g * 2 + j2, h, hks_, store_c)
        load_gate(0)
        cp_v = self.CP_tm[TC:, :].rearrange("(r c) f -> c r f", c=64)
        a_v = self.A_tm[TC:, :].rearrange("(r c) f -> c r f", c=64)
        hsrc = self.x[:, :] if self.h_in_src else self.hres[TC:, :]
        hs_v = hsrc.rearrange("(r c) f -> c r f", c=64)
        hd_v = self.hres[TC:, :].rearrange("(r c) f -> c r f", c=64)
        for m_ in range(64 // cpt):
            c0 = m_ * cpt
            cp_, cpk = cpb.next()
            a, ak = ab.next()
            h, hk = hb.next()
            for cc in range(cpt):
                rows = slice(cc * R, (cc + 1) * R)
                self.dma("q_sp", cp_[rows, :], cp_v[c0 + cc], [], [(cpk, cc)])
                self.dma("q_sp", a[rows, :], a_v[c0 + cc], [], [(ak, cc)])
                self.dma("q_sp", h[rows, :], hs_v[c0 + cc], [], [(hk, cc)])
            pt, ptk = ppt.next()
            cpks = [(cpk, cc) for cc in range(cpt)]
            aks = [(ak, cc) for cc in range(cpt)]
            hks = [(hk, cc) for cc in range(cpt)]
            for fc in range(8):
                g = fc // 2
                self.mm(pt[:, fc, :], cp_[:, fc * 128:(fc + 1) * 128], cb[:, 4 + g, :], True, False, cpks + ["cb"], [ptk])
                self.mm(pt[:, fc, :], a[:, fc * 128:(fc + 1) * 128], cb[:, 8 + g, :], False, True, aks + ["cb"], [ptk])

            def store_l(h, hks_, c0=c0):
                for cc in range(cpt):
                    self.dma("q_pool", hd_v[c0 + cc], h[cc * R:(cc + 1) * R, :], hks_, [])
            finish(pt, ptk, lambda g: g, h, hks, store_l)
        P.phase_end()
        self.h_in_src = False

    def build(self):
        self.setup()
        self.ada_phase()
        for l in range(self.depth):
            kind = self.kinds[l]
            last = l == self.depth - 1
            if kind == 0:
                self.gla_phase_p(l)
                self.gla_phase_s(l, 0)
                self.gla_phase_s(l, 1)
                self.gla_phase_o(l, need_ctx=not last)
            elif kind == 1:
                self.mlstm_phase_p1(l)
                self.mlstm_phase_p2(l)
                self.mlstm_phase_s(l, 0)
                self.mlstm_phase_s(l, 1)
                self.mlstm_phase_o(l, need_ctx=not last)
            elif kind == 2:
                self.pool_phase_p(l)
                self.pool_phase_q(l, need_ctx=not last)
            elif kind is not None:
                raise NotImplementedError
            self.mlp1_phase(l, skip_ctx=last)
            self.mlp2_phase(l, skip_ctx=last, final=last)
        self.P.emit()
        self.P.close()
        return self.nc


def _box(L, w):
    pos = np.arange(L)
    lo = np.maximum(pos - w // 2, 0)
    hi = np.minimum(pos + (w - w // 2), L)
    M = ((pos[:, None] >= lo[None, :]) & (pos[:, None] < hi[None, :])).astype(np.float32)
    return M, (hi - lo).astype(np.float32)


def _consts(T_lat):
    R = T_lat // 64
    cpt = 128 // R
    cb = np.zeros((128, 36, 128), np.float32)
    cf = np.zeros((128, 16, 128), np.float32)
    for g, w in enumerate((2, 4, 8, 16)):
        Mc, cc_ = _box(64, w)
        cb[:, g, :] = np.kron(np.eye(2, dtype=np.float32), Mc)
        cf[:, 12 + g, 0] = 1.0 / np.tile(cc_, 2)
        Mr, cr = _box(R, w)
        cb[:, 4 + g, :] = np.kron(np.eye(cpt, dtype=np.float32), Mr)
        cb[:, 8 + g, :] = -np.diag(np.tile(cr, cpt))
        cf[:, g, :] = (1.0 / np.tile(cr, cpt))[None, :]
        Mx, cx = _box(TC, w)
        for j in range(2):
            for j2 in range(2):
                cb[:, 12 + g * 4 + j * 2 + j2, :] = Mx[j * 128:(j + 1) * 128, j2 * 128:(j2 + 1) * 128]
        for j2 in range(2):
            cb[:, 28 + g * 2 + j2, :] = -np.diag(cx[j2 * 128:(j2 + 1) * 128])
            cf[:, 4 + g * 2 + j2, :] = (1.0 / cx[j2 * 128:(j2 + 1) * 128])[None, :]
    glacf = np.ones((128, 768), np.float32)
    glacf[:, 0:512:64] = 0.0
    si_, ti_ = np.meshgrid(np.arange(128), np.arange(128), indexing="ij")
    same = (si_ // 64) == (ti_ // 64)
    glacf[:, 512:640] = (same & (ti_ >= si_)).astype(np.float32)
    glacf[:, 640:768] = (same & (ti_ <= si_)).astype(np.float32)
    sel = np.zeros((64, 8, 128), np.float32)
    for r8 in range(8):
        sel[32 * (r8 // 4) + r8 % 4, r8, :] = 1.0
    mlmask = np.ones((128, 768), np.float32)
    mlmask[:, 0:512:MCH] = 0.0
    mlmask[:, 512:640] = (ti_ >= si_).astype(np.float32)
    mlmask[:, 640:768] = (ti_ <= si_).astype(np.float32)
    return {"mlmask": mlmask, "ml_sel": sel, "identf": np.eye(128, dtype=np.float32), "glacf": glacf, "onesb": np.ones((128, 128), np.float32).astype(ml_dtypes.bfloat16),
            "identb": np.eye(128, dtype=np.float32).astype(ml_dtypes.bfloat16),
            "poolcb": cb.astype(ml_dtypes.bfloat16), "poolcf": cf}


def _bd(w_qkv):
    n = w_qkv.shape[0]
    o = np.zeros((n, 3, 16, 128, 128), np.float32)
    w = w_qkv.reshape(n, 3, 16, 32, 4, 4)
    for b in range(32):
        o[:, :, :, 4 * b:4 * b + 4, 4 * b:4 * b + 4] = w[:, :, :, b]
    return o


def _wg(w_gate, off):
    n = w_gate.shape[0]
    o = np.zeros((n, 128, 48, 64), np.float32)
    w = w_gate.reshape(n, 2, 48, 128, 8)
    for d in range(2):
        o[:, :, :, 32 * d:32 * d + 4] = w[:, d, :, :, off:off + 4].transpose(0, 2, 1, 3)
    return o


def _bg(b_gate, off):
    n = b_gate.shape[0]
    o = np.zeros((n, 64, 1), np.float32)
    for d in range(2):
        o[:, 32 * d:32 * d + 4, 0] = b_gate[:, d, off:off + 4]
    return o


def _wa2blk(w_a2):
    n = w_a2.shape[0]
    o = np.zeros((n, 32, 1024), np.float32)
    o[:, 0:16, 0:512] = w_a2[:, 0]
    o[:, 16:32, 512:1024] = w_a2[:, 1]
    return o


def make_in_maps(inputs, T_lat, depth):
    B = inputs["x"].shape[0]
    consts = _consts(T_lat)
    maps = []
    for b in range(B):
        cT = np.stack([inputs["c"][b], inputs["c_ctx"]], axis=1)
        cT = np.ascontiguousarray(cT.reshape(KC, 128, 2).transpose(1, 0, 2))
        m = {
            "x": np.ascontiguousarray(inputs["x"][b]),
            "ctx": np.ascontiguousarray(inputs["ctx"][b]),
            "cT": cT.astype(np.float32),
            "ada_w": inputs["ada_w"], "ada_b": inputs["ada_b"],
            "norm1_g": inputs["norm1_g"], "norm2_g": inputs["norm2_g"],
            "mlp_w1": inputs["mlp_w1"], "mlp_w2": inputs["mlp_w2"],
            "final_g": inputs["final_g"].reshape(1, D),
            "gla_w_in": inputs["gla_w_in"],
            "gla_wa1": np.ascontiguousarray(np.concatenate([inputs["gla_w_a1"][:, 0], inputs["gla_w_a1"][:, 1]], axis=-1)),
            "gla_wa2": _wa2blk(inputs["gla_w_a2"]),
            "gla_ba": np.ascontiguousarray(inputs["gla_b_a"].reshape(-1, 8, 128).transpose(0, 2, 1)),
            "gla_gh": np.ascontiguousarray(inputs["gla_g_head"].reshape(-1, 8, 128).transpose(0, 2, 1)),
            "gla_w_o": inputs["gla_w_o"],
            "ml_w_up": inputs["mlstm_w_up"], "ml_w_down": inputs["mlstm_w_down"],
            "ml_bd": _bd(inputs["mlstm_w_qkv"]),
            "ml_wgI": _wg(inputs["mlstm_w_gate"], 0), "ml_wgF": _wg(inputs["mlstm_w_gate"], 4),
            "ml_convw": np.ascontiguousarray(inputs["mlstm_conv_w"].reshape(-1, 4, 16, 128).transpose(0, 3, 2, 1)),
            "ml_convb": np.ascontiguousarray(inputs["mlstm_conv_b"].reshape(-1, 16, 128).transpose(0, 2, 1)),
            "ml_bgI": _bg(inputs["mlstm_b_gate"], 0), "ml_bgF": _bg(inputs["mlstm_b_gate"], 4),
            "ml_gn": np.ascontiguousarray(inputs["mlstm_g_norm"].reshape(-1, 16, 128).transpose(0, 2, 1)),
            "ml_skip": np.ascontiguousarray(inputs["mlstm_skip"].reshape(-1, 16, 128).transpose(0, 2, 1)),
            "pool_w": inputs["pool_w"], "pool_b": inputs["pool_b"].reshape(-1, D),
            "pool_scale": inputs["pool_scale"],
        }
        m.update(consts)
        maps.append(m)
    return maps


def run(inputs, T_lat, depth, kinds=None, trace=False):
    inputs = {k: np.asarray(v) for k, v in inputs.items()}
    bld = Builder(T_lat, depth, kinds)
    nc = bld.build()
    maps = make_in_maps(inputs, T_lat, depth)
    maps = [{k: v for k, v in m.items() if k in bld.din} for m in maps]
    res = run_bass_kernel_spmd(nc, maps, core_ids=list(range(len(maps))), trace=trace)
    out = np.stack([r["out"] for r in res.results], axis=0)
    return out.astype(np.float32), res, bld


def kernel(**inputs):
    out, _, _ = run(inputs, 4096, 4)
    return out
```
